# Optimizing a Trainium2 kernel written in Bass

```python
import jax, jax.numpy as jnp
from jax import lax
import numpy as np

D_MODEL = 1024
BATCH = 16
SEQ = 4096
DEPTH = 1

MLA_HEADS = 8
MLA_NOPE = 64
MLA_ROPE = 32
MLA_V = 64
Q_LORA = 256
KV_LORA = 128
ATTN_QBLOCK = 128
RET_HEADS = 4
RET_DK = 64
RET_DV = 128
RET_CHUNK = 128
ROPE_BASE = 10000.0
NORM_EPS = 1e-6
MIX_WIDTH = MLA_HEADS * MLA_V + RET_HEADS * RET_DV
IN_WIDTH = Q_LORA + KV_LORA + MLA_ROPE + 2 * RET_HEADS * RET_DK + 2 * RET_HEADS * RET_DV
N_GROUPS = 4
EXPERTS_PER_GROUP = 8
N_EXPERTS = N_GROUPS * EXPERTS_PER_GROUP
TOP_K = 2
D_EXPERT = 256
MOE_BLOCK = 256

kernel_name = "hybrid_mla_retention_hmoe_adaln"


def rmsnorm(x, g):
    xf = x.astype(jnp.float32)
    y = xf * lax.rsqrt(jnp.mean(xf * xf, axis=-1, keepdims=True) + NORM_EPS)
    return (y * g.astype(jnp.float32)).astype(x.dtype)


def rope(x, positions):
    half = x.shape[-1] // 2
    inv_freq = ROPE_BASE ** (-(jnp.arange(half, dtype=jnp.float32) / half))
    ang = positions.astype(jnp.float32)[..., None] * inv_freq
    cos = jnp.cos(ang)[:, :, None, :]
    sin = jnp.sin(ang)[:, :, None, :]
    x1 = x[..., :half].astype(jnp.float32)
    x2 = x[..., half:].astype(jnp.float32)
    return jnp.concatenate([x1 * cos - x2 * sin, x1 * sin + x2 * cos], axis=-1).astype(x.dtype)


def mla_causal_attention(q, k, v):
    B, S, H, dqk = q.shape
    nqb = S // ATTN_QBLOCK
    scale = dqk ** -0.5
    q_blocks = q.reshape(B, nqb, ATTN_QBLOCK, H, dqk).transpose(1, 0, 2, 3, 4)
    key_idx = jnp.arange(S)

    def block(args):
        qb, i = args
        s = jnp.einsum('bqhd,bkhd->bhqk', qb, k).astype(jnp.float32) * scale
        q_idx = i * ATTN_QBLOCK + jnp.arange(ATTN_QBLOCK)
        mask = key_idx[None, :] <= q_idx[:, None]
        s = jnp.where(mask[None, None], s, jnp.finfo(jnp.float32).min)
        p = jax.nn.softmax(s, axis=-1).astype(v.dtype)
        return jnp.einsum('bhqk,bkhe->bqhe', p, v)

    o = lax.map(block, (q_blocks, jnp.arange(nqb)))
    return o.transpose(1, 0, 2, 3, 4).reshape(B, S, H * v.shape[-1])


def chunkwise_retention(q, k, v):
    B, S, H, dk = q.shape
    dv = v.shape[-1]
    C = RET_CHUNK
    n = S // C
    gamma = 1.0 - jnp.power(2.0, -5.0 - jnp.arange(H, dtype=jnp.float32))
    log_g = jnp.log(gamma)
    idx = jnp.arange(C, dtype=jnp.float32)
    diff = idx[:, None] - idx[None, :]
    dmask = jnp.where(diff[None] >= 0, jnp.exp(jnp.maximum(diff, 0.0)[None] * log_g[:, None, None]), 0.0)
    zeta = jnp.exp((C - 1.0 - idx)[None, :] * log_g[:, None])
    xi = jnp.exp((idx + 1.0)[None, :] * log_g[:, None])
    chunk_decay = jnp.exp(C * log_g)

    qc = q.reshape(B, n, C, H, dk)
    kc = k.reshape(B, n, C, H, dk)
    vc = v.reshape(B, n, C, H, dv)
    s = jnp.einsum('bnqhd,bnkhd->bnhqk', qc, kc) * dmask[None, None]
    o_intra = jnp.einsum('bnhqk,bnkhe->bnqhe', s, vc)
    u = jnp.einsum('bnkhd,hk,bnkhe->nbhde', kc, zeta, vc)

    def step(state, u_n):
        return state * chunk_decay[None, :, None, None] + u_n, state

    _, prev_states = lax.scan(step, jnp.zeros((B, H, dk, dv), u.dtype), u)
    o_cross = jnp.einsum('bnqhd,hq,nbhde->bnqhe', qc, xi, prev_states)
    return (o_intra + o_cross).reshape(B, S, H, dv)


def head_groupnorm(o):
    of = o.astype(jnp.float32)
    mu = jnp.mean(of, axis=-1, keepdims=True)
    var = jnp.mean(jnp.square(of - mu), axis=-1, keepdims=True)
    return ((of - mu) * lax.rsqrt(var + NORM_EPS)).astype(o.dtype)


def hybrid_mixer(h, positions, w_in, q_norm_g, w_uq, kv_norm_g, w_ukv, w_o):
    B, S, _ = h.shape
    proj = h @ w_in
    sizes = [Q_LORA, KV_LORA, MLA_ROPE, RET_HEADS * RET_DK, RET_HEADS * RET_DK,
             RET_HEADS * RET_DV, RET_HEADS * RET_DV]
    splits = [int(s) for s in np.cumsum(sizes)[:-1]]
    c_q, c_kv, k_rope, r_q, r_k, r_v, r_g = jnp.split(proj, splits, axis=-1)

    q = (rmsnorm(c_q, q_norm_g) @ w_uq).reshape(B, S, MLA_HEADS, MLA_NOPE + MLA_ROPE)
    q = jnp.concatenate([q[..., :MLA_NOPE], rope(q[..., MLA_NOPE:], positions)], axis=-1)
    kv = (rmsnorm(c_kv, kv_norm_g) @ w_ukv).reshape(B, S, MLA_HEADS, MLA_NOPE + MLA_V)
    k_nope, v = kv[..., :MLA_NOPE], kv[..., MLA_NOPE:]
    k_pe = rope(k_rope[:, :, None, :], positions)
    k = jnp.concatenate([k_nope, jnp.broadcast_to(k_pe, (B, S, MLA_HEADS, MLA_ROPE))], axis=-1)
    o_mla = mla_causal_attention(q, k, v)

    rq = rope(r_q.reshape(B, S, RET_HEADS, RET_DK), positions)
    rk = rope(r_k.reshape(B, S, RET_HEADS, RET_DK), positions) * (RET_DK ** -0.5)
    rv = r_v.reshape(B, S, RET_HEADS, RET_DV)
    o_ret = head_groupnorm(chunkwise_retention(rq, rk, rv)).reshape(B, S, RET_HEADS * RET_DV)
    o_ret = jax.nn.silu(r_g) * o_ret

    return jnp.concatenate([o_mla, o_ret], axis=-1) @ w_o


def hierarchical_moe(h, w_gr, b_gr, w_er, b_er, w1, w3, w2):
    B, S, D = h.shape
    T = B * S
    xf = h.reshape(T, D)
    p_group = jax.nn.softmax((xf @ w_gr).astype(jnp.float32) + b_gr, axis=-1)
    p_top, g_top = lax.top_k(p_group, 1)
    le = jnp.einsum('td,dge->tge', xf, w_er).astype(jnp.float32) + b_er
    le_sel = jnp.take_along_axis(le, g_top[:, :, None], axis=1)[:, 0]
    e_val, e_idx = lax.top_k(le_sel, TOP_K)
    gate = jax.nn.softmax(e_val, axis=-1) * p_top

    expert_id = (g_top * EXPERTS_PER_GROUP + e_idx).reshape(-1).astype(jnp.int32)
    token_id = jnp.repeat(jnp.arange(T, dtype=jnp.int32), TOP_K)
    weight = gate.reshape(-1)
    A = T * TOP_K

    order = jnp.argsort(expert_id)
    e_sorted, t_sorted, w_sorted = expert_id[order], token_id[order], weight[order]
    counts = jnp.bincount(expert_id, length=N_EXPERTS)
    start = jnp.cumsum(counts) - counts
    padded = ((counts + MOE_BLOCK - 1) // MOE_BLOCK) * MOE_BLOCK
    pend = jnp.cumsum(padded)
    pstart = pend - padded
    dest = pstart[e_sorted] + (jnp.arange(A) - start[e_sorted])
    P = A + N_EXPERTS * MOE_BLOCK
    nb = P // MOE_BLOCK
    tok_buf = jnp.zeros((P,), jnp.int32).at[dest].set(t_sorted)
    w_buf = jnp.zeros((P,), jnp.float32).at[dest].set(w_sorted)
    blk_expert = jnp.minimum(jnp.searchsorted(pend, jnp.arange(nb) * MOE_BLOCK, side='right'), N_EXPERTS - 1)
    xin = xf[tok_buf].reshape(nb, MOE_BLOCK, D)

    def expert_ffn(args):
        xb, e = args
        return (jax.nn.silu(xb @ w1[e]) * (xb @ w3[e])) @ w2[e]

    y = lax.map(expert_ffn, (xin, blk_expert)).reshape(P, D)
    out = jnp.zeros((T, D), h.dtype).at[tok_buf].add((y * w_buf[:, None]).astype(h.dtype))
    return out.reshape(B, S, D)


def setup_inputs(seed: int = 0) -> dict:
    key = jax.random.key(seed)
    ks = jax.random.split(key, 24)
    D, L = D_MODEL, DEPTH
    nrm = lambda k, shape, fan: jax.random.normal(k, shape, jnp.float32) * (fan ** -0.5)
    offset = jax.random.randint(ks[2], (BATCH, 1), 0, 1024, dtype=jnp.int32)
    return {
        "x": jax.random.normal(ks[0], (BATCH, SEQ, D), jnp.float32),
        "c": jax.random.normal(ks[1], (BATCH, D), jnp.float32),
        "positions": offset + jnp.arange(SEQ, dtype=jnp.int32)[None, :],
        "w_ada": nrm(ks[3], (L, D, 6 * D), D) * 0.5,
        "b_ada": jax.random.normal(ks[4], (L, 6 * D), jnp.float32) * 0.01,
        "norm1_g": 1.0 + 0.02 * jax.random.normal(ks[5], (L, D), jnp.float32),
        "w_in": nrm(ks[6], (L, D, IN_WIDTH), D),
        "q_norm_g": 1.0 + 0.02 * jax.random.normal(ks[7], (L, Q_LORA), jnp.float32),
        "w_uq": nrm(ks[8], (L, Q_LORA, MLA_HEADS * (MLA_NOPE + MLA_ROPE)), Q_LORA),
        "kv_norm_g": 1.0 + 0.02 * jax.random.normal(ks[9], (L, KV_LORA), jnp.float32),
        "w_ukv": nrm(ks[10], (L, KV_LORA, MLA_HEADS * (MLA_NOPE + MLA_V)), KV_LORA),
        "w_o": nrm(ks[11], (L, MIX_WIDTH, D), MIX_WIDTH),
        "norm2_g": 1.0 + 0.02 * jax.random.normal(ks[12], (L, D), jnp.float32),
        "w_gr": nrm(ks[13], (L, D, N_GROUPS), D),
        "b_gr": jax.random.normal(ks[14], (L, N_GROUPS), jnp.float32) * 0.01,
        "w_er": nrm(ks[15], (L, D, N_GROUPS, EXPERTS_PER_GROUP), D),
        "b_er": jax.random.normal(ks[16], (L, N_GROUPS, EXPERTS_PER_GROUP), jnp.float32) * 0.01,
        "w1": nrm(ks[17], (L, N_EXPERTS, D, D_EXPERT), D),
        "w3": nrm(ks[18], (L, N_EXPERTS, D, D_EXPERT), D),
        "w2": nrm(ks[19], (L, N_EXPERTS, D_EXPERT, D), D_EXPERT),
        "final_g": 1.0 + 0.02 * jax.random.normal(ks[20], (D,), jnp.float32),
    }


def reference(x, c, positions, w_ada, b_ada, norm1_g, w_in, q_norm_g, w_uq, kv_norm_g, w_ukv, w_o,
              norm2_g, w_gr, b_gr, w_er, b_er, w1, w3, w2, final_g):
    for l in range(DEPTH):
        mod = jax.nn.silu(c) @ w_ada[l] + b_ada[l]
        sh1, sc1, g1, sh2, sc2, g2 = [m[:, None, :] for m in jnp.split(mod, 6, axis=-1)]
        h = rmsnorm(x, norm1_g[l]) * (1.0 + sc1) + sh1
        x = x + g1 * hybrid_mixer(h, positions, w_in[l], q_norm_g[l], w_uq[l], kv_norm_g[l], w_ukv[l], w_o[l])
        h = rmsnorm(x, norm2_g[l]) * (1.0 + sc2) + sh2
        x = x + g2 * hierarchical_moe(h, w_gr[l], b_gr[l], w_er[l], b_er[l], w1[l], w3[l], w2[l])
    return rmsnorm(x, final_g)
```

```python
import math
import numpy as np
from contextlib import ExitStack
import concourse.bass as bass
import concourse.mybir as mybir
from concourse.bass_utils import run_bass_kernel_spmd

F32 = mybir.dt.float32
BF16 = mybir.dt.bfloat16
I32 = mybir.dt.int32
ALU = mybir.AluOpType
AF = mybir.ActivationFunctionType
AX = mybir.AxisListType

ENGS = ("pe", "act", "dve", "pool", "sp")
D = 1024
NE = 32
BLK = 256
EPS = 1e-6
MAGIC_RN = 12582912.0
TWO_PI = float(2 * np.pi)


class Tk:
    __slots__ = ("w", "r", "acc", "wd")

    def __init__(self, acc=False):
        self.w = None
        self.r = []
        self.acc = acc
        self.wd = {}


class Sched:
    def __init__(self, nc, stack):
        self.nc = nc
        self.stack = stack
        self.cnt = {}
        self.sems = {}
        for e in ENGS:
            self._mksem("E_" + e)
        self.nd = 0
        self.all = []
        self.tok2op = {}

    def _mksem(self, key):
        self.sems[key] = self.stack.enter_context(self.nc.semaphore(key))
        self.cnt[key] = 0
        return key

    def dma_sem(self, name=""):
        self.nd += 1
        return self._mksem("D%d_%s" % (self.nd, name))

    def _deps(self, reads, writes):
        deps = set()

        def add(tok):
            if tok is None:
                return
            k, v = tok
            if k[0] == "D":
                v = self.cnt[k]
            deps.add((k, v))
        for t in reads:
            add(t.w)
            if t.acc:
                for kv in t.wd.items():
                    add(kv)
        for t in writes:
            if t.acc:
                continue
            add(t.w)
            for tok in t.r:
                add(tok)
        return deps

    def _commit(self, tok, reads, writes):
        for t in reads:
            if not t.acc:
                t.r.append(tok)
        for t in writes:
            if t.acc:
                if t.wd.get(tok[0], 0) < tok[1]:
                    t.wd[tok[0]] = tok[1]
            else:
                t.w = tok
                t.r = []

    def op(self, eng, fn, reads=(), writes=(), cost=0.5):
        deps = self._deps(reads, writes)
        key = "E_" + eng
        self.cnt[key] += 1
        tok = (key, self.cnt[key])
        self.tok2op[tok] = len(self.all)
        self.all.append(dict(eng=eng, fn=fn, dma=False, tok=tok, deps=deps, cost=cost, lat=0.0))
        self._commit(tok, reads, writes)

    def dma(self, eng, fn, sem, reads=(), writes=(), lat=4.0):
        if eng == "pool":
            if sem + "_p" not in self.sems:
                self._mksem(sem + "_p")
            sem = sem + "_p"
        deps = self._deps(reads, writes)
        self.cnt[sem] += 16
        tok = (sem, self.cnt[sem])
        self.tok2op[tok] = len(self.all)
        self.all.append(dict(eng=eng, fn=fn, dma=True, tok=tok, deps=deps, cost=(1.2 if eng == "pool" else 0.12), lat=lat))
        self._commit(tok, reads, writes)

    def wait_all(self, eng, tks):
        deps = self._deps(tks, ())
        self.all.append(dict(eng=eng, fn=None, dma=False, tok=None, deps=deps, cost=0.0, lat=0.0))

    def _schedule(self, W=320):
        ops = self.all
        n = len(ops)
        prod = [None] * n
        dependents = [[] for _ in range(n)]
        ndeps = [0] * n
        for i, o in enumerate(ops):
            ps = set()
            for tok in o["deps"]:
                j = self.tok2op.get(tok)
                if j is not None:
                    ps.add(j)
            prod[i] = ps
            ndeps[i] = len(ps)
            for j in ps:
                dependents[j].append(i)
        pending = {e: [] for e in ENGS}
        for i, o in enumerate(ops):
            pending[o["eng"]].append(i)
        head = {e: 0 for e in ENGS}
        done = [False] * n
        comp = [0.0] * n
        ready = [0.0] * n
        free = {e: 0.0 for e in ENGS}
        semmax = {}
        order = {e: [] for e in ENGS}
        left = n
        while left:
            best = None
            for e in ENGS:
                lst = pending[e]
                h = head[e]
                while h < len(lst) and done[lst[h]]:
                    h += 1
                head[e] = h
                if h >= len(lst):
                    continue
                seen_dma = False
                cnt = 0
                k = h
                fe = free[e]
                while k < len(lst) and cnt < W:
                    i = lst[k]
                    k += 1
                    if done[i]:
                        continue
                    cnt += 1
                    o = ops[i]
                    isd = o["dma"] or o["fn"] is None
                    if isd and seen_dma:
                        continue
                    if isd:
                        seen_dma = True
                    if ndeps[i]:
                        continue
                    st = ready[i] if ready[i] > fe else fe
                    if best is None or st < best[0] or (st == best[0] and i < best[1]):
                        best = (st, i, e)
                    if st <= fe:
                        break
            st, i, e = best
            o = ops[i]
            done[i] = True
            left -= 1
            order[e].append(i)
            fin = st + o["cost"]
            free[e] = fin
            c = fin + o["lat"]
            if o["dma"]:
                sk = o["tok"][0]
                if semmax.get(sk, 0.0) > c:
                    c = semmax[sk]
                semmax[sk] = c
            comp[i] = c
            for d in dependents[i]:
                ndeps[d] -= 1
                if comp[i] > ready[d]:
                    ready[d] = comp[i]
        self.sim_time = max(free.values())
        return order

    def emit(self):
        import os
        ops = self.all
        if os.environ.get("K_REORDER", "1") == "1":
            order = self._schedule()
        else:
            order = {e: [] for e in ENGS}
            for i, o in enumerate(ops):
                order[o["eng"]].append(i)
        newtok = {}
        for e in ENGS:
            c = 0
            for i in order[e]:
                o = ops[i]
                if o["fn"] is not None and not o["dma"]:
                    c += 1
                    newtok[o["tok"]] = ("E_" + e, c)
        plan = {e: [] for e in ENGS}
        needed = {}
        for e in ENGS:
            wd = {}
            for i in order[e]:
                o = ops[i]
                mx = {}
                for tok in o["deps"]:
                    k, v = newtok.get(tok, tok)
                    if mx.get(k, 0) < v:
                        mx[k] = v
                waits = []
                for k, v in mx.items():
                    if wd.get(k, 0) < v:
                        wd[k] = v
                        waits.append((k, v))
                        if k[0] == "E":
                            needed.setdefault(k, set()).add(v)
                plan[e].append((waits, o))
        rank = {k: {v: r + 1 for r, v in enumerate(sorted(vs))} for k, vs in needed.items()}
        sems = self.sems

        def run(name, eng):
            for waits, o in plan[name]:
                for k, v in waits:
                    eng.wait_ge(sems[k], rank[k][v] if k[0] == "E" else v)
                if o["fn"] is not None:
                    ins = o["fn"](eng)
                    if o["dma"]:
                        ins.then_inc(sems[o["tok"][0]], 16)
                    else:
                        nt = newtok[o["tok"]]
                        if nt[1] in needed.get(nt[0], ()):
                            ins.then_inc(sems[nt[0]], 1)
        with self.nc.Block() as block:
            @block.tensor
            def _(e):
                run("pe", e)

            @block.scalar
            def _(e):
                run("act", e)

            @block.vector
            def _(e):
                run("dve", e)

            @block.gpsimd
            def _(e):
                run("pool", e)

            @block.sync
            def _(e):
                run("sp", e)


def make_consts(nblk):
    H = 4
    gam = 1.0 - 2.0 ** (-5.0 - np.arange(H))
    lg = np.log(gam)
    p = np.arange(128)
    cols = {}
    cols["ident"] = np.eye(128, dtype=np.float64)
    cols["triu"] = (p[:, None] < p[None, :]).astype(np.float64)
    cm = np.zeros((128, 4, 512))
    q = np.arange(512)
    for m in range(4):
        cm[:, m, :] = ((128 * m + p)[:, None] <= q[None, :])
    cmask_np = cm.reshape(128, -1)
    dm = np.zeros((128, 4, 128))
    for h in range(4):
        d = p[None, :] - p[:, None]
        dm[:, h, :] = np.where(d >= 0, np.exp(np.maximum(d, 0) * lg[h]), 0.0)
    cols["dmaskT"] = dm.reshape(128, -1)
    xi = np.zeros((128, 2, 128))
    for j in range(2):
        for half in range(2):
            h = 2 * j + half
            xi[half * 64:(half + 1) * 64, j, :] = np.exp((p + 1.0) * lg[h])[None, :]
    cols["xi"] = xi.reshape(128, -1)
    zt = np.zeros((128, 4, 64))
    for h in range(4):
        zt[:, h, :] = np.exp((127.0 - p) * lg[h])[:, None]
    cols["zeta"] = zt.reshape(128, -1)
    rp = np.zeros((128, 6))
    jr = p % 32
    rp[:, 0] = 10000.0 ** (-(jr / 32.0))
    blk64 = (p % 64) // 32
    rp[:, 1] = np.where(blk64 == 0, np.pi / 2, 0.0)
    rp[:, 2] = np.where(blk64 == 0, np.pi, np.pi / 2)
    jm = (p - 64) % 16
    rp[:, 3] = 10000.0 ** (-(jm / 16.0))
    b16 = ((p - 64) // 16) % 2
    rp[:, 4] = np.where(b16 == 0, np.pi / 2, 0.0)
    rp[:, 5] = np.where(b16 == 0, np.pi, np.pi / 2)
    cols["rp"] = rp
    cols["blkstart"] = np.broadcast_to((np.arange(nblk) * float(BLK))[None, :], (128, nblk))
    cols["iotap"] = p[:, None].astype(np.float64)
    cols["cd"] = np.broadcast_to(np.exp(128.0 * lg)[None, :], (128, 4))
    cols["cmask"] = cmask_np
    off = {}
    o = 0
    arrs = []
    for k, v in cols.items():
        off[k] = (o, o + v.shape[1])
        o += v.shape[1]
        arrs.append(v)
    return np.concatenate(arrs, axis=1).astype(np.float32), off


def build_nc(S, NSEQ, dbg=None):
    T = S * NSEQ
    NT = S // 128
    NTT = T // 128
    NG = S // 512
    NBLK = (2 * T + NE * (BLK - 1) + BLK - 1) // BLK
    PT = NBLK * BLK
    cst_np, coff = make_consts(NBLK)
    NC = cst_np.shape[1]
    nc = bass.Bass("TRN2", target_bir_lowering=False)

    def din(name, shape, dt=F32):
        return nc.dram_tensor(name, list(shape), dt, kind="ExternalInput").ap()

    def dscr(name, shape, dt):
        return nc.dram_tensor(name, list(shape), dt, kind="Internal").ap()

    x_d = din("x", [NSEQ, S, D])
    c_d = din("c", [NSEQ, D])
    pos_d = din("positions", [NSEQ, S], I32)
    wada_d = din("w_ada", [D, 6 * D])
    bada_d = din("b_ada", [1, 6 * D])
    n1g_d = din("norm1_g", [1, D])
    win_d = din("w_in", [D, 1952])
    qng_d = din("q_norm_g", [1, 256])
    wuq_d = din("w_uq", [256, 768])
    kvng_d = din("kv_norm_g", [1, 128])
    wukv_d = din("w_ukv", [128, 1024])
    wo_d = din("w_o", [D, D])
    n2g_d = din("norm2_g", [1, D])
    wgr_d = din("w_gr", [D, 4])
    bgr_d = din("b_gr", [1, 4])
    wer_d = din("w_er", [D, 32])
    ber_d = din("b_er", [1, 32])
    w1_d = din("w1", [NE, D, 256])
    w3_d = din("w3", [NE, D, 256])
    w2_d = din("w2", [NE, 256, D])
    fg_d = din("final_g", [1, D])
    cst_d = din("cst", [128, NC])
    out_d = nc.dram_tensor("out", [NSEQ, S, D], F32, kind="ExternalOutput").ap()

    mods_d = dscr("mods", [NSEQ, 6 * D], F32)
    mixm_d = dscr("mixm", [NSEQ, 8, 64, S], BF16)
    mixr_d = dscr("mixr", [NSEQ, 4, 128, S], BF16)
    x1s_d = dscr("x1s", [T, D], F32)
    h2s_d = dscr("h2s", [T, D], BF16)
    xs_d = dscr("xs", [PT, D], BF16)
    ys_d = dscr("ys", [PT, D], BF16)
    wall_d = dscr("wall", [NE * 128, 6144], BF16)
    csms_d = dscr("csms", [NSEQ, 2, 32, S], F32)
    dbg_out = {}
    if dbg:
        for name, shape in dbg.items():
            dbg_out[name] = nc.dram_tensor("dbg_" + name, list(shape), F32, kind="ExternalOutput").ap()

    st = ExitStack()
    with st:
        SC = Sched(nc, st)

        class Buf:
            def __init__(self, name, shape, dt, psum=False):
                if psum:
                    self.t = st.enter_context(nc.psum_tensor(name, list(shape), dt))
                else:
                    self.t = st.enter_context(nc.sbuf_tensor("sb_" + name, list(shape), dt))
                self.k = Tk()

            def __getitem__(self, idx):
                return self.t[idx]

        def sb(name, shape, dt=F32):
            return Buf(name, shape, dt)

        class Alias:
            def __init__(self, parent, off, shape, dt, own=False):
                n = 1
                for d_ in shape[1:]:
                    n *= d_
                nb = n * (4 if dt in (F32, I32) else 2)
                a = parent.t[0:shape[0], off // 4:(off + nb) // 4]
                v = a if dt == F32 else a.bitcast(dt)
                if len(shape) == 3:
                    v = v.rearrange("p (a b) -> p a b", a=shape[1])
                elif len(shape) == 4:
                    v = v.rearrange("p (a b c) -> p a b c", a=shape[1], b=shape[2])
                self.v = v
                self.k = Tk() if own else parent.k

            def __getitem__(self, idx):
                return self.v[idx]

        def _fsz(ap):
            try:
                return float(ap.free_size())
            except Exception:
                return 256.0

        def op(eng, method, reads, writes, *a, **kw):
            if eng == "pe":
                if method == "matmul":
                    cost = 0.31 + _fsz(kw["rhs"]) / 1200.0
                else:
                    cost = 0.42
            else:
                o_ = kw.get("out") if kw.get("out") is not None else (a[0] if a else None)
                f_ = _fsz(o_) if o_ is not None else 64.0
                if eng == "dve":
                    cost = 0.12 + f_ / 1100.0
                elif eng == "act":
                    cost = 0.28 + f_ / 1200.0
                else:
                    cost = 0.7 + f_ / 900.0
            SC.op(eng, lambda e: getattr(e, method)(*a, **kw), [b.k for b in reads], [b.k for b in writes], cost=cost)
            if kw.get("accum_out") is not None:
                SC.op(eng, lambda e: e.copy(out=adum[0:1, 0:2], in_=adum[0:1, 2:4]), [], [b.k for b in writes] + [adum.k], cost=0.2)

        def dma(eng, sem, reads, writes, **kw):
            try:
                nb = float(kw["out"].nbytes())
            except Exception:
                nb = 65536.0
            SC.dma(eng, lambda e: e.dma_start(**kw), sem, [b.k for b in reads], [b.k for b in writes], lat=2.5 + nb / 150e3)

        class DR:
            def __init__(self):
                self.k = Tk(acc=True)

        PS = [Buf("ps%d" % i, [128, 512], F32, psum=True) for i in range(8)]
        adum = sb("adum", [128, 4])
        SC.op("dve", lambda e: e.memset(adum[:], 0.0), [], [adum.k])

        def psbf(i):
            return PS[i].t[:].bitcast(BF16)

        NC0 = coff["cmask"][0]
        cst = sb("cst", [128, NC0])
        s_c = SC.dma_sem("cst")
        dma("sp", s_c, [], [cst], out=cst[:], in_=cst_d[:, 0:NC0])

        def cc(name):
            a, b = coff[name]
            return cst[:, a:b]
        ident_f = cc("ident")
        ident_b = sb("ident_b", [128, 128], BF16)
        triu_b = sb("triu_b", [128, 128], BF16)
        ones_b = sb("ones_b", [128, 128], BF16)
        ones_f = sb("ones_f", [128, 128], F32)
        cmask_b = sb("cmask_b", [128, 4, 512], BF16)
        op("dve", "tensor_copy", [cst], [ident_b], out=ident_b[:], in_=ident_f)
        op("dve", "tensor_copy", [cst], [triu_b], out=triu_b[:], in_=cc("triu"))
        op("dve", "memset", [], [ones_b], ones_b[:], 1.0)
        op("dve", "memset", [], [ones_f], ones_f[:], 1.0)
        dma("pool", s_c, [], [cmask_b], out=cmask_b[:].rearrange("p a b -> p (a b)"), in_=cst_d[:, NC0:NC0 + 2048])
        dmaskT = cc("dmaskT").rearrange("p (h q) -> p h q", h=4)
        xi_c = cc("xi").rearrange("p (j q) -> p j q", j=2)
        zeta_c = cc("zeta")
        rp = cc("rp")
        cd_c = cc("cd")

        nhalf = sb("nhalf", [128, 16])
        op("pool", "memset", [], [nhalf], nhalf[:], -0.5)
        fdum = sb("fdum", [128, 2])

        rs_i = sb("rs_i", [128, 16], I32)
        rs_t = sb("rs_t", [128, 16], F32)
        import os as _os
        USE_POW = _os.environ.get("K_POW", "1") == "1"

        def rsqrt(vbuf, vap, outbuf, outap, n):
            if USE_POW:
                op("pool", "tensor_tensor", [vbuf, nhalf], [outbuf], out=outap, in0=vap, in1=nhalf[:, 0:n], op=ALU.pow)
                return
            yi = rs_i[:, 0:n]
            y = yi.bitcast(F32)
            tt = rs_t[:, 0:n]
            op("dve", "tensor_single_scalar", [vbuf], [rs_i], out=yi, in_=vap.bitcast(I32), scalar=1, op=ALU.arith_shift_right)
            op("dve", "tensor_scalar", [rs_i], [rs_i], out=yi, in0=yi, scalar1=-1.0, scalar2=float(0x5f3759df), op0=ALU.mult, op1=ALU.add)
            for it in range(3):
                op("dve", "tensor_tensor", [rs_i], [rs_t], out=tt, in0=y, in1=y, op=ALU.mult)
                op("dve", "tensor_tensor", [rs_t, vbuf], [rs_t], out=tt, in0=tt, in1=vap, op=ALU.mult)
                op("dve", "tensor_scalar", [rs_t], [rs_t], out=tt, in0=tt, scalar1=-0.5, scalar2=1.5, op0=ALU.mult, op1=ALU.add)
                if it < 2:
                    op("dve", "tensor_tensor", [rs_t, rs_i], [rs_i], out=y, in0=y, in1=tt, op=ALU.mult)
                else:
                    op("dve", "tensor_tensor", [rs_t, rs_i], [outbuf], out=outap, in0=y, in1=tt, op=ALU.mult)

        def fence(frm, to):
            SC.op("pool", lambda e: e.memset(fdum[0:1, 0:1], 0.0), [], [b_.k for b_ in frm] + [b_.k for b_ in to] + [fdum.k])

        s_m = SC.dma_sem("mods")
        mods_k = DR()
        cT = sb("cT", [128, 8, NSEQ])
        cTe = sb("cTe", [128, 8, NSEQ])
        siluT = sb("siluT", [128, 8, NSEQ], BF16)
        for b0 in range(NSEQ):
            dma("sp", s_m, [], [cT], out=cT[:, :, b0], in_=c_d[b0:b0 + 1, :].rearrange("o (c p) -> p (o c)", p=128), allow_slow_non_contiguous=True)
        op("act", "activation", [cT], [cTe], out=cTe[:], in_=cT[:], func=AF.Exp, scale=-1.0)
        op("dve", "tensor_scalar", [cTe], [cTe], out=cTe[:], in0=cTe[:], scalar1=1.0, scalar2=None, op0=ALU.add)
        op("dve", "reciprocal", [cTe], [cTe], out=cTe[:], in_=cTe[:])
        op("dve", "tensor_tensor", [cTe, cT], [siluT], out=siluT[:], in0=cTe[:], in1=cT[:], op=ALU.mult)
        BIGW = sb("BIGW", [128, 12448])
        P0 = sb("P0", [128, 2048])
        wa = [Alias(BIGW, 0, [128, 8, 512], BF16, own=True), Alias(BIGW, 8192, [128, 8, 512], BF16, own=True)]
        s_wa = [SC.dma_sem("wa%d" % i) for i in range(2)]
        ba = sb("ba", [NSEQ, 512])
        mrow = sb("mrow", [NSEQ, 512])
        s_ba = SC.dma_sem("ba")
        for j in range(12):
            w = wa[j % 2]
            dma("pool", s_wa[j % 2], [], [w], out=w[:], in_=wada_d[:, j * 512:(j + 1) * 512].rearrange("(c p) n -> p c n", p=128))
            dma("sp", s_ba, [], [ba], out=ba[:], in_=bada_d[:, j * 512:(j + 1) * 512].partition_broadcast(NSEQ))
            for k in range(8):
                op("pe", "matmul", [siluT, w], [PS[0]], PS[0][0:NSEQ, :], lhsT=siluT[:, k, :], rhs=w[:, k, :], start=(k == 0), stop=(k == 7))
            op("dve", "tensor_tensor", [PS[0], ba], [mrow], out=mrow[:], in0=PS[0][0:NSEQ, :], in1=ba[:], op=ALU.add)
            dma("sp", s_m, [mrow], [mods_k], out=mods_d[:, j * 512:(j + 1) * 512], in_=mrow[:])

        wblk = [Alias(BIGW, 0, [128, 6144], BF16, own=True), Alias(BIGW, 12288, [128, 6144], BF16, own=True)]
        s_wl = SC.dma_sem("wl")
        s_ws = SC.dma_sem("ws")
        wall_k = DR()

        def relayout_expert(e):
            stg = wblk[e % 2]
            v13 = stg[:, 0:4096].rearrange("p (c f) -> p c f", c=8)
            dma("pool", s_wl, [], [stg], out=v13[:, :, 0:256], in_=w1_d[e].rearrange("(c p) f -> p c f", p=128))
            dma("pool", s_wl, [], [stg], out=v13[:, :, 256:512], in_=w3_d[e].rearrange("(c p) f -> p c f", p=128))
            dma("pool", s_wl, [], [stg], out=stg[:, 4096:6144].rearrange("p (c f) -> p c f", c=2),
                in_=w2_d[e].rearrange("(c p) f -> p c f", p=128))
            dma("pool", s_ws, [stg], [wall_k], out=wall_d[e * 128:(e + 1) * 128, :], in_=stg[:])
        n_slots = NSEQ * 8 * NG
        per_slot = (NE + n_slots - 1) // n_slots
        relay_state = [0]

        fence(wa, [BIGW])
        s_w = SC.dma_sem("w")
        NFM = 8 * 128 + 2 * 96
        w_fm = Alias(BIGW, 0, [128, 8, NFM], BF16)
        w_tm = Alias(BIGW, 19456, [128, 8, 1408], BF16)
        wst = [Alias(BIGW, 41984, [128, 1952], F32)] * 2
        s_wst = [SC.dma_sem("wst%d" % i) for i in range(2)]
        wuq_a = sb("wuq_a", [128, 2, 8, 192], BF16)
        STG = P0
        wuq_s = Alias(STG, 0, [128, 2, 768], F32)
        qng_c = sb("qng_c", [128, 2])
        dma("sp", s_w, [], [wuq_s], out=wuq_s[:], in_=wuq_d.rearrange("(c p) n -> p c n", p=128))
        dma("sp", s_w, [], [qng_c], out=qng_c[:], in_=qng_d.rearrange("o (c p) -> p (o c)", p=128), allow_slow_non_contiguous=True)
        op("pool", "memset", [], [wuq_a], wuq_a[:], 0.0)
        for c in range(2):
            s4 = wuq_s[:, c, :].rearrange("p (h f) -> p h f", h=8)
            op("dve", "tensor_scalar", [wuq_s, qng_c], [wuq_a], out=wuq_a[:, c, :, 0:64], in0=s4[:, :, 0:64],
               scalar1=qng_c[:, c:c + 1], scalar2=None, op0=ALU.mult)
            for ab in range(2):
                dst = wuq_a[:, c, :, ab * 96 + 64:ab * 96 + 96].rearrange("p h (dup j) -> p h dup j", dup=2)
                src = s4[:, :, 64 + ab * 16:64 + ab * 16 + 16].unsqueeze(2).to_broadcast([128, 8, 2, 16])
                op("dve", "tensor_scalar", [wuq_s, qng_c], [wuq_a], out=dst, in0=src,
                   scalar1=qng_c[:, c:c + 1], scalar2=None, op0=ALU.mult)
        wukv_s = Alias(STG, 0, [128, 1024], F32)
        kvng_c = sb("kvng_c", [128, 1])
        wk_b = sb("wk_b", [128, 8, 64], BF16)
        wv_b = sb("wv_b", [128, 8, 64], BF16)
        dma("sp", s_w, [], [wukv_s], out=wukv_s[:], in_=wukv_d)
        dma("sp", s_w, [], [kvng_c], out=kvng_c[:], in_=kvng_d.rearrange("o p -> p o"), allow_slow_non_contiguous=True)
        s3 = wukv_s[:].rearrange("p (h f) -> p h f", h=8)
        op("dve", "tensor_scalar", [wukv_s, kvng_c], [wk_b], out=wk_b[:], in0=s3[:, :, 0:64], scalar1=kvng_c[:, 0:1], scalar2=None, op0=ALU.mult)
        op("dve", "tensor_scalar", [wukv_s, kvng_c], [wv_b], out=wv_b[:], in0=s3[:, :, 64:128], scalar1=kvng_c[:, 0:1], scalar2=None, op0=ALU.mult)
        wo_m = Alias(BIGW, 0, [64, 8, D], BF16)
        wo_r = Alias(BIGW, 16384, [128, 4, D], BF16)
        w_rt = sb("w_rt", [128, 8, 36])
        b_rt = sb("b_rt", [128, 36])
        dma("sp", s_w, [], [w_rt], out=w_rt[:, :, 0:4], in_=wgr_d.rearrange("(c p) n -> p c n", p=128), allow_slow_non_contiguous=True)
        dma("sp", s_w, [], [w_rt], out=w_rt[:, :, 4:36], in_=wer_d.rearrange("(c p) n -> p c n", p=128), allow_slow_non_contiguous=True)
        dma("sp", s_w, [], [b_rt], out=b_rt[:, 0:4], in_=bgr_d.partition_broadcast(128))
        dma("sp", s_w, [], [b_rt], out=b_rt[:, 4:36], in_=ber_d.partition_broadcast(128))
        n1g_c = sb("n1g_c", [128, 8])
        dma("sp", s_w, [], [n1g_c], out=n1g_c[:], in_=n1g_d.rearrange("o (c p) -> p (o c)", p=128), allow_slow_non_contiguous=True)

        OH1 = sb("OH1", [128, NTT, 32], BF16)
        OH2 = sb("OH2", [128, NTT, 32], BF16)
        CUM = sb("CUM", [128, NTT, 32])
        GATE = sb("GATE", [128, NTT, 2])
        Macc = sb("Macc", [128, 32], BF16)
        op("pool", "memset", [], [Macc], Macc[:], 0.0)

        cqnT = sb("cqnT", [128, 2, S], BF16)
        ckvnT = sb("ckvnT", [128, S], BF16)
        kT = sb("kT", [96, S], BF16)
        s_csm = SC.dma_sem("csm")
        csms_k = DR()
        xin = [sb("xin%d" % i, [128, D]) for i in range(2)]
        s_xin = [SC.dma_sem("xin%d" % i) for i in range(2)]
        junk = sb("junk", [128, D], BF16)
        stat = sb("stat", [128, 16])
        xsb = sb("xsb", [128, D], BF16)
        xsb2 = [xsb, sb("xsb1", [128, D], BF16)]
        P1 = sb("P1", [128, 2048])
        h1T = Alias(P1, 0, [128, 8, 512], BF16)
        P2b = sb("P2b", [128, 1024])
        posi = Alias(P2b, 0, [128, 512], I32)
        posf = Alias(P2b, 2048, [128, 512], F32)
        s_pos = SC.dma_sem("pos")
        P2a = sb("P2a", [128, 1024])
        targ = Alias(P2a, 0, [128, 512], F32)
        ttmp = Alias(P2a, 2048, [128, 512], F32)
        P3a = sb("P3a", [128, 1024])
        csr1 = Alias(P3a, 0, [128, 512], F32)
        csr2 = Alias(P3a, 2048, [128, 512], F32)
        P3b = sb("P3b", [128, 1024])
        csm1f = Alias(P3b, 0, [96, 512], F32)
        csm2f = Alias(P3b, 2048, [96, 512], F32)
        colv = sb("colv", [128, 8, 4])
        s_col = SC.dma_sem("col")
        P6 = sb("P6", [128, 1024])
        P7 = sb("P7", [128, 512])
        rqT = Alias(P6, 0, [128, 2, 512], BF16)
        rqxT = Alias(P6, 2048, [128, 2, 512], BF16)
        rkT = Alias(P7, 0, [128, 2, 512], BF16)
        P4a = sb("P4a", [128, 1024])
        rt1 = Alias(P4a, 0, [128, 512], F32)
        rt2 = Alias(P4a, 2048, [128, 512], F32)
        cqn = sb("cqn", [128, 384], BF16)
        P4b = sb("P4b", [128, 1024])
        P4c = sb("P4c", [128, 1024])
        RVG = [Alias(P4b, 0, [128, 4, 512], BF16), Alias(P4c, 0, [128, 4, 512], BF16)]
        GTG = [Alias(P0, 0, [128, 4, 512], BF16), Alias(P0, 4096, [128, 4, 512], BF16)]
        GT4 = GTG[0]
        gsg = sb("gsg", [128, 512])
        rkz = sb("rkz", [128, 256], BF16)
        sdT = sb("sdT", [128, 4, 128], BF16)
        state = sb("state", [128, 2, 128])
        state_b = sb("state_b", [128, 2, 128], BF16)
        P8 = sb("P8", [128, 1024])
        osb = Alias(P8, 0, [128, 4, 128], F32)
        osq = Alias(P8, 2048, [128, 4, 128], F32)
        gst = sb("gst", [128, 16])
        oretb = sb("oretb", [128, 512], BF16)
        P5 = sb("P5", [128, 1024])
        oretT = Alias(P5, 0, [128, 4, 512], BF16)
        s_mixr = SC.dma_sem("mixr")
        mixr_k = DR()
        mixm_k = DR()

        def rope_table(dst, dstap, prow, invf_col, ph_col):
            a = targ[prow, :]
            b = ttmp[prow, :]
            op("dve", "tensor_scalar", [posf, cst], [targ], out=a, in0=posf[prow, :], scalar1=rp[prow, invf_col:invf_col + 1],
               scalar2=rp[prow, ph_col:ph_col + 1], op0=ALU.mult, op1=ALU.add)
            op("dve", "tensor_scalar", [targ], [ttmp], out=b, in0=a, scalar1=1.0 / TWO_PI, scalar2=MAGIC_RN, op0=ALU.mult, op1=ALU.add)
            op("dve", "tensor_scalar", [ttmp], [ttmp], out=b, in0=b, scalar1=MAGIC_RN, scalar2=-TWO_PI, op0=ALU.subtract, op1=ALU.mult)
            op("dve", "tensor_tensor", [ttmp, targ], [targ], out=a, in0=a, in1=b, op=ALU.add)
            op("dve", "tensor_scalar", [targ], [targ], out=a, in0=a, scalar1=-3.1415925, scalar2=3.1415925, op0=ALU.max, op1=ALU.min)
            op("act", "activation", [targ], [dst], out=dstap, in_=a, func=AF.Sin)


        vh = [sb("vh0", [128, NT, 65], BF16)] * 2
        for i in range(1):
            op("pool", "memset", [], [vh[i]], vh[i][:, :, 64:65], 1.0)
        pT = [Alias(P5, 0, [128, 512], BF16, own=True), Alias(P5, 1024, [128, 512], BF16, own=True)]
        qT = [Alias(P5, 2048, [96, 512], BF16, own=True), Alias(P5, 3072, [96, 512], BF16, own=True)]
        rrow = Alias(P8, 0, [65, 512], F32)
        bcs = Alias(P8, 2048, [64, 512], F32)
        oTm = Alias(P7, 0, [64, 512], BF16)
        s_mixm = SC.dma_sem("mixm")
        s_bc = SC.dma_sem("bc")
        s_mm = SC.dma_sem("mm")
        s_x1 = SC.dma_sem("x1")
        s_h2 = SC.dma_sem("h2")
        x1s_k = DR()
        h2s_k = DR()
        g1bc = Alias(P2a, 0, [128, D], F32)
        sh2bc = Alias(P2b, 0, [128, D], F32)
        A2bc = Alias(P3a, 0, [128, D], F32)
        n2gbc = Alias(P3b, 0, [128, D], F32)
        mm_t = Alias(P6, 0, [64, 8, 128], BF16)
        mr_t = Alias(P6, 2048, [128, 4, 128], BF16)
        x1 = Alias(P4a, 0, [128, D], F32)
        h2 = Alias(P4b, 0, [128, D], F32)
        h2b = Alias(P7, 0, [128, D], BF16)
        h2T = Alias(P5, 0, [128, 8, 128], F32)
        x1_2 = [x1, Alias(P0, 0, [128, D], F32, own=True)]
        h2_2 = [h2, Alias(P0, 4096, [128, D], F32, own=True)]
        h2b_2 = [h2b, Alias(P4c, 0, [128, D], BF16, own=True)]
        h2T_2 = [h2T, Alias(P1, 0, [128, 8, 128], F32, own=True)]
        mm_t_2 = [mm_t, Alias(P1, 4096, [64, 8, 128], BF16, own=True)]
        mr_t_2 = [mr_t, Alias(P1, 6144, [128, 4, 128], BF16, own=True)]
        c_alts = [x1_2[1], h2_2[1], h2b_2[1], h2T_2[1], mm_t_2[1], mr_t_2[1]]
        s_mm2 = [s_mm, SC.dma_sem("mm1")]
        lgt2 = [sb("lgt%d" % i, [128, 40]) for i in range(2)]
        for i in range(2):
            op("dve", "memset", [], [lgt2[i]], lgt2[i][:], -1e30)
        m8_2 = [sb("m8_%d" % i, [128, 16]) for i in range(2)]
        rst_2 = [sb("rst_%d" % i, [128, 20]) for i in range(2)]
        lem_2 = [sb("lem_%d" % i, [128, 32]) for i in range(2)]
        Mt_2 = [sb("Mt_%d" % i, [128, 32], BF16) for i in range(2)]
        ra = Alias(P2a, 0, [128, 32], F32)
        rb = Alias(P2a, 128, [128, 32], F32)
        pad_ = Alias(P2a, 256, [128, 32], F32)
        pst = Alias(P2a, 384, [128, 32], F32)
        cmp3 = Alias(BIGW, 0, [128, NBLK, 32], F32)
        ebf = sb("ebf", [128, NBLK])
        WIDX = sb("WIDX", [128, NBLK], I32)
        cmpd = Alias(BIGW, 16384, [128, NTT, 32], F32)
        destf = Alias(P2b, 0, [128, NTT, 2], F32)
        DEST = sb("DEST", [128, NTT, 2], I32)
        h2r = [Alias(P6, 0, [128, D], BF16, own=True), Alias(P6, 2048, [128, D], BF16, own=True)]
        s_h2r = [SC.dma_sem("h2r%d" % i) for i in range(2)]
        s_sc = SC.dma_sem("sc")
        s_wg = [SC.dma_sem("wg%d" % i) for i in range(2)]
        xblk = [Alias(P1, 0, [128, 2, D], BF16, own=True), Alias(P1, 4096, [128, 2, D], BF16, own=True)]
        s_xb = [SC.dma_sem("xb%d" % i) for i in range(2)]
        xTb = Alias(P8, 0, [128, 2, 8, 128], BF16)
        sg = Alias(P2a, 0, [128, 256], F32)
        actb = Alias(P2a, 1024, [128, 256], BF16)
        actT = Alias(P2a, 1536, [128, 2, 128], BF16)
        ysb = [Alias(P0, 0, [128, D], BF16, own=True), Alias(P0, 4096, [128, D], BF16, own=True)]
        ysc = [Alias(P1, 0, [128, D], BF16, own=True), Alias(P1, 4096, [128, D], BF16, own=True)]
        s_ys = [SC.dma_sem("ys%d" % i) for i in range(2)]
        s_yg = [SC.dma_sem("yg%d" % i) for i in range(2)]
        for b in range(NSEQ):
            op("pool", "memset", [], [w_fm], w_fm[:, :, 1024:NFM], 0.0)
            for k in range(8):
                ws = wst[k % 2]
                dma("sp", s_wst[k % 2], [], [ws], out=ws[:], in_=win_d[k * 128:(k + 1) * 128, :])
                op("act", "copy", [ws], [w_tm], out=w_tm[:, k, 0:384], in_=ws[:, 0:384])
                op("act", "copy", [ws], [w_tm], out=w_tm[:, k, 384:1408], in_=ws[:, 928:1952])
                for which, base, scale in ((0, 416, 1.0), (1, 672, 0.125)):
                    src = ws[:, base:base + 256].rearrange("p (h two j) -> p h two j", h=4, two=2)
                    for ab in range(2):
                        dst = w_fm[:, k, (which * 4 + ab * 2) * 128:(which * 4 + ab * 2 + 2) * 128].rearrange(
                            "p (h dup j) -> p h dup j", h=4, dup=2)
                        op("dve", "tensor_scalar", [ws], [w_fm], out=dst,
                           in0=src[:, :, ab:ab + 1, :].to_broadcast([128, 4, 2, 32]), scalar1=scale, scalar2=None, op0=ALU.mult)
                srck = ws[:, 384:416].rearrange("p (two j) -> p two j", two=2)
                for ab in range(2):
                    dst = w_fm[:, k, 1024 + ab * 96 + 64:1024 + ab * 96 + 96].rearrange("p (dup j) -> p dup j", dup=2)
                    op("dve", "tensor_copy", [ws], [w_fm], out=dst, in_=srck[:, ab:ab + 1, :].to_broadcast([128, 2, 16]))
            dma("sp", s_col, [mods_k], [colv], out=colv[:, :, 0], in_=mods_d[b:b + 1, 0:D].rearrange("o (c p) -> p (o c)", p=128),
                allow_slow_non_contiguous=True)
            dma("sp", s_col, [mods_k], [colv], out=colv[:, :, 1], in_=mods_d[b:b + 1, D:2 * D].rearrange("o (c p) -> p (o c)", p=128),
                allow_slow_non_contiguous=True)
            op("dve", "scalar_tensor_tensor", [colv, n1g_c], [colv], out=colv[:, :, 2], in0=colv[:, :, 1], scalar=1.0, in1=n1g_c[:],
               op0=ALU.add, op1=ALU.mult)
            op("dve", "memset", [], [state], state[:], 0.0)
            op("dve", "memset", [], [state_b], state_b[:], 0.0)

            def A_pos(g):
                t0 = g * 512
                dma("sp", s_pos, [], [posi], out=posi[:], in_=pos_d[b:b + 1, t0:t0 + 512].partition_broadcast(128))
                op("dve", "tensor_copy", [posi], [posf], out=posf[:], in_=posi[:])

            def A_table(g, k):
                t0 = g * 512
                if k == 0:
                    rope_table(csr1, csr1[:], slice(0, 128), 0, 1)
                elif k == 1:
                    rope_table(csr2, csr2[:], slice(0, 128), 0, 2)
                elif k == 2:
                    rope_table(csm1f, csm1f[64:96, :], slice(64, 96), 3, 4)
                    dma("sp", s_csm, [csm1f], [csms_k], out=csms_d[b, 0, :, t0:t0 + 512], in_=csm1f[64:96, :])
                else:
                    rope_table(csm2f, csm2f[64:96, :], slice(64, 96), 3, 5)
                    dma("sp", s_csm, [csm2f], [csms_k], out=csms_d[b, 1, :, t0:t0 + 512], in_=csm2f[64:96, :])

            def A_S1(g, tl):
                ti = g * 4 + tl
                tok0 = ti * 128
                xi_ = xin[ti % 2]
                xs_ = xsb2[ti % 2]
                so = 10 + 3 * (ti % 2)
                dma("sp", s_xin[ti % 2], [], [xi_], out=xi_[:], in_=x_d[b, tok0:tok0 + 128, :])
                op("act", "activation", [xi_], [junk, stat], out=junk[:], in_=xi_[:], func=AF.Square, accum_out=stat[:, so:so + 1])
                op("dve", "tensor_scalar", [stat], [stat], out=stat[:, so + 1:so + 2], in0=stat[:, so:so + 1], scalar1=1.0 / D, scalar2=EPS,
                   op0=ALU.mult, op1=ALU.add)
                rsqrt(stat, stat[:, so + 1:so + 2], stat, stat[:, so + 2:so + 3], 1)
                op("dve", "tensor_scalar", [xi_, stat], [xs_], out=xs_[:], in0=xi_[:], scalar1=stat[:, so + 2:so + 3], scalar2=None, op0=ALU.mult)

            def A_T8(g, tl):
                ti = g * 4 + tl
                xs_ = xsb2[ti % 2]
                for c in range(8):
                    op("pe", "transpose", [xs_, ident_b], [PS[0]], out=psbf(0)[:, c * 128:(c + 1) * 128],
                       in_=xs_[:, c * 128:(c + 1) * 128], identity=ident_b[:])
                for c in range(8):
                    if c % 2 == 0:
                        op("dve", "tensor_scalar", [PS[0], colv], [h1T], out=h1T[:, c, tl * 128:(tl + 1) * 128],
                           in0=psbf(0)[:, c * 128:(c + 1) * 128], scalar1=colv[:, c, 2:3], scalar2=colv[:, c, 0:1],
                           op0=ALU.mult, op1=ALU.add)
                    else:
                        op("act", "activation", [PS[0], colv], [h1T], out=h1T[:, c, tl * 128:(tl + 1) * 128],
                           in_=psbf(0)[:, c * 128:(c + 1) * 128], func=AF.Identity, scale=colv[:, c, 2:3], bias=colv[:, c, 0:1])

            def A_MM(g, tl):
                RV4 = RVG[g % 2]
                GT4 = GTG[g % 2]
                ti = g * 4 + tl
                tok0 = ti * 128
                for (pb, c0, n) in ((1, 0, 384), (2, 384, 512), (3, 896, 512)):
                    for k in range(8):
                        op("pe", "matmul", [h1T, w_tm], [PS[pb]], PS[pb][:, 0:n], lhsT=h1T[:, k, tl * 128:(tl + 1) * 128],
                           rhs=w_tm[:, k, c0:c0 + n], start=(k == 0), stop=(k == 7))
                op("act", "activation", [PS[1]], [junk, stat], out=junk[:, 0:256], in_=PS[1][:, 0:256], func=AF.Square, accum_out=stat[:, 4:5])
                op("act", "activation", [PS[1]], [junk, stat], out=junk[:, 256:384], in_=PS[1][:, 256:384], func=AF.Square, accum_out=stat[:, 5:6])
                op("dve", "tensor_scalar", [stat], [stat], out=stat[:, 6:7], in0=stat[:, 4:5], scalar1=1.0 / 256, scalar2=EPS, op0=ALU.mult, op1=ALU.add)
                op("dve", "tensor_scalar", [stat], [stat], out=stat[:, 7:8], in0=stat[:, 5:6], scalar1=1.0 / 128, scalar2=EPS, op0=ALU.mult, op1=ALU.add)
                rsqrt(stat, stat[:, 6:8], stat, stat[:, 8:10], 2)
                op("dve", "tensor_scalar", [PS[1], stat], [cqn], out=cqn[:, 0:256], in0=PS[1][:, 0:256], scalar1=stat[:, 8:9], scalar2=None, op0=ALU.mult)
                op("dve", "tensor_scalar", [PS[1], stat], [cqn], out=cqn[:, 256:384], in0=PS[1][:, 256:384], scalar1=stat[:, 9:10], scalar2=None, op0=ALU.mult)
                for c in range(3):
                    op("pe", "transpose", [cqn, ident_b], [PS[0]], out=psbf(0)[:, c * 128:(c + 1) * 128],
                       in_=cqn[:, c * 128:(c + 1) * 128], identity=ident_b[:])
                op("act", "copy", [PS[0]], [cqnT], out=cqnT[:, :, tok0:tok0 + 128],
                   in_=psbf(0)[:, 0:256].rearrange("p (c t) -> p c t", c=2))
                op("act", "copy", [PS[0]], [ckvnT], out=ckvnT[:, tok0:tok0 + 128], in_=psbf(0)[:, 256:384])
                op("act", "copy", [PS[2]], [RV4], out=RV4[:, tl, :], in_=PS[2][:])
                op("act", "activation", [PS[3]], [gsg], out=gsg[:], in_=PS[3][:], func=AF.Tanh, scale=0.5)
                op("dve", "scalar_tensor_tensor", [gsg, PS[3]], [GT4], out=GT4[:, tl, :], in0=gsg[:], scalar=1.0, in1=PS[3][:], op0=ALU.add, op1=ALU.mult)

            def A_FM(g):
                t0 = g * 512

                def fm_mm(pb, col0, ncols):
                    for k in range(8):
                        op("pe", "matmul", [h1T, w_fm], [PS[pb]], PS[pb][0:ncols, :], lhsT=w_fm[:, k, col0:col0 + ncols],
                           rhs=h1T[:, k, :], start=(k == 0), stop=(k == 7))
                for which, dst in ((0, rqT), (1, rkT)):
                    for j in range(2):
                        fm_mm(4, (which * 4 + j) * 128, 128)
                        fm_mm(5, (which * 4 + 2 + j) * 128, 128)
                        op("dve", "tensor_tensor", [PS[4], csr1], [rt1], out=rt1[:], in0=PS[4][:], in1=csr1[:], op=ALU.mult)
                        op("dve", "tensor_tensor", [PS[5], csr2], [rt2], out=rt2[:], in0=PS[5][:], in1=csr2[:], op=ALU.mult)
                        op("pool", "tensor_tensor", [rt1, rt2], [dst], out=dst[:, j, :], in0=rt1[:], in1=rt2[:], op=ALU.add)
                op("pool", "tensor_tensor", [rqT, cst], [rqxT], out=rqxT[:].rearrange("p j (n q) -> p j n q", n=4),
                   in0=rqT[:].rearrange("p j (n q) -> p j n q", n=4), in1=xi_c.unsqueeze(2).to_broadcast([128, 2, 4, 128]), op=ALU.mult)
                fm_mm(4, 1024, 96)
                fm_mm(5, 1120, 96)
                op("dve", "tensor_tensor", [PS[4], csm1f], [rt1], out=rt1[64:96, :], in0=PS[4][64:96, :], in1=csm1f[64:96, :], op=ALU.mult)
                op("dve", "tensor_tensor", [PS[5], csm2f], [rt2], out=rt2[64:96, :], in0=PS[5][64:96, :], in1=csm2f[64:96, :], op=ALU.mult)
                op("pool", "tensor_tensor", [rt1, rt2], [kT], out=kT[64:96, t0:t0 + 512], in0=rt1[64:96, :], in1=rt2[64:96, :], op=ALU.add)

            def A_RETa(g, tl):
                RV4 = RVG[g % 2]
                qs = slice(tl * 128, (tl + 1) * 128)
                for j in range(2):
                    op("pe", "transpose", [rkT, ident_b], [PS[4]], out=psbf(4)[:, j * 128:(j + 1) * 128], in_=rkT[:, j, qs], identity=ident_b[:])
                op("dve", "tensor_tensor", [PS[4], cst], [rkz], out=rkz[:], in0=psbf(4)[:, 0:256], in1=zeta_c, op=ALU.mult)
                for h in range(4):
                    j, half = h // 2, h % 2
                    pr = slice(half * 64, half * 64 + 64)
                    op("pe", "matmul", [rkT, rqT], [PS[6]], PS[6][:, h * 128:(h + 1) * 128], lhsT=rkT[pr, j, qs], rhs=rqT[pr, j, qs],
                       start=True, stop=True)
                op("dve", "tensor_tensor", [PS[6], cst], [sdT], out=sdT[:], in0=PS[6][:].rearrange("p (h q) -> p h q", h=4), in1=dmaskT, op=ALU.mult)
                for h in range(4):
                    j, half = h // 2, h % 2
                    pr = slice(half * 64, half * 64 + 64)
                    op("pe", "matmul", [sdT, RV4], [PS[7]], PS[7][:, h * 128:(h + 1) * 128], lhsT=sdT[:, h, :], rhs=RV4[:, tl, h * 128:(h + 1) * 128],
                       start=True, stop=False)
                    op("pe", "matmul", [rqxT, state_b], [PS[7]], PS[7][:, h * 128:(h + 1) * 128], lhsT=rqxT[pr, j, qs], rhs=state_b[pr, j, :],
                       start=False, stop=True)
                for h in range(4):
                    j = h // 2
                    op("pe", "matmul", [rkz, RV4], [PS[6]], PS[6][:, h * 128:(h + 1) * 128], lhsT=rkz[:, j * 128:(j + 1) * 128],
                       rhs=RV4[:, tl, h * 128:(h + 1) * 128], start=True, stop=True)
                for h in range(4):
                    j, half = h // 2, h % 2
                    pr = slice(half * 64, half * 64 + 64)
                    op("dve", "scalar_tensor_tensor", [state, cst, PS[6]], [state], out=state[pr, j, :], in0=state[pr, j, :],
                       scalar=cd_c[pr, h:h + 1], in1=PS[6][pr, h * 128:(h + 1) * 128], op0=ALU.mult, op1=ALU.add)
                op("pool", "tensor_copy", [state], [state_b], out=state_b[:], in_=state[:])

            def A_RETb_dve(g, tl):
                GT4 = GTG[g % 2]
                op("act", "copy", [PS[7]], [osb], out=osb[:], in_=PS[7][:].rearrange("p (h d) -> p h d", h=4))
                op("dve", "tensor_reduce", [osb], [gst], out=gst[:, 0:4], in_=osb[:], axis=AX.X, op=ALU.add)
                op("pool", "tensor_tensor", [osb], [osq], out=osq[:], in0=osb[:], in1=osb[:], op=ALU.mult)
                op("dve", "tensor_reduce", [osq], [gst], out=gst[:, 4:8], in_=osq[:], axis=AX.X, op=ALU.add)
                op("dve", "tensor_scalar", [gst], [gst], out=gst[:, 0:4], in0=gst[:, 0:4], scalar1=1.0 / 128, scalar2=None, op0=ALU.mult)
                op("dve", "tensor_tensor", [gst], [gst], out=gst[:, 8:12], in0=gst[:, 0:4], in1=gst[:, 0:4], op=ALU.mult)
                op("dve", "scalar_tensor_tensor", [gst], [gst], out=gst[:, 8:12], in0=gst[:, 4:8], scalar=1.0 / 128, in1=gst[:, 8:12],
                   op0=ALU.mult, op1=ALU.subtract)
                op("dve", "tensor_scalar", [gst], [gst], out=gst[:, 8:12], in0=gst[:, 8:12], scalar1=EPS, scalar2=None, op0=ALU.add)
                rsqrt(gst, gst[:, 8:12], gst, gst[:, 12:16], 4)
                op("dve", "tensor_scalar", [gst], [gst], out=gst[:, 12:16], in0=gst[:, 12:16], scalar1=0.5, scalar2=None, op0=ALU.mult)
                op("dve", "tensor_tensor", [osb, gst], [osb], out=osb[:], in0=osb[:], in1=gst[:, 0:4].unsqueeze(2).to_broadcast([128, 4, 128]), op=ALU.subtract)
                op("dve", "tensor_tensor", [osb, gst], [osb], out=osb[:], in0=osb[:], in1=gst[:, 12:16].unsqueeze(2).to_broadcast([128, 4, 128]), op=ALU.mult)
                op("pool", "tensor_tensor", [osb, GT4], [oretb], out=oretb[:], in0=osb[:].rearrange("p h d -> p (h d)"), in1=GT4[:, tl, :], op=ALU.mult)

            def A_RETb_pe(g, tl):
                qs = slice(tl * 128, (tl + 1) * 128)
                for h in range(4):
                    op("pe", "transpose", [oretb, ident_b], [PS[5]], out=psbf(5)[:, h * 128:(h + 1) * 128], in_=oretb[:, h * 128:(h + 1) * 128], identity=ident_b[:])
                op("act", "copy", [PS[5]], [oretT], out=oretT[:, :, qs], in_=psbf(5)[:, 0:512].rearrange("p (h t) -> p h t", h=4))

            A_S1(0, 0)
            for g in range(NG + 1):
                if g < NG:
                    A_pos(g)
                for tl in range(4):
                    if g < NG:
                        A_T8(g, tl)
                    nxt = g * 4 + tl + 1
                    if nxt < NG * 4:
                        A_S1(nxt // 4, nxt % 4)
                    if g >= 1:
                        A_RETa(g - 1, tl)
                    if g >= 1 and tl > 0:
                        A_RETb_pe(g - 1, tl - 1)
                    if g < NG:
                        A_MM(g, tl)
                    if g >= 1:
                        A_RETb_dve(g - 1, tl)
                    if g < NG:
                        A_table(g, tl)
                if g < NG:
                    A_FM(g)
                if g >= 1:
                    A_RETb_pe(g - 1, 3)
                    dma("sp", s_mixr, [oretT], [mixr_k], out=mixr_d[b, :, :, (g - 1) * 512:g * 512].rearrange("h p t -> p h t"), in_=oretT[:])
            fence([oretT, BIGW], pT + qT + wblk)

            def qprep(h, i):
                qsl = slice(i * 512, (i + 1) * 512)
                for c in range(2):
                    op("pe", "matmul", [wuq_a, cqnT], [PS[4]], PS[4][0:96, :], lhsT=wuq_a[:, c, h, 0:96], rhs=cqnT[:, c, qsl], start=(c == 0), stop=(c == 1))
                for c in range(2):
                    op("pe", "matmul", [wuq_a, cqnT], [PS[5]], PS[5][0:96, :], lhsT=wuq_a[:, c, h, 96:192], rhs=cqnT[:, c, qsl], start=(c == 0), stop=(c == 1))
                qt = qT[(h * NG + i) % 2]
                op("act", "copy", [PS[4]], [qt], out=qt[0:64, :], in_=PS[4][0:64, :])
                dma("sp", s_csm, [csms_k], [csm1f], out=csm1f[64:96, :], in_=csms_d[b, 0, :, qsl])
                dma("sp", s_csm, [csms_k], [csm2f], out=csm2f[64:96, :], in_=csms_d[b, 1, :, qsl])
                op("dve", "tensor_tensor", [PS[4], csm1f], [rt1], out=rt1[64:96, :], in0=PS[4][64:96, :], in1=csm1f[64:96, :], op=ALU.mult)
                op("dve", "tensor_tensor", [PS[5], csm2f], [rt2], out=rt2[64:96, :], in0=PS[5][64:96, :], in1=csm2f[64:96, :], op=ALU.mult)
                op("dve", "tensor_tensor", [rt1, rt2], [qt], out=qt[64:96, :], in0=rt1[64:96, :], in1=rt2[64:96, :], op=ALU.add)

            pend_epi = []
            for h in range(8):
                for g in range(NG):
                    op("pe", "matmul", [wk_b, ckvnT], [PS[6]], PS[6][0:64, :], lhsT=wk_b[:, h, :], rhs=ckvnT[:, g * 512:(g + 1) * 512], start=True, stop=True)
                    op("act", "copy", [PS[6]], [kT], out=kT[0:64, g * 512:(g + 1) * 512], in_=PS[6][0:64, :])
                vb = vh[h % 2]
                for t8 in range((NT + 7) // 8):
                    n8 = min(8, NT - t8 * 8)
                    for tt_ in range(n8):
                        ti = t8 * 8 + tt_
                        op("pe", "matmul", [ckvnT, wv_b], [PS[7]], PS[7][:, tt_ * 64:(tt_ + 1) * 64], lhsT=ckvnT[:, ti * 128:(ti + 1) * 128], rhs=wv_b[:, h, :],
                           start=True, stop=True)
                    op("dve", "tensor_copy", [PS[7]], [vb], out=vb[:, t8 * 8:t8 * 8 + n8, 0:64], in_=PS[7][:, 0:n8 * 64].rearrange("p (t d) -> p t d", d=64))
                qprep(h, 0)
                for i in range(NG):
                    qsl = slice(i * 512, (i + 1) * 512)
                    qt = qT[(h * NG + i) % 2]
                    if i + 1 < NG:
                        qprep(h, i + 1)
                    nk = 4 * i + 4
                    ob = 2 + ((h * NG + i) % 2)

                    def c0_of(j):
                        return 128 * (j - 4 * i) if j > 4 * i else 0

                    def qk(j):
                        c0 = c0_of(j)
                        op("pe", "matmul", [kT, qt], [PS[j % 2]], PS[j % 2][:, c0:512], lhsT=kT[0:96, j * 128:(j + 1) * 128], rhs=qt[0:96, c0:512], start=True, stop=True)
                    qk(0)
                    for j in range(nk):
                        if j + 1 < nk:
                            qk(j + 1)
                        p_ = pT[j % 2]
                        c0 = c0_of(j)
                        op("act", "activation", [PS[j % 2]], [p_], out=p_[:, c0:512], in_=PS[j % 2][:, c0:512], func=AF.Exp, scale=float(96 ** -0.5))
                        if j >= 4 * i:
                            m_ = j - 4 * i
                            op("dve", "tensor_tensor", [p_, cmask_b], [p_], out=p_[:, c0:c0 + 128], in0=p_[:, c0:c0 + 128], in1=cmask_b[:, m_, c0:c0 + 128], op=ALU.mult)
                        op("pe", "matmul", [vb, p_], [PS[ob]], PS[ob][0:65, c0:512], lhsT=vb[:, j, 0:65], rhs=p_[:, c0:512], start=(j == 0), stop=(j == nk - 1))
                        if j == 1 and pend_epi:
                            pend_epi.pop(0)()
                    def epilogue(ob=ob, h=h, qsl=qsl):
                        op("dve", "reciprocal", [PS[ob]], [rrow], out=rrow[64:65, :], in_=PS[ob][64:65, :])
                        op("pe", "matmul", [ones_f, rrow], [PS[7]], PS[7][0:64, :], lhsT=ones_f[64:65, 0:64], rhs=rrow[64:65, :], start=True, stop=True)
                        op("act", "copy", [PS[7]], [bcs], out=bcs[:], in_=PS[7][0:64, :])
                        op("dve", "tensor_tensor", [PS[ob], bcs], [oTm], out=oTm[:], in0=PS[ob][0:64, :], in1=bcs[:], op=ALU.mult)
                        dma("sp", s_mixm, [oTm], [mixm_k], out=mixm_d[b, h, :, qsl], in_=oTm[:])
                    pend_epi.append(epilogue)
                    for _ in range(per_slot):
                        if relay_state[0] < NE:
                            relayout_expert(relay_state[0])
                            relay_state[0] += 1
            while pend_epi:
                pend_epi.pop(0)()
            fence(pT + qT + wblk, [h2T, BIGW])

            fence([GTG[0], h1T, RVG[1]], c_alts)
            dma("pool", s_w, [], [wo_m], out=wo_m[:], in_=wo_d[0:512, :].rearrange("(h p) n -> p h n", p=64))
            dma("pool", s_w, [], [wo_r], out=wo_r[:], in_=wo_d[512:1024, :].rearrange("(h p) n -> p h n", p=128))
            dma("sp", s_bc, [mods_k], [g1bc], out=g1bc[:], in_=mods_d[b:b + 1, 2 * D:3 * D].partition_broadcast(128))
            dma("sp", s_bc, [mods_k], [sh2bc], out=sh2bc[:], in_=mods_d[b:b + 1, 3 * D:4 * D].partition_broadcast(128))
            dma("sp", s_bc, [mods_k], [A2bc], out=A2bc[:], in_=mods_d[b:b + 1, 4 * D:5 * D].partition_broadcast(128))
            dma("sp", s_bc, [], [n2gbc], out=n2gbc[:], in_=n2g_d.partition_broadcast(128))
            op("dve", "scalar_tensor_tensor", [A2bc, n2gbc], [A2bc], out=A2bc[:], in0=A2bc[:], scalar=1.0, in1=n2gbc[:], op0=ALU.add, op1=ALU.mult)
            def C1(ti):
                tok0 = ti * 128
                gt = b * NT + ti
                p2 = ti % 2
                x1 = x1_2[p2]
                h2 = h2_2[p2]
                h2b = h2b_2[p2]
                h2T = h2T_2[p2]
                mm_t = mm_t_2[p2]
                mr_t = mr_t_2[p2]
                pa, pbk = (0, 1) if p2 == 0 else (6, 7)
                so = 0 if p2 == 0 else 10
                xi_ = xin[ti % 2]
                dma("sp", s_xin[ti % 2], [], [xi_], out=xi_[:], in_=x_d[b, tok0:tok0 + 128, :])
                dma("sp", s_mm2[p2], [mixm_k], [mm_t], out=mm_t[:], in_=mixm_d[b, :, :, tok0:tok0 + 128].rearrange("h p t -> p h t"))
                dma("sp", s_mm2[p2], [mixr_k], [mr_t], out=mr_t[:], in_=mixr_d[b, :, :, tok0:tok0 + 128].rearrange("h p t -> p h t"))
                for nh, pbank in ((0, pa), (1, pbk)):
                    for hh in range(8):
                        op("pe", "matmul", [mm_t, wo_m], [PS[pbank]], PS[pbank][:, :], lhsT=mm_t[:, hh, :], rhs=wo_m[:, hh, nh * 512:(nh + 1) * 512], start=(hh == 0), stop=False)
                    for hh in range(4):
                        op("pe", "matmul", [mr_t, wo_r], [PS[pbank]], PS[pbank][:, :], lhsT=mr_t[:, hh, :], rhs=wo_r[:, hh, nh * 512:(nh + 1) * 512], start=False, stop=(hh == 3))
                for nh, pbank in ((0, pa), (1, pbk)):
                    op("dve", "tensor_tensor", [PS[pbank], g1bc], [x1], out=x1[:, nh * 512:(nh + 1) * 512], in0=PS[pbank][:, :], in1=g1bc[:, nh * 512:(nh + 1) * 512], op=ALU.mult)
                op("pool", "tensor_tensor", [x1, xi_], [x1], out=x1[:], in0=x1[:], in1=xi_[:], op=ALU.add)
                dma("sp", s_x1, [x1], [x1s_k], out=x1s_d[gt * 128:(gt + 1) * 128, :], in_=x1[:])
                op("act", "activation", [x1], [junk, stat], out=junk[:], in_=x1[:], func=AF.Square, accum_out=stat[:, so:so + 1])
                op("dve", "tensor_scalar", [stat], [stat], out=stat[:, so + 1:so + 2], in0=stat[:, so:so + 1], scalar1=1.0 / D, scalar2=EPS, op0=ALU.mult, op1=ALU.add)
                rsqrt(stat, stat[:, so + 1:so + 2], stat, stat[:, so + 2:so + 3], 1)
                op("dve", "scalar_tensor_tensor", [x1, stat, A2bc], [h2], out=h2[:], in0=x1[:], scalar=stat[:, so + 2:so + 3], in1=A2bc[:], op0=ALU.mult, op1=ALU.mult)
                op("pool", "tensor_tensor", [h2, sh2bc], [h2], out=h2[:], in0=h2[:], in1=sh2bc[:], op=ALU.add)
                op("act", "copy", [h2], [h2b], out=h2b[:], in_=h2[:])
                dma("sp", s_h2, [h2b], [h2s_k], out=h2s_d[gt * 128:(gt + 1) * 128, :], in_=h2b[:])
                for c in range(8):
                    pb = 2 + c // 4
                    op("pe", "transpose", [h2, cst], [PS[pb]], out=PS[pb][:, (c % 4) * 128:(c % 4 + 1) * 128], in_=h2[:, c * 128:(c + 1) * 128], identity=ident_f)
                op("act", "copy", [PS[2]], [h2T], out=h2T[:, 0:4, :], in_=PS[2][:, :].rearrange("p (c t) -> p c t", c=4))
                op("dve", "tensor_copy", [PS[3]], [h2T], out=h2T[:, 4:8, :], in_=PS[3][:, :].rearrange("p (c t) -> p c t", c=4))
                for c in range(8):
                    op("pe", "matmul", [h2T, w_rt], [PS[4]], PS[4][:, 0:36], lhsT=h2T[:, c, :], rhs=w_rt[:, c, :], start=(c == 0), stop=(c == 7))

                lgt = lgt2[ti % 2]
                op("dve", "tensor_tensor", [PS[4], b_rt], [lgt], out=lgt[:, 0:4], in0=PS[4][:, 0:4], in1=b_rt[:, 0:4], op=ALU.add)
                op("dve", "tensor_tensor", [PS[4], b_rt], [lgt], out=lgt[:, 8:40], in0=PS[4][:, 4:36], in1=b_rt[:, 4:36], op=ALU.add)

            def C2(ti):
                gt = b * NT + ti
                lgt = lgt2[ti % 2]
                m8 = m8_2[ti % 2]
                rst = rst_2[ti % 2]
                lem = lem_2[ti % 2]
                Mt = Mt_2[ti % 2]
                op("dve", "max", [lgt], [m8], out=m8[:, 0:8], in_=lgt[:, 0:8])
                op("dve", "tensor_scalar", [m8], [rst], out=rst[:, 0:1], in0=m8[:, 0:1], scalar1=-1.0, scalar2=None, op0=ALU.mult)
                op("act", "activation", [lgt, rst], [rst], out=rst[:, 8:16], in_=lgt[:, 0:8], func=AF.Exp, bias=rst[:, 0:1], scale=1.0, accum_out=rst[:, 1:2])
                op("dve", "reciprocal", [rst], [rst], out=rst[:, 2:3], in_=rst[:, 1:2])
                op("dve", "tensor_scalar", [lgt, m8], [rst], out=rst[:, 16:20], in0=lgt[:, 0:4], scalar1=m8[:, 0:1], scalar2=None, op0=ALU.is_equal)
                op("dve", "tensor_scalar", [rst], [rst], out=rst[:, 16:20], in0=rst[:, 16:20], scalar1=-1.0, scalar2=1e30, op0=ALU.add, op1=ALU.mult)
                op("dve", "tensor_tensor", [lgt, rst], [lem], out=lem[:].rearrange("p (g e) -> p g e", g=4), in0=lgt[:, 8:40].rearrange("p (g e) -> p g e", g=4),
                   in1=rst[:, 16:20].unsqueeze(2).to_broadcast([128, 4, 8]), op=ALU.add)
                op("dve", "max", [lem], [m8], out=m8[:, 8:16], in_=lem[:])
                op("dve", "tensor_scalar", [lem, m8], [OH1], out=OH1[:, gt, :], in0=lem[:], scalar1=m8[:, 8:9], scalar2=None, op0=ALU.is_equal)
                op("dve", "tensor_scalar", [lem, m8], [OH2], out=OH2[:, gt, :], in0=lem[:], scalar1=m8[:, 9:10], scalar2=None, op0=ALU.is_equal)
                op("dve", "tensor_tensor", [m8], [rst], out=rst[:, 3:4], in0=m8[:, 9:10], in1=m8[:, 8:9], op=ALU.subtract)
                op("act", "activation", [rst], [rst], out=rst[:, 4:5], in_=rst[:, 3:4], func=AF.Exp)
                op("dve", "tensor_scalar", [rst], [rst], out=rst[:, 4:5], in0=rst[:, 4:5], scalar1=1.0, scalar2=None, op0=ALU.add)
                op("dve", "reciprocal", [rst], [rst], out=rst[:, 5:6], in_=rst[:, 4:5])
                op("dve", "tensor_tensor", [rst], [GATE], out=GATE[:, gt, 0:1], in0=rst[:, 5:6], in1=rst[:, 2:3], op=ALU.mult)
                op("dve", "tensor_tensor", [rst, GATE], [GATE], out=GATE[:, gt, 1:2], in0=rst[:, 2:3], in1=GATE[:, gt, 0:1], op=ALU.subtract)
                op("pool", "tensor_tensor", [OH1, OH2], [Mt], out=Mt[:], in0=OH1[:, gt, :], in1=OH2[:, gt, :], op=ALU.add)
                op("pe", "matmul", [triu_b, Mt], [PS[5]], PS[5][:, 0:32], lhsT=triu_b[:], rhs=Mt[:], start=True, stop=False)
                op("pe", "matmul", [ones_b, Macc], [PS[5]], PS[5][:, 0:32], lhsT=ones_b[:], rhs=Macc[:], start=False, stop=True)
                op("act", "copy", [PS[5]], [CUM], out=CUM[:, gt, :], in_=PS[5][:, 0:32])
                op("pool", "tensor_tensor", [Macc, Mt], [Macc], out=Macc[:], in0=Macc[:], in1=Mt[:], op=ALU.add)


            import os as _os2
            if _os2.environ.get("K_CSKEW", "1") == "1":
                C1(0)
                for ti in range(NT):
                    if ti + 1 < NT:
                        C1(ti + 1)
                    C2(ti)
            else:
                for ti in range(NT):
                    C1(ti)
                    C2(ti)
            fence(c_alts, [P0, P1, P4c])

        op("pe", "matmul", [ones_b, Macc], [PS[5]], PS[5][:, 0:32], lhsT=ones_b[:], rhs=Macc[:], start=True, stop=True)
        op("dve", "tensor_scalar", [PS[5]], [ra], out=ra[:], in0=PS[5][:, 0:32], scalar1=1.0 / BLK, scalar2=(BLK - 1 - (BLK / 2 - 0.5)) / BLK, op0=ALU.mult, op1=ALU.add)
        op("dve", "tensor_scalar", [ra], [ra], out=ra[:], in0=ra[:], scalar1=MAGIC_RN, scalar2=None, op0=ALU.add)
        op("dve", "tensor_scalar", [ra], [pad_], out=pad_[:], in0=ra[:], scalar1=MAGIC_RN, scalar2=float(BLK), op0=ALU.subtract, op1=ALU.mult)
        op("dve", "tensor_copy", [pad_], [ra], out=ra[:], in_=pad_[:])
        cur, oth = ra, rb
        for sft in (1, 2, 4, 8, 16):
            op("dve", "tensor_copy", [cur], [oth], out=oth[:, 0:sft], in_=cur[:, 0:sft])
            op("dve", "tensor_tensor", [cur], [oth], out=oth[:, sft:32], in0=cur[:, sft:32], in1=cur[:, 0:32 - sft], op=ALU.add)
            cur, oth = oth, cur
        pend = cur
        op("dve", "tensor_tensor", [pend, pad_], [pst], out=pst[:], in0=pend[:], in1=pad_[:], op=ALU.subtract)
        a0, a1 = coff["blkstart"]
        op("dve", "tensor_tensor", [pend, cst], [cmp3], out=cmp3[:], in0=pend[:].unsqueeze(1).to_broadcast([128, NBLK, 32]),
           in1=cst[:, a0:a1].unsqueeze(2).to_broadcast([128, NBLK, 32]), op=ALU.is_le)
        op("dve", "tensor_reduce", [cmp3], [ebf], out=ebf[:], in_=cmp3[:], axis=AX.X, op=ALU.add)
        i0, i1 = coff["iotap"]
        op("dve", "tensor_scalar", [ebf], [ebf], out=ebf[:], in0=ebf[:], scalar1=31.0, scalar2=128.0, op0=ALU.min, op1=ALU.mult)
        op("dve", "tensor_scalar", [ebf, cst], [WIDX], out=WIDX[:], in0=ebf[:], scalar1=cst[:, i0:i1], scalar2=None, op0=ALU.add)
        op("pool", "tensor_tensor", [CUM, pst], [CUM], out=CUM[:], in0=CUM[:], in1=pst[:].unsqueeze(1).to_broadcast([128, NTT, 32]), op=ALU.add)
        for k_, OH in ((0, OH1), (1, OH2)):
            op("dve", "tensor_tensor", [OH, CUM], [cmpd], out=cmpd[:], in0=OH[:], in1=CUM[:], op=ALU.mult)
            op("dve", "tensor_reduce", [cmpd], [destf], out=destf[:, :, k_], in_=cmpd[:], axis=AX.X, op=ALU.add)
        op("dve", "tensor_copy", [destf], [DEST], out=DEST[:], in_=destf[:])

        xs_k = DR()
        ys_k = DR()
        h2r = h2r + [Alias(P7, 0, [128, D], BF16, own=True), Alias(P4c, 0, [128, D], BF16, own=True)]
        s_h2r = s_h2r + [SC.dma_sem("h2r2"), SC.dma_sem("h2r3")]
        fence([rqT, rkT, RVG[1], h1T, GT4, BIGW], h2r + xblk + ysb + wblk)
        for gt in range(NTT):
            hr = h2r[gt % 4]
            dma("sp", s_h2r[gt % 4], [h2s_k], [hr], out=hr[:], in_=h2s_d[gt * 128:(gt + 1) * 128, :])
            for k_ in range(2):
                SC.dma("pool", (lambda e, hr=hr, gt=gt, k_=k_: e.indirect_dma_start(
                    out=xs_d, out_offset=bass.IndirectOffsetOnAxis(ap=DEST[:, gt, k_:k_ + 1], axis=0), in_=hr[:, :], in_offset=None)),
                    s_sc, [hr.k, DEST.k], [xs_k.k], lat=6.0)

        xTb2 = [xTb, Alias(P4b, 0, [128, 2, 8, 128], BF16)]
        actb2 = [[Alias(P2a, 1024 + 512 * (2 * pq + r), [128, 256], BF16, own=True) for r in range(2)] for pq in range(2)]
        sg2 = [sg, Alias(P2a, 3072, [128, 256], F32, own=True)]
        actT2 = [Alias(P3a, 512 * r, [128, 2, 128], BF16, own=True) for r in range(2)]
        fence([g1bc, A2bc], [a_ for l_ in actb2 for a_ in l_] + sg2 + actT2)

        def stage1(blk):
            pq = blk % 2
            wb = wblk[pq]
            SC.dma("pool", (lambda e, wb=wb, blk=blk: e.indirect_dma_start(
                out=wb[:, :], out_offset=None, in_=wall_d, in_offset=bass.IndirectOffsetOnAxis(ap=WIDX[:, blk:blk + 1], axis=0))),
                s_wg[pq], [WIDX.k, wall_k.k], [wb.k], lat=14.0)
            xb_ = xblk[pq]
            dma("sp", s_xb[pq], [xs_k], [xb_], out=xb_[:], in_=xs_d[blk * BLK:(blk + 1) * BLK, :].rearrange("(r p) d -> p r d", p=128))
            xt_ = xTb2[pq]
            for r in range(2):
                for c in range(8):
                    op("pe", "transpose", [xb_, ident_b], [PS[0]], out=psbf(0)[:, c * 128:(c + 1) * 128], in_=xb_[:, r, c * 128:(c + 1) * 128], identity=ident_b[:])
                if r == 0:
                    op("act", "copy", [PS[0]], [xt_], out=xt_[:, r, :, :], in_=psbf(0)[:, 0:1024].rearrange("p (c t) -> p c t", c=8))
                else:
                    op("dve", "tensor_copy", [PS[0]], [xt_], out=xt_[:, r, :, :], in_=psbf(0)[:, 0:1024].rearrange("p (c t) -> p c t", c=8))
            for r in range(2):
                hb = 1 + 2 * pq + r
                for c in range(8):
                    op("pe", "matmul", [xt_, wb], [PS[hb]], PS[hb][:, :], lhsT=xt_[:, r, c, :], rhs=wb[:, c * 512:(c + 1) * 512], start=(c == 0), stop=(c == 7))
            for r in range(2):
                hb = 1 + 2 * pq + r
                sg_ = sg2[r]
                ab = actb2[pq][r]
                op("act", "activation", [PS[hb]], [sg_], out=sg_[:], in_=PS[hb][:, 0:256], func=AF.Tanh, scale=0.5)
                op("dve", "scalar_tensor_tensor", [sg_, PS[hb]], [sg_], out=sg_[:], in0=sg_[:], scalar=1.0, in1=PS[hb][:, 0:256], op0=ALU.add, op1=ALU.mult)
                op("dve", "scalar_tensor_tensor", [sg_, PS[hb]], [ab], out=ab[:], in0=sg_[:], scalar=0.5, in1=PS[hb][:, 256:512], op0=ALU.mult, op1=ALU.mult)

        def stage2(blk):
            pq = blk % 2
            wb = wblk[pq]
            for r in range(2):
                ab = actb2[pq][r]
                at = actT2[r]
                for fc in range(2):
                    op("pe", "transpose", [ab, ident_b], [PS[5]], out=psbf(5)[:, (2 * r + fc) * 128:(2 * r + fc + 1) * 128], in_=ab[:, fc * 128:(fc + 1) * 128], identity=ident_b[:])
                op("act", "copy", [PS[5]], [at], out=at[:], in_=psbf(5)[:, 2 * r * 128:(2 * r + 2) * 128].rearrange("p (c t) -> p c t", c=2))
            for r in range(2):
                at = actT2[r]
                yb = ysb[r]
                for nh in range(2):
                    for fc in range(2):
                        op("pe", "matmul", [at, wb], [PS[6 + nh]], PS[6 + nh][:, :], lhsT=at[:, fc, :],
                           rhs=wb[:, 4096 + fc * 1024 + nh * 512:4096 + fc * 1024 + (nh + 1) * 512], start=(fc == 0), stop=(fc == 1))
                    if nh == 0:
                        op("act", "copy", [PS[6]], [yb], out=yb[:, 0:512], in_=PS[6][:, :])
                    else:
                        op("dve", "tensor_copy", [PS[7]], [yb], out=yb[:, 512:1024], in_=PS[7][:, :])
                dma("sp", s_ys[r], [yb], [ys_k], out=ys_d[blk * BLK + r * 128:blk * BLK + (r + 1) * 128, :], in_=yb[:])

        stage1(0)
        for blk in range(NBLK):
            if blk + 1 < NBLK:
                stage1(blk + 1)
            stage2(blk)

        out_k = DR()
        fg_bc = Alias(P3b, 0, [128, D], F32)
        fence(xblk + [a_ for l_ in actb2 for a_ in l_] + sg2 + actT2, ysc + [g1bc, A2bc])
        dma("sp", s_bc, [], [fg_bc], out=fg_bc[:], in_=fg_d.partition_broadcast(128))
        s_yg2 = [SC.dma_sem("yg2_%d" % i) for i in range(2)]
        s_x1f = [SC.dma_sem("x1f%d" % i) for i in range(2)]
        h2alt = Alias(P2b, 0, [128, D], F32)
        x1alt = Alias(P5, 0, [128, D], F32)
        def F_pre(gt):
            yp = ysb if gt % 2 == 0 else ysc
            sy = s_yg if gt % 2 == 0 else s_yg2
            for k_ in range(2):
                SC.dma("pool", (lambda e, gt=gt, k_=k_, yy=yp[k_]: e.indirect_dma_start(
                    out=yy[:, :], out_offset=None, in_=ys_d, in_offset=bass.IndirectOffsetOnAxis(ap=DEST[:, gt, k_:k_ + 1], axis=0))),
                    sy[k_], [DEST.k, ys_k.k], [yp[k_].k], lat=7.0)
            xx = x1 if gt % 2 == 0 else x1alt
            dma("sp", s_x1f[gt % 2], [x1s_k], [xx], out=xx[:], in_=x1s_d[gt * 128:(gt + 1) * 128, :])

        def F_main(gt):
            b = gt // NT
            ti = gt % NT
            if ti == 0:
                dma("sp", s_bc, [mods_k], [g1bc], out=g1bc[:], in_=mods_d[b:b + 1, 5 * D:6 * D].partition_broadcast(128))
            yp = ysb if gt % 2 == 0 else ysc
            y1, y2 = yp[0], yp[1]
            hh = h2 if gt % 2 == 0 else h2alt
            xx = x1 if gt % 2 == 0 else x1alt
            sc_ = (gt % 2) * 4
            op("act", "activation", [y1, GATE], [hh], out=hh[:], in_=y1[:], func=AF.Identity, scale=GATE[:, gt, 0:1])
            op("dve", "scalar_tensor_tensor", [y2, GATE, hh], [hh], out=hh[:], in0=y2[:], scalar=GATE[:, gt, 1:2], in1=hh[:], op0=ALU.mult, op1=ALU.add)
            op("dve", "tensor_tensor", [hh, g1bc], [hh], out=hh[:], in0=hh[:], in1=g1bc[:], op=ALU.mult)
            op("pool", "tensor_tensor", [hh, xx], [hh], out=hh[:], in0=hh[:], in1=xx[:], op=ALU.add)
            op("act", "activation", [hh], [junk, stat], out=junk[:], in_=hh[:], func=AF.Square, accum_out=stat[:, sc_:sc_ + 1])
            op("dve", "tensor_scalar", [stat], [stat], out=stat[:, sc_ + 1:sc_ + 2], in0=stat[:, sc_:sc_ + 1], scalar1=1.0 / D, scalar2=EPS, op0=ALU.mult, op1=ALU.add)
            rsqrt(stat, stat[:, sc_ + 1:sc_ + 2], stat, stat[:, sc_ + 2:sc_ + 3], 1)
            xo = xin[gt % 2]
            op("dve", "scalar_tensor_tensor", [hh, stat, fg_bc], [xo], out=xo[:], in0=hh[:], scalar=stat[:, sc_ + 2:sc_ + 3], in1=fg_bc[:], op0=ALU.mult, op1=ALU.mult)
            dma("sp", s_xin[gt % 2], [xo], [out_k], out=out_d[b, ti * 128:(ti + 1) * 128, :], in_=xo[:])

        F_pre(0)
        for gt in range(NTT):
            if gt + 1 < NTT:
                F_pre(gt + 1)
            F_main(gt)
        SC.wait_all("sp", [out_k.k])
        SC.emit()
    return nc


_CACHE = {}


def kernel(**inputs):
    NCORES = 8
    x = np.asarray(inputs["x"], dtype=np.float32)
    B, S, _ = x.shape
    NSEQ = B // NCORES
    key = (S, NSEQ)
    if key not in _CACHE:
        _CACHE[key] = build_nc(S, NSEQ)
    nc = _CACHE[key]
    T = S * NSEQ
    NBLK = (2 * T + NE * (BLK - 1) + BLK - 1) // BLK
    cst_np, _ = make_consts(NBLK)
    f = lambda k: np.ascontiguousarray(np.asarray(inputs[k], dtype=np.float32))
    shared = {
        "w_ada": f("w_ada")[0], "b_ada": f("b_ada"), "norm1_g": f("norm1_g"), "w_in": f("w_in")[0],
        "q_norm_g": f("q_norm_g"), "w_uq": f("w_uq")[0], "kv_norm_g": f("kv_norm_g"), "w_ukv": f("w_ukv")[0],
        "w_o": f("w_o")[0], "norm2_g": f("norm2_g"), "w_gr": f("w_gr")[0], "b_gr": f("b_gr"),
        "w_er": f("w_er")[0].reshape(D, 32), "b_er": f("b_er").reshape(1, 32), "w1": f("w1")[0], "w3": f("w3")[0],
        "w2": f("w2")[0], "final_g": f("final_g").reshape(1, D), "cst": cst_np,
    }
    c = f("c")
    pos = np.ascontiguousarray(np.asarray(inputs["positions"], dtype=np.int32))
    in_maps = []
    for i in range(NCORES):
        m = dict(shared)
        m["x"] = np.ascontiguousarray(x[i * NSEQ:(i + 1) * NSEQ])
        m["c"] = np.ascontiguousarray(c[i * NSEQ:(i + 1) * NSEQ])
        m["positions"] = np.ascontiguousarray(pos[i * NSEQ:(i + 1) * NSEQ])
        in_maps.append(m)
    res = run_bass_kernel_spmd(nc, in_maps, core_ids=list(range(NCORES)))
    return np.concatenate([np.asarray(r["out"]) for r in res.results], axis=0).astype(np.float32)
```

```python
import math
import numpy as np
from contextlib import ExitStack
import concourse.bass as bass
import concourse.mybir as mybir
from concourse.bass_utils import run_bass_kernel_spmd

F32 = mybir.dt.float32
BF16 = mybir.dt.bfloat16
I32 = mybir.dt.int32
ALU = mybir.AluOpType
AF = mybir.ActivationFunctionType
AX = mybir.AxisListType

ENGS = ("pe", "act", "dve", "pool", "sp")
D = 1024
NE = 32
BLK = 256
EPS = 1e-6
MAGIC_RN = 12582912.0
TWO_PI = float(2 * np.pi)


class Tk:
    __slots__ = ("w", "r", "acc", "wd")

    def __init__(self, acc=False):
        self.w = None
        self.r = []
        self.acc = acc
        self.wd = {}


class Sched:
    def __init__(self, nc, stack):
        self.nc = nc
        self.stack = stack
        self.cnt = {}
        self.sems = {}
        for e in ENGS:
            self._mksem("E_" + e)
        self.nd = 0
        self.all = []
        self.tok2op = {}

    def _mksem(self, key):
        self.sems[key] = self.stack.enter_context(self.nc.semaphore(key))
        self.cnt[key] = 0
        return key

    def dma_sem(self, name=""):
        self.nd += 1
        return self._mksem("D%d_%s" % (self.nd, name))

    def _deps(self, reads, writes):
        deps = set()

        def add(tok):
            if tok is None:
                return
            k, v = tok
            if k[0] == "D":
                v = self.cnt[k]
            deps.add((k, v))
        for t in reads:
            add(t.w)
            if t.acc:
                for kv in t.wd.items():
                    add(kv)
        for t in writes:
            if t.acc:
                continue
            add(t.w)
            for tok in t.r:
                add(tok)
        return deps

    def _commit(self, tok, reads, writes):
        for t in reads:
            if not t.acc:
                t.r.append(tok)
        for t in writes:
            if t.acc:
                if t.wd.get(tok[0], 0) < tok[1]:
                    t.wd[tok[0]] = tok[1]
            else:
                t.w = tok
                t.r = []

    def op(self, eng, fn, reads=(), writes=(), cost=0.5):
        deps = self._deps(reads, writes)
        key = "E_" + eng
        self.cnt[key] += 1
        tok = (key, self.cnt[key])
        self.tok2op[tok] = len(self.all)
        self.all.append(dict(eng=eng, fn=fn, dma=False, tok=tok, deps=deps, cost=cost, lat=0.0))
        self._commit(tok, reads, writes)

    def dma(self, eng, fn, sem, reads=(), writes=(), lat=4.0):
        if eng == "pool":
            if sem + "_p" not in self.sems:
                self._mksem(sem + "_p")
            sem = sem + "_p"
        deps = self._deps(reads, writes)
        self.cnt[sem] += 16
        tok = (sem, self.cnt[sem])
        self.tok2op[tok] = len(self.all)
        self.all.append(dict(eng=eng, fn=fn, dma=True, tok=tok, deps=deps, cost=(1.2 if eng == "pool" else 0.12), lat=lat))
        self._commit(tok, reads, writes)

    def wait_all(self, eng, tks):
        deps = self._deps(tks, ())
        self.all.append(dict(eng=eng, fn=None, dma=False, tok=None, deps=deps, cost=0.0, lat=0.0))

    def _schedule(self, W=320):
        ops = self.all
        n = len(ops)
        prod = [None] * n
        dependents = [[] for _ in range(n)]
        ndeps = [0] * n
        for i, o in enumerate(ops):
            ps = set()
            for tok in o["deps"]:
                j = self.tok2op.get(tok)
                if j is not None:
                    ps.add(j)
            prod[i] = ps
            ndeps[i] = len(ps)
            for j in ps:
                dependents[j].append(i)
        pending = {e: [] for e in ENGS}
        for i, o in enumerate(ops):
            pending[o["eng"]].append(i)
        head = {e: 0 for e in ENGS}
        done = [False] * n
        comp = [0.0] * n
        ready = [0.0] * n
        free = {e: 0.0 for e in ENGS}
        semmax = {}
        order = {e: [] for e in ENGS}
        left = n
        while left:
            best = None
            for e in ENGS:
                lst = pending[e]
                h = head[e]
                while h < len(lst) and done[lst[h]]:
                    h += 1
                head[e] = h
                if h >= len(lst):
                    continue
                seen_sems = set()
                cnt = 0
                k = h
                fe = free[e]
                while k < len(lst) and cnt < W:
                    i = lst[k]
                    k += 1
                    if done[i]:
                        continue
                    cnt += 1
                    o = ops[i]
                    if o["fn"] is None:
                        if cnt > 1:
                            continue
                    elif o["dma"]:
                        sk_ = o["tok"][0]
                        if sk_ in seen_sems:
                            continue
                        seen_sems.add(sk_)
                    if ndeps[i]:
                        continue
                    st = ready[i] if ready[i] > fe else fe
                    if best is None or st < best[0] or (st == best[0] and i < best[1]):
                        best = (st, i, e)
                    if st <= fe:
                        break
            st, i, e = best
            o = ops[i]
            done[i] = True
            left -= 1
            order[e].append(i)
            fin = st + o["cost"]
            free[e] = fin
            c = fin + o["lat"]
            if o["dma"]:
                sk = o["tok"][0]
                if semmax.get(sk, 0.0) > c:
                    c = semmax[sk]
                semmax[sk] = c
            comp[i] = c
            for d in dependents[i]:
                ndeps[d] -= 1
                if comp[i] > ready[d]:
                    ready[d] = comp[i]
        self.sim_time = max(free.values())
        return order

    def emit(self):
        import os
        ops = self.all
        if os.environ.get("K_REORDER", "1") == "1":
            order = self._schedule()
        else:
            order = {e: [] for e in ENGS}
            for i, o in enumerate(ops):
                order[o["eng"]].append(i)
        newtok = {}
        for e in ENGS:
            c = 0
            for i in order[e]:
                o = ops[i]
                if o["fn"] is not None and not o["dma"]:
                    c += 1
                    newtok[o["tok"]] = ("E_" + e, c)
        plan = {e: [] for e in ENGS}
        needed = {}
        for e in ENGS:
            wd = {}
            for i in order[e]:
                o = ops[i]
                mx = {}
                for tok in o["deps"]:
                    k, v = newtok.get(tok, tok)
                    if mx.get(k, 0) < v:
                        mx[k] = v
                waits = []
                for k, v in mx.items():
                    if wd.get(k, 0) < v:
                        wd[k] = v
                        waits.append((k, v))
                        if k[0] == "E":
                            needed.setdefault(k, set()).add(v)
                plan[e].append((waits, o))
        rank = {k: {v: r + 1 for r, v in enumerate(sorted(vs))} for k, vs in needed.items()}
        sems = self.sems

        def run(name, eng):
            for waits, o in plan[name]:
                for k, v in waits:
                    eng.wait_ge(sems[k], rank[k][v] if k[0] == "E" else v)
                if o["fn"] is not None:
                    ins = o["fn"](eng)
                    if o["dma"]:
                        ins.then_inc(sems[o["tok"][0]], 16)
                    else:
                        nt = newtok[o["tok"]]
                        if nt[1] in needed.get(nt[0], ()):
                            ins.then_inc(sems[nt[0]], 1)
        with self.nc.Block() as block:
            @block.tensor
            def _(e):
                run("pe", e)

            @block.scalar
            def _(e):
                run("act", e)

            @block.vector
            def _(e):
                run("dve", e)

            @block.gpsimd
            def _(e):
                run("pool", e)

            @block.sync
            def _(e):
                run("sp", e)


def make_consts(nblk):
    H = 4
    gam = 1.0 - 2.0 ** (-5.0 - np.arange(H))
    lg = np.log(gam)
    p = np.arange(128)
    cols = {}
    cols["ident"] = np.eye(128, dtype=np.float64)
    cols["triu"] = (p[:, None] < p[None, :]).astype(np.float64)
    cm = np.zeros((128, 4, 512))
    q = np.arange(512)
    for m in range(4):
        cm[:, m, :] = ((128 * m + p)[:, None] <= q[None, :])
    cmask_np = cm.reshape(128, -1)
    dm = np.zeros((128, 4, 128))
    for h in range(4):
        d = p[None, :] - p[:, None]
        dm[:, h, :] = np.where(d >= 0, np.exp(np.maximum(d, 0) * lg[h]), 0.0)
    cols["dmaskT"] = dm.reshape(128, -1)
    xi = np.zeros((128, 2, 128))
    for j in range(2):
        for half in range(2):
            h = 2 * j + half
            xi[half * 64:(half + 1) * 64, j, :] = np.exp((p + 1.0) * lg[h])[None, :]
    cols["xi"] = xi.reshape(128, -1)
    zt = np.zeros((128, 4, 64))
    for h in range(4):
        zt[:, h, :] = np.exp((127.0 - p) * lg[h])[:, None]
    cols["zeta"] = zt.reshape(128, -1)
    rp = np.zeros((128, 6))
    jr = p % 32
    rp[:, 0] = 10000.0 ** (-(jr / 32.0))
    blk64 = (p % 64) // 32
    rp[:, 1] = np.where(blk64 == 0, np.pi / 2, 0.0)
    rp[:, 2] = np.where(blk64 == 0, np.pi, np.pi / 2)
    jm = (p - 64) % 16
    rp[:, 3] = 10000.0 ** (-(jm / 16.0))
    b16 = ((p - 64) // 16) % 2
    rp[:, 4] = np.where(b16 == 0, np.pi / 2, 0.0)
    rp[:, 5] = np.where(b16 == 0, np.pi, np.pi / 2)
    cols["rp"] = rp
    cols["blkstart"] = np.broadcast_to((np.arange(nblk) * float(BLK))[None, :], (128, nblk))
    cols["iotap"] = p[:, None].astype(np.float64)
    cols["cd"] = np.broadcast_to(np.exp(128.0 * lg)[None, :], (128, 4))
    cols["cmask"] = cmask_np
    off = {}
    o = 0
    arrs = []
    for k, v in cols.items():
        off[k] = (o, o + v.shape[1])
        o += v.shape[1]
        arrs.append(v)
    return np.concatenate(arrs, axis=1).astype(np.float32), off


def build_nc(S, NSEQ, dbg=None):
    T = S * NSEQ
    NT = S // 128
    NTT = T // 128
    NG = S // 512
    NBLK = (2 * T + NE * (BLK - 1) + BLK - 1) // BLK
    PT = NBLK * BLK
    cst_np, coff = make_consts(NBLK)
    NC = cst_np.shape[1]
    nc = bass.Bass("TRN2", target_bir_lowering=False)

    def din(name, shape, dt=F32):
        return nc.dram_tensor(name, list(shape), dt, kind="ExternalInput").ap()

    def dscr(name, shape, dt):
        return nc.dram_tensor(name, list(shape), dt, kind="Internal").ap()

    x_d = din("x", [NSEQ, S, D])
    c_d = din("c", [NSEQ, D])
    pos_d = din("positions", [NSEQ, S], I32)
    wada_d = din("w_ada", [D, 6 * D])
    bada_d = din("b_ada", [1, 6 * D])
    n1g_d = din("norm1_g", [1, D])
    win_d = din("w_in", [D, 1952])
    qng_d = din("q_norm_g", [1, 256])
    wuq_d = din("w_uq", [256, 768])
    kvng_d = din("kv_norm_g", [1, 128])
    wukv_d = din("w_ukv", [128, 1024])
    wo_d = din("w_o", [D, D])
    n2g_d = din("norm2_g", [1, D])
    wgr_d = din("w_gr", [D, 4])
    bgr_d = din("b_gr", [1, 4])
    wer_d = din("w_er", [D, 32])
    ber_d = din("b_er", [1, 32])
    w1_d = din("w1", [NE, D, 256])
    w3_d = din("w3", [NE, D, 256])
    w2_d = din("w2", [NE, 256, D])
    fg_d = din("final_g", [1, D])
    cst_d = din("cst", [128, NC])
    out_d = nc.dram_tensor("out", [NSEQ, S, D], F32, kind="ExternalOutput").ap()

    mods_d = dscr("mods", [NSEQ, 6 * D], F32)
    mixm_d = dscr("mixm", [NSEQ, 8, 64, S], BF16)
    mixr_d = dscr("mixr", [NSEQ, 4, 128, S], BF16)
    x1s_d = dscr("x1s", [T, D], F32)
    h2s_d = dscr("h2s", [T, D], BF16)
    xs_d = dscr("xs", [PT, D], BF16)
    ys_d = dscr("ys", [PT, D], BF16)
    wall_d = dscr("wall", [NE * 128, 6144], BF16)
    csms_d = dscr("csms", [NSEQ, 2, 32, S], F32)
    dbg_out = {}
    if dbg:
        for name, shape in dbg.items():
            dbg_out[name] = nc.dram_tensor("dbg_" + name, list(shape), F32, kind="ExternalOutput").ap()

    st = ExitStack()
    with st:
        SC = Sched(nc, st)

        class Buf:
            def __init__(self, name, shape, dt, psum=False):
                if psum:
                    self.t = st.enter_context(nc.psum_tensor(name, list(shape), dt))
                else:
                    self.t = st.enter_context(nc.sbuf_tensor("sb_" + name, list(shape), dt))
                self.k = Tk()

            def __getitem__(self, idx):
                return self.t[idx]

        def sb(name, shape, dt=F32):
            return Buf(name, shape, dt)

        class Alias:
            def __init__(self, parent, off, shape, dt, own=False):
                n = 1
                for d_ in shape[1:]:
                    n *= d_
                nb = n * (4 if dt in (F32, I32) else 2)
                a = parent.t[0:shape[0], off // 4:(off + nb) // 4]
                v = a if dt == F32 else a.bitcast(dt)
                if len(shape) == 3:
                    v = v.rearrange("p (a b) -> p a b", a=shape[1])
                elif len(shape) == 4:
                    v = v.rearrange("p (a b c) -> p a b c", a=shape[1], b=shape[2])
                self.v = v
                self.k = Tk() if own else parent.k

            def __getitem__(self, idx):
                return self.v[idx]

        def _fsz(ap):
            try:
                return float(ap.free_size())
            except Exception:
                return 256.0

        def op(eng, method, reads, writes, *a, **kw):
            if eng == "pe":
                if method == "matmul":
                    cost = 0.31 + _fsz(kw["rhs"]) / 1200.0
                else:
                    cost = 0.42
            else:
                o_ = kw.get("out") if kw.get("out") is not None else (a[0] if a else None)
                f_ = _fsz(o_) if o_ is not None else 64.0
                if eng == "dve":
                    cost = 0.12 + f_ / 1100.0
                elif eng == "act":
                    cost = 0.28 + f_ / 1200.0
                else:
                    cost = 0.7 + f_ / 900.0
            SC.op(eng, lambda e: getattr(e, method)(*a, **kw), [b.k for b in reads], [b.k for b in writes], cost=cost)
            if kw.get("accum_out") is not None:
                SC.op(eng, lambda e: e.copy(out=adum[0:1, 0:2], in_=adum[0:1, 2:4]), [], [b.k for b in writes] + [adum.k], cost=0.2)

        def dma(eng, sem, reads, writes, **kw):
            try:
                nb = float(kw["out"].nbytes())
            except Exception:
                nb = 65536.0
            SC.dma(eng, lambda e: e.dma_start(**kw), sem, [b.k for b in reads], [b.k for b in writes], lat=2.5 + nb / 150e3)

        class DR:
            def __init__(self):
                self.k = Tk(acc=True)

        PS = [Buf("ps%d" % i, [128, 512], F32, psum=True) for i in range(8)]
        adum = sb("adum", [128, 4])
        SC.op("dve", lambda e: e.memset(adum[:], 0.0), [], [adum.k])

        def psbf(i):
            return PS[i].t[:].bitcast(BF16)

        NC0 = coff["cmask"][0]
        cst = sb("cst", [128, NC0])
        s_c = SC.dma_sem("cst")
        dma("sp", s_c, [], [cst], out=cst[:], in_=cst_d[:, 0:NC0])

        def cc(name):
            a, b = coff[name]
            return cst[:, a:b]
        ident_f = cc("ident")
        ident_b = sb("ident_b", [128, 128], BF16)
        triu_b = sb("triu_b", [128, 128], BF16)
        ones_b = sb("ones_b", [128, 128], BF16)
        ones_f = sb("ones_f", [128, 128], F32)
        cmask_b = sb("cmask_b", [128, 4, 512], BF16)
        op("dve", "tensor_copy", [cst], [ident_b], out=ident_b[:], in_=ident_f)
        op("dve", "tensor_copy", [cst], [triu_b], out=triu_b[:], in_=cc("triu"))
        op("dve", "memset", [], [ones_b], ones_b[:], 1.0)
        op("dve", "memset", [], [ones_f], ones_f[:], 1.0)
        dma("pool", s_c, [], [cmask_b], out=cmask_b[:].rearrange("p a b -> p (a b)"), in_=cst_d[:, NC0:NC0 + 2048])
        dmaskT = cc("dmaskT").rearrange("p (h q) -> p h q", h=4)
        xi_c = cc("xi").rearrange("p (j q) -> p j q", j=2)
        zeta_c = cc("zeta")
        rp = cc("rp")
        cd_c = cc("cd")

        nhalf = sb("nhalf", [128, 16])
        op("pool", "memset", [], [nhalf], nhalf[:], -0.5)
        fdum = sb("fdum", [128, 2])

        rs_i = sb("rs_i", [128, 16], I32)
        rs_t = sb("rs_t", [128, 16], F32)
        import os as _os
        USE_POW = _os.environ.get("K_POW", "1") == "1"

        def rsqrt(vbuf, vap, outbuf, outap, n):
            if USE_POW:
                op("pool", "tensor_tensor", [vbuf, nhalf], [outbuf], out=outap, in0=vap, in1=nhalf[:, 0:n], op=ALU.pow)
                return
            yi = rs_i[:, 0:n]
            y = yi.bitcast(F32)
            tt = rs_t[:, 0:n]
            op("dve", "tensor_single_scalar", [vbuf], [rs_i], out=yi, in_=vap.bitcast(I32), scalar=1, op=ALU.arith_shift_right)
            op("dve", "tensor_scalar", [rs_i], [rs_i], out=yi, in0=yi, scalar1=-1.0, scalar2=float(0x5f3759df), op0=ALU.mult, op1=ALU.add)
            for it in range(3):
                op("dve", "tensor_tensor", [rs_i], [rs_t], out=tt, in0=y, in1=y, op=ALU.mult)
                op("dve", "tensor_tensor", [rs_t, vbuf], [rs_t], out=tt, in0=tt, in1=vap, op=ALU.mult)
                op("dve", "tensor_scalar", [rs_t], [rs_t], out=tt, in0=tt, scalar1=-0.5, scalar2=1.5, op0=ALU.mult, op1=ALU.add)
                if it < 2:
                    op("dve", "tensor_tensor", [rs_t, rs_i], [rs_i], out=y, in0=y, in1=tt, op=ALU.mult)
                else:
                    op("dve", "tensor_tensor", [rs_t, rs_i], [outbuf], out=outap, in0=y, in1=tt, op=ALU.mult)

        def fence(frm, to):
            SC.op("pool", lambda e: e.memset(fdum[0:1, 0:1], 0.0), [], [b_.k for b_ in frm] + [b_.k for b_ in to] + [fdum.k])

        s_m = SC.dma_sem("mods")
        mods_k = DR()
        cT = sb("cT", [128, 8, NSEQ])
        cTe = sb("cTe", [128, 8, NSEQ])
        siluT = sb("siluT", [128, 8, NSEQ], BF16)
        for b0 in range(NSEQ):
            dma("sp", s_m, [], [cT], out=cT[:, :, b0], in_=c_d[b0:b0 + 1, :].rearrange("o (c p) -> p (o c)", p=128), allow_slow_non_contiguous=True)
        op("act", "activation", [cT], [cTe], out=cTe[:], in_=cT[:], func=AF.Exp, scale=-1.0)
        op("dve", "tensor_scalar", [cTe], [cTe], out=cTe[:], in0=cTe[:], scalar1=1.0, scalar2=None, op0=ALU.add)
        op("dve", "reciprocal", [cTe], [cTe], out=cTe[:], in_=cTe[:])
        op("dve", "tensor_tensor", [cTe, cT], [siluT], out=siluT[:], in0=cTe[:], in1=cT[:], op=ALU.mult)
        BIGW = sb("BIGW", [128, 12448])
        P0 = sb("P0", [128, 2048])
        wa = [Alias(BIGW, 0, [128, 8, 512], BF16, own=True), Alias(BIGW, 8192, [128, 8, 512], BF16, own=True)]
        s_wa = [SC.dma_sem("wa%d" % i) for i in range(2)]
        ba = sb("ba", [NSEQ, 512])
        mrow = sb("mrow", [NSEQ, 512])
        s_ba = SC.dma_sem("ba")
        for j in range(12):
            w = wa[j % 2]
            dma("pool", s_wa[j % 2], [], [w], out=w[:], in_=wada_d[:, j * 512:(j + 1) * 512].rearrange("(c p) n -> p c n", p=128))
            dma("sp", s_ba, [], [ba], out=ba[:], in_=bada_d[:, j * 512:(j + 1) * 512].partition_broadcast(NSEQ))
            for k in range(8):
                op("pe", "matmul", [siluT, w], [PS[0]], PS[0][0:NSEQ, :], lhsT=siluT[:, k, :], rhs=w[:, k, :], start=(k == 0), stop=(k == 7))
            op("dve", "tensor_tensor", [PS[0], ba], [mrow], out=mrow[:], in0=PS[0][0:NSEQ, :], in1=ba[:], op=ALU.add)
            dma("sp", s_m, [mrow], [mods_k], out=mods_d[:, j * 512:(j + 1) * 512], in_=mrow[:])

        wblk = [Alias(BIGW, 0, [128, 6144], BF16, own=True), Alias(BIGW, 12288, [128, 6144], BF16, own=True)]
        s_wl = SC.dma_sem("wl")
        s_ws = SC.dma_sem("ws")
        wall_k = DR()

        def relayout_expert(e):
            stg = wblk[e % 2]
            v13 = stg[:, 0:4096].rearrange("p (c f) -> p c f", c=8)
            dma("pool", s_wl, [], [stg], out=v13[:, :, 0:256], in_=w1_d[e].rearrange("(c p) f -> p c f", p=128))
            dma("pool", s_wl, [], [stg], out=v13[:, :, 256:512], in_=w3_d[e].rearrange("(c p) f -> p c f", p=128))
            dma("pool", s_wl, [], [stg], out=stg[:, 4096:6144].rearrange("p (c f) -> p c f", c=2),
                in_=w2_d[e].rearrange("(c p) f -> p c f", p=128))
            dma("pool", s_ws, [stg], [wall_k], out=wall_d[e * 128:(e + 1) * 128, :], in_=stg[:])
        n_slots = NSEQ * 8 * NG
        per_slot = (NE + n_slots - 1) // n_slots
        relay_state = [0]

        fence(wa, [BIGW])
        s_w = SC.dma_sem("w")
        NFM = 8 * 128 + 2 * 96
        w_fm = Alias(BIGW, 0, [128, 8, NFM], BF16)
        w_tm = Alias(BIGW, 19456, [128, 8, 1408], BF16)
        wst = [Alias(BIGW, 41984, [128, 1952], F32)] * 2
        s_wst = [SC.dma_sem("wst%d" % i) for i in range(2)]
        wuq_a = sb("wuq_a", [128, 2, 8, 192], BF16)
        STG = P0
        wuq_s = Alias(STG, 0, [128, 2, 768], F32)
        qng_c = sb("qng_c", [128, 2])
        dma("sp", s_w, [], [wuq_s], out=wuq_s[:], in_=wuq_d.rearrange("(c p) n -> p c n", p=128))
        dma("sp", s_w, [], [qng_c], out=qng_c[:], in_=qng_d.rearrange("o (c p) -> p (o c)", p=128), allow_slow_non_contiguous=True)
        op("pool", "memset", [], [wuq_a], wuq_a[:], 0.0)
        for c in range(2):
            s4 = wuq_s[:, c, :].rearrange("p (h f) -> p h f", h=8)
            op("dve", "tensor_scalar", [wuq_s, qng_c], [wuq_a], out=wuq_a[:, c, :, 0:64], in0=s4[:, :, 0:64],
               scalar1=qng_c[:, c:c + 1], scalar2=None, op0=ALU.mult)
            for ab in range(2):
                dst = wuq_a[:, c, :, ab * 96 + 64:ab * 96 + 96].rearrange("p h (dup j) -> p h dup j", dup=2)
                src = s4[:, :, 64 + ab * 16:64 + ab * 16 + 16].unsqueeze(2).to_broadcast([128, 8, 2, 16])
                op("dve", "tensor_scalar", [wuq_s, qng_c], [wuq_a], out=dst, in0=src,
                   scalar1=qng_c[:, c:c + 1], scalar2=None, op0=ALU.mult)
        wukv_s = Alias(STG, 0, [128, 1024], F32)
        kvng_c = sb("kvng_c", [128, 1])
        wk_b = sb("wk_b", [128, 8, 64], BF16)
        wv_b = sb("wv_b", [128, 8, 64], BF16)
        dma("sp", s_w, [], [wukv_s], out=wukv_s[:], in_=wukv_d)
        dma("sp", s_w, [], [kvng_c], out=kvng_c[:], in_=kvng_d.rearrange("o p -> p o"), allow_slow_non_contiguous=True)
        s3 = wukv_s[:].rearrange("p (h f) -> p h f", h=8)
        op("dve", "tensor_scalar", [wukv_s, kvng_c], [wk_b], out=wk_b[:], in0=s3[:, :, 0:64], scalar1=kvng_c[:, 0:1], scalar2=None, op0=ALU.mult)
        op("dve", "tensor_scalar", [wukv_s, kvng_c], [wv_b], out=wv_b[:], in0=s3[:, :, 64:128], scalar1=kvng_c[:, 0:1], scalar2=None, op0=ALU.mult)
        wo_m = Alias(BIGW, 0, [64, 8, D], BF16)
        wo_r = Alias(BIGW, 16384, [128, 4, D], BF16)
        w_rt = sb("w_rt", [128, 8, 36])
        b_rt = sb("b_rt", [128, 36])
        dma("sp", s_w, [], [w_rt], out=w_rt[:, :, 0:4], in_=wgr_d.rearrange("(c p) n -> p c n", p=128), allow_slow_non_contiguous=True)
        dma("sp", s_w, [], [w_rt], out=w_rt[:, :, 4:36], in_=wer_d.rearrange("(c p) n -> p c n", p=128), allow_slow_non_contiguous=True)
        dma("sp", s_w, [], [b_rt], out=b_rt[:, 0:4], in_=bgr_d.partition_broadcast(128))
        dma("sp", s_w, [], [b_rt], out=b_rt[:, 4:36], in_=ber_d.partition_broadcast(128))
        n1g_c = sb("n1g_c", [128, 8])
        dma("sp", s_w, [], [n1g_c], out=n1g_c[:], in_=n1g_d.rearrange("o (c p) -> p (o c)", p=128), allow_slow_non_contiguous=True)

        OH1 = sb("OH1", [128, NTT, 32], BF16)
        OH2 = sb("OH2", [128, NTT, 32], BF16)
        CUM = sb("CUM", [128, NTT, 32])
        GATE = sb("GATE", [128, NTT, 2])
        Macc = sb("Macc", [128, 32], BF16)
        op("pool", "memset", [], [Macc], Macc[:], 0.0)

        cqnT = sb("cqnT", [128, 2, S], BF16)
        ckvnT = sb("ckvnT", [128, S], BF16)
        kT = sb("kT", [96, S], BF16)
        s_csm = SC.dma_sem("csm")
        csms_k = DR()
        xin = [sb("xin%d" % i, [128, D]) for i in range(2)]
        s_xin = [SC.dma_sem("xin%d" % i) for i in range(2)]
        junk = sb("junk", [128, D], BF16)
        stat = sb("stat", [128, 16])
        xsb = sb("xsb", [128, D], BF16)
        xsb2 = [xsb, sb("xsb1", [128, D], BF16)]
        P1 = sb("P1", [128, 2048])
        h1T = Alias(P1, 0, [128, 8, 512], BF16)
        P2b = sb("P2b", [128, 1024])
        posi = Alias(P2b, 0, [128, 512], I32)
        posf = Alias(P2b, 2048, [128, 512], F32)
        s_pos = SC.dma_sem("pos")
        P2a = sb("P2a", [128, 1024])
        targ = Alias(P2a, 0, [128, 512], F32)
        ttmp = Alias(P2a, 2048, [128, 512], F32)
        P3a = sb("P3a", [128, 1024])
        csr1 = Alias(P3a, 0, [128, 512], F32)
        csr2 = Alias(P3a, 2048, [128, 512], F32)
        P3b = sb("P3b", [128, 1024])
        csm1f = Alias(P3b, 0, [96, 512], F32)
        csm2f = Alias(P3b, 2048, [96, 512], F32)
        colv = sb("colv", [128, 8, 4])
        s_col = SC.dma_sem("col")
        P6 = sb("P6", [128, 1024])
        P7 = sb("P7", [128, 512])
        rqT = Alias(P6, 0, [128, 2, 512], BF16)
        rqxT = Alias(P6, 2048, [128, 2, 512], BF16)
        rkT = Alias(P7, 0, [128, 2, 512], BF16)
        P4a = sb("P4a", [128, 1024])
        rt1 = Alias(P4a, 0, [128, 512], F32)
        rt2 = Alias(P4a, 2048, [128, 512], F32)
        cqn = sb("cqn", [128, 384], BF16)
        P4b = sb("P4b", [128, 1024])
        P4c = sb("P4c", [128, 1024])
        RVG = [Alias(P4b, 0, [128, 4, 512], BF16), Alias(P4c, 0, [128, 4, 512], BF16)]
        GTG = [Alias(P0, 0, [128, 4, 512], BF16), Alias(P0, 4096, [128, 4, 512], BF16)]
        GT4 = GTG[0]
        gsg = sb("gsg", [128, 512])
        rkz = sb("rkz", [128, 256], BF16)
        sdT = sb("sdT", [128, 4, 128], BF16)
        state = sb("state", [128, 2, 128])
        state_b = sb("state_b", [128, 2, 128], BF16)
        P8 = sb("P8", [128, 1024])
        osb = Alias(P8, 0, [128, 4, 128], F32)
        osq = Alias(P8, 2048, [128, 4, 128], F32)
        gst = sb("gst", [128, 16])
        oretb = sb("oretb", [128, 512], BF16)
        P5 = sb("P5", [128, 1024])
        oretT = Alias(P5, 0, [128, 4, 512], BF16)
        s_mixr = SC.dma_sem("mixr")
        mixr_k = DR()
        mixm_k = DR()

        def rope_table(dst, dstap, prow, invf_col, ph_col):
            a = targ[prow, :]
            b = ttmp[prow, :]
            op("dve", "tensor_scalar", [posf, cst], [targ], out=a, in0=posf[prow, :], scalar1=rp[prow, invf_col:invf_col + 1],
               scalar2=rp[prow, ph_col:ph_col + 1], op0=ALU.mult, op1=ALU.add)
            op("dve", "tensor_scalar", [targ], [ttmp], out=b, in0=a, scalar1=1.0 / TWO_PI, scalar2=MAGIC_RN, op0=ALU.mult, op1=ALU.add)
            op("dve", "tensor_scalar", [ttmp], [ttmp], out=b, in0=b, scalar1=MAGIC_RN, scalar2=-TWO_PI, op0=ALU.subtract, op1=ALU.mult)
            op("dve", "tensor_tensor", [ttmp, targ], [targ], out=a, in0=a, in1=b, op=ALU.add)
            op("dve", "tensor_scalar", [targ], [targ], out=a, in0=a, scalar1=-3.1415925, scalar2=3.1415925, op0=ALU.max, op1=ALU.min)
            op("act", "activation", [targ], [dst], out=dstap, in_=a, func=AF.Sin)


        vh = [sb("vh0", [128, NT, 65], BF16)] * 2
        for i in range(1):
            op("pool", "memset", [], [vh[i]], vh[i][:, :, 64:65], 1.0)
        pT = [Alias(P5, 0, [128, 512], BF16, own=True), Alias(P5, 1024, [128, 512], BF16, own=True)]
        qT = [Alias(P5, 2048, [96, 512], BF16, own=True), Alias(P5, 3072, [96, 512], BF16, own=True)]
        rrow = Alias(P8, 0, [65, 512], F32)
        bcs = Alias(P8, 2048, [64, 512], F32)
        oTm = Alias(P7, 0, [64, 512], BF16)
        s_mixm = SC.dma_sem("mixm")
        s_bc = SC.dma_sem("bc")
        s_mm = SC.dma_sem("mm")
        s_x1 = SC.dma_sem("x1")
        s_h2 = SC.dma_sem("h2")
        x1s_k = DR()
        h2s_k = DR()
        g1bc = Alias(P2a, 0, [128, D], F32)
        sh2bc = Alias(P2b, 0, [128, D], F32)
        A2bc = Alias(P3a, 0, [128, D], F32)
        n2gbc = Alias(P3b, 0, [128, D], F32)
        mm_t = Alias(P6, 0, [64, 8, 128], BF16)
        mr_t = Alias(P6, 2048, [128, 4, 128], BF16)
        x1 = Alias(P4a, 0, [128, D], F32)
        h2 = Alias(P4b, 0, [128, D], F32)
        h2b = Alias(P7, 0, [128, D], BF16)
        h2T = Alias(P5, 0, [128, 8, 128], F32)
        x1_2 = [x1, Alias(P0, 0, [128, D], F32, own=True)]
        h2_2 = [h2, Alias(P0, 4096, [128, D], F32, own=True)]
        h2b_2 = [h2b, Alias(P4c, 0, [128, D], BF16, own=True)]
        h2T_2 = [h2T, Alias(P1, 0, [128, 8, 128], F32, own=True)]
        mm_t_2 = [mm_t, Alias(P1, 4096, [64, 8, 128], BF16, own=True)]
        mr_t_2 = [mr_t, Alias(P1, 6144, [128, 4, 128], BF16, own=True)]
        c_alts = [x1_2[1], h2_2[1], h2b_2[1], h2T_2[1], mm_t_2[1], mr_t_2[1]]
        s_mm2 = [s_mm, SC.dma_sem("mm1")]
        lgt2 = [sb("lgt%d" % i, [128, 40]) for i in range(2)]
        for i in range(2):
            op("dve", "memset", [], [lgt2[i]], lgt2[i][:], -1e30)
        m8_2 = [sb("m8_%d" % i, [128, 16]) for i in range(2)]
        rst_2 = [sb("rst_%d" % i, [128, 20]) for i in range(2)]
        lem_2 = [sb("lem_%d" % i, [128, 32]) for i in range(2)]
        Mt_2 = [sb("Mt_%d" % i, [128, 32], BF16) for i in range(2)]
        ra = Alias(P2a, 0, [128, 32], F32)
        rb = Alias(P2a, 128, [128, 32], F32)
        pad_ = Alias(P2a, 256, [128, 32], F32)
        pst = Alias(P2a, 384, [128, 32], F32)
        cmp3 = Alias(BIGW, 0, [128, NBLK, 32], F32)
        ebf = sb("ebf", [128, NBLK])
        WIDX = sb("WIDX", [128, NBLK], I32)
        cmpd = Alias(BIGW, 16384, [128, NTT, 32], F32)
        destf = Alias(P2b, 0, [128, NTT, 2], F32)
        DEST = sb("DEST", [128, NTT, 2], I32)
        h2r = [Alias(P6, 0, [128, D], BF16, own=True), Alias(P6, 2048, [128, D], BF16, own=True)]
        s_h2r = [SC.dma_sem("h2r%d" % i) for i in range(2)]
        s_sc = SC.dma_sem("sc")
        s_wg = [SC.dma_sem("wg%d" % i) for i in range(2)]
        xblk = [Alias(P1, 0, [128, 2, D], BF16, own=True), Alias(P1, 4096, [128, 2, D], BF16, own=True)]
        s_xb = [SC.dma_sem("xb%d" % i) for i in range(2)]
        xTb = Alias(P8, 0, [128, 2, 8, 128], BF16)
        sg = Alias(P2a, 0, [128, 256], F32)
        actb = Alias(P2a, 1024, [128, 256], BF16)
        actT = Alias(P2a, 1536, [128, 2, 128], BF16)
        ysb = [Alias(P0, 0, [128, D], BF16, own=True), Alias(P0, 4096, [128, D], BF16, own=True)]
        ysc = [Alias(P1, 0, [128, D], BF16, own=True), Alias(P1, 4096, [128, D], BF16, own=True)]
        s_ys = [SC.dma_sem("ys%d" % i) for i in range(2)]
        s_yg = [SC.dma_sem("yg%d" % i) for i in range(2)]
        for b in range(NSEQ):
            op("pool", "memset", [], [w_fm], w_fm[:, :, 1024:NFM], 0.0)
            for k in range(8):
                ws = wst[k % 2]
                dma("sp", s_wst[k % 2], [], [ws], out=ws[:], in_=win_d[k * 128:(k + 1) * 128, :])
                op("act", "copy", [ws], [w_tm], out=w_tm[:, k, 0:384], in_=ws[:, 0:384])
                op("act", "copy", [ws], [w_tm], out=w_tm[:, k, 384:1408], in_=ws[:, 928:1952])
                for which, base, scale in ((0, 416, 1.0), (1, 672, 0.125)):
                    src = ws[:, base:base + 256].rearrange("p (h two j) -> p h two j", h=4, two=2)
                    for ab in range(2):
                        dst = w_fm[:, k, (which * 4 + ab * 2) * 128:(which * 4 + ab * 2 + 2) * 128].rearrange(
                            "p (h dup j) -> p h dup j", h=4, dup=2)
                        op("dve", "tensor_scalar", [ws], [w_fm], out=dst,
                           in0=src[:, :, ab:ab + 1, :].to_broadcast([128, 4, 2, 32]), scalar1=scale, scalar2=None, op0=ALU.mult)
                srck = ws[:, 384:416].rearrange("p (two j) -> p two j", two=2)
                for ab in range(2):
                    dst = w_fm[:, k, 1024 + ab * 96 + 64:1024 + ab * 96 + 96].rearrange("p (dup j) -> p dup j", dup=2)
                    op("dve", "tensor_copy", [ws], [w_fm], out=dst, in_=srck[:, ab:ab + 1, :].to_broadcast([128, 2, 16]))
            dma("sp", s_col, [mods_k], [colv], out=colv[:, :, 0], in_=mods_d[b:b + 1, 0:D].rearrange("o (c p) -> p (o c)", p=128),
                allow_slow_non_contiguous=True)
            dma("sp", s_col, [mods_k], [colv], out=colv[:, :, 1], in_=mods_d[b:b + 1, D:2 * D].rearrange("o (c p) -> p (o c)", p=128),
                allow_slow_non_contiguous=True)
            op("dve", "scalar_tensor_tensor", [colv, n1g_c], [colv], out=colv[:, :, 2], in0=colv[:, :, 1], scalar=1.0, in1=n1g_c[:],
               op0=ALU.add, op1=ALU.mult)
            op("dve", "memset", [], [state], state[:], 0.0)
            op("dve", "memset", [], [state_b], state_b[:], 0.0)

            def A_pos(g):
                t0 = g * 512
                dma("sp", s_pos, [], [posi], out=posi[:], in_=pos_d[b:b + 1, t0:t0 + 512].partition_broadcast(128))
                op("dve", "tensor_copy", [posi], [posf], out=posf[:], in_=posi[:])

            def A_table(g, k):
                t0 = g * 512
                if k == 0:
                    rope_table(csr1, csr1[:], slice(0, 128), 0, 1)
                elif k == 1:
                    rope_table(csr2, csr2[:], slice(0, 128), 0, 2)
                elif k == 2:
                    rope_table(csm1f, csm1f[64:96, :], slice(64, 96), 3, 4)
                    dma("sp", s_csm, [csm1f], [csms_k], out=csms_d[b, 0, :, t0:t0 + 512], in_=csm1f[64:96, :])
                else:
                    rope_table(csm2f, csm2f[64:96, :], slice(64, 96), 3, 5)
                    dma("sp", s_csm, [csm2f], [csms_k], out=csms_d[b, 1, :, t0:t0 + 512], in_=csm2f[64:96, :])

            def A_S1(g, tl):
                ti = g * 4 + tl
                tok0 = ti * 128
                xi_ = xin[ti % 2]
                xs_ = xsb2[ti % 2]
                so = 10 + 3 * (ti % 2)
                dma("sp", s_xin[ti % 2], [], [xi_], out=xi_[:], in_=x_d[b, tok0:tok0 + 128, :])
                op("act", "activation", [xi_], [junk, stat], out=junk[:], in_=xi_[:], func=AF.Square, accum_out=stat[:, so:so + 1])
                op("dve", "tensor_scalar", [stat], [stat], out=stat[:, so + 1:so + 2], in0=stat[:, so:so + 1], scalar1=1.0 / D, scalar2=EPS,
                   op0=ALU.mult, op1=ALU.add)
                rsqrt(stat, stat[:, so + 1:so + 2], stat, stat[:, so + 2:so + 3], 1)
                op("dve", "tensor_scalar", [xi_, stat], [xs_], out=xs_[:], in0=xi_[:], scalar1=stat[:, so + 2:so + 3], scalar2=None, op0=ALU.mult)

            def A_T8(g, tl):
                ti = g * 4 + tl
                xs_ = xsb2[ti % 2]
                for c in range(8):
                    op("pe", "transpose", [xs_, ident_b], [PS[0]], out=psbf(0)[:, c * 128:(c + 1) * 128],
                       in_=xs_[:, c * 128:(c + 1) * 128], identity=ident_b[:])
                for c in range(8):
                    if c % 2 == 0:
                        op("dve", "tensor_scalar", [PS[0], colv], [h1T], out=h1T[:, c, tl * 128:(tl + 1) * 128],
                           in0=psbf(0)[:, c * 128:(c + 1) * 128], scalar1=colv[:, c, 2:3], scalar2=colv[:, c, 0:1],
                           op0=ALU.mult, op1=ALU.add)
                    else:
                        op("act", "activation", [PS[0], colv], [h1T], out=h1T[:, c, tl * 128:(tl + 1) * 128],
                           in_=psbf(0)[:, c * 128:(c + 1) * 128], func=AF.Identity, scale=colv[:, c, 2:3], bias=colv[:, c, 0:1])

            def A_MM(g, tl):
                RV4 = RVG[g % 2]
                GT4 = GTG[g % 2]
                ti = g * 4 + tl
                tok0 = ti * 128
                for (pb, c0, n) in ((1, 0, 384), (2, 384, 512), (3, 896, 512)):
                    for k in range(8):
                        op("pe", "matmul", [h1T, w_tm], [PS[pb]], PS[pb][:, 0:n], lhsT=h1T[:, k, tl * 128:(tl + 1) * 128],
                           rhs=w_tm[:, k, c0:c0 + n], start=(k == 0), stop=(k == 7))
                op("act", "activation", [PS[1]], [junk, stat], out=junk[:, 0:256], in_=PS[1][:, 0:256], func=AF.Square, accum_out=stat[:, 4:5])
                op("act", "activation", [PS[1]], [junk, stat], out=junk[:, 256:384], in_=PS[1][:, 256:384], func=AF.Square, accum_out=stat[:, 5:6])
                op("dve", "tensor_scalar", [stat], [stat], out=stat[:, 6:7], in0=stat[:, 4:5], scalar1=1.0 / 256, scalar2=EPS, op0=ALU.mult, op1=ALU.add)
                op("dve", "tensor_scalar", [stat], [stat], out=stat[:, 7:8], in0=stat[:, 5:6], scalar1=1.0 / 128, scalar2=EPS, op0=ALU.mult, op1=ALU.add)
                rsqrt(stat, stat[:, 6:8], stat, stat[:, 8:10], 2)
                op("dve", "tensor_scalar", [PS[1], stat], [cqn], out=cqn[:, 0:256], in0=PS[1][:, 0:256], scalar1=stat[:, 8:9], scalar2=None, op0=ALU.mult)
                op("dve", "tensor_scalar", [PS[1], stat], [cqn], out=cqn[:, 256:384], in0=PS[1][:, 256:384], scalar1=stat[:, 9:10], scalar2=None, op0=ALU.mult)
                for c in range(3):
                    op("pe", "transpose", [cqn, ident_b], [PS[0]], out=psbf(0)[:, c * 128:(c + 1) * 128],
                       in_=cqn[:, c * 128:(c + 1) * 128], identity=ident_b[:])
                op("act", "copy", [PS[0]], [cqnT], out=cqnT[:, :, tok0:tok0 + 128],
                   in_=psbf(0)[:, 0:256].rearrange("p (c t) -> p c t", c=2))
                op("act", "copy", [PS[0]], [ckvnT], out=ckvnT[:, tok0:tok0 + 128], in_=psbf(0)[:, 256:384])
                op("act", "copy", [PS[2]], [RV4], out=RV4[:, tl, :], in_=PS[2][:])
                op("act", "activation", [PS[3]], [gsg], out=gsg[:], in_=PS[3][:], func=AF.Tanh, scale=0.5)
                op("dve", "scalar_tensor_tensor", [gsg, PS[3]], [GT4], out=GT4[:, tl, :], in0=gsg[:], scalar=1.0, in1=PS[3][:], op0=ALU.add, op1=ALU.mult)

            def A_FM(g):
                t0 = g * 512

                def fm_mm(pb, col0, ncols):
                    for k in range(8):
                        op("pe", "matmul", [h1T, w_fm], [PS[pb]], PS[pb][0:ncols, :], lhsT=w_fm[:, k, col0:col0 + ncols],
                           rhs=h1T[:, k, :], start=(k == 0), stop=(k == 7))
                for which, dst in ((0, rqT), (1, rkT)):
                    for j in range(2):
                        fm_mm(4, (which * 4 + j) * 128, 128)
                        fm_mm(5, (which * 4 + 2 + j) * 128, 128)
                        op("dve", "tensor_tensor", [PS[4], csr1], [rt1], out=rt1[:], in0=PS[4][:], in1=csr1[:], op=ALU.mult)
                        op("dve", "tensor_tensor", [PS[5], csr2], [rt2], out=rt2[:], in0=PS[5][:], in1=csr2[:], op=ALU.mult)
                        op("pool", "tensor_tensor", [rt1, rt2], [dst], out=dst[:, j, :], in0=rt1[:], in1=rt2[:], op=ALU.add)
                op("pool", "tensor_tensor", [rqT, cst], [rqxT], out=rqxT[:].rearrange("p j (n q) -> p j n q", n=4),
                   in0=rqT[:].rearrange("p j (n q) -> p j n q", n=4), in1=xi_c.unsqueeze(2).to_broadcast([128, 2, 4, 128]), op=ALU.mult)
                fm_mm(4, 1024, 96)
                fm_mm(5, 1120, 96)
                op("dve", "tensor_tensor", [PS[4], csm1f], [rt1], out=rt1[64:96, :], in0=PS[4][64:96, :], in1=csm1f[64:96, :], op=ALU.mult)
                op("dve", "tensor_tensor", [PS[5], csm2f], [rt2], out=rt2[64:96, :], in0=PS[5][64:96, :], in1=csm2f[64:96, :], op=ALU.mult)
                op("pool", "tensor_tensor", [rt1, rt2], [kT], out=kT[64:96, t0:t0 + 512], in0=rt1[64:96, :], in1=rt2[64:96, :], op=ALU.add)

            def A_RETa(g, tl):
                RV4 = RVG[g % 2]
                qs = slice(tl * 128, (tl + 1) * 128)
                for j in range(2):
                    op("pe", "transpose", [rkT, ident_b], [PS[4]], out=psbf(4)[:, j * 128:(j + 1) * 128], in_=rkT[:, j, qs], identity=ident_b[:])
                op("dve", "tensor_tensor", [PS[4], cst], [rkz], out=rkz[:], in0=psbf(4)[:, 0:256], in1=zeta_c, op=ALU.mult)
                for h in range(4):
                    j, half = h // 2, h % 2
                    pr = slice(half * 64, half * 64 + 64)
                    op("pe", "matmul", [rkT, rqT], [PS[6]], PS[6][:, h * 128:(h + 1) * 128], lhsT=rkT[pr, j, qs], rhs=rqT[pr, j, qs],
                       start=True, stop=True)
                op("dve", "tensor_tensor", [PS[6], cst], [sdT], out=sdT[:], in0=PS[6][:].rearrange("p (h q) -> p h q", h=4), in1=dmaskT, op=ALU.mult)
                for h in range(4):
                    j, half = h // 2, h % 2
                    pr = slice(half * 64, half * 64 + 64)
                    op("pe", "matmul", [sdT, RV4], [PS[7]], PS[7][:, h * 128:(h + 1) * 128], lhsT=sdT[:, h, :], rhs=RV4[:, tl, h * 128:(h + 1) * 128],
                       start=True, stop=False)
                    op("pe", "matmul", [rqxT, state_b], [PS[7]], PS[7][:, h * 128:(h + 1) * 128], lhsT=rqxT[pr, j, qs], rhs=state_b[pr, j, :],
                       start=False, stop=True)
                for h in range(4):
                    j = h // 2
                    op("pe", "matmul", [rkz, RV4], [PS[6]], PS[6][:, h * 128:(h + 1) * 128], lhsT=rkz[:, j * 128:(j + 1) * 128],
                       rhs=RV4[:, tl, h * 128:(h + 1) * 128], start=True, stop=True)
                for h in range(4):
                    j, half = h // 2, h % 2
                    pr = slice(half * 64, half * 64 + 64)
                    op("dve", "scalar_tensor_tensor", [state, cst, PS[6]], [state], out=state[pr, j, :], in0=state[pr, j, :],
                       scalar=cd_c[pr, h:h + 1], in1=PS[6][pr, h * 128:(h + 1) * 128], op0=ALU.mult, op1=ALU.add)
                op("pool", "tensor_copy", [state], [state_b], out=state_b[:], in_=state[:])

            def A_RETb_dve(g, tl):
                GT4 = GTG[g % 2]
                op("act", "copy", [PS[7]], [osb], out=osb[:], in_=PS[7][:].rearrange("p (h d) -> p h d", h=4))
                op("dve", "tensor_reduce", [osb], [gst], out=gst[:, 0:4], in_=osb[:], axis=AX.X, op=ALU.add)
                op("pool", "tensor_tensor", [osb], [osq], out=osq[:], in0=osb[:], in1=osb[:], op=ALU.mult)
                op("dve", "tensor_reduce", [osq], [gst], out=gst[:, 4:8], in_=osq[:], axis=AX.X, op=ALU.add)
                op("dve", "tensor_scalar", [gst], [gst], out=gst[:, 0:4], in0=gst[:, 0:4], scalar1=1.0 / 128, scalar2=None, op0=ALU.mult)
                op("dve", "tensor_tensor", [gst], [gst], out=gst[:, 8:12], in0=gst[:, 0:4], in1=gst[:, 0:4], op=ALU.mult)
                op("dve", "scalar_tensor_tensor", [gst], [gst], out=gst[:, 8:12], in0=gst[:, 4:8], scalar=1.0 / 128, in1=gst[:, 8:12],
                   op0=ALU.mult, op1=ALU.subtract)
                op("dve", "tensor_scalar", [gst], [gst], out=gst[:, 8:12], in0=gst[:, 8:12], scalar1=EPS, scalar2=None, op0=ALU.add)
                rsqrt(gst, gst[:, 8:12], gst, gst[:, 12:16], 4)
                op("dve", "tensor_scalar", [gst], [gst], out=gst[:, 12:16], in0=gst[:, 12:16], scalar1=0.5, scalar2=None, op0=ALU.mult)
                op("dve", "tensor_tensor", [osb, gst], [osb], out=osb[:], in0=osb[:], in1=gst[:, 0:4].unsqueeze(2).to_broadcast([128, 4, 128]), op=ALU.subtract)
                op("dve", "tensor_tensor", [osb, gst], [osb], out=osb[:], in0=osb[:], in1=gst[:, 12:16].unsqueeze(2).to_broadcast([128, 4, 128]), op=ALU.mult)
                op("pool", "tensor_tensor", [osb, GT4], [oretb], out=oretb[:], in0=osb[:].rearrange("p h d -> p (h d)"), in1=GT4[:, tl, :], op=ALU.mult)

            def A_RETb_pe(g, tl):
                qs = slice(tl * 128, (tl + 1) * 128)
                for h in range(4):
                    op("pe", "transpose", [oretb, ident_b], [PS[5]], out=psbf(5)[:, h * 128:(h + 1) * 128], in_=oretb[:, h * 128:(h + 1) * 128], identity=ident_b[:])
                op("act", "copy", [PS[5]], [oretT], out=oretT[:, :, qs], in_=psbf(5)[:, 0:512].rearrange("p (h t) -> p h t", h=4))

            A_S1(0, 0)
            for g in range(NG + 1):
                if g < NG:
                    A_pos(g)
                for tl in range(4):
                    if g < NG:
                        A_T8(g, tl)
                    nxt = g * 4 + tl + 1
                    if nxt < NG * 4:
                        A_S1(nxt // 4, nxt % 4)
                    if g >= 1:
                        A_RETa(g - 1, tl)
                    if g >= 1 and tl > 0:
                        A_RETb_pe(g - 1, tl - 1)
                    if g < NG:
                        A_MM(g, tl)
                    if g >= 1:
                        A_RETb_dve(g - 1, tl)
                    if g < NG:
                        A_table(g, tl)
                if g < NG:
                    A_FM(g)
                if g >= 1:
                    A_RETb_pe(g - 1, 3)
                    dma("sp", s_mixr, [oretT], [mixr_k], out=mixr_d[b, :, :, (g - 1) * 512:g * 512].rearrange("h p t -> p h t"), in_=oretT[:])
            fence([oretT, BIGW], pT + qT + wblk)

            def qprep(h, i):
                qsl = slice(i * 512, (i + 1) * 512)
                for c in range(2):
                    op("pe", "matmul", [wuq_a, cqnT], [PS[4]], PS[4][0:96, :], lhsT=wuq_a[:, c, h, 0:96], rhs=cqnT[:, c, qsl], start=(c == 0), stop=(c == 1))
                for c in range(2):
                    op("pe", "matmul", [wuq_a, cqnT], [PS[5]], PS[5][0:96, :], lhsT=wuq_a[:, c, h, 96:192], rhs=cqnT[:, c, qsl], start=(c == 0), stop=(c == 1))
                qt = qT[(h * NG + i) % 2]
                op("act", "copy", [PS[4]], [qt], out=qt[0:64, :], in_=PS[4][0:64, :])
                dma("sp", s_csm, [csms_k], [csm1f], out=csm1f[64:96, :], in_=csms_d[b, 0, :, qsl])
                dma("sp", s_csm, [csms_k], [csm2f], out=csm2f[64:96, :], in_=csms_d[b, 1, :, qsl])
                op("dve", "tensor_tensor", [PS[4], csm1f], [rt1], out=rt1[64:96, :], in0=PS[4][64:96, :], in1=csm1f[64:96, :], op=ALU.mult)
                op("dve", "tensor_tensor", [PS[5], csm2f], [rt2], out=rt2[64:96, :], in0=PS[5][64:96, :], in1=csm2f[64:96, :], op=ALU.mult)
                op("dve", "tensor_tensor", [rt1, rt2], [qt], out=qt[64:96, :], in0=rt1[64:96, :], in1=rt2[64:96, :], op=ALU.add)

            pend_epi = []
            for h in range(8):
                for g in range(NG):
                    op("pe", "matmul", [wk_b, ckvnT], [PS[6]], PS[6][0:64, :], lhsT=wk_b[:, h, :], rhs=ckvnT[:, g * 512:(g + 1) * 512], start=True, stop=True)
                    op("act", "copy", [PS[6]], [kT], out=kT[0:64, g * 512:(g + 1) * 512], in_=PS[6][0:64, :])
                vb = vh[h % 2]
                for t8 in range((NT + 7) // 8):
                    n8 = min(8, NT - t8 * 8)
                    for tt_ in range(n8):
                        ti = t8 * 8 + tt_
                        op("pe", "matmul", [ckvnT, wv_b], [PS[7]], PS[7][:, tt_ * 64:(tt_ + 1) * 64], lhsT=ckvnT[:, ti * 128:(ti + 1) * 128], rhs=wv_b[:, h, :],
                           start=True, stop=True)
                    op("dve", "tensor_copy", [PS[7]], [vb], out=vb[:, t8 * 8:t8 * 8 + n8, 0:64], in_=PS[7][:, 0:n8 * 64].rearrange("p (t d) -> p t d", d=64))
                qprep(h, 0)
                for i in range(NG):
                    qsl = slice(i * 512, (i + 1) * 512)
                    qt = qT[(h * NG + i) % 2]
                    if i + 1 < NG:
                        qprep(h, i + 1)
                    nk = 4 * i + 4
                    ob = 2 + ((h * NG + i) % 2)

                    def c0_of(j):
                        return 128 * (j - 4 * i) if j > 4 * i else 0

                    def qk(j):
                        c0 = c0_of(j)
                        op("pe", "matmul", [kT, qt], [PS[j % 2]], PS[j % 2][:, c0:512], lhsT=kT[0:96, j * 128:(j + 1) * 128], rhs=qt[0:96, c0:512], start=True, stop=True)
                    qk(0)
                    for j in range(nk):
                        if j + 1 < nk:
                            qk(j + 1)
                        p_ = pT[j % 2]
                        c0 = c0_of(j)
                        op("act", "activation", [PS[j % 2]], [p_], out=p_[:, c0:512], in_=PS[j % 2][:, c0:512], func=AF.Exp, scale=float(96 ** -0.5))
                        if j >= 4 * i:
                            m_ = j - 4 * i
                            op("dve", "tensor_tensor", [p_, cmask_b], [p_], out=p_[:, c0:c0 + 128], in0=p_[:, c0:c0 + 128], in1=cmask_b[:, m_, c0:c0 + 128], op=ALU.mult)
                        op("pe", "matmul", [vb, p_], [PS[ob]], PS[ob][0:65, c0:512], lhsT=vb[:, j, 0:65], rhs=p_[:, c0:512], start=(j == 0), stop=(j == nk - 1))
                        if j == 1 and pend_epi:
                            pend_epi.pop(0)()
                    def epilogue(ob=ob, h=h, qsl=qsl):
                        op("dve", "reciprocal", [PS[ob]], [rrow], out=rrow[64:65, :], in_=PS[ob][64:65, :])
                        op("pe", "matmul", [ones_f, rrow], [PS[7]], PS[7][0:64, :], lhsT=ones_f[64:65, 0:64], rhs=rrow[64:65, :], start=True, stop=True)
                        op("act", "copy", [PS[7]], [bcs], out=bcs[:], in_=PS[7][0:64, :])
                        op("dve", "tensor_tensor", [PS[ob], bcs], [oTm], out=oTm[:], in0=PS[ob][0:64, :], in1=bcs[:], op=ALU.mult)
                        dma("sp", s_mixm, [oTm], [mixm_k], out=mixm_d[b, h, :, qsl], in_=oTm[:])
                    pend_epi.append(epilogue)
                    for _ in range(per_slot):
                        if relay_state[0] < NE:
                            relayout_expert(relay_state[0])
                            relay_state[0] += 1
            while pend_epi:
                pend_epi.pop(0)()
            fence(pT + qT + wblk, [h2T, BIGW])

            fence([GTG[0], h1T, RVG[1]], c_alts)
            dma("pool", s_w, [], [wo_m], out=wo_m[:], in_=wo_d[0:512, :].rearrange("(h p) n -> p h n", p=64))
            dma("pool", s_w, [], [wo_r], out=wo_r[:], in_=wo_d[512:1024, :].rearrange("(h p) n -> p h n", p=128))
            dma("sp", s_bc, [mods_k], [g1bc], out=g1bc[:], in_=mods_d[b:b + 1, 2 * D:3 * D].partition_broadcast(128))
            dma("sp", s_bc, [mods_k], [sh2bc], out=sh2bc[:], in_=mods_d[b:b + 1, 3 * D:4 * D].partition_broadcast(128))
            dma("sp", s_bc, [mods_k], [A2bc], out=A2bc[:], in_=mods_d[b:b + 1, 4 * D:5 * D].partition_broadcast(128))
            dma("sp", s_bc, [], [n2gbc], out=n2gbc[:], in_=n2g_d.partition_broadcast(128))
            op("dve", "scalar_tensor_tensor", [A2bc, n2gbc], [A2bc], out=A2bc[:], in0=A2bc[:], scalar=1.0, in1=n2gbc[:], op0=ALU.add, op1=ALU.mult)
            def C1(ti):
                tok0 = ti * 128
                gt = b * NT + ti
                p2 = ti % 2
                x1 = x1_2[p2]
                h2 = h2_2[p2]
                h2b = h2b_2[p2]
                h2T = h2T_2[p2]
                mm_t = mm_t_2[p2]
                mr_t = mr_t_2[p2]
                pa, pbk = (0, 1) if p2 == 0 else (6, 7)
                so = 0 if p2 == 0 else 10
                xi_ = xin[ti % 2]
                dma("sp", s_xin[ti % 2], [], [xi_], out=xi_[:], in_=x_d[b, tok0:tok0 + 128, :])
                dma("sp", s_mm2[p2], [mixm_k], [mm_t], out=mm_t[:], in_=mixm_d[b, :, :, tok0:tok0 + 128].rearrange("h p t -> p h t"))
                dma("sp", s_mm2[p2], [mixr_k], [mr_t], out=mr_t[:], in_=mixr_d[b, :, :, tok0:tok0 + 128].rearrange("h p t -> p h t"))
                for nh, pbank in ((0, pa), (1, pbk)):
                    for hh in range(8):
                        op("pe", "matmul", [mm_t, wo_m], [PS[pbank]], PS[pbank][:, :], lhsT=mm_t[:, hh, :], rhs=wo_m[:, hh, nh * 512:(nh + 1) * 512], start=(hh == 0), stop=False)
                    for hh in range(4):
                        op("pe", "matmul", [mr_t, wo_r], [PS[pbank]], PS[pbank][:, :], lhsT=mr_t[:, hh, :], rhs=wo_r[:, hh, nh * 512:(nh + 1) * 512], start=False, stop=(hh == 3))
                for nh, pbank in ((0, pa), (1, pbk)):
                    op("dve", "tensor_tensor", [PS[pbank], g1bc], [x1], out=x1[:, nh * 512:(nh + 1) * 512], in0=PS[pbank][:, :], in1=g1bc[:, nh * 512:(nh + 1) * 512], op=ALU.mult)
                op("pool", "tensor_tensor", [x1, xi_], [x1], out=x1[:], in0=x1[:], in1=xi_[:], op=ALU.add)
                dma("sp", s_x1, [x1], [x1s_k], out=x1s_d[gt * 128:(gt + 1) * 128, :], in_=x1[:])
                op("act", "activation", [x1], [junk, stat], out=junk[:], in_=x1[:], func=AF.Square, accum_out=stat[:, so:so + 1])
                op("dve", "tensor_scalar", [stat], [stat], out=stat[:, so + 1:so + 2], in0=stat[:, so:so + 1], scalar1=1.0 / D, scalar2=EPS, op0=ALU.mult, op1=ALU.add)
                rsqrt(stat, stat[:, so + 1:so + 2], stat, stat[:, so + 2:so + 3], 1)
                op("dve", "scalar_tensor_tensor", [x1, stat, A2bc], [h2], out=h2[:], in0=x1[:], scalar=stat[:, so + 2:so + 3], in1=A2bc[:], op0=ALU.mult, op1=ALU.mult)
                op("pool", "tensor_tensor", [h2, sh2bc], [h2], out=h2[:], in0=h2[:], in1=sh2bc[:], op=ALU.add)
                op("act", "copy", [h2], [h2b], out=h2b[:], in_=h2[:])
                dma("sp", s_h2, [h2b], [h2s_k], out=h2s_d[gt * 128:(gt + 1) * 128, :], in_=h2b[:])
                for c in range(8):
                    pb = 2 + c // 4
                    op("pe", "transpose", [h2, cst], [PS[pb]], out=PS[pb][:, (c % 4) * 128:(c % 4 + 1) * 128], in_=h2[:, c * 128:(c + 1) * 128], identity=ident_f)
                op("act", "copy", [PS[2]], [h2T], out=h2T[:, 0:4, :], in_=PS[2][:, :].rearrange("p (c t) -> p c t", c=4))
                op("dve", "tensor_copy", [PS[3]], [h2T], out=h2T[:, 4:8, :], in_=PS[3][:, :].rearrange("p (c t) -> p c t", c=4))
                for c in range(8):
                    op("pe", "matmul", [h2T, w_rt], [PS[4]], PS[4][:, 0:36], lhsT=h2T[:, c, :], rhs=w_rt[:, c, :], start=(c == 0), stop=(c == 7))

                lgt = lgt2[ti % 2]
                op("dve", "tensor_tensor", [PS[4], b_rt], [lgt], out=lgt[:, 0:4], in0=PS[4][:, 0:4], in1=b_rt[:, 0:4], op=ALU.add)
                op("dve", "tensor_tensor", [PS[4], b_rt], [lgt], out=lgt[:, 8:40], in0=PS[4][:, 4:36], in1=b_rt[:, 4:36], op=ALU.add)

            def C2(ti):
                gt = b * NT + ti
                lgt = lgt2[ti % 2]
                m8 = m8_2[ti % 2]
                rst = rst_2[ti % 2]
                lem = lem_2[ti % 2]
                Mt = Mt_2[ti % 2]
                op("dve", "max", [lgt], [m8], out=m8[:, 0:8], in_=lgt[:, 0:8])
                op("dve", "tensor_scalar", [m8], [rst], out=rst[:, 0:1], in0=m8[:, 0:1], scalar1=-1.0, scalar2=None, op0=ALU.mult)
                op("act", "activation", [lgt, rst], [rst], out=rst[:, 8:16], in_=lgt[:, 0:8], func=AF.Exp, bias=rst[:, 0:1], scale=1.0, accum_out=rst[:, 1:2])
                op("dve", "reciprocal", [rst], [rst], out=rst[:, 2:3], in_=rst[:, 1:2])
                op("dve", "tensor_scalar", [lgt, m8], [rst], out=rst[:, 16:20], in0=lgt[:, 0:4], scalar1=m8[:, 0:1], scalar2=None, op0=ALU.is_equal)
                op("dve", "tensor_scalar", [rst], [rst], out=rst[:, 16:20], in0=rst[:, 16:20], scalar1=-1.0, scalar2=1e30, op0=ALU.add, op1=ALU.mult)
                op("dve", "tensor_tensor", [lgt, rst], [lem], out=lem[:].rearrange("p (g e) -> p g e", g=4), in0=lgt[:, 8:40].rearrange("p (g e) -> p g e", g=4),
                   in1=rst[:, 16:20].unsqueeze(2).to_broadcast([128, 4, 8]), op=ALU.add)
                op("dve", "max", [lem], [m8], out=m8[:, 8:16], in_=lem[:])
                op("dve", "tensor_scalar", [lem, m8], [OH1], out=OH1[:, gt, :], in0=lem[:], scalar1=m8[:, 8:9], scalar2=None, op0=ALU.is_equal)
                op("dve", "tensor_scalar", [lem, m8], [OH2], out=OH2[:, gt, :], in0=lem[:], scalar1=m8[:, 9:10], scalar2=None, op0=ALU.is_equal)
                op("dve", "tensor_tensor", [m8], [rst], out=rst[:, 3:4], in0=m8[:, 9:10], in1=m8[:, 8:9], op=ALU.subtract)
                op("act", "activation", [rst], [rst], out=rst[:, 4:5], in_=rst[:, 3:4], func=AF.Exp)
                op("dve", "tensor_scalar", [rst], [rst], out=rst[:, 4:5], in0=rst[:, 4:5], scalar1=1.0, scalar2=None, op0=ALU.add)
                op("dve", "reciprocal", [rst], [rst], out=rst[:, 5:6], in_=rst[:, 4:5])
                op("dve", "tensor_tensor", [rst], [GATE], out=GATE[:, gt, 0:1], in0=rst[:, 5:6], in1=rst[:, 2:3], op=ALU.mult)
                op("dve", "tensor_tensor", [rst, GATE], [GATE], out=GATE[:, gt, 1:2], in0=rst[:, 2:3], in1=GATE[:, gt, 0:1], op=ALU.subtract)
                op("pool", "tensor_tensor", [OH1, OH2], [Mt], out=Mt[:], in0=OH1[:, gt, :], in1=OH2[:, gt, :], op=ALU.add)
                op("pe", "matmul", [triu_b, Mt], [PS[5]], PS[5][:, 0:32], lhsT=triu_b[:], rhs=Mt[:], start=True, stop=False)
                op("pe", "matmul", [ones_b, Macc], [PS[5]], PS[5][:, 0:32], lhsT=ones_b[:], rhs=Macc[:], start=False, stop=True)
                op("act", "copy", [PS[5]], [CUM], out=CUM[:, gt, :], in_=PS[5][:, 0:32])
                op("pool", "tensor_tensor", [Macc, Mt], [Macc], out=Macc[:], in0=Macc[:], in1=Mt[:], op=ALU.add)


            import os as _os2
            if _os2.environ.get("K_CSKEW", "1") == "1":
                C1(0)
                for ti in range(NT):
                    if ti + 1 < NT:
                        C1(ti + 1)
                    C2(ti)
            else:
                for ti in range(NT):
                    C1(ti)
                    C2(ti)
            fence(c_alts, [P0, P1, P4c])

        op("pe", "matmul", [ones_b, Macc], [PS[5]], PS[5][:, 0:32], lhsT=ones_b[:], rhs=Macc[:], start=True, stop=True)
        op("dve", "tensor_scalar", [PS[5]], [ra], out=ra[:], in0=PS[5][:, 0:32], scalar1=1.0 / BLK, scalar2=(BLK - 1 - (BLK / 2 - 0.5)) / BLK, op0=ALU.mult, op1=ALU.add)
        op("dve", "tensor_scalar", [ra], [ra], out=ra[:], in0=ra[:], scalar1=MAGIC_RN, scalar2=None, op0=ALU.add)
        op("dve", "tensor_scalar", [ra], [pad_], out=pad_[:], in0=ra[:], scalar1=MAGIC_RN, scalar2=float(BLK), op0=ALU.subtract, op1=ALU.mult)
        op("dve", "tensor_copy", [pad_], [ra], out=ra[:], in_=pad_[:])
        cur, oth = ra, rb
        for sft in (1, 2, 4, 8, 16):
            op("dve", "tensor_copy", [cur], [oth], out=oth[:, 0:sft], in_=cur[:, 0:sft])
            op("dve", "tensor_tensor", [cur], [oth], out=oth[:, sft:32], in0=cur[:, sft:32], in1=cur[:, 0:32 - sft], op=ALU.add)
            cur, oth = oth, cur
        pend = cur
        op("dve", "tensor_tensor", [pend, pad_], [pst], out=pst[:], in0=pend[:], in1=pad_[:], op=ALU.subtract)
        a0, a1 = coff["blkstart"]
        op("dve", "tensor_tensor", [pend, cst], [cmp3], out=cmp3[:], in0=pend[:].unsqueeze(1).to_broadcast([128, NBLK, 32]),
           in1=cst[:, a0:a1].unsqueeze(2).to_broadcast([128, NBLK, 32]), op=ALU.is_le)
        op("dve", "tensor_reduce", [cmp3], [ebf], out=ebf[:], in_=cmp3[:], axis=AX.X, op=ALU.add)
        i0, i1 = coff["iotap"]
        op("dve", "tensor_scalar", [ebf], [ebf], out=ebf[:], in0=ebf[:], scalar1=31.0, scalar2=128.0, op0=ALU.min, op1=ALU.mult)
        op("dve", "tensor_scalar", [ebf, cst], [WIDX], out=WIDX[:], in0=ebf[:], scalar1=cst[:, i0:i1], scalar2=None, op0=ALU.add)
        op("pool", "tensor_tensor", [CUM, pst], [CUM], out=CUM[:], in0=CUM[:], in1=pst[:].unsqueeze(1).to_broadcast([128, NTT, 32]), op=ALU.add)
        for k_, OH in ((0, OH1), (1, OH2)):
            op("dve", "tensor_tensor", [OH, CUM], [cmpd], out=cmpd[:], in0=OH[:], in1=CUM[:], op=ALU.mult)
            op("dve", "tensor_reduce", [cmpd], [destf], out=destf[:, :, k_], in_=cmpd[:], axis=AX.X, op=ALU.add)
        op("dve", "tensor_copy", [destf], [DEST], out=DEST[:], in_=destf[:])

        xs_k = DR()
        ys_k = DR()
        h2r = h2r + [Alias(P7, 0, [128, D], BF16, own=True), Alias(P4c, 0, [128, D], BF16, own=True)]
        s_h2r = s_h2r + [SC.dma_sem("h2r2"), SC.dma_sem("h2r3")]
        fence([rqT, rkT, RVG[1], h1T, GT4, BIGW], h2r + xblk + ysb + wblk)
        for gt in range(NTT):
            hr = h2r[gt % 4]
            dma("sp", s_h2r[gt % 4], [h2s_k], [hr], out=hr[:], in_=h2s_d[gt * 128:(gt + 1) * 128, :])
            for k_ in range(2):
                SC.dma("pool", (lambda e, hr=hr, gt=gt, k_=k_: e.indirect_dma_start(
                    out=xs_d, out_offset=bass.IndirectOffsetOnAxis(ap=DEST[:, gt, k_:k_ + 1], axis=0), in_=hr[:, :], in_offset=None)),
                    s_sc, [hr.k, DEST.k], [xs_k.k], lat=6.0)

        xTb2 = [xTb, Alias(P4b, 0, [128, 2, 8, 128], BF16)]
        actb2 = [[Alias(P2a, 1024 + 512 * (2 * pq + r), [128, 256], BF16, own=True) for r in range(2)] for pq in range(2)]
        sg2 = [sg, Alias(P2a, 3072, [128, 256], F32, own=True)]
        actT2 = [Alias(P3a, 512 * r, [128, 2, 128], BF16, own=True) for r in range(2)]
        fence([g1bc, A2bc], [a_ for l_ in actb2 for a_ in l_] + sg2 + actT2)

        def stage1(blk):
            pq = blk % 2
            wb = wblk[pq]
            SC.dma("pool", (lambda e, wb=wb, blk=blk: e.indirect_dma_start(
                out=wb[:, :], out_offset=None, in_=wall_d, in_offset=bass.IndirectOffsetOnAxis(ap=WIDX[:, blk:blk + 1], axis=0))),
                s_wg[pq], [WIDX.k, wall_k.k], [wb.k], lat=14.0)
            xb_ = xblk[pq]
            dma("sp", s_xb[pq], [xs_k], [xb_], out=xb_[:], in_=xs_d[blk * BLK:(blk + 1) * BLK, :].rearrange("(r p) d -> p r d", p=128))
            xt_ = xTb2[pq]
            for r in range(2):
                for c in range(8):
                    op("pe", "transpose", [xb_, ident_b], [PS[0]], out=psbf(0)[:, c * 128:(c + 1) * 128], in_=xb_[:, r, c * 128:(c + 1) * 128], identity=ident_b[:])
                if r == 0:
                    op("act", "copy", [PS[0]], [xt_], out=xt_[:, r, :, :], in_=psbf(0)[:, 0:1024].rearrange("p (c t) -> p c t", c=8))
                else:
                    op("dve", "tensor_copy", [PS[0]], [xt_], out=xt_[:, r, :, :], in_=psbf(0)[:, 0:1024].rearrange("p (c t) -> p c t", c=8))
            for r in range(2):
                hb = 1 + 2 * pq + r
                for c in range(8):
                    op("pe", "matmul", [xt_, wb], [PS[hb]], PS[hb][:, :], lhsT=xt_[:, r, c, :], rhs=wb[:, c * 512:(c + 1) * 512], start=(c == 0), stop=(c == 7))
            for r in range(2):
                hb = 1 + 2 * pq + r
                sg_ = sg2[r]
                ab = actb2[pq][r]
                op("act", "activation", [PS[hb]], [sg_], out=sg_[:], in_=PS[hb][:, 0:256], func=AF.Tanh, scale=0.5)
                op("dve", "scalar_tensor_tensor", [sg_, PS[hb]], [sg_], out=sg_[:], in0=sg_[:], scalar=1.0, in1=PS[hb][:, 0:256], op0=ALU.add, op1=ALU.mult)
                op("dve", "scalar_tensor_tensor", [sg_, PS[hb]], [ab], out=ab[:], in0=sg_[:], scalar=0.5, in1=PS[hb][:, 256:512], op0=ALU.mult, op1=ALU.mult)

        def stage2(blk):
            pq = blk % 2
            wb = wblk[pq]
            for r in range(2):
                ab = actb2[pq][r]
                at = actT2[r]
                for fc in range(2):
                    op("pe", "transpose", [ab, ident_b], [PS[5]], out=psbf(5)[:, (2 * r + fc) * 128:(2 * r + fc + 1) * 128], in_=ab[:, fc * 128:(fc + 1) * 128], identity=ident_b[:])
                op("act", "copy", [PS[5]], [at], out=at[:], in_=psbf(5)[:, 2 * r * 128:(2 * r + 2) * 128].rearrange("p (c t) -> p c t", c=2))
            for r in range(2):
                at = actT2[r]
                yb = ysb[r]
                for nh in range(2):
                    for fc in range(2):
                        op("pe", "matmul", [at, wb], [PS[6 + nh]], PS[6 + nh][:, :], lhsT=at[:, fc, :],
                           rhs=wb[:, 4096 + fc * 1024 + nh * 512:4096 + fc * 1024 + (nh + 1) * 512], start=(fc == 0), stop=(fc == 1))
                    if nh == 0:
                        op("act", "copy", [PS[6]], [yb], out=yb[:, 0:512], in_=PS[6][:, :])
                    else:
                        op("dve", "tensor_copy", [PS[7]], [yb], out=yb[:, 512:1024], in_=PS[7][:, :])
                dma("sp", s_ys[r], [yb], [ys_k], out=ys_d[blk * BLK + r * 128:blk * BLK + (r + 1) * 128, :], in_=yb[:])

        stage1(0)
        for blk in range(NBLK):
            if blk + 1 < NBLK:
                stage1(blk + 1)
            stage2(blk)

        out_k = DR()
        fg_bc = Alias(P3b, 0, [128, D], F32)
        fence(xblk + [a_ for l_ in actb2 for a_ in l_] + sg2 + actT2, ysc + [g1bc, A2bc])
        dma("sp", s_bc, [], [fg_bc], out=fg_bc[:], in_=fg_d.partition_broadcast(128))
        s_yg2 = [SC.dma_sem("yg2_%d" % i) for i in range(2)]
        s_x1f = [SC.dma_sem("x1f%d" % i) for i in range(2)]
        h2alt = Alias(P2b, 0, [128, D], F32)
        x1alt = Alias(P5, 0, [128, D], F32)
        def F_pre(gt):
            yp = ysb if gt % 2 == 0 else ysc
            sy = s_yg if gt % 2 == 0 else s_yg2
            for k_ in range(2):
                SC.dma("pool", (lambda e, gt=gt, k_=k_, yy=yp[k_]: e.indirect_dma_start(
                    out=yy[:, :], out_offset=None, in_=ys_d, in_offset=bass.IndirectOffsetOnAxis(ap=DEST[:, gt, k_:k_ + 1], axis=0))),
                    sy[k_], [DEST.k, ys_k.k], [yp[k_].k], lat=7.0)
            xx = x1 if gt % 2 == 0 else x1alt
            dma("sp", s_x1f[gt % 2], [x1s_k], [xx], out=xx[:], in_=x1s_d[gt * 128:(gt + 1) * 128, :])

        def F_main(gt):
            b = gt // NT
            ti = gt % NT
            if ti == 0:
                dma("sp", s_bc, [mods_k], [g1bc], out=g1bc[:], in_=mods_d[b:b + 1, 5 * D:6 * D].partition_broadcast(128))
            yp = ysb if gt % 2 == 0 else ysc
            y1, y2 = yp[0], yp[1]
            hh = h2 if gt % 2 == 0 else h2alt
            xx = x1 if gt % 2 == 0 else x1alt
            sc_ = (gt % 2) * 4
            op("act", "activation", [y1, GATE], [hh], out=hh[:], in_=y1[:], func=AF.Identity, scale=GATE[:, gt, 0:1])
            op("dve", "scalar_tensor_tensor", [y2, GATE, hh], [hh], out=hh[:], in0=y2[:], scalar=GATE[:, gt, 1:2], in1=hh[:], op0=ALU.mult, op1=ALU.add)
            op("dve", "tensor_tensor", [hh, g1bc], [hh], out=hh[:], in0=hh[:], in1=g1bc[:], op=ALU.mult)
            op("pool", "tensor_tensor", [hh, xx], [hh], out=hh[:], in0=hh[:], in1=xx[:], op=ALU.add)
            op("act", "activation", [hh], [junk, stat], out=junk[:], in_=hh[:], func=AF.Square, accum_out=stat[:, sc_:sc_ + 1])
            op("dve", "tensor_scalar", [stat], [stat], out=stat[:, sc_ + 1:sc_ + 2], in0=stat[:, sc_:sc_ + 1], scalar1=1.0 / D, scalar2=EPS, op0=ALU.mult, op1=ALU.add)
            rsqrt(stat, stat[:, sc_ + 1:sc_ + 2], stat, stat[:, sc_ + 2:sc_ + 3], 1)
            xo = xin[gt % 2]
            op("dve", "scalar_tensor_tensor", [hh, stat, fg_bc], [xo], out=xo[:], in0=hh[:], scalar=stat[:, sc_ + 2:sc_ + 3], in1=fg_bc[:], op0=ALU.mult, op1=ALU.mult)
            dma("sp", s_xin[gt % 2], [xo], [out_k], out=out_d[b, ti * 128:(ti + 1) * 128, :], in_=xo[:])

        F_pre(0)
        for gt in range(NTT):
            if gt + 1 < NTT:
                F_pre(gt + 1)
            F_main(gt)
        SC.wait_all("sp", [out_k.k])
        SC.emit()
    return nc


_CACHE = {}


def kernel(**inputs):
    NCORES = 8
    x = np.asarray(inputs["x"], dtype=np.float32)
    B, S, _ = x.shape
    NSEQ = B // NCORES
    key = (S, NSEQ)
    if key not in _CACHE:
        _CACHE[key] = build_nc(S, NSEQ)
    nc = _CACHE[key]
    T = S * NSEQ
    NBLK = (2 * T + NE * (BLK - 1) + BLK - 1) // BLK
    cst_np, _ = make_consts(NBLK)
    f = lambda k: np.ascontiguousarray(np.asarray(inputs[k], dtype=np.float32))
    shared = {
        "w_ada": f("w_ada")[0], "b_ada": f("b_ada"), "norm1_g": f("norm1_g"), "w_in": f("w_in")[0],
        "q_norm_g": f("q_norm_g"), "w_uq": f("w_uq")[0], "kv_norm_g": f("kv_norm_g"), "w_ukv": f("w_ukv")[0],
        "w_o": f("w_o")[0], "norm2_g": f("norm2_g"), "w_gr": f("w_gr")[0], "b_gr": f("b_gr"),
        "w_er": f("w_er")[0].reshape(D, 32), "b_er": f("b_er").reshape(1, 32), "w1": f("w1")[0], "w3": f("w3")[0],
        "w2": f("w2")[0], "final_g": f("final_g").reshape(1, D), "cst": cst_np,
    }
    c = f("c")
    pos = np.ascontiguousarray(np.asarray(inputs["positions"], dtype=np.int32))
    in_maps = []
    for i in range(NCORES):
        m = dict(shared)
        m["x"] = np.ascontiguousarray(x[i * NSEQ:(i + 1) * NSEQ])
        m["c"] = np.ascontiguousarray(c[i * NSEQ:(i + 1) * NSEQ])
        m["positions"] = np.ascontiguousarray(pos[i * NSEQ:(i + 1) * NSEQ])
        in_maps.append(m)
    res = run_bass_kernel_spmd(nc, in_maps, core_ids=list(range(NCORES)))
    return np.concatenate([np.asarray(r["out"]) for r in res.results], axis=0).astype(np.float32)
```

```python
import math
import numpy as np
from contextlib import ExitStack
import concourse.bass as bass
import concourse.mybir as mybir
from concourse.bass_utils import run_bass_kernel_spmd

F32 = mybir.dt.float32
BF16 = mybir.dt.bfloat16
I32 = mybir.dt.int32
ALU = mybir.AluOpType
AF = mybir.ActivationFunctionType
AX = mybir.AxisListType

ENGS = ("pe", "act", "dve", "pool", "sp")
D = 1024
NE = 32
BLK = 256
EPS = 1e-6
MAGIC_RN = 12582912.0
TWO_PI = float(2 * np.pi)


class Tk:
    __slots__ = ("w", "r", "acc", "wd")

    def __init__(self, acc=False):
        self.w = None
        self.r = []
        self.acc = acc
        self.wd = {}


class Sched:
    def __init__(self, nc, stack):
        self.nc = nc
        self.stack = stack
        self.cnt = {}
        self.sems = {}
        for e in ENGS:
            self._mksem("E_" + e)
        self.nd = 0
        self.all = []
        self.tk_sems = {}
        self.tok2op = {}

    def _mksem(self, key):
        self.sems[key] = self.stack.enter_context(self.nc.semaphore(key))
        self.cnt[key] = 0
        return key

    def dma_sem(self, name=""):
        self.nd += 1
        return self._mksem("D%d_%s" % (self.nd, name))

    def _deps(self, reads, writes):
        deps = set()

        def add(tok):
            if tok is None:
                return
            k, v = tok
            if k[0] == "D":
                v = self.cnt[k]
            deps.add((k, v))
        for t in reads:
            add(t.w)
            if t.acc:
                for kv in t.wd.items():
                    add(kv)
        for t in writes:
            if t.acc:
                continue
            add(t.w)
            for tok in t.r:
                add(tok)
        return deps

    def _commit(self, tok, reads, writes):
        for t in reads:
            if not t.acc:
                t.r.append(tok)
        for t in writes:
            if t.acc:
                if t.wd.get(tok[0], 0) < tok[1]:
                    t.wd[tok[0]] = tok[1]
            else:
                t.w = tok
                t.r = []

    def op(self, eng, fn, reads=(), writes=(), cost=0.5):
        deps = self._deps(reads, writes)
        key = "E_" + eng
        self.cnt[key] += 1
        tok = (key, self.cnt[key])
        self.tok2op[tok] = len(self.all)
        self.all.append(dict(eng=eng, fn=fn, dma=False, tok=tok, deps=deps, cost=cost, lat=0.0))
        self._commit(tok, reads, writes)

    def dma(self, eng, fn, sem, reads=(), writes=(), lat=4.0):
        anchor = None
        for t in list(writes) + list(reads):
            if not t.acc:
                anchor = t
                break
        if anchor is not None:
            key = "DT%d_%s" % (id(anchor), eng)
            if key not in self.tk_sems:
                self.tk_sems[key] = self.dma_sem("t")
            sem = self.tk_sems[key]
        elif eng == "pool":
            if sem + "_p" not in self.sems:
                self._mksem(sem + "_p")
            sem = sem + "_p"
        deps = self._deps(reads, writes)
        self.cnt[sem] += 16
        tok = (sem, self.cnt[sem])
        self.tok2op[tok] = len(self.all)
        self.all.append(dict(eng=eng, fn=fn, dma=True, tok=tok, deps=deps, cost=(1.2 if eng == "pool" else 0.12), lat=lat))
        self._commit(tok, reads, writes)

    def wait_all(self, eng, tks):
        deps = self._deps(tks, ())
        self.all.append(dict(eng=eng, fn=None, dma=False, tok=None, deps=deps, cost=0.0, lat=0.0))

    def _schedule(self, W=320):
        ops = self.all
        n = len(ops)
        prod = [None] * n
        dependents = [[] for _ in range(n)]
        ndeps = [0] * n
        for i, o in enumerate(ops):
            ps = set()
            for tok in o["deps"]:
                j = self.tok2op.get(tok)
                if j is not None:
                    ps.add(j)
            prod[i] = ps
            ndeps[i] = len(ps)
            for j in ps:
                dependents[j].append(i)
        pending = {e: [] for e in ENGS}
        for i, o in enumerate(ops):
            pending[o["eng"]].append(i)
        head = {e: 0 for e in ENGS}
        done = [False] * n
        comp = [0.0] * n
        ready = [0.0] * n
        free = {e: 0.0 for e in ENGS}
        semmax = {}
        order = {e: [] for e in ENGS}
        left = n
        while left:
            best = None
            for e in ENGS:
                lst = pending[e]
                h = head[e]
                while h < len(lst) and done[lst[h]]:
                    h += 1
                head[e] = h
                if h >= len(lst):
                    continue
                seen_sems = set()
                cnt = 0
                k = h
                fe = free[e]
                while k < len(lst) and cnt < W:
                    i = lst[k]
                    k += 1
                    if done[i]:
                        continue
                    cnt += 1
                    o = ops[i]
                    if o["fn"] is None:
                        if cnt > 1:
                            continue
                    elif o["dma"]:
                        sk_ = o["tok"][0]
                        if sk_ in seen_sems:
                            continue
                        seen_sems.add(sk_)
                    if ndeps[i]:
                        continue
                    st = ready[i] if ready[i] > fe else fe
                    if best is None or st < best[0] or (st == best[0] and i < best[1]):
                        best = (st, i, e)
                    if st <= fe:
                        break
            st, i, e = best
            o = ops[i]
            done[i] = True
            left -= 1
            order[e].append(i)
            fin = st + o["cost"]
            free[e] = fin
            c = fin + o["lat"]
            if o["dma"]:
                sk = o["tok"][0]
                if semmax.get(sk, 0.0) > c:
                    c = semmax[sk]
                semmax[sk] = c
            comp[i] = c
            for d in dependents[i]:
                ndeps[d] -= 1
                if comp[i] > ready[d]:
                    ready[d] = comp[i]
        self.sim_time = max(free.values())
        return order

    def emit(self):
        import os
        ops = self.all
        if os.environ.get("K_REORDER", "1") == "1":
            order = self._schedule()
        else:
            order = {e: [] for e in ENGS}
            for i, o in enumerate(ops):
                order[o["eng"]].append(i)
        newtok = {}
        for e in ENGS:
            c = 0
            for i in order[e]:
                o = ops[i]
                if o["fn"] is not None and not o["dma"]:
                    c += 1
                    newtok[o["tok"]] = ("E_" + e, c)
        plan = {e: [] for e in ENGS}
        needed = {}
        for e in ENGS:
            wd = {}
            for i in order[e]:
                o = ops[i]
                mx = {}
                for tok in o["deps"]:
                    k, v = newtok.get(tok, tok)
                    if mx.get(k, 0) < v:
                        mx[k] = v
                waits = []
                for k, v in mx.items():
                    if wd.get(k, 0) < v:
                        wd[k] = v
                        waits.append((k, v))
                        if k[0] == "E":
                            needed.setdefault(k, set()).add(v)
                plan[e].append((waits, o))
        rank = {k: {v: r + 1 for r, v in enumerate(sorted(vs))} for k, vs in needed.items()}
        sems = self.sems

        def run(name, eng):
            for waits, o in plan[name]:
                for k, v in waits:
                    eng.wait_ge(sems[k], rank[k][v] if k[0] == "E" else v)
                if o["fn"] is not None:
                    ins = o["fn"](eng)
                    if o["dma"]:
                        ins.then_inc(sems[o["tok"][0]], 16)
                    else:
                        nt = newtok[o["tok"]]
                        if nt[1] in needed.get(nt[0], ()):
                            ins.then_inc(sems[nt[0]], 1)
        with self.nc.Block() as block:
            @block.tensor
            def _(e):
                run("pe", e)

            @block.scalar
            def _(e):
                run("act", e)

            @block.vector
            def _(e):
                run("dve", e)

            @block.gpsimd
            def _(e):
                run("pool", e)

            @block.sync
            def _(e):
                run("sp", e)


def make_consts(nblk):
    H = 4
    gam = 1.0 - 2.0 ** (-5.0 - np.arange(H))
    lg = np.log(gam)
    p = np.arange(128)
    cols = {}
    cols["ident"] = np.eye(128, dtype=np.float64)
    cols["triu"] = (p[:, None] < p[None, :]).astype(np.float64)
    cm = np.zeros((128, 4, 512))
    q = np.arange(512)
    for m in range(4):
        cm[:, m, :] = ((128 * m + p)[:, None] <= q[None, :])
    cmask_np = cm.reshape(128, -1)
    dm = np.zeros((128, 4, 128))
    for h in range(4):
        d = p[None, :] - p[:, None]
        dm[:, h, :] = np.where(d >= 0, np.exp(np.maximum(d, 0) * lg[h]), 0.0)
    cols["dmaskT"] = dm.reshape(128, -1)
    xi = np.zeros((128, 2, 128))
    for j in range(2):
        for half in range(2):
            h = 2 * j + half
            xi[half * 64:(half + 1) * 64, j, :] = np.exp((p + 1.0) * lg[h])[None, :]
    cols["xi"] = xi.reshape(128, -1)
    zt = np.zeros((128, 4, 64))
    for h in range(4):
        zt[:, h, :] = np.exp((127.0 - p) * lg[h])[:, None]
    cols["zeta"] = zt.reshape(128, -1)
    rp = np.zeros((128, 6))
    jr = p % 32
    rp[:, 0] = 10000.0 ** (-(jr / 32.0))
    blk64 = (p % 64) // 32
    rp[:, 1] = np.where(blk64 == 0, np.pi / 2, 0.0)
    rp[:, 2] = np.where(blk64 == 0, np.pi, np.pi / 2)
    jm = (p - 64) % 16
    rp[:, 3] = 10000.0 ** (-(jm / 16.0))
    b16 = ((p - 64) // 16) % 2
    rp[:, 4] = np.where(b16 == 0, np.pi / 2, 0.0)
    rp[:, 5] = np.where(b16 == 0, np.pi, np.pi / 2)
    cols["rp"] = rp
    cols["blkstart"] = np.broadcast_to((np.arange(nblk) * float(BLK))[None, :], (128, nblk))
    cols["iotap"] = p[:, None].astype(np.float64)
    cols["cd"] = np.broadcast_to(np.exp(128.0 * lg)[None, :], (128, 4))
    cols["cmask"] = cmask_np
    off = {}
    o = 0
    arrs = []
    for k, v in cols.items():
        off[k] = (o, o + v.shape[1])
        o += v.shape[1]
        arrs.append(v)
    return np.concatenate(arrs, axis=1).astype(np.float32), off


def build_nc(S, NSEQ, dbg=None):
    T = S * NSEQ
    NT = S // 128
    NTT = T // 128
    NG = S // 512
    NBLK = (2 * T + NE * (BLK - 1) + BLK - 1) // BLK
    PT = NBLK * BLK
    cst_np, coff = make_consts(NBLK)
    NC = cst_np.shape[1]
    nc = bass.Bass("TRN2", target_bir_lowering=False)

    def din(name, shape, dt=F32):
        return nc.dram_tensor(name, list(shape), dt, kind="ExternalInput").ap()

    def dscr(name, shape, dt):
        return nc.dram_tensor(name, list(shape), dt, kind="Internal").ap()

    x_d = din("x", [NSEQ, S, D])
    c_d = din("c", [NSEQ, D])
    pos_d = din("positions", [NSEQ, S], I32)
    wada_d = din("w_ada", [D, 6 * D])
    bada_d = din("b_ada", [1, 6 * D])
    n1g_d = din("norm1_g", [1, D])
    win_d = din("w_in", [D, 1952])
    qng_d = din("q_norm_g", [1, 256])
    wuq_d = din("w_uq", [256, 768])
    kvng_d = din("kv_norm_g", [1, 128])
    wukv_d = din("w_ukv", [128, 1024])
    wo_d = din("w_o", [D, D])
    n2g_d = din("norm2_g", [1, D])
    wgr_d = din("w_gr", [D, 4])
    bgr_d = din("b_gr", [1, 4])
    wer_d = din("w_er", [D, 32])
    ber_d = din("b_er", [1, 32])
    w1_d = din("w1", [NE, D, 256])
    w3_d = din("w3", [NE, D, 256])
    w2_d = din("w2", [NE, 256, D])
    fg_d = din("final_g", [1, D])
    cst_d = din("cst", [128, NC])
    out_d = nc.dram_tensor("out", [NSEQ, S, D], F32, kind="ExternalOutput").ap()

    mods_d = dscr("mods", [NSEQ, 6 * D], F32)
    mixm_d = dscr("mixm", [NSEQ, 8, 64, S], BF16)
    mixr_d = dscr("mixr", [NSEQ, 4, 128, S], BF16)
    x1s_d = dscr("x1s", [T, D], F32)
    h2s_d = dscr("h2s", [T, D], BF16)
    xs_d = dscr("xs", [PT, D], BF16)
    ys_d = dscr("ys", [PT, D], BF16)
    wall_d = dscr("wall", [NE * 128, 6144], BF16)
    csms_d = dscr("csms", [NSEQ, 2, 32, S], F32)
    dbg_out = {}
    if dbg:
        for name, shape in dbg.items():
            dbg_out[name] = nc.dram_tensor("dbg_" + name, list(shape), F32, kind="ExternalOutput").ap()

    st = ExitStack()
    with st:
        SC = Sched(nc, st)

        class Buf:
            def __init__(self, name, shape, dt, psum=False):
                if psum:
                    self.t = st.enter_context(nc.psum_tensor(name, list(shape), dt))
                else:
                    self.t = st.enter_context(nc.sbuf_tensor("sb_" + name, list(shape), dt))
                self.k = Tk()

            def __getitem__(self, idx):
                return self.t[idx]

        def sb(name, shape, dt=F32):
            return Buf(name, shape, dt)

        class Alias:
            def __init__(self, parent, off, shape, dt, own=False):
                n = 1
                for d_ in shape[1:]:
                    n *= d_
                nb = n * (4 if dt in (F32, I32) else 2)
                a = parent.t[0:shape[0], off // 4:(off + nb) // 4]
                v = a if dt == F32 else a.bitcast(dt)
                if len(shape) == 3:
                    v = v.rearrange("p (a b) -> p a b", a=shape[1])
                elif len(shape) == 4:
                    v = v.rearrange("p (a b c) -> p a b c", a=shape[1], b=shape[2])
                self.v = v
                self.k = Tk() if own else parent.k

            def __getitem__(self, idx):
                return self.v[idx]

        def _fsz(ap):
            try:
                return float(ap.free_size())
            except Exception:
                return 256.0

        def op(eng, method, reads, writes, *a, **kw):
            if eng == "pe":
                if method == "matmul":
                    cost = 0.31 + _fsz(kw["rhs"]) / 1200.0
                else:
                    cost = 0.42
            else:
                o_ = kw.get("out") if kw.get("out") is not None else (a[0] if a else None)
                f_ = _fsz(o_) if o_ is not None else 64.0
                if eng == "dve":
                    cost = 0.12 + f_ / 1100.0
                elif eng == "act":
                    cost = 0.28 + f_ / 1200.0
                else:
                    cost = 0.7 + f_ / 900.0
            SC.op(eng, lambda e: getattr(e, method)(*a, **kw), [b.k for b in reads], [b.k for b in writes], cost=cost)
            if kw.get("accum_out") is not None:
                SC.op(eng, lambda e: e.copy(out=adum[0:1, 0:2], in_=adum[0:1, 2:4]), [], [b.k for b in writes] + [adum.k], cost=0.2)

        def dma(eng, sem, reads, writes, **kw):
            try:
                nb = float(kw["out"].nbytes())
            except Exception:
                nb = 65536.0
            SC.dma(eng, lambda e: e.dma_start(**kw), sem, [b.k for b in reads], [b.k for b in writes], lat=2.5 + nb / 150e3)

        class DR:
            def __init__(self):
                self.k = Tk(acc=True)

        PS = [Buf("ps%d" % i, [128, 512], F32, psum=True) for i in range(8)]
        adum = sb("adum", [128, 4])
        SC.op("dve", lambda e: e.memset(adum[:], 0.0), [], [adum.k])

        def psbf(i):
            return PS[i].t[:].bitcast(BF16)

        NC0 = coff["cmask"][0]
        cst = sb("cst", [128, NC0])
        s_c = SC.dma_sem("cst")
        dma("sp", s_c, [], [cst], out=cst[:], in_=cst_d[:, 0:NC0])

        def cc(name):
            a, b = coff[name]
            return cst[:, a:b]
        ident_f = cc("ident")
        ident_b = sb("ident_b", [128, 128], BF16)
        triu_b = sb("triu_b", [128, 128], BF16)
        ones_b = sb("ones_b", [128, 128], BF16)
        ones_f = sb("ones_f", [128, 128], F32)
        cmask_b = sb("cmask_b", [128, 4, 512], BF16)
        op("dve", "tensor_copy", [cst], [ident_b], out=ident_b[:], in_=ident_f)
        op("dve", "tensor_copy", [cst], [triu_b], out=triu_b[:], in_=cc("triu"))
        op("dve", "memset", [], [ones_b], ones_b[:], 1.0)
        op("dve", "memset", [], [ones_f], ones_f[:], 1.0)
        dma("pool", s_c, [], [cmask_b], out=cmask_b[:].rearrange("p a b -> p (a b)"), in_=cst_d[:, NC0:NC0 + 2048])
        dmaskT = cc("dmaskT").rearrange("p (h q) -> p h q", h=4)
        xi_c = cc("xi").rearrange("p (j q) -> p j q", j=2)
        zeta_c = cc("zeta")
        rp = cc("rp")
        cd_c = cc("cd")

        nhalf = sb("nhalf", [128, 16])
        op("pool", "memset", [], [nhalf], nhalf[:], -0.5)
        fdum = sb("fdum", [128, 2])

        rs_i = sb("rs_i", [128, 16], I32)
        rs_t = sb("rs_t", [128, 16], F32)
        import os as _os
        USE_POW = _os.environ.get("K_POW", "1") == "1"

        def rsqrt(vbuf, vap, outbuf, outap, n):
            if USE_POW:
                op("pool", "tensor_tensor", [vbuf, nhalf], [outbuf], out=outap, in0=vap, in1=nhalf[:, 0:n], op=ALU.pow)
                return
            yi = rs_i[:, 0:n]
            y = yi.bitcast(F32)
            tt = rs_t[:, 0:n]
            op("dve", "tensor_single_scalar", [vbuf], [rs_i], out=yi, in_=vap.bitcast(I32), scalar=1, op=ALU.arith_shift_right)
            op("dve", "tensor_scalar", [rs_i], [rs_i], out=yi, in0=yi, scalar1=-1.0, scalar2=float(0x5f3759df), op0=ALU.mult, op1=ALU.add)
            for it in range(3):
                op("dve", "tensor_tensor", [rs_i], [rs_t], out=tt, in0=y, in1=y, op=ALU.mult)
                op("dve", "tensor_tensor", [rs_t, vbuf], [rs_t], out=tt, in0=tt, in1=vap, op=ALU.mult)
                op("dve", "tensor_scalar", [rs_t], [rs_t], out=tt, in0=tt, scalar1=-0.5, scalar2=1.5, op0=ALU.mult, op1=ALU.add)
                if it < 2:
                    op("dve", "tensor_tensor", [rs_t, rs_i], [rs_i], out=y, in0=y, in1=tt, op=ALU.mult)
                else:
                    op("dve", "tensor_tensor", [rs_t, rs_i], [outbuf], out=outap, in0=y, in1=tt, op=ALU.mult)

        def fence(frm, to):
            SC.op("pool", lambda e: e.memset(fdum[0:1, 0:1], 0.0), [], [b_.k for b_ in frm] + [b_.k for b_ in to] + [fdum.k])

        s_m = SC.dma_sem("mods")
        mods_k = DR()
        cT = sb("cT", [128, 8, NSEQ])
        cTe = sb("cTe", [128, 8, NSEQ])
        siluT = sb("siluT", [128, 8, NSEQ], BF16)
        for b0 in range(NSEQ):
            dma("sp", s_m, [], [cT], out=cT[:, :, b0], in_=c_d[b0:b0 + 1, :].rearrange("o (c p) -> p (o c)", p=128), allow_slow_non_contiguous=True)
        op("act", "activation", [cT], [cTe], out=cTe[:], in_=cT[:], func=AF.Exp, scale=-1.0)
        op("dve", "tensor_scalar", [cTe], [cTe], out=cTe[:], in0=cTe[:], scalar1=1.0, scalar2=None, op0=ALU.add)
        op("dve", "reciprocal", [cTe], [cTe], out=cTe[:], in_=cTe[:])
        op("dve", "tensor_tensor", [cTe, cT], [siluT], out=siluT[:], in0=cTe[:], in1=cT[:], op=ALU.mult)
        BIGW = sb("BIGW", [128, 12448])
        P0 = sb("P0", [128, 2048])
        wa = [Alias(BIGW, 0, [128, 8, 512], BF16, own=True), Alias(BIGW, 8192, [128, 8, 512], BF16, own=True)]
        s_wa = [SC.dma_sem("wa%d" % i) for i in range(2)]
        ba = sb("ba", [NSEQ, 512])
        mrow = sb("mrow", [NSEQ, 512])
        s_ba = SC.dma_sem("ba")
        for j in range(12):
            w = wa[j % 2]
            dma("pool", s_wa[j % 2], [], [w], out=w[:], in_=wada_d[:, j * 512:(j + 1) * 512].rearrange("(c p) n -> p c n", p=128))
            dma("sp", s_ba, [], [ba], out=ba[:], in_=bada_d[:, j * 512:(j + 1) * 512].partition_broadcast(NSEQ))
            for k in range(8):
                op("pe", "matmul", [siluT, w], [PS[0]], PS[0][0:NSEQ, :], lhsT=siluT[:, k, :], rhs=w[:, k, :], start=(k == 0), stop=(k == 7))
            op("dve", "tensor_tensor", [PS[0], ba], [mrow], out=mrow[:], in0=PS[0][0:NSEQ, :], in1=ba[:], op=ALU.add)
            dma("sp", s_m, [mrow], [mods_k], out=mods_d[:, j * 512:(j + 1) * 512], in_=mrow[:])

        wblk = [Alias(BIGW, 0, [128, 6144], BF16, own=True), Alias(BIGW, 12288, [128, 6144], BF16, own=True)]
        s_wl = SC.dma_sem("wl")
        s_ws = SC.dma_sem("ws")
        wall_k = DR()

        def relayout_expert(e):
            stg = wblk[e % 2]
            v13 = stg[:, 0:4096].rearrange("p (c f) -> p c f", c=8)
            dma("pool", s_wl, [], [stg], out=v13[:, :, 0:256], in_=w1_d[e].rearrange("(c p) f -> p c f", p=128))
            dma("pool", s_wl, [], [stg], out=v13[:, :, 256:512], in_=w3_d[e].rearrange("(c p) f -> p c f", p=128))
            dma("pool", s_wl, [], [stg], out=stg[:, 4096:6144].rearrange("p (c f) -> p c f", c=2),
                in_=w2_d[e].rearrange("(c p) f -> p c f", p=128))
            dma("pool", s_ws, [stg], [wall_k], out=wall_d[e * 128:(e + 1) * 128, :], in_=stg[:])
        n_slots = NSEQ * 8 * NG
        per_slot = (NE + n_slots - 1) // n_slots
        relay_state = [0]

        fence(wa, [BIGW])
        s_w = SC.dma_sem("w")
        NFM = 8 * 128 + 2 * 96
        w_fm = Alias(BIGW, 0, [128, 8, NFM], BF16)
        w_tm = Alias(BIGW, 19456, [128, 8, 1408], BF16)
        wst = [Alias(BIGW, 41984, [128, 1952], F32)] * 2
        s_wst = [SC.dma_sem("wst%d" % i) for i in range(2)]
        wuq_a = sb("wuq_a", [128, 2, 8, 192], BF16)
        STG = P0
        wuq_s = Alias(STG, 0, [128, 2, 768], F32)
        qng_c = sb("qng_c", [128, 2])
        dma("sp", s_w, [], [wuq_s], out=wuq_s[:], in_=wuq_d.rearrange("(c p) n -> p c n", p=128))
        dma("sp", s_w, [], [qng_c], out=qng_c[:], in_=qng_d.rearrange("o (c p) -> p (o c)", p=128), allow_slow_non_contiguous=True)
        op("pool", "memset", [], [wuq_a], wuq_a[:], 0.0)
        for c in range(2):
            s4 = wuq_s[:, c, :].rearrange("p (h f) -> p h f", h=8)
            op("dve", "tensor_scalar", [wuq_s, qng_c], [wuq_a], out=wuq_a[:, c, :, 0:64], in0=s4[:, :, 0:64],
               scalar1=qng_c[:, c:c + 1], scalar2=None, op0=ALU.mult)
            for ab in range(2):
                dst = wuq_a[:, c, :, ab * 96 + 64:ab * 96 + 96].rearrange("p h (dup j) -> p h dup j", dup=2)
                src = s4[:, :, 64 + ab * 16:64 + ab * 16 + 16].unsqueeze(2).to_broadcast([128, 8, 2, 16])
                op("dve", "tensor_scalar", [wuq_s, qng_c], [wuq_a], out=dst, in0=src,
                   scalar1=qng_c[:, c:c + 1], scalar2=None, op0=ALU.mult)
        wukv_s = Alias(STG, 0, [128, 1024], F32)
        kvng_c = sb("kvng_c", [128, 1])
        wk_b = sb("wk_b", [128, 8, 64], BF16)
        wv_b = sb("wv_b", [128, 8, 64], BF16)
        dma("sp", s_w, [], [wukv_s], out=wukv_s[:], in_=wukv_d)
        dma("sp", s_w, [], [kvng_c], out=kvng_c[:], in_=kvng_d.rearrange("o p -> p o"), allow_slow_non_contiguous=True)
        s3 = wukv_s[:].rearrange("p (h f) -> p h f", h=8)
        op("dve", "tensor_scalar", [wukv_s, kvng_c], [wk_b], out=wk_b[:], in0=s3[:, :, 0:64], scalar1=kvng_c[:, 0:1], scalar2=None, op0=ALU.mult)
        op("dve", "tensor_scalar", [wukv_s, kvng_c], [wv_b], out=wv_b[:], in0=s3[:, :, 64:128], scalar1=kvng_c[:, 0:1], scalar2=None, op0=ALU.mult)
        wo_m = Alias(BIGW, 0, [64, 8, D], BF16)
        wo_r = Alias(BIGW, 16384, [128, 4, D], BF16)
        w_rt = sb("w_rt", [128, 8, 36])
        b_rt = sb("b_rt", [128, 36])
        dma("sp", s_w, [], [w_rt], out=w_rt[:, :, 0:4], in_=wgr_d.rearrange("(c p) n -> p c n", p=128), allow_slow_non_contiguous=True)
        dma("sp", s_w, [], [w_rt], out=w_rt[:, :, 4:36], in_=wer_d.rearrange("(c p) n -> p c n", p=128), allow_slow_non_contiguous=True)
        dma("sp", s_w, [], [b_rt], out=b_rt[:, 0:4], in_=bgr_d.partition_broadcast(128))
        dma("sp", s_w, [], [b_rt], out=b_rt[:, 4:36], in_=ber_d.partition_broadcast(128))
        n1g_c = sb("n1g_c", [128, 8])
        dma("sp", s_w, [], [n1g_c], out=n1g_c[:], in_=n1g_d.rearrange("o (c p) -> p (o c)", p=128), allow_slow_non_contiguous=True)

        OH1 = sb("OH1", [128, NTT, 32], BF16)
        OH2 = sb("OH2", [128, NTT, 32], BF16)
        CUM = sb("CUM", [128, NTT, 32])
        GATE = sb("GATE", [128, NTT, 2])
        Macc = sb("Macc", [128, 32], BF16)
        op("pool", "memset", [], [Macc], Macc[:], 0.0)

        cqnT = sb("cqnT", [128, 2, S], BF16)
        ckvnT = sb("ckvnT", [128, S], BF16)
        kT = sb("kT", [96, S], BF16)
        s_csm = SC.dma_sem("csm")
        csms_k = DR()
        xin = [sb("xin%d" % i, [128, D]) for i in range(2)]
        s_xin = [SC.dma_sem("xin%d" % i) for i in range(2)]
        junk = sb("junk", [128, D], BF16)
        stat = sb("stat", [128, 16])
        xsb = sb("xsb", [128, D], BF16)
        xsb2 = [xsb, sb("xsb1", [128, D], BF16)]
        P1 = sb("P1", [128, 2048])
        h1T = Alias(P1, 0, [128, 8, 512], BF16)
        P2b = sb("P2b", [128, 1024])
        posi = Alias(P2b, 0, [128, 512], I32)
        posf = Alias(P2b, 2048, [128, 512], F32)
        s_pos = SC.dma_sem("pos")
        P2a = sb("P2a", [128, 1024])
        targ = Alias(P2a, 0, [128, 512], F32)
        ttmp = Alias(P2a, 2048, [128, 512], F32)
        P3a = sb("P3a", [128, 1024])
        csr1 = Alias(P3a, 0, [128, 512], F32)
        csr2 = Alias(P3a, 2048, [128, 512], F32)
        P3b = sb("P3b", [128, 1024])
        csm1f = Alias(P3b, 0, [96, 512], F32)
        csm2f = Alias(P3b, 2048, [96, 512], F32)
        colv = sb("colv", [128, 8, 4])
        s_col = SC.dma_sem("col")
        P6 = sb("P6", [128, 1024])
        P7 = sb("P7", [128, 512])
        rqT = Alias(P6, 0, [128, 2, 512], BF16)
        rqxT = Alias(P6, 2048, [128, 2, 512], BF16)
        rkT = Alias(P7, 0, [128, 2, 512], BF16)
        P4a = sb("P4a", [128, 1024])
        rt1 = Alias(P4a, 0, [128, 512], F32)
        rt2 = Alias(P4a, 2048, [128, 512], F32)
        cqn = sb("cqn", [128, 384], BF16)
        P4b = sb("P4b", [128, 1024])
        P4c = sb("P4c", [128, 1024])
        RVG = [Alias(P4b, 0, [128, 4, 512], BF16), Alias(P4c, 0, [128, 4, 512], BF16)]
        GTG = [Alias(P0, 0, [128, 4, 512], BF16), Alias(P0, 4096, [128, 4, 512], BF16)]
        GT4 = GTG[0]
        gsg = sb("gsg", [128, 512])
        rkz = sb("rkz", [128, 256], BF16)
        sdT = sb("sdT", [128, 4, 128], BF16)
        state = sb("state", [128, 2, 128])
        state_b = sb("state_b", [128, 2, 128], BF16)
        P8 = sb("P8", [128, 1024])
        osb = Alias(P8, 0, [128, 4, 128], F32)
        osq = Alias(P8, 2048, [128, 4, 128], F32)
        gst = sb("gst", [128, 16])
        oretb = sb("oretb", [128, 512], BF16)
        P5 = sb("P5", [128, 1024])
        oretT = Alias(P5, 0, [128, 4, 512], BF16)
        s_mixr = SC.dma_sem("mixr")
        mixr_k = DR()
        mixm_k = DR()

        def rope_table(dst, dstap, prow, invf_col, ph_col):
            a = targ[prow, :]
            b = ttmp[prow, :]
            op("dve", "tensor_scalar", [posf, cst], [targ], out=a, in0=posf[prow, :], scalar1=rp[prow, invf_col:invf_col + 1],
               scalar2=rp[prow, ph_col:ph_col + 1], op0=ALU.mult, op1=ALU.add)
            op("dve", "tensor_scalar", [targ], [ttmp], out=b, in0=a, scalar1=1.0 / TWO_PI, scalar2=MAGIC_RN, op0=ALU.mult, op1=ALU.add)
            op("dve", "tensor_scalar", [ttmp], [ttmp], out=b, in0=b, scalar1=MAGIC_RN, scalar2=-TWO_PI, op0=ALU.subtract, op1=ALU.mult)
            op("dve", "tensor_tensor", [ttmp, targ], [targ], out=a, in0=a, in1=b, op=ALU.add)
            op("dve", "tensor_scalar", [targ], [targ], out=a, in0=a, scalar1=-3.1415925, scalar2=3.1415925, op0=ALU.max, op1=ALU.min)
            op("act", "activation", [targ], [dst], out=dstap, in_=a, func=AF.Sin)


        vh = [sb("vh0", [128, NT, 65], BF16)] * 2
        for i in range(1):
            op("pool", "memset", [], [vh[i]], vh[i][:, :, 64:65], 1.0)
        pT = [Alias(P5, 0, [128, 512], BF16, own=True), Alias(P5, 1024, [128, 512], BF16, own=True)]
        qT = [Alias(P5, 2048, [96, 512], BF16, own=True), Alias(P5, 3072, [96, 512], BF16, own=True)]
        rrow = Alias(P8, 0, [65, 512], F32)
        bcs = Alias(P8, 2048, [64, 512], F32)
        oTm = Alias(P7, 0, [64, 512], BF16)
        s_mixm = SC.dma_sem("mixm")
        s_bc = SC.dma_sem("bc")
        s_mm = SC.dma_sem("mm")
        s_x1 = SC.dma_sem("x1")
        s_h2 = SC.dma_sem("h2")
        x1s_k = DR()
        h2s_k = DR()
        g1bc = Alias(P2a, 0, [128, D], F32)
        sh2bc = Alias(P2b, 0, [128, D], F32)
        A2bc = Alias(P3a, 0, [128, D], F32)
        n2gbc = Alias(P3b, 0, [128, D], F32)
        mm_t = Alias(P6, 0, [64, 8, 128], BF16)
        mr_t = Alias(P6, 2048, [128, 4, 128], BF16)
        x1 = Alias(P4a, 0, [128, D], F32)
        h2 = Alias(P4b, 0, [128, D], F32)
        h2b = Alias(P7, 0, [128, D], BF16)
        h2T = Alias(P5, 0, [128, 8, 128], F32)
        x1_2 = [x1, Alias(P0, 0, [128, D], F32, own=True)]
        h2_2 = [h2, Alias(P0, 4096, [128, D], F32, own=True)]
        h2b_2 = [h2b, Alias(P4c, 0, [128, D], BF16, own=True)]
        h2T_2 = [h2T, Alias(P1, 0, [128, 8, 128], F32, own=True)]
        mm_t_2 = [mm_t, Alias(P1, 4096, [64, 8, 128], BF16, own=True)]
        mr_t_2 = [mr_t, Alias(P1, 6144, [128, 4, 128], BF16, own=True)]
        c_alts = [x1_2[1], h2_2[1], h2b_2[1], h2T_2[1], mm_t_2[1], mr_t_2[1]]
        s_mm2 = [s_mm, SC.dma_sem("mm1")]
        lgt2 = [sb("lgt%d" % i, [128, 40]) for i in range(2)]
        for i in range(2):
            op("dve", "memset", [], [lgt2[i]], lgt2[i][:], -1e30)
        m8_2 = [sb("m8_%d" % i, [128, 16]) for i in range(2)]
        rst_2 = [sb("rst_%d" % i, [128, 20]) for i in range(2)]
        lem_2 = [sb("lem_%d" % i, [128, 32]) for i in range(2)]
        Mt_2 = [sb("Mt_%d" % i, [128, 32], BF16) for i in range(2)]
        ra = Alias(P2a, 0, [128, 32], F32)
        rb = Alias(P2a, 128, [128, 32], F32)
        pad_ = Alias(P2a, 256, [128, 32], F32)
        pst = Alias(P2a, 384, [128, 32], F32)
        cmp3 = Alias(BIGW, 0, [128, NBLK, 32], F32)
        ebf = sb("ebf", [128, NBLK])
        WIDX = sb("WIDX", [128, NBLK], I32)
        cmpd = Alias(BIGW, 16384, [128, NTT, 32], F32)
        destf = Alias(P2b, 0, [128, NTT, 2], F32)
        DEST = sb("DEST", [128, NTT, 2], I32)
        h2r = [Alias(P6, 0, [128, D], BF16, own=True), Alias(P6, 2048, [128, D], BF16, own=True)]
        s_h2r = [SC.dma_sem("h2r%d" % i) for i in range(2)]
        s_sc = SC.dma_sem("sc")
        s_wg = [SC.dma_sem("wg%d" % i) for i in range(2)]
        xblk = [Alias(P1, 0, [128, 2, D], BF16, own=True), Alias(P1, 4096, [128, 2, D], BF16, own=True)]
        s_xb = [SC.dma_sem("xb%d" % i) for i in range(2)]
        xTb = Alias(P8, 0, [128, 2, 8, 128], BF16)
        sg = Alias(P2a, 0, [128, 256], F32)
        actb = Alias(P2a, 1024, [128, 256], BF16)
        actT = Alias(P2a, 1536, [128, 2, 128], BF16)
        ysb = [Alias(P0, 0, [128, D], BF16, own=True), Alias(P0, 4096, [128, D], BF16, own=True)]
        ysc = [Alias(P1, 0, [128, D], BF16, own=True), Alias(P1, 4096, [128, D], BF16, own=True)]
        s_ys = [SC.dma_sem("ys%d" % i) for i in range(2)]
        s_yg = [SC.dma_sem("yg%d" % i) for i in range(2)]
        for b in range(NSEQ):
            op("pool", "memset", [], [w_fm], w_fm[:, :, 1024:NFM], 0.0)
            for k in range(8):
                ws = wst[k % 2]
                dma("sp", s_wst[k % 2], [], [ws], out=ws[:], in_=win_d[k * 128:(k + 1) * 128, :])
                op("act", "copy", [ws], [w_tm], out=w_tm[:, k, 0:384], in_=ws[:, 0:384])
                op("act", "copy", [ws], [w_tm], out=w_tm[:, k, 384:1408], in_=ws[:, 928:1952])
                for which, base, scale in ((0, 416, 1.0), (1, 672, 0.125)):
                    src = ws[:, base:base + 256].rearrange("p (h two j) -> p h two j", h=4, two=2)
                    for ab in range(2):
                        dst = w_fm[:, k, (which * 4 + ab * 2) * 128:(which * 4 + ab * 2 + 2) * 128].rearrange(
                            "p (h dup j) -> p h dup j", h=4, dup=2)
                        op("dve", "tensor_scalar", [ws], [w_fm], out=dst,
                           in0=src[:, :, ab:ab + 1, :].to_broadcast([128, 4, 2, 32]), scalar1=scale, scalar2=None, op0=ALU.mult)
                srck = ws[:, 384:416].rearrange("p (two j) -> p two j", two=2)
                for ab in range(2):
                    dst = w_fm[:, k, 1024 + ab * 96 + 64:1024 + ab * 96 + 96].rearrange("p (dup j) -> p dup j", dup=2)
                    op("dve", "tensor_copy", [ws], [w_fm], out=dst, in_=srck[:, ab:ab + 1, :].to_broadcast([128, 2, 16]))
            dma("sp", s_col, [mods_k], [colv], out=colv[:, :, 0], in_=mods_d[b:b + 1, 0:D].rearrange("o (c p) -> p (o c)", p=128),
                allow_slow_non_contiguous=True)
            dma("sp", s_col, [mods_k], [colv], out=colv[:, :, 1], in_=mods_d[b:b + 1, D:2 * D].rearrange("o (c p) -> p (o c)", p=128),
                allow_slow_non_contiguous=True)
            op("dve", "scalar_tensor_tensor", [colv, n1g_c], [colv], out=colv[:, :, 2], in0=colv[:, :, 1], scalar=1.0, in1=n1g_c[:],
               op0=ALU.add, op1=ALU.mult)
            op("dve", "memset", [], [state], state[:], 0.0)
            op("dve", "memset", [], [state_b], state_b[:], 0.0)

            def A_pos(g):
                t0 = g * 512
                dma("sp", s_pos, [], [posi], out=posi[:], in_=pos_d[b:b + 1, t0:t0 + 512].partition_broadcast(128))
                op("dve", "tensor_copy", [posi], [posf], out=posf[:], in_=posi[:])

            def A_table(g, k):
                t0 = g * 512
                if k == 0:
                    rope_table(csr1, csr1[:], slice(0, 128), 0, 1)
                elif k == 1:
                    rope_table(csr2, csr2[:], slice(0, 128), 0, 2)
                elif k == 2:
                    rope_table(csm1f, csm1f[64:96, :], slice(64, 96), 3, 4)
                    dma("sp", s_csm, [csm1f], [csms_k], out=csms_d[b, 0, :, t0:t0 + 512], in_=csm1f[64:96, :])
                else:
                    rope_table(csm2f, csm2f[64:96, :], slice(64, 96), 3, 5)
                    dma("sp", s_csm, [csm2f], [csms_k], out=csms_d[b, 1, :, t0:t0 + 512], in_=csm2f[64:96, :])

            def A_S1(g, tl):
                ti = g * 4 + tl
                tok0 = ti * 128
                xi_ = xin[ti % 2]
                xs_ = xsb2[ti % 2]
                so = 10 + 3 * (ti % 2)
                dma("sp", s_xin[ti % 2], [], [xi_], out=xi_[:], in_=x_d[b, tok0:tok0 + 128, :])
                op("act", "activation", [xi_], [junk, stat], out=junk[:], in_=xi_[:], func=AF.Square, accum_out=stat[:, so:so + 1])
                op("dve", "tensor_scalar", [stat], [stat], out=stat[:, so + 1:so + 2], in0=stat[:, so:so + 1], scalar1=1.0 / D, scalar2=EPS,
                   op0=ALU.mult, op1=ALU.add)
                rsqrt(stat, stat[:, so + 1:so + 2], stat, stat[:, so + 2:so + 3], 1)
                op("dve", "tensor_scalar", [xi_, stat], [xs_], out=xs_[:], in0=xi_[:], scalar1=stat[:, so + 2:so + 3], scalar2=None, op0=ALU.mult)

            def A_T8(g, tl):
                ti = g * 4 + tl
                xs_ = xsb2[ti % 2]
                for c in range(8):
                    op("pe", "transpose", [xs_, ident_b], [PS[0]], out=psbf(0)[:, c * 128:(c + 1) * 128],
                       in_=xs_[:, c * 128:(c + 1) * 128], identity=ident_b[:])
                for c in range(8):
                    if c % 2 == 0:
                        op("dve", "tensor_scalar", [PS[0], colv], [h1T], out=h1T[:, c, tl * 128:(tl + 1) * 128],
                           in0=psbf(0)[:, c * 128:(c + 1) * 128], scalar1=colv[:, c, 2:3], scalar2=colv[:, c, 0:1],
                           op0=ALU.mult, op1=ALU.add)
                    else:
                        op("act", "activation", [PS[0], colv], [h1T], out=h1T[:, c, tl * 128:(tl + 1) * 128],
                           in_=psbf(0)[:, c * 128:(c + 1) * 128], func=AF.Identity, scale=colv[:, c, 2:3], bias=colv[:, c, 0:1])

            def A_MM(g, tl):
                RV4 = RVG[g % 2]
                GT4 = GTG[g % 2]
                ti = g * 4 + tl
                tok0 = ti * 128
                for (pb, c0, n) in ((1, 0, 384), (2, 384, 512), (3, 896, 512)):
                    for k in range(8):
                        op("pe", "matmul", [h1T, w_tm], [PS[pb]], PS[pb][:, 0:n], lhsT=h1T[:, k, tl * 128:(tl + 1) * 128],
                           rhs=w_tm[:, k, c0:c0 + n], start=(k == 0), stop=(k == 7))
                op("act", "activation", [PS[1]], [junk, stat], out=junk[:, 0:256], in_=PS[1][:, 0:256], func=AF.Square, accum_out=stat[:, 4:5])
                op("act", "activation", [PS[1]], [junk, stat], out=junk[:, 256:384], in_=PS[1][:, 256:384], func=AF.Square, accum_out=stat[:, 5:6])
                op("dve", "tensor_scalar", [stat], [stat], out=stat[:, 6:7], in0=stat[:, 4:5], scalar1=1.0 / 256, scalar2=EPS, op0=ALU.mult, op1=ALU.add)
                op("dve", "tensor_scalar", [stat], [stat], out=stat[:, 7:8], in0=stat[:, 5:6], scalar1=1.0 / 128, scalar2=EPS, op0=ALU.mult, op1=ALU.add)
                rsqrt(stat, stat[:, 6:8], stat, stat[:, 8:10], 2)
                op("dve", "tensor_scalar", [PS[1], stat], [cqn], out=cqn[:, 0:256], in0=PS[1][:, 0:256], scalar1=stat[:, 8:9], scalar2=None, op0=ALU.mult)
                op("dve", "tensor_scalar", [PS[1], stat], [cqn], out=cqn[:, 256:384], in0=PS[1][:, 256:384], scalar1=stat[:, 9:10], scalar2=None, op0=ALU.mult)
                for c in range(3):
                    op("pe", "transpose", [cqn, ident_b], [PS[0]], out=psbf(0)[:, c * 128:(c + 1) * 128],
                       in_=cqn[:, c * 128:(c + 1) * 128], identity=ident_b[:])
                op("act", "copy", [PS[0]], [cqnT], out=cqnT[:, :, tok0:tok0 + 128],
                   in_=psbf(0)[:, 0:256].rearrange("p (c t) -> p c t", c=2))
                op("act", "copy", [PS[0]], [ckvnT], out=ckvnT[:, tok0:tok0 + 128], in_=psbf(0)[:, 256:384])
                op("act", "copy", [PS[2]], [RV4], out=RV4[:, tl, :], in_=PS[2][:])
                op("act", "activation", [PS[3]], [gsg], out=gsg[:], in_=PS[3][:], func=AF.Tanh, scale=0.5)
                op("dve", "scalar_tensor_tensor", [gsg, PS[3]], [GT4], out=GT4[:, tl, :], in0=gsg[:], scalar=1.0, in1=PS[3][:], op0=ALU.add, op1=ALU.mult)

            def A_FM(g):
                t0 = g * 512

                def fm_mm(pb, col0, ncols):
                    for k in range(8):
                        op("pe", "matmul", [h1T, w_fm], [PS[pb]], PS[pb][0:ncols, :], lhsT=w_fm[:, k, col0:col0 + ncols],
                           rhs=h1T[:, k, :], start=(k == 0), stop=(k == 7))
                for which, dst in ((0, rqT), (1, rkT)):
                    for j in range(2):
                        fm_mm(4, (which * 4 + j) * 128, 128)
                        fm_mm(5, (which * 4 + 2 + j) * 128, 128)
                        op("dve", "tensor_tensor", [PS[4], csr1], [rt1], out=rt1[:], in0=PS[4][:], in1=csr1[:], op=ALU.mult)
                        op("dve", "tensor_tensor", [PS[5], csr2], [rt2], out=rt2[:], in0=PS[5][:], in1=csr2[:], op=ALU.mult)
                        op("pool", "tensor_tensor", [rt1, rt2], [dst], out=dst[:, j, :], in0=rt1[:], in1=rt2[:], op=ALU.add)
                op("pool", "tensor_tensor", [rqT, cst], [rqxT], out=rqxT[:].rearrange("p j (n q) -> p j n q", n=4),
                   in0=rqT[:].rearrange("p j (n q) -> p j n q", n=4), in1=xi_c.unsqueeze(2).to_broadcast([128, 2, 4, 128]), op=ALU.mult)
                fm_mm(4, 1024, 96)
                fm_mm(5, 1120, 96)
                op("dve", "tensor_tensor", [PS[4], csm1f], [rt1], out=rt1[64:96, :], in0=PS[4][64:96, :], in1=csm1f[64:96, :], op=ALU.mult)
                op("dve", "tensor_tensor", [PS[5], csm2f], [rt2], out=rt2[64:96, :], in0=PS[5][64:96, :], in1=csm2f[64:96, :], op=ALU.mult)
                op("pool", "tensor_tensor", [rt1, rt2], [kT], out=kT[64:96, t0:t0 + 512], in0=rt1[64:96, :], in1=rt2[64:96, :], op=ALU.add)

            def A_RETa(g, tl):
                RV4 = RVG[g % 2]
                qs = slice(tl * 128, (tl + 1) * 128)
                for j in range(2):
                    op("pe", "transpose", [rkT, ident_b], [PS[4]], out=psbf(4)[:, j * 128:(j + 1) * 128], in_=rkT[:, j, qs], identity=ident_b[:])
                op("dve", "tensor_tensor", [PS[4], cst], [rkz], out=rkz[:], in0=psbf(4)[:, 0:256], in1=zeta_c, op=ALU.mult)
                for h in range(4):
                    j, half = h // 2, h % 2
                    pr = slice(half * 64, half * 64 + 64)
                    op("pe", "matmul", [rkT, rqT], [PS[6]], PS[6][:, h * 128:(h + 1) * 128], lhsT=rkT[pr, j, qs], rhs=rqT[pr, j, qs],
                       start=True, stop=True)
                op("dve", "tensor_tensor", [PS[6], cst], [sdT], out=sdT[:], in0=PS[6][:].rearrange("p (h q) -> p h q", h=4), in1=dmaskT, op=ALU.mult)
                for h in range(4):
                    j, half = h // 2, h % 2
                    pr = slice(half * 64, half * 64 + 64)
                    op("pe", "matmul", [sdT, RV4], [PS[7]], PS[7][:, h * 128:(h + 1) * 128], lhsT=sdT[:, h, :], rhs=RV4[:, tl, h * 128:(h + 1) * 128],
                       start=True, stop=False)
                    op("pe", "matmul", [rqxT, state_b], [PS[7]], PS[7][:, h * 128:(h + 1) * 128], lhsT=rqxT[pr, j, qs], rhs=state_b[pr, j, :],
                       start=False, stop=True)
                for h in range(4):
                    j = h // 2
                    op("pe", "matmul", [rkz, RV4], [PS[6]], PS[6][:, h * 128:(h + 1) * 128], lhsT=rkz[:, j * 128:(j + 1) * 128],
                       rhs=RV4[:, tl, h * 128:(h + 1) * 128], start=True, stop=True)
                for h in range(4):
                    j, half = h // 2, h % 2
                    pr = slice(half * 64, half * 64 + 64)
                    op("dve", "scalar_tensor_tensor", [state, cst, PS[6]], [state], out=state[pr, j, :], in0=state[pr, j, :],
                       scalar=cd_c[pr, h:h + 1], in1=PS[6][pr, h * 128:(h + 1) * 128], op0=ALU.mult, op1=ALU.add)
                op("pool", "tensor_copy", [state], [state_b], out=state_b[:], in_=state[:])

            def A_RETb_dve(g, tl):
                GT4 = GTG[g % 2]
                op("act", "copy", [PS[7]], [osb], out=osb[:], in_=PS[7][:].rearrange("p (h d) -> p h d", h=4))
                op("dve", "tensor_reduce", [osb], [gst], out=gst[:, 0:4], in_=osb[:], axis=AX.X, op=ALU.add)
                op("pool", "tensor_tensor", [osb], [osq], out=osq[:], in0=osb[:], in1=osb[:], op=ALU.mult)
                op("dve", "tensor_reduce", [osq], [gst], out=gst[:, 4:8], in_=osq[:], axis=AX.X, op=ALU.add)
                op("dve", "tensor_scalar", [gst], [gst], out=gst[:, 0:4], in0=gst[:, 0:4], scalar1=1.0 / 128, scalar2=None, op0=ALU.mult)
                op("dve", "tensor_tensor", [gst], [gst], out=gst[:, 8:12], in0=gst[:, 0:4], in1=gst[:, 0:4], op=ALU.mult)
                op("dve", "scalar_tensor_tensor", [gst], [gst], out=gst[:, 8:12], in0=gst[:, 4:8], scalar=1.0 / 128, in1=gst[:, 8:12],
                   op0=ALU.mult, op1=ALU.subtract)
                op("dve", "tensor_scalar", [gst], [gst], out=gst[:, 8:12], in0=gst[:, 8:12], scalar1=EPS, scalar2=None, op0=ALU.add)
                rsqrt(gst, gst[:, 8:12], gst, gst[:, 12:16], 4)
                op("dve", "tensor_scalar", [gst], [gst], out=gst[:, 12:16], in0=gst[:, 12:16], scalar1=0.5, scalar2=None, op0=ALU.mult)
                op("dve", "tensor_tensor", [osb, gst], [osb], out=osb[:], in0=osb[:], in1=gst[:, 0:4].unsqueeze(2).to_broadcast([128, 4, 128]), op=ALU.subtract)
                op("dve", "tensor_tensor", [osb, gst], [osb], out=osb[:], in0=osb[:], in1=gst[:, 12:16].unsqueeze(2).to_broadcast([128, 4, 128]), op=ALU.mult)
                op("pool", "tensor_tensor", [osb, GT4], [oretb], out=oretb[:], in0=osb[:].rearrange("p h d -> p (h d)"), in1=GT4[:, tl, :], op=ALU.mult)

            def A_RETb_pe(g, tl):
                qs = slice(tl * 128, (tl + 1) * 128)
                for h in range(4):
                    op("pe", "transpose", [oretb, ident_b], [PS[5]], out=psbf(5)[:, h * 128:(h + 1) * 128], in_=oretb[:, h * 128:(h + 1) * 128], identity=ident_b[:])
                op("act", "copy", [PS[5]], [oretT], out=oretT[:, :, qs], in_=psbf(5)[:, 0:512].rearrange("p (h t) -> p h t", h=4))

            A_S1(0, 0)
            for g in range(NG + 1):
                if g < NG:
                    A_pos(g)
                for tl in range(4):
                    if g < NG:
                        A_T8(g, tl)
                    nxt = g * 4 + tl + 1
                    if nxt < NG * 4:
                        A_S1(nxt // 4, nxt % 4)
                    if g >= 1:
                        A_RETa(g - 1, tl)
                    if g >= 1 and tl > 0:
                        A_RETb_pe(g - 1, tl - 1)
                    if g < NG:
                        A_MM(g, tl)
                    if g >= 1:
                        A_RETb_dve(g - 1, tl)
                    if g < NG:
                        A_table(g, tl)
                if g < NG:
                    A_FM(g)
                if g >= 1:
                    A_RETb_pe(g - 1, 3)
                    dma("sp", s_mixr, [oretT], [mixr_k], out=mixr_d[b, :, :, (g - 1) * 512:g * 512].rearrange("h p t -> p h t"), in_=oretT[:])
            fence([oretT, BIGW], pT + qT + wblk)

            def qprep(h, i):
                qsl = slice(i * 512, (i + 1) * 512)
                for c in range(2):
                    op("pe", "matmul", [wuq_a, cqnT], [PS[4]], PS[4][0:96, :], lhsT=wuq_a[:, c, h, 0:96], rhs=cqnT[:, c, qsl], start=(c == 0), stop=(c == 1))
                for c in range(2):
                    op("pe", "matmul", [wuq_a, cqnT], [PS[5]], PS[5][0:96, :], lhsT=wuq_a[:, c, h, 96:192], rhs=cqnT[:, c, qsl], start=(c == 0), stop=(c == 1))
                qt = qT[(h * NG + i) % 2]
                op("act", "copy", [PS[4]], [qt], out=qt[0:64, :], in_=PS[4][0:64, :])
                dma("sp", s_csm, [csms_k], [csm1f], out=csm1f[64:96, :], in_=csms_d[b, 0, :, qsl])
                dma("sp", s_csm, [csms_k], [csm2f], out=csm2f[64:96, :], in_=csms_d[b, 1, :, qsl])
                op("dve", "tensor_tensor", [PS[4], csm1f], [rt1], out=rt1[64:96, :], in0=PS[4][64:96, :], in1=csm1f[64:96, :], op=ALU.mult)
                op("dve", "tensor_tensor", [PS[5], csm2f], [rt2], out=rt2[64:96, :], in0=PS[5][64:96, :], in1=csm2f[64:96, :], op=ALU.mult)
                op("dve", "tensor_tensor", [rt1, rt2], [qt], out=qt[64:96, :], in0=rt1[64:96, :], in1=rt2[64:96, :], op=ALU.add)

            pend_epi = []
            for h in range(8):
                for g in range(NG):
                    op("pe", "matmul", [wk_b, ckvnT], [PS[6]], PS[6][0:64, :], lhsT=wk_b[:, h, :], rhs=ckvnT[:, g * 512:(g + 1) * 512], start=True, stop=True)
                    op("act", "copy", [PS[6]], [kT], out=kT[0:64, g * 512:(g + 1) * 512], in_=PS[6][0:64, :])
                vb = vh[h % 2]
                for t8 in range((NT + 7) // 8):
                    n8 = min(8, NT - t8 * 8)
                    for tt_ in range(n8):
                        ti = t8 * 8 + tt_
                        op("pe", "matmul", [ckvnT, wv_b], [PS[7]], PS[7][:, tt_ * 64:(tt_ + 1) * 64], lhsT=ckvnT[:, ti * 128:(ti + 1) * 128], rhs=wv_b[:, h, :],
                           start=True, stop=True)
                    op("dve", "tensor_copy", [PS[7]], [vb], out=vb[:, t8 * 8:t8 * 8 + n8, 0:64], in_=PS[7][:, 0:n8 * 64].rearrange("p (t d) -> p t d", d=64))
                qprep(h, 0)
                for i in range(NG):
                    qsl = slice(i * 512, (i + 1) * 512)
                    qt = qT[(h * NG + i) % 2]
                    if i + 1 < NG:
                        qprep(h, i + 1)
                    nk = 4 * i + 4
                    ob = 2 + ((h * NG + i) % 2)

                    def c0_of(j):
                        return 128 * (j - 4 * i) if j > 4 * i else 0

                    def qk(j):
                        c0 = c0_of(j)
                        op("pe", "matmul", [kT, qt], [PS[j % 2]], PS[j % 2][:, c0:512], lhsT=kT[0:96, j * 128:(j + 1) * 128], rhs=qt[0:96, c0:512], start=True, stop=True)
                    qk(0)
                    for j in range(nk):
                        if j + 1 < nk:
                            qk(j + 1)
                        p_ = pT[j % 2]
                        c0 = c0_of(j)
                        op("act", "activation", [PS[j % 2]], [p_], out=p_[:, c0:512], in_=PS[j % 2][:, c0:512], func=AF.Exp, scale=float(96 ** -0.5))
                        if j >= 4 * i:
                            m_ = j - 4 * i
                            op("dve", "tensor_tensor", [p_, cmask_b], [p_], out=p_[:, c0:c0 + 128], in0=p_[:, c0:c0 + 128], in1=cmask_b[:, m_, c0:c0 + 128], op=ALU.mult)
                        op("pe", "matmul", [vb, p_], [PS[ob]], PS[ob][0:65, c0:512], lhsT=vb[:, j, 0:65], rhs=p_[:, c0:512], start=(j == 0), stop=(j == nk - 1))
                        if j == 1 and pend_epi:
                            pend_epi.pop(0)()
                    def epilogue(ob=ob, h=h, qsl=qsl):
                        op("dve", "reciprocal", [PS[ob]], [rrow], out=rrow[64:65, :], in_=PS[ob][64:65, :])
                        op("pe", "matmul", [ones_f, rrow], [PS[7]], PS[7][0:64, :], lhsT=ones_f[64:65, 0:64], rhs=rrow[64:65, :], start=True, stop=True)
                        op("act", "copy", [PS[7]], [bcs], out=bcs[:], in_=PS[7][0:64, :])
                        op("dve", "tensor_tensor", [PS[ob], bcs], [oTm], out=oTm[:], in0=PS[ob][0:64, :], in1=bcs[:], op=ALU.mult)
                        dma("sp", s_mixm, [oTm], [mixm_k], out=mixm_d[b, h, :, qsl], in_=oTm[:])
                    pend_epi.append(epilogue)
                    for _ in range(per_slot):
                        if relay_state[0] < NE:
                            relayout_expert(relay_state[0])
                            relay_state[0] += 1
            while pend_epi:
                pend_epi.pop(0)()
            fence(pT + qT + wblk, [h2T, BIGW])

            fence([GTG[0], h1T, RVG[1]], c_alts)
            dma("pool", s_w, [], [wo_m], out=wo_m[:], in_=wo_d[0:512, :].rearrange("(h p) n -> p h n", p=64))
            dma("pool", s_w, [], [wo_r], out=wo_r[:], in_=wo_d[512:1024, :].rearrange("(h p) n -> p h n", p=128))
            dma("sp", s_bc, [mods_k], [g1bc], out=g1bc[:], in_=mods_d[b:b + 1, 2 * D:3 * D].partition_broadcast(128))
            dma("sp", s_bc, [mods_k], [sh2bc], out=sh2bc[:], in_=mods_d[b:b + 1, 3 * D:4 * D].partition_broadcast(128))
            dma("sp", s_bc, [mods_k], [A2bc], out=A2bc[:], in_=mods_d[b:b + 1, 4 * D:5 * D].partition_broadcast(128))
            dma("sp", s_bc, [], [n2gbc], out=n2gbc[:], in_=n2g_d.partition_broadcast(128))
            op("dve", "scalar_tensor_tensor", [A2bc, n2gbc], [A2bc], out=A2bc[:], in0=A2bc[:], scalar=1.0, in1=n2gbc[:], op0=ALU.add, op1=ALU.mult)
            def C1(ti):
                tok0 = ti * 128
                gt = b * NT + ti
                p2 = ti % 2
                x1 = x1_2[p2]
                h2 = h2_2[p2]
                h2b = h2b_2[p2]
                h2T = h2T_2[p2]
                mm_t = mm_t_2[p2]
                mr_t = mr_t_2[p2]
                pa, pbk = (0, 1) if p2 == 0 else (6, 7)
                so = 0 if p2 == 0 else 10
                xi_ = xin[ti % 2]
                dma("sp", s_xin[ti % 2], [], [xi_], out=xi_[:], in_=x_d[b, tok0:tok0 + 128, :])
                dma("sp", s_mm2[p2], [mixm_k], [mm_t], out=mm_t[:], in_=mixm_d[b, :, :, tok0:tok0 + 128].rearrange("h p t -> p h t"))
                dma("sp", s_mm2[p2], [mixr_k], [mr_t], out=mr_t[:], in_=mixr_d[b, :, :, tok0:tok0 + 128].rearrange("h p t -> p h t"))
                for nh, pbank in ((0, pa), (1, pbk)):
                    for hh in range(8):
                        op("pe", "matmul", [mm_t, wo_m], [PS[pbank]], PS[pbank][:, :], lhsT=mm_t[:, hh, :], rhs=wo_m[:, hh, nh * 512:(nh + 1) * 512], start=(hh == 0), stop=False)
                    for hh in range(4):
                        op("pe", "matmul", [mr_t, wo_r], [PS[pbank]], PS[pbank][:, :], lhsT=mr_t[:, hh, :], rhs=wo_r[:, hh, nh * 512:(nh + 1) * 512], start=False, stop=(hh == 3))
                for nh, pbank in ((0, pa), (1, pbk)):
                    op("dve", "tensor_tensor", [PS[pbank], g1bc], [x1], out=x1[:, nh * 512:(nh + 1) * 512], in0=PS[pbank][:, :], in1=g1bc[:, nh * 512:(nh + 1) * 512], op=ALU.mult)
                op("pool", "tensor_tensor", [x1, xi_], [x1], out=x1[:], in0=x1[:], in1=xi_[:], op=ALU.add)
                dma("sp", s_x1, [x1], [x1s_k], out=x1s_d[gt * 128:(gt + 1) * 128, :], in_=x1[:])
                op("act", "activation", [x1], [junk, stat], out=junk[:], in_=x1[:], func=AF.Square, accum_out=stat[:, so:so + 1])
                op("dve", "tensor_scalar", [stat], [stat], out=stat[:, so + 1:so + 2], in0=stat[:, so:so + 1], scalar1=1.0 / D, scalar2=EPS, op0=ALU.mult, op1=ALU.add)
                rsqrt(stat, stat[:, so + 1:so + 2], stat, stat[:, so + 2:so + 3], 1)
                op("dve", "scalar_tensor_tensor", [x1, stat, A2bc], [h2], out=h2[:], in0=x1[:], scalar=stat[:, so + 2:so + 3], in1=A2bc[:], op0=ALU.mult, op1=ALU.mult)
                op("pool", "tensor_tensor", [h2, sh2bc], [h2], out=h2[:], in0=h2[:], in1=sh2bc[:], op=ALU.add)
                op("act", "copy", [h2], [h2b], out=h2b[:], in_=h2[:])
                dma("sp", s_h2, [h2b], [h2s_k], out=h2s_d[gt * 128:(gt + 1) * 128, :], in_=h2b[:])
                for c in range(8):
                    pb = 2 + c // 4
                    op("pe", "transpose", [h2, cst], [PS[pb]], out=PS[pb][:, (c % 4) * 128:(c % 4 + 1) * 128], in_=h2[:, c * 128:(c + 1) * 128], identity=ident_f)
                op("act", "copy", [PS[2]], [h2T], out=h2T[:, 0:4, :], in_=PS[2][:, :].rearrange("p (c t) -> p c t", c=4))
                op("dve", "tensor_copy", [PS[3]], [h2T], out=h2T[:, 4:8, :], in_=PS[3][:, :].rearrange("p (c t) -> p c t", c=4))
                for c in range(8):
                    op("pe", "matmul", [h2T, w_rt], [PS[4]], PS[4][:, 0:36], lhsT=h2T[:, c, :], rhs=w_rt[:, c, :], start=(c == 0), stop=(c == 7))

                lgt = lgt2[ti % 2]
                op("dve", "tensor_tensor", [PS[4], b_rt], [lgt], out=lgt[:, 0:4], in0=PS[4][:, 0:4], in1=b_rt[:, 0:4], op=ALU.add)
                op("dve", "tensor_tensor", [PS[4], b_rt], [lgt], out=lgt[:, 8:40], in0=PS[4][:, 4:36], in1=b_rt[:, 4:36], op=ALU.add)

            def C2(ti):
                gt = b * NT + ti
                lgt = lgt2[ti % 2]
                m8 = m8_2[ti % 2]
                rst = rst_2[ti % 2]
                lem = lem_2[ti % 2]
                Mt = Mt_2[ti % 2]
                op("dve", "max", [lgt], [m8], out=m8[:, 0:8], in_=lgt[:, 0:8])
                op("dve", "tensor_scalar", [m8], [rst], out=rst[:, 0:1], in0=m8[:, 0:1], scalar1=-1.0, scalar2=None, op0=ALU.mult)
                op("act", "activation", [lgt, rst], [rst], out=rst[:, 8:16], in_=lgt[:, 0:8], func=AF.Exp, bias=rst[:, 0:1], scale=1.0, accum_out=rst[:, 1:2])
                op("dve", "reciprocal", [rst], [rst], out=rst[:, 2:3], in_=rst[:, 1:2])
                op("dve", "tensor_scalar", [lgt, m8], [rst], out=rst[:, 16:20], in0=lgt[:, 0:4], scalar1=m8[:, 0:1], scalar2=None, op0=ALU.is_equal)
                op("dve", "tensor_scalar", [rst], [rst], out=rst[:, 16:20], in0=rst[:, 16:20], scalar1=-1.0, scalar2=1e30, op0=ALU.add, op1=ALU.mult)
                op("dve", "tensor_tensor", [lgt, rst], [lem], out=lem[:].rearrange("p (g e) -> p g e", g=4), in0=lgt[:, 8:40].rearrange("p (g e) -> p g e", g=4),
                   in1=rst[:, 16:20].unsqueeze(2).to_broadcast([128, 4, 8]), op=ALU.add)
                op("dve", "max", [lem], [m8], out=m8[:, 8:16], in_=lem[:])
                op("dve", "tensor_scalar", [lem, m8], [OH1], out=OH1[:, gt, :], in0=lem[:], scalar1=m8[:, 8:9], scalar2=None, op0=ALU.is_equal)
                op("dve", "tensor_scalar", [lem, m8], [OH2], out=OH2[:, gt, :], in0=lem[:], scalar1=m8[:, 9:10], scalar2=None, op0=ALU.is_equal)
                op("dve", "tensor_tensor", [m8], [rst], out=rst[:, 3:4], in0=m8[:, 9:10], in1=m8[:, 8:9], op=ALU.subtract)
                op("act", "activation", [rst], [rst], out=rst[:, 4:5], in_=rst[:, 3:4], func=AF.Exp)
                op("dve", "tensor_scalar", [rst], [rst], out=rst[:, 4:5], in0=rst[:, 4:5], scalar1=1.0, scalar2=None, op0=ALU.add)
                op("dve", "reciprocal", [rst], [rst], out=rst[:, 5:6], in_=rst[:, 4:5])
                op("dve", "tensor_tensor", [rst], [GATE], out=GATE[:, gt, 0:1], in0=rst[:, 5:6], in1=rst[:, 2:3], op=ALU.mult)
                op("dve", "tensor_tensor", [rst, GATE], [GATE], out=GATE[:, gt, 1:2], in0=rst[:, 2:3], in1=GATE[:, gt, 0:1], op=ALU.subtract)
                op("pool", "tensor_tensor", [OH1, OH2], [Mt], out=Mt[:], in0=OH1[:, gt, :], in1=OH2[:, gt, :], op=ALU.add)
                op("pe", "matmul", [triu_b, Mt], [PS[5]], PS[5][:, 0:32], lhsT=triu_b[:], rhs=Mt[:], start=True, stop=False)
                op("pe", "matmul", [ones_b, Macc], [PS[5]], PS[5][:, 0:32], lhsT=ones_b[:], rhs=Macc[:], start=False, stop=True)
                op("act", "copy", [PS[5]], [CUM], out=CUM[:, gt, :], in_=PS[5][:, 0:32])
                op("pool", "tensor_tensor", [Macc, Mt], [Macc], out=Macc[:], in0=Macc[:], in1=Mt[:], op=ALU.add)


            import os as _os2
            if _os2.environ.get("K_CSKEW", "1") == "1":
                C1(0)
                for ti in range(NT):
                    if ti + 1 < NT:
                        C1(ti + 1)
                    C2(ti)
            else:
                for ti in range(NT):
                    C1(ti)
                    C2(ti)
            fence(c_alts, [P0, P1, P4c])

        op("pe", "matmul", [ones_b, Macc], [PS[5]], PS[5][:, 0:32], lhsT=ones_b[:], rhs=Macc[:], start=True, stop=True)
        op("dve", "tensor_scalar", [PS[5]], [ra], out=ra[:], in0=PS[5][:, 0:32], scalar1=1.0 / BLK, scalar2=(BLK - 1 - (BLK / 2 - 0.5)) / BLK, op0=ALU.mult, op1=ALU.add)
        op("dve", "tensor_scalar", [ra], [ra], out=ra[:], in0=ra[:], scalar1=MAGIC_RN, scalar2=None, op0=ALU.add)
        op("dve", "tensor_scalar", [ra], [pad_], out=pad_[:], in0=ra[:], scalar1=MAGIC_RN, scalar2=float(BLK), op0=ALU.subtract, op1=ALU.mult)
        op("dve", "tensor_copy", [pad_], [ra], out=ra[:], in_=pad_[:])
        cur, oth = ra, rb
        for sft in (1, 2, 4, 8, 16):
            op("dve", "tensor_copy", [cur], [oth], out=oth[:, 0:sft], in_=cur[:, 0:sft])
            op("dve", "tensor_tensor", [cur], [oth], out=oth[:, sft:32], in0=cur[:, sft:32], in1=cur[:, 0:32 - sft], op=ALU.add)
            cur, oth = oth, cur
        pend = cur
        op("dve", "tensor_tensor", [pend, pad_], [pst], out=pst[:], in0=pend[:], in1=pad_[:], op=ALU.subtract)
        a0, a1 = coff["blkstart"]
        op("dve", "tensor_tensor", [pend, cst], [cmp3], out=cmp3[:], in0=pend[:].unsqueeze(1).to_broadcast([128, NBLK, 32]),
           in1=cst[:, a0:a1].unsqueeze(2).to_broadcast([128, NBLK, 32]), op=ALU.is_le)
        op("dve", "tensor_reduce", [cmp3], [ebf], out=ebf[:], in_=cmp3[:], axis=AX.X, op=ALU.add)
        i0, i1 = coff["iotap"]
        op("dve", "tensor_scalar", [ebf], [ebf], out=ebf[:], in0=ebf[:], scalar1=31.0, scalar2=128.0, op0=ALU.min, op1=ALU.mult)
        op("dve", "tensor_scalar", [ebf, cst], [WIDX], out=WIDX[:], in0=ebf[:], scalar1=cst[:, i0:i1], scalar2=None, op0=ALU.add)
        op("pool", "tensor_tensor", [CUM, pst], [CUM], out=CUM[:], in0=CUM[:], in1=pst[:].unsqueeze(1).to_broadcast([128, NTT, 32]), op=ALU.add)
        for k_, OH in ((0, OH1), (1, OH2)):
            op("dve", "tensor_tensor", [OH, CUM], [cmpd], out=cmpd[:], in0=OH[:], in1=CUM[:], op=ALU.mult)
            op("dve", "tensor_reduce", [cmpd], [destf], out=destf[:, :, k_], in_=cmpd[:], axis=AX.X, op=ALU.add)
        op("dve", "tensor_copy", [destf], [DEST], out=DEST[:], in_=destf[:])

        xs_k = DR()
        ys_k = DR()
        h2r = h2r + [Alias(P7, 0, [128, D], BF16, own=True), Alias(P4c, 0, [128, D], BF16, own=True)]
        s_h2r = s_h2r + [SC.dma_sem("h2r2"), SC.dma_sem("h2r3")]
        fence([rqT, rkT, RVG[1], h1T, GT4, BIGW], h2r + xblk + ysb + wblk)
        for gt in range(NTT):
            hr = h2r[gt % 4]
            dma("sp", s_h2r[gt % 4], [h2s_k], [hr], out=hr[:], in_=h2s_d[gt * 128:(gt + 1) * 128, :])
            for k_ in range(2):
                SC.dma("pool", (lambda e, hr=hr, gt=gt, k_=k_: e.indirect_dma_start(
                    out=xs_d, out_offset=bass.IndirectOffsetOnAxis(ap=DEST[:, gt, k_:k_ + 1], axis=0), in_=hr[:, :], in_offset=None)),
                    s_sc, [hr.k, DEST.k], [xs_k.k], lat=6.0)

        xTb2 = [xTb, Alias(P4b, 0, [128, 2, 8, 128], BF16)]
        actb2 = [[Alias(P2a, 1024 + 512 * (2 * pq + r), [128, 256], BF16, own=True) for r in range(2)] for pq in range(2)]
        sg2 = [sg, Alias(P2a, 3072, [128, 256], F32, own=True)]
        actT2 = [Alias(P3a, 512 * r, [128, 2, 128], BF16, own=True) for r in range(2)]
        fence([g1bc, A2bc], [a_ for l_ in actb2 for a_ in l_] + sg2 + actT2)

        def stage1(blk):
            pq = blk % 2
            wb = wblk[pq]
            SC.dma("pool", (lambda e, wb=wb, blk=blk: e.indirect_dma_start(
                out=wb[:, :], out_offset=None, in_=wall_d, in_offset=bass.IndirectOffsetOnAxis(ap=WIDX[:, blk:blk + 1], axis=0))),
                s_wg[pq], [WIDX.k, wall_k.k], [wb.k], lat=14.0)
            xb_ = xblk[pq]
            dma("sp", s_xb[pq], [xs_k], [xb_], out=xb_[:], in_=xs_d[blk * BLK:(blk + 1) * BLK, :].rearrange("(r p) d -> p r d", p=128))
            xt_ = xTb2[pq]
            for r in range(2):
                for c in range(8):
                    op("pe", "transpose", [xb_, ident_b], [PS[0]], out=psbf(0)[:, c * 128:(c + 1) * 128], in_=xb_[:, r, c * 128:(c + 1) * 128], identity=ident_b[:])
                if r == 0:
                    op("act", "copy", [PS[0]], [xt_], out=xt_[:, r, :, :], in_=psbf(0)[:, 0:1024].rearrange("p (c t) -> p c t", c=8))
                else:
                    op("dve", "tensor_copy", [PS[0]], [xt_], out=xt_[:, r, :, :], in_=psbf(0)[:, 0:1024].rearrange("p (c t) -> p c t", c=8))
            for r in range(2):
                hb = 1 + 2 * pq + r
                for c in range(8):
                    op("pe", "matmul", [xt_, wb], [PS[hb]], PS[hb][:, :], lhsT=xt_[:, r, c, :], rhs=wb[:, c * 512:(c + 1) * 512], start=(c == 0), stop=(c == 7))
            for r in range(2):
                hb = 1 + 2 * pq + r
                sg_ = sg2[r]
                ab = actb2[pq][r]
                op("act", "activation", [PS[hb]], [sg_], out=sg_[:], in_=PS[hb][:, 0:256], func=AF.Tanh, scale=0.5)
                op("dve", "scalar_tensor_tensor", [sg_, PS[hb]], [sg_], out=sg_[:], in0=sg_[:], scalar=1.0, in1=PS[hb][:, 0:256], op0=ALU.add, op1=ALU.mult)
                op("dve", "scalar_tensor_tensor", [sg_, PS[hb]], [ab], out=ab[:], in0=sg_[:], scalar=0.5, in1=PS[hb][:, 256:512], op0=ALU.mult, op1=ALU.mult)

        def stage2(blk):
            pq = blk % 2
            wb = wblk[pq]
            for r in range(2):
                ab = actb2[pq][r]
                at = actT2[r]
                for fc in range(2):
                    op("pe", "transpose", [ab, ident_b], [PS[5]], out=psbf(5)[:, (2 * r + fc) * 128:(2 * r + fc + 1) * 128], in_=ab[:, fc * 128:(fc + 1) * 128], identity=ident_b[:])
                op("act", "copy", [PS[5]], [at], out=at[:], in_=psbf(5)[:, 2 * r * 128:(2 * r + 2) * 128].rearrange("p (c t) -> p c t", c=2))
            for r in range(2):
                at = actT2[r]
                yb = ysb[r]
                for nh in range(2):
                    for fc in range(2):
                        op("pe", "matmul", [at, wb], [PS[6 + nh]], PS[6 + nh][:, :], lhsT=at[:, fc, :],
                           rhs=wb[:, 4096 + fc * 1024 + nh * 512:4096 + fc * 1024 + (nh + 1) * 512], start=(fc == 0), stop=(fc == 1))
                    if nh == 0:
                        op("act", "copy", [PS[6]], [yb], out=yb[:, 0:512], in_=PS[6][:, :])
                    else:
                        op("dve", "tensor_copy", [PS[7]], [yb], out=yb[:, 512:1024], in_=PS[7][:, :])
                dma("sp", s_ys[r], [yb], [ys_k], out=ys_d[blk * BLK + r * 128:blk * BLK + (r + 1) * 128, :], in_=yb[:])

        stage1(0)
        for blk in range(NBLK):
            if blk + 1 < NBLK:
                stage1(blk + 1)
            stage2(blk)

        out_k = DR()
        fg_bc = Alias(P3b, 0, [128, D], F32)
        fence(xblk + [a_ for l_ in actb2 for a_ in l_] + sg2 + actT2, ysc + [g1bc, A2bc])
        dma("sp", s_bc, [], [fg_bc], out=fg_bc[:], in_=fg_d.partition_broadcast(128))
        s_yg2 = [SC.dma_sem("yg2_%d" % i) for i in range(2)]
        s_x1f = [SC.dma_sem("x1f%d" % i) for i in range(2)]
        h2alt = Alias(P2b, 0, [128, D], F32)
        x1alt = Alias(P5, 0, [128, D], F32)
        def F_pre(gt):
            yp = ysb if gt % 2 == 0 else ysc
            sy = s_yg if gt % 2 == 0 else s_yg2
            for k_ in range(2):
                SC.dma("pool", (lambda e, gt=gt, k_=k_, yy=yp[k_]: e.indirect_dma_start(
                    out=yy[:, :], out_offset=None, in_=ys_d, in_offset=bass.IndirectOffsetOnAxis(ap=DEST[:, gt, k_:k_ + 1], axis=0))),
                    sy[k_], [DEST.k, ys_k.k], [yp[k_].k], lat=7.0)
            xx = x1 if gt % 2 == 0 else x1alt
            dma("sp", s_x1f[gt % 2], [x1s_k], [xx], out=xx[:], in_=x1s_d[gt * 128:(gt + 1) * 128, :])

        def F_main(gt):
            b = gt // NT
            ti = gt % NT
            if ti == 0:
                dma("sp", s_bc, [mods_k], [g1bc], out=g1bc[:], in_=mods_d[b:b + 1, 5 * D:6 * D].partition_broadcast(128))
            yp = ysb if gt % 2 == 0 else ysc
            y1, y2 = yp[0], yp[1]
            hh = h2 if gt % 2 == 0 else h2alt
            xx = x1 if gt % 2 == 0 else x1alt
            sc_ = (gt % 2) * 4
            op("act", "activation", [y1, GATE], [hh], out=hh[:], in_=y1[:], func=AF.Identity, scale=GATE[:, gt, 0:1])
            op("dve", "scalar_tensor_tensor", [y2, GATE, hh], [hh], out=hh[:], in0=y2[:], scalar=GATE[:, gt, 1:2], in1=hh[:], op0=ALU.mult, op1=ALU.add)
            op("dve", "tensor_tensor", [hh, g1bc], [hh], out=hh[:], in0=hh[:], in1=g1bc[:], op=ALU.mult)
            op("pool", "tensor_tensor", [hh, xx], [hh], out=hh[:], in0=hh[:], in1=xx[:], op=ALU.add)
            op("act", "activation", [hh], [junk, stat], out=junk[:], in_=hh[:], func=AF.Square, accum_out=stat[:, sc_:sc_ + 1])
            op("dve", "tensor_scalar", [stat], [stat], out=stat[:, sc_ + 1:sc_ + 2], in0=stat[:, sc_:sc_ + 1], scalar1=1.0 / D, scalar2=EPS, op0=ALU.mult, op1=ALU.add)
            rsqrt(stat, stat[:, sc_ + 1:sc_ + 2], stat, stat[:, sc_ + 2:sc_ + 3], 1)
            xo = xin[gt % 2]
            op("dve", "scalar_tensor_tensor", [hh, stat, fg_bc], [xo], out=xo[:], in0=hh[:], scalar=stat[:, sc_ + 2:sc_ + 3], in1=fg_bc[:], op0=ALU.mult, op1=ALU.mult)
            dma("sp", s_xin[gt % 2], [xo], [out_k], out=out_d[b, ti * 128:(ti + 1) * 128, :], in_=xo[:])

        F_pre(0)
        for gt in range(NTT):
            if gt + 1 < NTT:
                F_pre(gt + 1)
            F_main(gt)
        SC.wait_all("sp", [out_k.k])
        SC.emit()
    return nc


_CACHE = {}


def kernel(**inputs):
    NCORES = 8
    x = np.asarray(inputs["x"], dtype=np.float32)
    B, S, _ = x.shape
    NSEQ = B // NCORES
    key = (S, NSEQ)
    if key not in _CACHE:
        _CACHE[key] = build_nc(S, NSEQ)
    nc = _CACHE[key]
    T = S * NSEQ
    NBLK = (2 * T + NE * (BLK - 1) + BLK - 1) // BLK
    cst_np, _ = make_consts(NBLK)
    f = lambda k: np.ascontiguousarray(np.asarray(inputs[k], dtype=np.float32))
    shared = {
        "w_ada": f("w_ada")[0], "b_ada": f("b_ada"), "norm1_g": f("norm1_g"), "w_in": f("w_in")[0],
        "q_norm_g": f("q_norm_g"), "w_uq": f("w_uq")[0], "kv_norm_g": f("kv_norm_g"), "w_ukv": f("w_ukv")[0],
        "w_o": f("w_o")[0], "norm2_g": f("norm2_g"), "w_gr": f("w_gr")[0], "b_gr": f("b_gr"),
        "w_er": f("w_er")[0].reshape(D, 32), "b_er": f("b_er").reshape(1, 32), "w1": f("w1")[0], "w3": f("w3")[0],
        "w2": f("w2")[0], "final_g": f("final_g").reshape(1, D), "cst": cst_np,
    }
    c = f("c")
    pos = np.ascontiguousarray(np.asarray(inputs["positions"], dtype=np.int32))
    in_maps = []
    for i in range(NCORES):
        m = dict(shared)
        m["x"] = np.ascontiguousarray(x[i * NSEQ:(i + 1) * NSEQ])
        m["c"] = np.ascontiguousarray(c[i * NSEQ:(i + 1) * NSEQ])
        m["positions"] = np.ascontiguousarray(pos[i * NSEQ:(i + 1) * NSEQ])
        in_maps.append(m)
    res = run_bass_kernel_spmd(nc, in_maps, core_ids=list(range(NCORES)))
    return np.concatenate([np.asarray(r["out"]) for r in res.results], axis=0).astype(np.float32)
```

```python
import math
import numpy as np
from contextlib import ExitStack
import concourse.bass as bass
import concourse.mybir as mybir
from concourse.bass_utils import run_bass_kernel_spmd

F32 = mybir.dt.float32
BF16 = mybir.dt.bfloat16
I32 = mybir.dt.int32
ALU = mybir.AluOpType
AF = mybir.ActivationFunctionType
AX = mybir.AxisListType

ENGS = ("pe", "act", "dve", "pool", "sp")
D = 1024
NE = 32
BLK = 256
EPS = 1e-6
MAGIC_RN = 12582912.0
TWO_PI = float(2 * np.pi)


class Tk:
    __slots__ = ("w", "r", "acc", "wd")

    def __init__(self, acc=False):
        self.w = None
        self.r = []
        self.acc = acc
        self.wd = {}


class Sched:
    def __init__(self, nc, stack):
        self.nc = nc
        self.stack = stack
        self.cnt = {}
        self.sems = {}
        for e in ENGS:
            self._mksem("E_" + e)
        self.nd = 0
        self.all = []
        self.tk_sems = {}
        self.tok2op = {}

    def _mksem(self, key):
        self.sems[key] = self.stack.enter_context(self.nc.semaphore(key))
        self.cnt[key] = 0
        return key

    def dma_sem(self, name=""):
        self.nd += 1
        return self._mksem("D%d_%s" % (self.nd, name))

    def _deps(self, reads, writes):
        deps = set()

        def add(tok):
            if tok is None:
                return
            k, v = tok
            if k[0] == "D":
                v = self.cnt[k]
            deps.add((k, v))
        for t in reads:
            add(t.w)
            if t.acc:
                for kv in t.wd.items():
                    add(kv)
        for t in writes:
            if t.acc:
                continue
            add(t.w)
            for tok in t.r:
                add(tok)
        return deps

    def _commit(self, tok, reads, writes):
        for t in reads:
            if not t.acc:
                t.r.append(tok)
        for t in writes:
            if t.acc:
                if t.wd.get(tok[0], 0) < tok[1]:
                    t.wd[tok[0]] = tok[1]
            else:
                t.w = tok
                t.r = []

    def op(self, eng, fn, reads=(), writes=(), cost=0.5):
        deps = self._deps(reads, writes)
        key = "E_" + eng
        self.cnt[key] += 1
        tok = (key, self.cnt[key])
        self.tok2op[tok] = len(self.all)
        self.all.append(dict(eng=eng, fn=fn, dma=False, tok=tok, deps=deps, cost=cost, lat=0.0))
        self._commit(tok, reads, writes)

    def dma(self, eng, fn, sem, reads=(), writes=(), lat=4.0):
        anchor = None
        for t in list(writes) + list(reads):
            if not t.acc:
                anchor = t
                break
        if anchor is not None:
            key = "DT%d_%s" % (id(anchor), eng)
            if key not in self.tk_sems:
                self.tk_sems[key] = self.dma_sem("t")
            sem = self.tk_sems[key]
        elif eng == "pool":
            if sem + "_p" not in self.sems:
                self._mksem(sem + "_p")
            sem = sem + "_p"
        deps = self._deps(reads, writes)
        self.cnt[sem] += 16
        tok = (sem, self.cnt[sem])
        self.tok2op[tok] = len(self.all)
        self.all.append(dict(eng=eng, fn=fn, dma=True, tok=tok, deps=deps, cost=(1.2 if eng == "pool" else 0.12), lat=lat))
        self._commit(tok, reads, writes)

    def wait_all(self, eng, tks):
        deps = self._deps(tks, ())
        self.all.append(dict(eng=eng, fn=None, dma=False, tok=None, deps=deps, cost=0.0, lat=0.0))

    def _schedule(self, W=320):
        ops = self.all
        n = len(ops)
        prod = [None] * n
        dependents = [[] for _ in range(n)]
        ndeps = [0] * n
        for i, o in enumerate(ops):
            ps = set()
            for tok in o["deps"]:
                j = self.tok2op.get(tok)
                if j is not None:
                    ps.add(j)
            prod[i] = ps
            ndeps[i] = len(ps)
            for j in ps:
                dependents[j].append(i)
        pending = {e: [] for e in ENGS}
        for i, o in enumerate(ops):
            pending[o["eng"]].append(i)
        head = {e: 0 for e in ENGS}
        done = [False] * n
        comp = [0.0] * n
        ready = [0.0] * n
        free = {e: 0.0 for e in ENGS}
        semmax = {}
        order = {e: [] for e in ENGS}
        left = n
        while left:
            best = None
            for e in ENGS:
                lst = pending[e]
                h = head[e]
                while h < len(lst) and done[lst[h]]:
                    h += 1
                head[e] = h
                if h >= len(lst):
                    continue
                seen_sems = set()
                cnt = 0
                k = h
                fe = free[e]
                while k < len(lst) and cnt < W:
                    i = lst[k]
                    k += 1
                    if done[i]:
                        continue
                    cnt += 1
                    o = ops[i]
                    if o["fn"] is None:
                        if cnt > 1:
                            continue
                    elif o["dma"]:
                        sk_ = o["tok"][0]
                        if sk_ in seen_sems:
                            continue
                        seen_sems.add(sk_)
                    if ndeps[i]:
                        continue
                    st = ready[i] if ready[i] > fe else fe
                    if best is None or st < best[0] or (st == best[0] and i < best[1]):
                        best = (st, i, e)
                    if st <= fe:
                        break
            st, i, e = best
            o = ops[i]
            done[i] = True
            left -= 1
            order[e].append(i)
            fin = st + o["cost"]
            free[e] = fin
            c = fin + o["lat"]
            if o["dma"]:
                sk = o["tok"][0]
                if semmax.get(sk, 0.0) > c:
                    c = semmax[sk]
                semmax[sk] = c
            comp[i] = c
            for d in dependents[i]:
                ndeps[d] -= 1
                if comp[i] > ready[d]:
                    ready[d] = comp[i]
        self.sim_time = max(free.values())
        return order

    def emit(self):
        import os
        ops = self.all
        if os.environ.get("K_REORDER", "1") == "1":
            order = self._schedule()
        else:
            order = {e: [] for e in ENGS}
            for i, o in enumerate(ops):
                order[o["eng"]].append(i)
        newtok = {}
        for e in ENGS:
            c = 0
            for i in order[e]:
                o = ops[i]
                if o["fn"] is not None and not o["dma"]:
                    c += 1
                    newtok[o["tok"]] = ("E_" + e, c)
        plan = {e: [] for e in ENGS}
        needed = {}
        for e in ENGS:
            wd = {}
            for i in order[e]:
                o = ops[i]
                mx = {}
                for tok in o["deps"]:
                    k, v = newtok.get(tok, tok)
                    if mx.get(k, 0) < v:
                        mx[k] = v
                waits = []
                for k, v in mx.items():
                    if wd.get(k, 0) < v:
                        wd[k] = v
                        waits.append((k, v))
                        if k[0] == "E":
                            needed.setdefault(k, set()).add(v)
                plan[e].append((waits, o))
        rank = {k: {v: r + 1 for r, v in enumerate(sorted(vs))} for k, vs in needed.items()}
        sems = self.sems

        def run(name, eng):
            for waits, o in plan[name]:
                for k, v in waits:
                    eng.wait_ge(sems[k], rank[k][v] if k[0] == "E" else v)
                if o["fn"] is not None:
                    ins = o["fn"](eng)
                    if o["dma"]:
                        ins.then_inc(sems[o["tok"][0]], 16)
                    else:
                        nt = newtok[o["tok"]]
                        if nt[1] in needed.get(nt[0], ()):
                            ins.then_inc(sems[nt[0]], 1)
        with self.nc.Block() as block:
            @block.tensor
            def _(e):
                run("pe", e)

            @block.scalar
            def _(e):
                run("act", e)

            @block.vector
            def _(e):
                run("dve", e)

            @block.gpsimd
            def _(e):
                run("pool", e)

            @block.sync
            def _(e):
                run("sp", e)


def make_consts(nblk):
    H = 4
    gam = 1.0 - 2.0 ** (-5.0 - np.arange(H))
    lg = np.log(gam)
    p = np.arange(128)
    cols = {}
    cols["ident"] = np.eye(128, dtype=np.float64)
    cols["triu"] = (p[:, None] < p[None, :]).astype(np.float64)
    cm = np.zeros((128, 4, 512))
    q = np.arange(512)
    for m in range(4):
        cm[:, m, :] = ((128 * m + p)[:, None] <= q[None, :])
    cmask_np = cm.reshape(128, -1)
    dm = np.zeros((128, 4, 128))
    for h in range(4):
        d = p[None, :] - p[:, None]
        dm[:, h, :] = np.where(d >= 0, np.exp(np.maximum(d, 0) * lg[h]), 0.0)
    cols["dmaskT"] = dm.reshape(128, -1)
    xi = np.zeros((128, 2, 128))
    for j in range(2):
        for half in range(2):
            h = 2 * j + half
            xi[half * 64:(half + 1) * 64, j, :] = np.exp((p + 1.0) * lg[h])[None, :]
    cols["xi"] = xi.reshape(128, -1)
    zt = np.zeros((128, 4, 64))
    for h in range(4):
        zt[:, h, :] = np.exp((127.0 - p) * lg[h])[:, None]
    cols["zeta"] = zt.reshape(128, -1)
    rp = np.zeros((128, 6))
    jr = p % 32
    rp[:, 0] = 10000.0 ** (-(jr / 32.0))
    blk64 = (p % 64) // 32
    rp[:, 1] = np.where(blk64 == 0, np.pi / 2, 0.0)
    rp[:, 2] = np.where(blk64 == 0, np.pi, np.pi / 2)
    jm = (p - 64) % 16
    rp[:, 3] = 10000.0 ** (-(jm / 16.0))
    b16 = ((p - 64) // 16) % 2
    rp[:, 4] = np.where(b16 == 0, np.pi / 2, 0.0)
    rp[:, 5] = np.where(b16 == 0, np.pi, np.pi / 2)
    cols["rp"] = rp
    cols["blkstart"] = np.broadcast_to((np.arange(nblk) * float(BLK))[None, :], (128, nblk))
    cols["iotap"] = p[:, None].astype(np.float64)
    cols["cd"] = np.broadcast_to(np.exp(128.0 * lg)[None, :], (128, 4))
    cols["cmask"] = cmask_np
    off = {}
    o = 0
    arrs = []
    for k, v in cols.items():
        off[k] = (o, o + v.shape[1])
        o += v.shape[1]
        arrs.append(v)
    return np.concatenate(arrs, axis=1).astype(np.float32), off


def build_nc(S, NSEQ, dbg=None):
    T = S * NSEQ
    NT = S // 128
    NTT = T // 128
    NG = S // 512
    NBLK = (2 * T + NE * (BLK - 1) + BLK - 1) // BLK
    PT = NBLK * BLK
    cst_np, coff = make_consts(NBLK)
    NC = cst_np.shape[1]
    nc = bass.Bass("TRN2", target_bir_lowering=False)

    def din(name, shape, dt=F32):
        return nc.dram_tensor(name, list(shape), dt, kind="ExternalInput").ap()

    def dscr(name, shape, dt):
        return nc.dram_tensor(name, list(shape), dt, kind="Internal").ap()

    x_d = din("x", [NSEQ, S, D])
    c_d = din("c", [NSEQ, D])
    pos_d = din("positions", [NSEQ, S], I32)
    wada_d = din("w_ada", [D, 6 * D])
    bada_d = din("b_ada", [1, 6 * D])
    n1g_d = din("norm1_g", [1, D])
    win_d = din("w_in", [D, 1952])
    qng_d = din("q_norm_g", [1, 256])
    wuq_d = din("w_uq", [256, 768])
    kvng_d = din("kv_norm_g", [1, 128])
    wukv_d = din("w_ukv", [128, 1024])
    wo_d = din("w_o", [D, D])
    n2g_d = din("norm2_g", [1, D])
    wgr_d = din("w_gr", [D, 4])
    bgr_d = din("b_gr", [1, 4])
    wer_d = din("w_er", [D, 32])
    ber_d = din("b_er", [1, 32])
    w1_d = din("w1", [NE, D, 256])
    w3_d = din("w3", [NE, D, 256])
    w2_d = din("w2", [NE, 256, D])
    fg_d = din("final_g", [1, D])
    cst_d = din("cst", [128, NC])
    out_d = nc.dram_tensor("out", [NSEQ, S, D], F32, kind="ExternalOutput").ap()

    mods_d = dscr("mods", [NSEQ, 6 * D], F32)
    mixm_d = dscr("mixm", [NSEQ, 8, 64, S], BF16)
    mixr_d = dscr("mixr", [NSEQ, 4, 128, S], BF16)
    x1s_d = dscr("x1s", [T, D], F32)
    h2s_d = dscr("h2s", [T, D], BF16)
    xs_d = dscr("xs", [PT, D], BF16)
    ys_d = dscr("ys", [PT, D], BF16)
    wall_d = dscr("wall", [NE * 128, 6144], BF16)
    csms_d = dscr("csms", [NSEQ, 2, 32, S], F32)
    dbg_out = {}
    if dbg:
        for name, shape in dbg.items():
            dbg_out[name] = nc.dram_tensor("dbg_" + name, list(shape), F32, kind="ExternalOutput").ap()

    st = ExitStack()
    with st:
        SC = Sched(nc, st)

        class Buf:
            def __init__(self, name, shape, dt, psum=False):
                if psum:
                    self.t = st.enter_context(nc.psum_tensor(name, list(shape), dt))
                else:
                    self.t = st.enter_context(nc.sbuf_tensor("sb_" + name, list(shape), dt))
                self.k = Tk()

            def __getitem__(self, idx):
                return self.t[idx]

        def sb(name, shape, dt=F32):
            return Buf(name, shape, dt)

        class Alias:
            def __init__(self, parent, off, shape, dt, own=False):
                n = 1
                for d_ in shape[1:]:
                    n *= d_
                nb = n * (4 if dt in (F32, I32) else 2)
                a = parent.t[0:shape[0], off // 4:(off + nb) // 4]
                v = a if dt == F32 else a.bitcast(dt)
                if len(shape) == 3:
                    v = v.rearrange("p (a b) -> p a b", a=shape[1])
                elif len(shape) == 4:
                    v = v.rearrange("p (a b c) -> p a b c", a=shape[1], b=shape[2])
                self.v = v
                self.k = Tk() if own else parent.k

            def __getitem__(self, idx):
                return self.v[idx]

        def _fsz(ap):
            try:
                return float(ap.free_size())
            except Exception:
                return 256.0

        def op(eng, method, reads, writes, *a, **kw):
            if eng == "pe":
                if method == "matmul":
                    cost = 0.31 + _fsz(kw["rhs"]) / 1200.0
                else:
                    cost = 0.42
            else:
                o_ = kw.get("out") if kw.get("out") is not None else (a[0] if a else None)
                f_ = _fsz(o_) if o_ is not None else 64.0
                if eng == "dve":
                    cost = 0.12 + f_ / 1100.0
                elif eng == "act":
                    cost = 0.28 + f_ / 1200.0
                else:
                    cost = 0.7 + f_ / 900.0
            SC.op(eng, lambda e: getattr(e, method)(*a, **kw), [b.k for b in reads], [b.k for b in writes], cost=cost)
            if kw.get("accum_out") is not None:
                SC.op(eng, lambda e: e.copy(out=adum[0:1, 0:2], in_=adum[0:1, 2:4]), [], [b.k for b in writes] + [adum.k], cost=0.2)

        def dma(eng, sem, reads, writes, **kw):
            try:
                nb = float(kw["out"].nbytes())
            except Exception:
                nb = 65536.0
            SC.dma(eng, lambda e: e.dma_start(**kw), sem, [b.k for b in reads], [b.k for b in writes], lat=2.5 + nb / 150e3)

        class DR:
            def __init__(self):
                self.k = Tk(acc=True)

        PS = [Buf("ps%d" % i, [128, 512], F32, psum=True) for i in range(8)]
        adum = sb("adum", [128, 4])
        SC.op("dve", lambda e: e.memset(adum[:], 0.0), [], [adum.k])

        def psbf(i):
            return PS[i].t[:].bitcast(BF16)

        NC0 = coff["cmask"][0]
        cst = sb("cst", [128, NC0])
        s_c = SC.dma_sem("cst")
        dma("sp", s_c, [], [cst], out=cst[:], in_=cst_d[:, 0:NC0])

        def cc(name):
            a, b = coff[name]
            return cst[:, a:b]
        ident_f = cc("ident")
        ident_b = sb("ident_b", [128, 128], BF16)
        triu_b = sb("triu_b", [128, 128], BF16)
        ones_b = sb("ones_b", [128, 128], BF16)
        ones_f = sb("ones_f", [128, 128], F32)
        cmask_b = sb("cmask_b", [128, 4, 512], BF16)
        op("dve", "tensor_copy", [cst], [ident_b], out=ident_b[:], in_=ident_f)
        op("dve", "tensor_copy", [cst], [triu_b], out=triu_b[:], in_=cc("triu"))
        op("dve", "memset", [], [ones_b], ones_b[:], 1.0)
        op("dve", "memset", [], [ones_f], ones_f[:], 1.0)
        dma("pool", s_c, [], [cmask_b], out=cmask_b[:].rearrange("p a b -> p (a b)"), in_=cst_d[:, NC0:NC0 + 2048])
        dmaskT = cc("dmaskT").rearrange("p (h q) -> p h q", h=4)
        xi_c = cc("xi").rearrange("p (j q) -> p j q", j=2)
        zeta_c = cc("zeta")
        rp = cc("rp")
        cd_c = cc("cd")

        nhalf = sb("nhalf", [128, 16])
        op("pool", "memset", [], [nhalf], nhalf[:], -0.5)
        fdum = sb("fdum", [128, 2])

        rs_i = nhalf
        rs_t = nhalf
        import os as _os
        USE_POW = _os.environ.get("K_POW", "1") == "1"

        def rsqrt(vbuf, vap, outbuf, outap, n):
            if USE_POW:
                op("pool", "tensor_tensor", [vbuf, nhalf], [outbuf], out=outap, in0=vap, in1=nhalf[:, 0:n], op=ALU.pow)
                return
            yi = rs_i[:, 0:n]
            y = yi.bitcast(F32)
            tt = rs_t[:, 0:n]
            op("dve", "tensor_single_scalar", [vbuf], [rs_i], out=yi, in_=vap.bitcast(I32), scalar=1, op=ALU.arith_shift_right)
            op("dve", "tensor_scalar", [rs_i], [rs_i], out=yi, in0=yi, scalar1=-1.0, scalar2=float(0x5f3759df), op0=ALU.mult, op1=ALU.add)
            for it in range(3):
                op("dve", "tensor_tensor", [rs_i], [rs_t], out=tt, in0=y, in1=y, op=ALU.mult)
                op("dve", "tensor_tensor", [rs_t, vbuf], [rs_t], out=tt, in0=tt, in1=vap, op=ALU.mult)
                op("dve", "tensor_scalar", [rs_t], [rs_t], out=tt, in0=tt, scalar1=-0.5, scalar2=1.5, op0=ALU.mult, op1=ALU.add)
                if it < 2:
                    op("dve", "tensor_tensor", [rs_t, rs_i], [rs_i], out=y, in0=y, in1=tt, op=ALU.mult)
                else:
                    op("dve", "tensor_tensor", [rs_t, rs_i], [outbuf], out=outap, in0=y, in1=tt, op=ALU.mult)

        def fence(frm, to):
            SC.op("pool", lambda e: e.memset(fdum[0:1, 0:1], 0.0), [], [b_.k for b_ in frm] + [b_.k for b_ in to] + [fdum.k])

        s_m = SC.dma_sem("mods")
        mods_k = DR()
        cT = sb("cT", [128, 8, NSEQ])
        cTe = sb("cTe", [128, 8, NSEQ])
        siluT = sb("siluT", [128, 8, NSEQ], BF16)
        for b0 in range(NSEQ):
            dma("sp", s_m, [], [cT], out=cT[:, :, b0], in_=c_d[b0:b0 + 1, :].rearrange("o (c p) -> p (o c)", p=128), allow_slow_non_contiguous=True)
        op("act", "activation", [cT], [cTe], out=cTe[:], in_=cT[:], func=AF.Exp, scale=-1.0)
        op("dve", "tensor_scalar", [cTe], [cTe], out=cTe[:], in0=cTe[:], scalar1=1.0, scalar2=None, op0=ALU.add)
        op("dve", "reciprocal", [cTe], [cTe], out=cTe[:], in_=cTe[:])
        op("dve", "tensor_tensor", [cTe, cT], [siluT], out=siluT[:], in0=cTe[:], in1=cT[:], op=ALU.mult)
        BIGW = sb("BIGW", [128, 12448])
        P0 = sb("P0", [128, 2048])
        wa = [Alias(BIGW, 0, [128, 8, 512], BF16, own=True), Alias(BIGW, 8192, [128, 8, 512], BF16, own=True)]
        s_wa = [SC.dma_sem("wa%d" % i) for i in range(2)]
        ba = sb("ba", [NSEQ, 512])
        mrow = sb("mrow", [NSEQ, 512])
        s_ba = SC.dma_sem("ba")
        for j in range(12):
            w = wa[j % 2]
            dma("pool", s_wa[j % 2], [], [w], out=w[:], in_=wada_d[:, j * 512:(j + 1) * 512].rearrange("(c p) n -> p c n", p=128))
            dma("sp", s_ba, [], [ba], out=ba[:], in_=bada_d[:, j * 512:(j + 1) * 512].partition_broadcast(NSEQ))
            for k in range(8):
                op("pe", "matmul", [siluT, w], [PS[0]], PS[0][0:NSEQ, :], lhsT=siluT[:, k, :], rhs=w[:, k, :], start=(k == 0), stop=(k == 7))
            op("dve", "tensor_tensor", [PS[0], ba], [mrow], out=mrow[:], in0=PS[0][0:NSEQ, :], in1=ba[:], op=ALU.add)
            dma("sp", s_m, [mrow], [mods_k], out=mods_d[:, j * 512:(j + 1) * 512], in_=mrow[:])

        wblk = [Alias(BIGW, 0, [128, 6144], BF16, own=True), Alias(BIGW, 12288, [128, 6144], BF16, own=True)]
        s_wl = SC.dma_sem("wl")
        s_ws = SC.dma_sem("ws")
        wall_k = DR()

        def relayout_expert(e):
            stg = wblk[e % 2]
            v13 = stg[:, 0:4096].rearrange("p (c f) -> p c f", c=8)
            dma("pool", s_wl, [], [stg], out=v13[:, :, 0:256], in_=w1_d[e].rearrange("(c p) f -> p c f", p=128))
            dma("pool", s_wl, [], [stg], out=v13[:, :, 256:512], in_=w3_d[e].rearrange("(c p) f -> p c f", p=128))
            dma("pool", s_wl, [], [stg], out=stg[:, 4096:6144].rearrange("p (c f) -> p c f", c=2),
                in_=w2_d[e].rearrange("(c p) f -> p c f", p=128))
            dma("pool", s_ws, [stg], [wall_k], out=wall_d[e * 128:(e + 1) * 128, :], in_=stg[:])
        n_slots = NSEQ * 8 * NG
        per_slot = (NE + n_slots - 1) // n_slots
        relay_state = [0]

        fence(wa, [BIGW])
        s_w = SC.dma_sem("w")
        NFM = 8 * 128 + 2 * 96
        w_fm = Alias(BIGW, 0, [128, 8, NFM], BF16)
        w_tm = Alias(BIGW, 19456, [128, 8, 1408], BF16)
        wst = [Alias(BIGW, 41984, [128, 1952], F32)] * 2
        s_wst = [SC.dma_sem("wst%d" % i) for i in range(2)]
        wuq_a = sb("wuq_a", [128, 2, 8, 192], BF16)
        STG = P0
        wuq_s = Alias(STG, 0, [128, 2, 768], F32)
        qng_c = sb("qng_c", [128, 2])
        dma("sp", s_w, [], [wuq_s], out=wuq_s[:], in_=wuq_d.rearrange("(c p) n -> p c n", p=128))
        dma("sp", s_w, [], [qng_c], out=qng_c[:], in_=qng_d.rearrange("o (c p) -> p (o c)", p=128), allow_slow_non_contiguous=True)
        op("pool", "memset", [], [wuq_a], wuq_a[:], 0.0)
        for c in range(2):
            s4 = wuq_s[:, c, :].rearrange("p (h f) -> p h f", h=8)
            op("dve", "tensor_scalar", [wuq_s, qng_c], [wuq_a], out=wuq_a[:, c, :, 0:64], in0=s4[:, :, 0:64],
               scalar1=qng_c[:, c:c + 1], scalar2=None, op0=ALU.mult)
            for ab in range(2):
                dst = wuq_a[:, c, :, ab * 96 + 64:ab * 96 + 96].rearrange("p h (dup j) -> p h dup j", dup=2)
                src = s4[:, :, 64 + ab * 16:64 + ab * 16 + 16].unsqueeze(2).to_broadcast([128, 8, 2, 16])
                op("dve", "tensor_scalar", [wuq_s, qng_c], [wuq_a], out=dst, in0=src,
                   scalar1=qng_c[:, c:c + 1], scalar2=None, op0=ALU.mult)
        wukv_s = Alias(STG, 0, [128, 1024], F32)
        kvng_c = sb("kvng_c", [128, 1])
        wk_b = sb("wk_b", [128, 8, 64], BF16)
        wv_b = sb("wv_b", [128, 8, 64], BF16)
        dma("sp", s_w, [], [wukv_s], out=wukv_s[:], in_=wukv_d)
        dma("sp", s_w, [], [kvng_c], out=kvng_c[:], in_=kvng_d.rearrange("o p -> p o"), allow_slow_non_contiguous=True)
        s3 = wukv_s[:].rearrange("p (h f) -> p h f", h=8)
        op("dve", "tensor_scalar", [wukv_s, kvng_c], [wk_b], out=wk_b[:], in0=s3[:, :, 0:64], scalar1=kvng_c[:, 0:1], scalar2=None, op0=ALU.mult)
        op("dve", "tensor_scalar", [wukv_s, kvng_c], [wv_b], out=wv_b[:], in0=s3[:, :, 64:128], scalar1=kvng_c[:, 0:1], scalar2=None, op0=ALU.mult)
        wo_m = Alias(BIGW, 0, [64, 8, D], BF16)
        wo_r = Alias(BIGW, 16384, [128, 4, D], BF16)
        w_rt = sb("w_rt", [128, 8, 36])
        b_rt = sb("b_rt", [128, 36])
        dma("sp", s_w, [], [w_rt], out=w_rt[:, :, 0:4], in_=wgr_d.rearrange("(c p) n -> p c n", p=128), allow_slow_non_contiguous=True)
        dma("sp", s_w, [], [w_rt], out=w_rt[:, :, 4:36], in_=wer_d.rearrange("(c p) n -> p c n", p=128), allow_slow_non_contiguous=True)
        w_rtb = sb("w_rtb", [128, 8, 36], BF16)
        op("dve", "tensor_copy", [w_rt], [w_rtb], out=w_rtb[:], in_=w_rt[:])
        dma("sp", s_w, [], [b_rt], out=b_rt[:, 0:4], in_=bgr_d.partition_broadcast(128))
        dma("sp", s_w, [], [b_rt], out=b_rt[:, 4:36], in_=ber_d.partition_broadcast(128))
        n1g_c = sb("n1g_c", [128, 8])
        dma("sp", s_w, [], [n1g_c], out=n1g_c[:], in_=n1g_d.rearrange("o (c p) -> p (o c)", p=128), allow_slow_non_contiguous=True)

        OH1 = sb("OH1", [128, NTT, 32], BF16)
        OH2 = sb("OH2", [128, NTT, 32], BF16)
        CUM = sb("CUM", [128, NTT, 32])
        GATE = sb("GATE", [128, NTT, 2])
        Macc = sb("Macc", [128, 32], BF16)
        op("pool", "memset", [], [Macc], Macc[:], 0.0)

        cqnT = sb("cqnT", [128, 2, S], BF16)
        ckvnT = sb("ckvnT", [128, S], BF16)
        kT = sb("kT", [96, S], BF16)
        s_csm = SC.dma_sem("csm")
        csms_k = DR()
        xin = [sb("xin%d" % i, [128, D]) for i in range(2)]
        s_xin = [SC.dma_sem("xin%d" % i) for i in range(2)]
        junk = sb("junk", [128, D], BF16)
        stat = sb("stat", [128, 16])
        xsb = sb("xsb", [128, D], BF16)
        xsb2 = [xsb, sb("xsb1", [128, D], BF16)]
        P1 = sb("P1", [128, 2048])
        h1T = Alias(P1, 0, [128, 8, 512], BF16)
        P2b = sb("P2b", [128, 1024])
        posi = Alias(P2b, 0, [128, 512], I32)
        posf = Alias(P2b, 2048, [128, 512], F32)
        s_pos = SC.dma_sem("pos")
        P2a = sb("P2a", [128, 1024])
        targ = Alias(P2a, 0, [128, 512], F32)
        ttmp = Alias(P2a, 2048, [128, 512], F32)
        P3a = sb("P3a", [128, 1024])
        csr1 = Alias(P3a, 0, [128, 512], F32)
        csr2 = Alias(P3a, 2048, [128, 512], F32)
        P3b = sb("P3b", [128, 1024])
        csm1f = Alias(P3b, 0, [96, 512], F32)
        csm2f = Alias(P3b, 2048, [96, 512], F32)
        colv = sb("colv", [128, 8, 4])
        s_col = SC.dma_sem("col")
        P6 = sb("P6", [128, 1024])
        P7 = sb("P7", [128, 512])
        rqT = Alias(P6, 0, [128, 2, 512], BF16)
        rqxT = Alias(P6, 2048, [128, 2, 512], BF16)
        rkT = Alias(P7, 0, [128, 2, 512], BF16)
        P4a = sb("P4a", [128, 1024])
        rt1 = Alias(P4a, 0, [128, 512], F32)
        rt2 = Alias(P4a, 2048, [128, 512], F32)
        cqn = sb("cqn", [128, 384], BF16)
        P4b = sb("P4b", [128, 1024])
        P4c = sb("P4c", [128, 1024])
        RVG = [Alias(P4b, 0, [128, 4, 512], BF16), Alias(P4c, 0, [128, 4, 512], BF16)]
        GTG = [Alias(P0, 0, [128, 4, 512], BF16), Alias(P0, 4096, [128, 4, 512], BF16)]
        GT4 = GTG[0]
        gsg = sb("gsg", [128, 512])
        rkz = sb("rkz", [128, 256], BF16)
        sdT = sb("sdT", [128, 4, 128], BF16)
        state = sb("state", [128, 2, 128])
        state_b = sb("state_b", [128, 2, 128], BF16)
        P8 = sb("P8", [128, 1024])
        osb = Alias(P8, 0, [128, 4, 128], F32)
        osq = Alias(P8, 2048, [128, 4, 128], F32)
        gst = sb("gst", [128, 16])
        oretb = sb("oretb", [128, 512], BF16)
        P5 = sb("P5", [128, 1024])
        oretT = Alias(P5, 0, [128, 4, 512], BF16)
        s_mixr = SC.dma_sem("mixr")
        mixr_k = DR()
        mixm_k = DR()

        def rope_table(dst, dstap, prow, invf_col, ph_col):
            a = targ[prow, :]
            b = ttmp[prow, :]
            op("dve", "tensor_scalar", [posf, cst], [targ], out=a, in0=posf[prow, :], scalar1=rp[prow, invf_col:invf_col + 1],
               scalar2=rp[prow, ph_col:ph_col + 1], op0=ALU.mult, op1=ALU.add)
            op("dve", "tensor_scalar", [targ], [ttmp], out=b, in0=a, scalar1=1.0 / TWO_PI, scalar2=MAGIC_RN, op0=ALU.mult, op1=ALU.add)
            op("dve", "tensor_scalar", [ttmp], [ttmp], out=b, in0=b, scalar1=MAGIC_RN, scalar2=-TWO_PI, op0=ALU.subtract, op1=ALU.mult)
            op("dve", "tensor_tensor", [ttmp, targ], [targ], out=a, in0=a, in1=b, op=ALU.add)
            op("dve", "tensor_scalar", [targ], [targ], out=a, in0=a, scalar1=-3.1415925, scalar2=3.1415925, op0=ALU.max, op1=ALU.min)
            op("act", "activation", [targ], [dst], out=dstap, in_=a, func=AF.Sin)


        vh = [sb("vh0", [128, NT, 65], BF16)] * 2
        for i in range(1):
            op("pool", "memset", [], [vh[i]], vh[i][:, :, 64:65], 1.0)
        pT = [Alias(P5, 0, [128, 512], BF16, own=True), Alias(P5, 1024, [128, 512], BF16, own=True)]
        qT = [Alias(P5, 2048, [96, 512], BF16, own=True), Alias(P5, 3072, [96, 512], BF16, own=True)]
        rrow = Alias(P8, 0, [65, 512], F32)
        bcs = Alias(P8, 2048, [64, 512], F32)
        oTm = Alias(P7, 0, [64, 512], BF16)
        s_mixm = SC.dma_sem("mixm")
        s_bc = SC.dma_sem("bc")
        s_mm = SC.dma_sem("mm")
        s_x1 = SC.dma_sem("x1")
        s_h2 = SC.dma_sem("h2")
        x1s_k = DR()
        h2s_k = DR()
        g1bc = Alias(P2a, 0, [128, D], F32)
        sh2bc = Alias(P2b, 0, [128, D], F32)
        A2bc = Alias(P3a, 0, [128, D], F32)
        n2gbc = Alias(P3b, 0, [128, D], F32)
        mm_t = Alias(P6, 0, [64, 8, 128], BF16)
        mr_t = Alias(P6, 2048, [128, 4, 128], BF16)
        x1 = Alias(P4a, 0, [128, D], F32)
        h2 = Alias(P4b, 0, [128, D], F32)
        h2b = Alias(P7, 0, [128, D], BF16)
        h2T = Alias(P5, 0, [128, 8, 128], F32)
        x1_2 = [x1, Alias(P0, 0, [128, D], F32, own=True)]
        h2_2 = [h2, Alias(P0, 4096, [128, D], F32, own=True)]
        h2b_2 = [h2b, Alias(P4c, 0, [128, D], BF16, own=True)]
        h2T_2 = [h2T, Alias(P1, 0, [128, 8, 128], F32, own=True)]
        mm_t_2 = [mm_t, Alias(P1, 4096, [64, 8, 128], BF16, own=True)]
        mr_t_2 = [mr_t, Alias(P1, 6144, [128, 4, 128], BF16, own=True)]
        c_alts = [x1_2[1], h2_2[1], h2b_2[1], h2T_2[1], mm_t_2[1], mr_t_2[1]]
        h2Tb_2 = [Alias(P5, 0, [128, 8, 128], BF16), Alias(P1, 0, [128, 8, 128], BF16)]
        h2Tb_2[1].k = h2T_2[1].k
        s_mm2 = [s_mm, SC.dma_sem("mm1")]
        lgt2 = [sb("lgt%d" % i, [128, 40]) for i in range(2)]
        for i in range(2):
            op("dve", "memset", [], [lgt2[i]], lgt2[i][:], -1e30)
        m8_2 = [sb("m8_%d" % i, [128, 16]) for i in range(2)]
        rst_2 = [sb("rst_%d" % i, [128, 20]) for i in range(2)]
        lem_2 = [sb("lem_%d" % i, [128, 32]) for i in range(2)]
        Mt_2 = [sb("Mt_%d" % i, [128, 32], BF16) for i in range(2)]
        ra = Alias(P2a, 0, [128, 32], F32)
        rb = Alias(P2a, 128, [128, 32], F32)
        pad_ = Alias(P2a, 256, [128, 32], F32)
        pst = Alias(P2a, 384, [128, 32], F32)
        cmp3 = Alias(BIGW, 0, [128, NBLK, 32], F32)
        ebf = sb("ebf", [128, NBLK])
        WIDX = sb("WIDX", [128, NBLK], I32)
        cmpd = Alias(BIGW, 16384, [128, NTT, 32], F32)
        destf = Alias(P2b, 0, [128, NTT, 2], F32)
        DEST = sb("DEST", [128, NTT, 2], I32)
        h2r = [Alias(P6, 0, [128, D], BF16, own=True), Alias(P6, 2048, [128, D], BF16, own=True)]
        s_h2r = [SC.dma_sem("h2r%d" % i) for i in range(2)]
        s_sc = SC.dma_sem("sc")
        s_wg = [SC.dma_sem("wg%d" % i) for i in range(2)]
        xblk = [Alias(P1, 0, [128, 2, D], BF16, own=True), Alias(P1, 4096, [128, 2, D], BF16, own=True)]
        s_xb = [SC.dma_sem("xb%d" % i) for i in range(2)]
        xTb = Alias(P8, 0, [128, 2, 8, 128], BF16)
        sg = Alias(P2a, 0, [128, 256], F32)
        actb = Alias(P2a, 1024, [128, 256], BF16)
        actT = Alias(P2a, 1536, [128, 2, 128], BF16)
        ysb = [Alias(P0, 0, [128, D], BF16, own=True), Alias(P0, 4096, [128, D], BF16, own=True)]
        ysc = [Alias(P1, 0, [128, D], BF16, own=True), Alias(P1, 4096, [128, D], BF16, own=True)]
        s_ys = [SC.dma_sem("ys%d" % i) for i in range(2)]
        s_yg = [SC.dma_sem("yg%d" % i) for i in range(2)]
        for b in range(NSEQ):
            op("pool", "memset", [], [w_fm], w_fm[:, :, 1024:NFM], 0.0)
            for k in range(8):
                ws = wst[k % 2]
                dma("sp", s_wst[k % 2], [], [ws], out=ws[:], in_=win_d[k * 128:(k + 1) * 128, :])
                op("act", "copy", [ws], [w_tm], out=w_tm[:, k, 0:384], in_=ws[:, 0:384])
                op("act", "copy", [ws], [w_tm], out=w_tm[:, k, 384:1408], in_=ws[:, 928:1952])
                for which, base, scale in ((0, 416, 1.0), (1, 672, 0.125)):
                    src = ws[:, base:base + 256].rearrange("p (h two j) -> p h two j", h=4, two=2)
                    for ab in range(2):
                        dst = w_fm[:, k, (which * 4 + ab * 2) * 128:(which * 4 + ab * 2 + 2) * 128].rearrange(
                            "p (h dup j) -> p h dup j", h=4, dup=2)
                        op("dve", "tensor_scalar", [ws], [w_fm], out=dst,
                           in0=src[:, :, ab:ab + 1, :].to_broadcast([128, 4, 2, 32]), scalar1=scale, scalar2=None, op0=ALU.mult)
                srck = ws[:, 384:416].rearrange("p (two j) -> p two j", two=2)
                for ab in range(2):
                    dst = w_fm[:, k, 1024 + ab * 96 + 64:1024 + ab * 96 + 96].rearrange("p (dup j) -> p dup j", dup=2)
                    op("dve", "tensor_copy", [ws], [w_fm], out=dst, in_=srck[:, ab:ab + 1, :].to_broadcast([128, 2, 16]))
            dma("sp", s_col, [mods_k], [colv], out=colv[:, :, 0], in_=mods_d[b:b + 1, 0:D].rearrange("o (c p) -> p (o c)", p=128),
                allow_slow_non_contiguous=True)
            dma("sp", s_col, [mods_k], [colv], out=colv[:, :, 1], in_=mods_d[b:b + 1, D:2 * D].rearrange("o (c p) -> p (o c)", p=128),
                allow_slow_non_contiguous=True)
            op("dve", "scalar_tensor_tensor", [colv, n1g_c], [colv], out=colv[:, :, 2], in0=colv[:, :, 1], scalar=1.0, in1=n1g_c[:],
               op0=ALU.add, op1=ALU.mult)
            op("dve", "memset", [], [state], state[:], 0.0)
            op("dve", "memset", [], [state_b], state_b[:], 0.0)

            def A_pos(g):
                t0 = g * 512
                dma("sp", s_pos, [], [posi], out=posi[:], in_=pos_d[b:b + 1, t0:t0 + 512].partition_broadcast(128))
                op("dve", "tensor_copy", [posi], [posf], out=posf[:], in_=posi[:])

            def A_table(g, k):
                t0 = g * 512
                if k == 0:
                    rope_table(csr1, csr1[:], slice(0, 128), 0, 1)
                elif k == 1:
                    rope_table(csr2, csr2[:], slice(0, 128), 0, 2)
                elif k == 2:
                    rope_table(csm1f, csm1f[64:96, :], slice(64, 96), 3, 4)
                    dma("sp", s_csm, [csm1f], [csms_k], out=csms_d[b, 0, :, t0:t0 + 512], in_=csm1f[64:96, :])
                else:
                    rope_table(csm2f, csm2f[64:96, :], slice(64, 96), 3, 5)
                    dma("sp", s_csm, [csm2f], [csms_k], out=csms_d[b, 1, :, t0:t0 + 512], in_=csm2f[64:96, :])

            def A_S1(g, tl):
                ti = g * 4 + tl
                tok0 = ti * 128
                xi_ = xin[ti % 2]
                xs_ = xsb2[ti % 2]
                so = 10 + 3 * (ti % 2)
                dma("sp", s_xin[ti % 2], [], [xi_], out=xi_[:], in_=x_d[b, tok0:tok0 + 128, :])
                op("act", "activation", [xi_], [junk, stat], out=junk[:], in_=xi_[:], func=AF.Square, accum_out=stat[:, so:so + 1])
                op("dve", "tensor_scalar", [stat], [stat], out=stat[:, so + 1:so + 2], in0=stat[:, so:so + 1], scalar1=1.0 / D, scalar2=EPS,
                   op0=ALU.mult, op1=ALU.add)
                rsqrt(stat, stat[:, so + 1:so + 2], stat, stat[:, so + 2:so + 3], 1)
                op("dve", "tensor_scalar", [xi_, stat], [xs_], out=xs_[:], in0=xi_[:], scalar1=stat[:, so + 2:so + 3], scalar2=None, op0=ALU.mult)

            def A_T8(g, tl):
                ti = g * 4 + tl
                xs_ = xsb2[ti % 2]
                for c in range(8):
                    op("pe", "transpose", [xs_, ident_b], [PS[0]], out=psbf(0)[:, c * 128:(c + 1) * 128],
                       in_=xs_[:, c * 128:(c + 1) * 128], identity=ident_b[:])
                for c in range(8):
                    if c % 2 == 0:
                        op("dve", "tensor_scalar", [PS[0], colv], [h1T], out=h1T[:, c, tl * 128:(tl + 1) * 128],
                           in0=psbf(0)[:, c * 128:(c + 1) * 128], scalar1=colv[:, c, 2:3], scalar2=colv[:, c, 0:1],
                           op0=ALU.mult, op1=ALU.add)
                    else:
                        op("act", "activation", [PS[0], colv], [h1T], out=h1T[:, c, tl * 128:(tl + 1) * 128],
                           in_=psbf(0)[:, c * 128:(c + 1) * 128], func=AF.Identity, scale=colv[:, c, 2:3], bias=colv[:, c, 0:1])

            def A_MM(g, tl):
                RV4 = RVG[g % 2]
                GT4 = GTG[g % 2]
                ti = g * 4 + tl
                tok0 = ti * 128
                for (pb, c0, n) in ((1, 0, 384), (2, 384, 512), (3, 896, 512)):
                    for k in range(8):
                        op("pe", "matmul", [h1T, w_tm], [PS[pb]], PS[pb][:, 0:n], lhsT=h1T[:, k, tl * 128:(tl + 1) * 128],
                           rhs=w_tm[:, k, c0:c0 + n], start=(k == 0), stop=(k == 7))
                op("act", "activation", [PS[1]], [junk, stat], out=junk[:, 0:256], in_=PS[1][:, 0:256], func=AF.Square, accum_out=stat[:, 4:5])
                op("act", "activation", [PS[1]], [junk, stat], out=junk[:, 256:384], in_=PS[1][:, 256:384], func=AF.Square, accum_out=stat[:, 5:6])
                op("dve", "tensor_scalar", [stat], [stat], out=stat[:, 6:7], in0=stat[:, 4:5], scalar1=1.0 / 256, scalar2=EPS, op0=ALU.mult, op1=ALU.add)
                op("dve", "tensor_scalar", [stat], [stat], out=stat[:, 7:8], in0=stat[:, 5:6], scalar1=1.0 / 128, scalar2=EPS, op0=ALU.mult, op1=ALU.add)
                rsqrt(stat, stat[:, 6:8], stat, stat[:, 8:10], 2)
                op("dve", "tensor_scalar", [PS[1], stat], [cqn], out=cqn[:, 0:256], in0=PS[1][:, 0:256], scalar1=stat[:, 8:9], scalar2=None, op0=ALU.mult)
                op("dve", "tensor_scalar", [PS[1], stat], [cqn], out=cqn[:, 256:384], in0=PS[1][:, 256:384], scalar1=stat[:, 9:10], scalar2=None, op0=ALU.mult)
                for c in range(3):
                    op("pe", "transpose", [cqn, ident_b], [PS[0]], out=psbf(0)[:, c * 128:(c + 1) * 128],
                       in_=cqn[:, c * 128:(c + 1) * 128], identity=ident_b[:])
                op("act", "copy", [PS[0]], [cqnT], out=cqnT[:, :, tok0:tok0 + 128],
                   in_=psbf(0)[:, 0:256].rearrange("p (c t) -> p c t", c=2))
                op("act", "copy", [PS[0]], [ckvnT], out=ckvnT[:, tok0:tok0 + 128], in_=psbf(0)[:, 256:384])
                op("act", "copy", [PS[2]], [RV4], out=RV4[:, tl, :], in_=PS[2][:])
                op("act", "activation", [PS[3]], [gsg], out=gsg[:], in_=PS[3][:], func=AF.Tanh, scale=0.5)
                op("dve", "scalar_tensor_tensor", [gsg, PS[3]], [GT4], out=GT4[:, tl, :], in0=gsg[:], scalar=1.0, in1=PS[3][:], op0=ALU.add, op1=ALU.mult)

            def A_FM(g):
                t0 = g * 512

                def fm_mm(pb, col0, ncols):
                    for k in range(8):
                        op("pe", "matmul", [h1T, w_fm], [PS[pb]], PS[pb][0:ncols, :], lhsT=w_fm[:, k, col0:col0 + ncols],
                           rhs=h1T[:, k, :], start=(k == 0), stop=(k == 7))
                for which, dst in ((0, rqT), (1, rkT)):
                    for j in range(2):
                        fm_mm(4, (which * 4 + j) * 128, 128)
                        fm_mm(5, (which * 4 + 2 + j) * 128, 128)
                        op("dve", "tensor_tensor", [PS[4], csr1], [rt1], out=rt1[:], in0=PS[4][:], in1=csr1[:], op=ALU.mult)
                        op("dve", "tensor_tensor", [PS[5], csr2], [rt2], out=rt2[:], in0=PS[5][:], in1=csr2[:], op=ALU.mult)
                        op("pool", "tensor_tensor", [rt1, rt2], [dst], out=dst[:, j, :], in0=rt1[:], in1=rt2[:], op=ALU.add)
                op("pool", "tensor_tensor", [rqT, cst], [rqxT], out=rqxT[:].rearrange("p j (n q) -> p j n q", n=4),
                   in0=rqT[:].rearrange("p j (n q) -> p j n q", n=4), in1=xi_c.unsqueeze(2).to_broadcast([128, 2, 4, 128]), op=ALU.mult)
                fm_mm(4, 1024, 96)
                fm_mm(5, 1120, 96)
                op("dve", "tensor_tensor", [PS[4], csm1f], [rt1], out=rt1[64:96, :], in0=PS[4][64:96, :], in1=csm1f[64:96, :], op=ALU.mult)
                op("dve", "tensor_tensor", [PS[5], csm2f], [rt2], out=rt2[64:96, :], in0=PS[5][64:96, :], in1=csm2f[64:96, :], op=ALU.mult)
                op("pool", "tensor_tensor", [rt1, rt2], [kT], out=kT[64:96, t0:t0 + 512], in0=rt1[64:96, :], in1=rt2[64:96, :], op=ALU.add)

            def A_RETa(g, tl):
                RV4 = RVG[g % 2]
                qs = slice(tl * 128, (tl + 1) * 128)
                for j in range(2):
                    op("pe", "transpose", [rkT, ident_b], [PS[4]], out=psbf(4)[:, j * 128:(j + 1) * 128], in_=rkT[:, j, qs], identity=ident_b[:])
                op("dve", "tensor_tensor", [PS[4], cst], [rkz], out=rkz[:], in0=psbf(4)[:, 0:256], in1=zeta_c, op=ALU.mult)
                for h in range(4):
                    j, half = h // 2, h % 2
                    pr = slice(half * 64, half * 64 + 64)
                    op("pe", "matmul", [rkT, rqT], [PS[6]], PS[6][:, h * 128:(h + 1) * 128], lhsT=rkT[pr, j, qs], rhs=rqT[pr, j, qs],
                       start=True, stop=True)
                op("dve", "tensor_tensor", [PS[6], cst], [sdT], out=sdT[:], in0=PS[6][:].rearrange("p (h q) -> p h q", h=4), in1=dmaskT, op=ALU.mult)
                for h in range(4):
                    j, half = h // 2, h % 2
                    pr = slice(half * 64, half * 64 + 64)
                    op("pe", "matmul", [sdT, RV4], [PS[7]], PS[7][:, h * 128:(h + 1) * 128], lhsT=sdT[:, h, :], rhs=RV4[:, tl, h * 128:(h + 1) * 128],
                       start=True, stop=False)
                    op("pe", "matmul", [rqxT, state_b], [PS[7]], PS[7][:, h * 128:(h + 1) * 128], lhsT=rqxT[pr, j, qs], rhs=state_b[pr, j, :],
                       start=False, stop=True)
                for h in range(4):
                    j = h // 2
                    op("pe", "matmul", [rkz, RV4], [PS[6]], PS[6][:, h * 128:(h + 1) * 128], lhsT=rkz[:, j * 128:(j + 1) * 128],
                       rhs=RV4[:, tl, h * 128:(h + 1) * 128], start=True, stop=True)
                for h in range(4):
                    j, half = h // 2, h % 2
                    pr = slice(half * 64, half * 64 + 64)
                    op("dve", "scalar_tensor_tensor", [state, cst, PS[6]], [state], out=state[pr, j, :], in0=state[pr, j, :],
                       scalar=cd_c[pr, h:h + 1], in1=PS[6][pr, h * 128:(h + 1) * 128], op0=ALU.mult, op1=ALU.add)
                op("pool", "tensor_copy", [state], [state_b], out=state_b[:], in_=state[:])

            def A_RETb_dve(g, tl):
                GT4 = GTG[g % 2]
                op("act", "copy", [PS[7]], [osb], out=osb[:], in_=PS[7][:].rearrange("p (h d) -> p h d", h=4))
                op("dve", "tensor_reduce", [osb], [gst], out=gst[:, 0:4], in_=osb[:], axis=AX.X, op=ALU.add)
                op("pool", "tensor_tensor", [osb], [osq], out=osq[:], in0=osb[:], in1=osb[:], op=ALU.mult)
                op("dve", "tensor_reduce", [osq], [gst], out=gst[:, 4:8], in_=osq[:], axis=AX.X, op=ALU.add)
                op("dve", "tensor_scalar", [gst], [gst], out=gst[:, 0:4], in0=gst[:, 0:4], scalar1=1.0 / 128, scalar2=None, op0=ALU.mult)
                op("dve", "tensor_tensor", [gst], [gst], out=gst[:, 8:12], in0=gst[:, 0:4], in1=gst[:, 0:4], op=ALU.mult)
                op("dve", "scalar_tensor_tensor", [gst], [gst], out=gst[:, 8:12], in0=gst[:, 4:8], scalar=1.0 / 128, in1=gst[:, 8:12],
                   op0=ALU.mult, op1=ALU.subtract)
                op("dve", "tensor_scalar", [gst], [gst], out=gst[:, 8:12], in0=gst[:, 8:12], scalar1=EPS, scalar2=None, op0=ALU.add)
                rsqrt(gst, gst[:, 8:12], gst, gst[:, 12:16], 4)
                op("dve", "tensor_scalar", [gst], [gst], out=gst[:, 12:16], in0=gst[:, 12:16], scalar1=0.5, scalar2=None, op0=ALU.mult)
                op("dve", "tensor_tensor", [osb, gst], [osb], out=osb[:], in0=osb[:], in1=gst[:, 0:4].unsqueeze(2).to_broadcast([128, 4, 128]), op=ALU.subtract)
                op("dve", "tensor_tensor", [osb, gst], [osb], out=osb[:], in0=osb[:], in1=gst[:, 12:16].unsqueeze(2).to_broadcast([128, 4, 128]), op=ALU.mult)
                op("pool", "tensor_tensor", [osb, GT4], [oretb], out=oretb[:], in0=osb[:].rearrange("p h d -> p (h d)"), in1=GT4[:, tl, :], op=ALU.mult)

            def A_RETb_pe(g, tl):
                qs = slice(tl * 128, (tl + 1) * 128)
                for h in range(4):
                    op("pe", "transpose", [oretb, ident_b], [PS[5]], out=psbf(5)[:, h * 128:(h + 1) * 128], in_=oretb[:, h * 128:(h + 1) * 128], identity=ident_b[:])
                op("act", "copy", [PS[5]], [oretT], out=oretT[:, :, qs], in_=psbf(5)[:, 0:512].rearrange("p (h t) -> p h t", h=4))

            A_S1(0, 0)
            for g in range(NG + 1):
                if g < NG:
                    A_pos(g)
                for tl in range(4):
                    if g < NG:
                        A_T8(g, tl)
                    nxt = g * 4 + tl + 1
                    if nxt < NG * 4:
                        A_S1(nxt // 4, nxt % 4)
                    if g >= 1:
                        A_RETa(g - 1, tl)
                    if g >= 1 and tl > 0:
                        A_RETb_pe(g - 1, tl - 1)
                    if g < NG:
                        A_MM(g, tl)
                    if g >= 1:
                        A_RETb_dve(g - 1, tl)
                    if g < NG:
                        A_table(g, tl)
                if g < NG:
                    A_FM(g)
                if g >= 1:
                    A_RETb_pe(g - 1, 3)
                    dma("sp", s_mixr, [oretT], [mixr_k], out=mixr_d[b, :, :, (g - 1) * 512:g * 512].rearrange("h p t -> p h t"), in_=oretT[:])
            fence([oretT, BIGW], pT + qT + wblk)

            def qprep(h, i):
                qsl = slice(i * 512, (i + 1) * 512)
                for c in range(2):
                    op("pe", "matmul", [wuq_a, cqnT], [PS[4]], PS[4][0:96, :], lhsT=wuq_a[:, c, h, 0:96], rhs=cqnT[:, c, qsl], start=(c == 0), stop=(c == 1))
                for c in range(2):
                    op("pe", "matmul", [wuq_a, cqnT], [PS[5]], PS[5][0:96, :], lhsT=wuq_a[:, c, h, 96:192], rhs=cqnT[:, c, qsl], start=(c == 0), stop=(c == 1))
                qt = qT[(h * NG + i) % 2]
                op("act", "copy", [PS[4]], [qt], out=qt[0:64, :], in_=PS[4][0:64, :])
                dma("sp", s_csm, [csms_k], [csm1f], out=csm1f[64:96, :], in_=csms_d[b, 0, :, qsl])
                dma("sp", s_csm, [csms_k], [csm2f], out=csm2f[64:96, :], in_=csms_d[b, 1, :, qsl])
                op("dve", "tensor_tensor", [PS[4], csm1f], [rt1], out=rt1[64:96, :], in0=PS[4][64:96, :], in1=csm1f[64:96, :], op=ALU.mult)
                op("dve", "tensor_tensor", [PS[5], csm2f], [rt2], out=rt2[64:96, :], in0=PS[5][64:96, :], in1=csm2f[64:96, :], op=ALU.mult)
                op("dve", "tensor_tensor", [rt1, rt2], [qt], out=qt[64:96, :], in0=rt1[64:96, :], in1=rt2[64:96, :], op=ALU.add)

            pend_epi = []
            for h in range(8):
                for g in range(NG):
                    op("pe", "matmul", [wk_b, ckvnT], [PS[6]], PS[6][0:64, :], lhsT=wk_b[:, h, :], rhs=ckvnT[:, g * 512:(g + 1) * 512], start=True, stop=True)
                    op("act", "copy", [PS[6]], [kT], out=kT[0:64, g * 512:(g + 1) * 512], in_=PS[6][0:64, :])
                vb = vh[h % 2]
                for t8 in range((NT + 7) // 8):
                    n8 = min(8, NT - t8 * 8)
                    for tt_ in range(n8):
                        ti = t8 * 8 + tt_
                        op("pe", "matmul", [ckvnT, wv_b], [PS[7]], PS[7][:, tt_ * 64:(tt_ + 1) * 64], lhsT=ckvnT[:, ti * 128:(ti + 1) * 128], rhs=wv_b[:, h, :],
                           start=True, stop=True)
                    op("dve", "tensor_copy", [PS[7]], [vb], out=vb[:, t8 * 8:t8 * 8 + n8, 0:64], in_=PS[7][:, 0:n8 * 64].rearrange("p (t d) -> p t d", d=64))
                qprep(h, 0)
                for i in range(NG):
                    qsl = slice(i * 512, (i + 1) * 512)
                    qt = qT[(h * NG + i) % 2]
                    if i + 1 < NG:
                        qprep(h, i + 1)
                    nk = 4 * i + 4
                    ob = 2 + ((h * NG + i) % 2)

                    def c0_of(j):
                        return 128 * (j - 4 * i) if j > 4 * i else 0

                    def qk(j):
                        c0 = c0_of(j)
                        op("pe", "matmul", [kT, qt], [PS[j % 2]], PS[j % 2][:, c0:512], lhsT=kT[0:96, j * 128:(j + 1) * 128], rhs=qt[0:96, c0:512], start=True, stop=True)
                    qk(0)
                    for j in range(nk):
                        if j + 1 < nk:
                            qk(j + 1)
                        p_ = pT[j % 2]
                        c0 = c0_of(j)
                        op("act", "activation", [PS[j % 2]], [p_], out=p_[:, c0:512], in_=PS[j % 2][:, c0:512], func=AF.Exp, scale=float(96 ** -0.5))
                        if j >= 4 * i:
                            m_ = j - 4 * i
                            op("dve", "tensor_tensor", [p_, cmask_b], [p_], out=p_[:, c0:c0 + 128], in0=p_[:, c0:c0 + 128], in1=cmask_b[:, m_, c0:c0 + 128], op=ALU.mult)
                        op("pe", "matmul", [vb, p_], [PS[ob]], PS[ob][0:65, c0:512], lhsT=vb[:, j, 0:65], rhs=p_[:, c0:512], start=(j == 0), stop=(j == nk - 1))
                        if j == 1 and pend_epi:
                            pend_epi.pop(0)()
                    def epilogue(ob=ob, h=h, qsl=qsl):
                        op("dve", "reciprocal", [PS[ob]], [rrow], out=rrow[64:65, :], in_=PS[ob][64:65, :])
                        op("pe", "matmul", [ones_f, rrow], [PS[7]], PS[7][0:64, :], lhsT=ones_f[64:65, 0:64], rhs=rrow[64:65, :], start=True, stop=True)
                        op("act", "copy", [PS[7]], [bcs], out=bcs[:], in_=PS[7][0:64, :])
                        op("dve", "tensor_tensor", [PS[ob], bcs], [oTm], out=oTm[:], in0=PS[ob][0:64, :], in1=bcs[:], op=ALU.mult)
                        dma("sp", s_mixm, [oTm], [mixm_k], out=mixm_d[b, h, :, qsl], in_=oTm[:])
                    pend_epi.append(epilogue)
                    for _ in range(per_slot):
                        if relay_state[0] < NE:
                            relayout_expert(relay_state[0])
                            relay_state[0] += 1
            while pend_epi:
                pend_epi.pop(0)()
            fence(pT + qT + wblk, [h2T, BIGW])

            fence([GTG[0], h1T, RVG[1]], c_alts)
            dma("pool", s_w, [], [wo_m], out=wo_m[:], in_=wo_d[0:512, :].rearrange("(h p) n -> p h n", p=64))
            dma("pool", s_w, [], [wo_r], out=wo_r[:], in_=wo_d[512:1024, :].rearrange("(h p) n -> p h n", p=128))
            dma("sp", s_bc, [mods_k], [g1bc], out=g1bc[:], in_=mods_d[b:b + 1, 2 * D:3 * D].partition_broadcast(128))
            dma("sp", s_bc, [mods_k], [sh2bc], out=sh2bc[:], in_=mods_d[b:b + 1, 3 * D:4 * D].partition_broadcast(128))
            dma("sp", s_bc, [mods_k], [A2bc], out=A2bc[:], in_=mods_d[b:b + 1, 4 * D:5 * D].partition_broadcast(128))
            dma("sp", s_bc, [], [n2gbc], out=n2gbc[:], in_=n2g_d.partition_broadcast(128))
            op("dve", "scalar_tensor_tensor", [A2bc, n2gbc], [A2bc], out=A2bc[:], in0=A2bc[:], scalar=1.0, in1=n2gbc[:], op0=ALU.add, op1=ALU.mult)
            def C1(ti):
                tok0 = ti * 128
                gt = b * NT + ti
                p2 = ti % 2
                x1 = x1_2[p2]
                h2 = h2_2[p2]
                h2b = h2b_2[p2]
                h2T = h2T_2[p2]
                mm_t = mm_t_2[p2]
                mr_t = mr_t_2[p2]
                pa, pbk = (0, 1) if p2 == 0 else (6, 7)
                so = 0 if p2 == 0 else 10
                xi_ = xin[ti % 2]
                dma("sp", s_xin[ti % 2], [], [xi_], out=xi_[:], in_=x_d[b, tok0:tok0 + 128, :])
                dma("sp", s_mm2[p2], [mixm_k], [mm_t], out=mm_t[:], in_=mixm_d[b, :, :, tok0:tok0 + 128].rearrange("h p t -> p h t"))
                dma("sp", s_mm2[p2], [mixr_k], [mr_t], out=mr_t[:], in_=mixr_d[b, :, :, tok0:tok0 + 128].rearrange("h p t -> p h t"))
                for nh, pbank in ((0, pa), (1, pbk)):
                    for hh in range(8):
                        op("pe", "matmul", [mm_t, wo_m], [PS[pbank]], PS[pbank][:, :], lhsT=mm_t[:, hh, :], rhs=wo_m[:, hh, nh * 512:(nh + 1) * 512], start=(hh == 0), stop=False)
                    for hh in range(4):
                        op("pe", "matmul", [mr_t, wo_r], [PS[pbank]], PS[pbank][:, :], lhsT=mr_t[:, hh, :], rhs=wo_r[:, hh, nh * 512:(nh + 1) * 512], start=False, stop=(hh == 3))
                for nh, pbank in ((0, pa), (1, pbk)):
                    op("dve", "tensor_tensor", [PS[pbank], g1bc], [x1], out=x1[:, nh * 512:(nh + 1) * 512], in0=PS[pbank][:, :], in1=g1bc[:, nh * 512:(nh + 1) * 512], op=ALU.mult)
                op("pool", "tensor_tensor", [x1, xi_], [x1], out=x1[:], in0=x1[:], in1=xi_[:], op=ALU.add)
                dma("sp", s_x1, [x1], [x1s_k], out=x1s_d[gt * 128:(gt + 1) * 128, :], in_=x1[:])
                op("act", "activation", [x1], [junk, stat], out=junk[:], in_=x1[:], func=AF.Square, accum_out=stat[:, so:so + 1])
                op("dve", "tensor_scalar", [stat], [stat], out=stat[:, so + 1:so + 2], in0=stat[:, so:so + 1], scalar1=1.0 / D, scalar2=EPS, op0=ALU.mult, op1=ALU.add)
                rsqrt(stat, stat[:, so + 1:so + 2], stat, stat[:, so + 2:so + 3], 1)
                op("dve", "scalar_tensor_tensor", [x1, stat, A2bc], [h2], out=h2[:], in0=x1[:], scalar=stat[:, so + 2:so + 3], in1=A2bc[:], op0=ALU.mult, op1=ALU.mult)
                op("pool", "tensor_tensor", [h2, sh2bc], [h2], out=h2[:], in0=h2[:], in1=sh2bc[:], op=ALU.add)
                op("act", "copy", [h2], [h2b], out=h2b[:], in_=h2[:])
                dma("sp", s_h2, [h2b], [h2s_k], out=h2s_d[gt * 128:(gt + 1) * 128, :], in_=h2b[:])
                h2Tb = h2Tb_2[p2]
                for c in range(8):
                    op("pe", "transpose", [h2b, ident_b], [PS[2 + p2]], out=psbf(2 + p2)[:, c * 128:(c + 1) * 128], in_=h2b[:, c * 128:(c + 1) * 128], identity=ident_b[:])
                op("act", "copy", [PS[2 + p2]], [h2T], out=h2Tb[:], in_=psbf(2 + p2)[:, 0:1024].rearrange("p (c t) -> p c t", c=8))
                for c in range(8):
                    op("pe", "matmul", [h2T, w_rtb], [PS[4]], PS[4][:, 0:36], lhsT=h2Tb[:, c, :], rhs=w_rtb[:, c, :], start=(c == 0), stop=(c == 7))

                lgt = lgt2[ti % 2]
                op("dve", "tensor_tensor", [PS[4], b_rt], [lgt], out=lgt[:, 0:4], in0=PS[4][:, 0:4], in1=b_rt[:, 0:4], op=ALU.add)
                op("dve", "tensor_tensor", [PS[4], b_rt], [lgt], out=lgt[:, 8:40], in0=PS[4][:, 4:36], in1=b_rt[:, 4:36], op=ALU.add)

            def C2(ti):
                gt = b * NT + ti
                lgt = lgt2[ti % 2]
                m8 = m8_2[ti % 2]
                rst = rst_2[ti % 2]
                lem = lem_2[ti % 2]
                Mt = Mt_2[ti % 2]
                op("dve", "max", [lgt], [m8], out=m8[:, 0:8], in_=lgt[:, 0:8])
                op("dve", "tensor_scalar", [m8], [rst], out=rst[:, 0:1], in0=m8[:, 0:1], scalar1=-1.0, scalar2=None, op0=ALU.mult)
                op("act", "activation", [lgt, rst], [rst], out=rst[:, 8:16], in_=lgt[:, 0:8], func=AF.Exp, bias=rst[:, 0:1], scale=1.0, accum_out=rst[:, 1:2])
                op("dve", "reciprocal", [rst], [rst], out=rst[:, 2:3], in_=rst[:, 1:2])
                op("dve", "tensor_scalar", [lgt, m8], [rst], out=rst[:, 16:20], in0=lgt[:, 0:4], scalar1=m8[:, 0:1], scalar2=None, op0=ALU.is_equal)
                op("dve", "tensor_scalar", [rst], [rst], out=rst[:, 16:20], in0=rst[:, 16:20], scalar1=-1.0, scalar2=1e30, op0=ALU.add, op1=ALU.mult)
                op("dve", "tensor_tensor", [lgt, rst], [lem], out=lem[:].rearrange("p (g e) -> p g e", g=4), in0=lgt[:, 8:40].rearrange("p (g e) -> p g e", g=4),
                   in1=rst[:, 16:20].unsqueeze(2).to_broadcast([128, 4, 8]), op=ALU.add)
                op("dve", "max", [lem], [m8], out=m8[:, 8:16], in_=lem[:])
                op("dve", "tensor_scalar", [lem, m8], [OH1], out=OH1[:, gt, :], in0=lem[:], scalar1=m8[:, 8:9], scalar2=None, op0=ALU.is_equal)
                op("dve", "tensor_scalar", [lem, m8], [OH2], out=OH2[:, gt, :], in0=lem[:], scalar1=m8[:, 9:10], scalar2=None, op0=ALU.is_equal)
                op("dve", "tensor_tensor", [m8], [rst], out=rst[:, 3:4], in0=m8[:, 9:10], in1=m8[:, 8:9], op=ALU.subtract)
                op("act", "activation", [rst], [rst], out=rst[:, 4:5], in_=rst[:, 3:4], func=AF.Exp)
                op("dve", "tensor_scalar", [rst], [rst], out=rst[:, 4:5], in0=rst[:, 4:5], scalar1=1.0, scalar2=None, op0=ALU.add)
                op("dve", "reciprocal", [rst], [rst], out=rst[:, 5:6], in_=rst[:, 4:5])
                op("dve", "tensor_tensor", [rst], [GATE], out=GATE[:, gt, 0:1], in0=rst[:, 5:6], in1=rst[:, 2:3], op=ALU.mult)
                op("dve", "tensor_tensor", [rst, GATE], [GATE], out=GATE[:, gt, 1:2], in0=rst[:, 2:3], in1=GATE[:, gt, 0:1], op=ALU.subtract)
                op("pool", "tensor_tensor", [OH1, OH2], [Mt], out=Mt[:], in0=OH1[:, gt, :], in1=OH2[:, gt, :], op=ALU.add)
                op("pe", "matmul", [triu_b, Mt], [PS[5]], PS[5][:, 0:32], lhsT=triu_b[:], rhs=Mt[:], start=True, stop=False)
                op("pe", "matmul", [ones_b, Macc], [PS[5]], PS[5][:, 0:32], lhsT=ones_b[:], rhs=Macc[:], start=False, stop=True)
                op("act", "copy", [PS[5]], [CUM], out=CUM[:, gt, :], in_=PS[5][:, 0:32])
                op("pool", "tensor_tensor", [Macc, Mt], [Macc], out=Macc[:], in0=Macc[:], in1=Mt[:], op=ALU.add)


            import os as _os2
            if _os2.environ.get("K_CSKEW", "1") == "1":
                C1(0)
                for ti in range(NT):
                    if ti + 1 < NT:
                        C1(ti + 1)
                    C2(ti)
            else:
                for ti in range(NT):
                    C1(ti)
                    C2(ti)
            fence(c_alts, [P0, P1, P4c])

        op("pe", "matmul", [ones_b, Macc], [PS[5]], PS[5][:, 0:32], lhsT=ones_b[:], rhs=Macc[:], start=True, stop=True)
        op("dve", "tensor_scalar", [PS[5]], [ra], out=ra[:], in0=PS[5][:, 0:32], scalar1=1.0 / BLK, scalar2=(BLK - 1 - (BLK / 2 - 0.5)) / BLK, op0=ALU.mult, op1=ALU.add)
        op("dve", "tensor_scalar", [ra], [ra], out=ra[:], in0=ra[:], scalar1=MAGIC_RN, scalar2=None, op0=ALU.add)
        op("dve", "tensor_scalar", [ra], [pad_], out=pad_[:], in0=ra[:], scalar1=MAGIC_RN, scalar2=float(BLK), op0=ALU.subtract, op1=ALU.mult)
        op("dve", "tensor_copy", [pad_], [ra], out=ra[:], in_=pad_[:])
        cur, oth = ra, rb
        for sft in (1, 2, 4, 8, 16):
            op("dve", "tensor_copy", [cur], [oth], out=oth[:, 0:sft], in_=cur[:, 0:sft])
            op("dve", "tensor_tensor", [cur], [oth], out=oth[:, sft:32], in0=cur[:, sft:32], in1=cur[:, 0:32 - sft], op=ALU.add)
            cur, oth = oth, cur
        pend = cur
        op("dve", "tensor_tensor", [pend, pad_], [pst], out=pst[:], in0=pend[:], in1=pad_[:], op=ALU.subtract)
        a0, a1 = coff["blkstart"]
        op("dve", "tensor_tensor", [pend, cst], [cmp3], out=cmp3[:], in0=pend[:].unsqueeze(1).to_broadcast([128, NBLK, 32]),
           in1=cst[:, a0:a1].unsqueeze(2).to_broadcast([128, NBLK, 32]), op=ALU.is_le)
        op("dve", "tensor_reduce", [cmp3], [ebf], out=ebf[:], in_=cmp3[:], axis=AX.X, op=ALU.add)
        i0, i1 = coff["iotap"]
        op("dve", "tensor_scalar", [ebf], [ebf], out=ebf[:], in0=ebf[:], scalar1=31.0, scalar2=128.0, op0=ALU.min, op1=ALU.mult)
        op("dve", "tensor_scalar", [ebf, cst], [WIDX], out=WIDX[:], in0=ebf[:], scalar1=cst[:, i0:i1], scalar2=None, op0=ALU.add)
        op("pool", "tensor_tensor", [CUM, pst], [CUM], out=CUM[:], in0=CUM[:], in1=pst[:].unsqueeze(1).to_broadcast([128, NTT, 32]), op=ALU.add)
        for k_, OH in ((0, OH1), (1, OH2)):
            op("dve", "tensor_tensor", [OH, CUM], [cmpd], out=cmpd[:], in0=OH[:], in1=CUM[:], op=ALU.mult)
            op("dve", "tensor_reduce", [cmpd], [destf], out=destf[:, :, k_], in_=cmpd[:], axis=AX.X, op=ALU.add)
        op("dve", "tensor_copy", [destf], [DEST], out=DEST[:], in_=destf[:])

        xs_k = DR()
        ys_k = DR()
        h2r = h2r + [Alias(P7, 0, [128, D], BF16, own=True), Alias(P4c, 0, [128, D], BF16, own=True)]
        s_h2r = s_h2r + [SC.dma_sem("h2r2"), SC.dma_sem("h2r3")]
        fence([rqT, rkT, RVG[1], h1T, GT4, BIGW], h2r + xblk + ysb + wblk)
        for gt in range(NTT):
            hr = h2r[gt % 4]
            dma("sp", s_h2r[gt % 4], [h2s_k], [hr], out=hr[:], in_=h2s_d[gt * 128:(gt + 1) * 128, :])
            for k_ in range(2):
                SC.dma("pool", (lambda e, hr=hr, gt=gt, k_=k_: e.indirect_dma_start(
                    out=xs_d, out_offset=bass.IndirectOffsetOnAxis(ap=DEST[:, gt, k_:k_ + 1], axis=0), in_=hr[:, :], in_offset=None)),
                    s_sc, [hr.k, DEST.k], [xs_k.k], lat=6.0)

        xTb2 = [xTb, Alias(P4b, 0, [128, 2, 8, 128], BF16)]
        actb2 = [[Alias(P2a, 1024 + 512 * (2 * pq + r), [128, 256], BF16, own=True) for r in range(2)] for pq in range(2)]
        sg2 = [sg, Alias(P2a, 3072, [128, 256], F32, own=True)]
        actT2 = [Alias(P3a, 512 * r, [128, 2, 128], BF16, own=True) for r in range(2)]
        fence([g1bc, A2bc], [a_ for l_ in actb2 for a_ in l_] + sg2 + actT2)

        def stage1(blk):
            pq = blk % 2
            wb = wblk[pq]
            SC.dma("pool", (lambda e, wb=wb, blk=blk: e.indirect_dma_start(
                out=wb[:, :], out_offset=None, in_=wall_d, in_offset=bass.IndirectOffsetOnAxis(ap=WIDX[:, blk:blk + 1], axis=0))),
                s_wg[pq], [WIDX.k, wall_k.k], [wb.k], lat=14.0)
            xb_ = xblk[pq]
            dma("sp", s_xb[pq], [xs_k], [xb_], out=xb_[:], in_=xs_d[blk * BLK:(blk + 1) * BLK, :].rearrange("(r p) d -> p r d", p=128))
            xt_ = xTb2[pq]
            for r in range(2):
                for c in range(8):
                    op("pe", "transpose", [xb_, ident_b], [PS[0]], out=psbf(0)[:, c * 128:(c + 1) * 128], in_=xb_[:, r, c * 128:(c + 1) * 128], identity=ident_b[:])
                if r == 0:
                    op("act", "copy", [PS[0]], [xt_], out=xt_[:, r, :, :], in_=psbf(0)[:, 0:1024].rearrange("p (c t) -> p c t", c=8))
                else:
                    op("dve", "tensor_copy", [PS[0]], [xt_], out=xt_[:, r, :, :], in_=psbf(0)[:, 0:1024].rearrange("p (c t) -> p c t", c=8))
            for r in range(2):
                hb = 1 + 2 * pq + r
                for c in range(8):
                    op("pe", "matmul", [xt_, wb], [PS[hb]], PS[hb][:, :], lhsT=xt_[:, r, c, :], rhs=wb[:, c * 512:(c + 1) * 512], start=(c == 0), stop=(c == 7))
            for r in range(2):
                hb = 1 + 2 * pq + r
                sg_ = sg2[r]
                ab = actb2[pq][r]
                op("act", "activation", [PS[hb]], [sg_], out=sg_[:], in_=PS[hb][:, 0:256], func=AF.Tanh, scale=0.5)
                op("dve", "scalar_tensor_tensor", [sg_, PS[hb]], [sg_], out=sg_[:], in0=sg_[:], scalar=1.0, in1=PS[hb][:, 0:256], op0=ALU.add, op1=ALU.mult)
                op("dve", "scalar_tensor_tensor", [sg_, PS[hb]], [ab], out=ab[:], in0=sg_[:], scalar=0.5, in1=PS[hb][:, 256:512], op0=ALU.mult, op1=ALU.mult)

        def stage2(blk):
            pq = blk % 2
            wb = wblk[pq]
            for r in range(2):
                ab = actb2[pq][r]
                at = actT2[r]
                for fc in range(2):
                    op("pe", "transpose", [ab, ident_b], [PS[5]], out=psbf(5)[:, (2 * r + fc) * 128:(2 * r + fc + 1) * 128], in_=ab[:, fc * 128:(fc + 1) * 128], identity=ident_b[:])
                op("act", "copy", [PS[5]], [at], out=at[:], in_=psbf(5)[:, 2 * r * 128:(2 * r + 2) * 128].rearrange("p (c t) -> p c t", c=2))
            for r in range(2):
                at = actT2[r]
                yb = ysb[r]
                for nh in range(2):
                    for fc in range(2):
                        op("pe", "matmul", [at, wb], [PS[6 + nh]], PS[6 + nh][:, :], lhsT=at[:, fc, :],
                           rhs=wb[:, 4096 + fc * 1024 + nh * 512:4096 + fc * 1024 + (nh + 1) * 512], start=(fc == 0), stop=(fc == 1))
                    if nh == 0:
                        op("act", "copy", [PS[6]], [yb], out=yb[:, 0:512], in_=PS[6][:, :])
                    else:
                        op("dve", "tensor_copy", [PS[7]], [yb], out=yb[:, 512:1024], in_=PS[7][:, :])
                dma("sp", s_ys[r], [yb], [ys_k], out=ys_d[blk * BLK + r * 128:blk * BLK + (r + 1) * 128, :], in_=yb[:])

        stage1(0)
        for blk in range(NBLK):
            if blk + 1 < NBLK:
                stage1(blk + 1)
            stage2(blk)

        out_k = DR()
        fg_bc = Alias(P3b, 0, [128, D], F32)
        fence(xblk + [a_ for l_ in actb2 for a_ in l_] + sg2 + actT2, ysc + [g1bc, A2bc])
        dma("sp", s_bc, [], [fg_bc], out=fg_bc[:], in_=fg_d.partition_broadcast(128))
        s_yg2 = [SC.dma_sem("yg2_%d" % i) for i in range(2)]
        s_x1f = [SC.dma_sem("x1f%d" % i) for i in range(2)]
        h2alt = Alias(P2b, 0, [128, D], F32)
        x1alt = Alias(P5, 0, [128, D], F32)
        def F_pre(gt):
            yp = ysb if gt % 2 == 0 else ysc
            sy = s_yg if gt % 2 == 0 else s_yg2
            for k_ in range(2):
                SC.dma("pool", (lambda e, gt=gt, k_=k_, yy=yp[k_]: e.indirect_dma_start(
                    out=yy[:, :], out_offset=None, in_=ys_d, in_offset=bass.IndirectOffsetOnAxis(ap=DEST[:, gt, k_:k_ + 1], axis=0))),
                    sy[k_], [DEST.k, ys_k.k], [yp[k_].k], lat=7.0)
            xx = x1 if gt % 2 == 0 else x1alt
            dma("sp", s_x1f[gt % 2], [x1s_k], [xx], out=xx[:], in_=x1s_d[gt * 128:(gt + 1) * 128, :])

        def F_main(gt):
            b = gt // NT
            ti = gt % NT
            if ti == 0:
                dma("sp", s_bc, [mods_k], [g1bc], out=g1bc[:], in_=mods_d[b:b + 1, 5 * D:6 * D].partition_broadcast(128))
            yp = ysb if gt % 2 == 0 else ysc
            y1, y2 = yp[0], yp[1]
            hh = h2 if gt % 2 == 0 else h2alt
            xx = x1 if gt % 2 == 0 else x1alt
            sc_ = (gt % 2) * 4
            op("act", "activation", [y1, GATE], [hh], out=hh[:], in_=y1[:], func=AF.Identity, scale=GATE[:, gt, 0:1])
            op("dve", "scalar_tensor_tensor", [y2, GATE, hh], [hh], out=hh[:], in0=y2[:], scalar=GATE[:, gt, 1:2], in1=hh[:], op0=ALU.mult, op1=ALU.add)
            op("dve", "tensor_tensor", [hh, g1bc], [hh], out=hh[:], in0=hh[:], in1=g1bc[:], op=ALU.mult)
            op("pool", "tensor_tensor", [hh, xx], [hh], out=hh[:], in0=hh[:], in1=xx[:], op=ALU.add)
            op("act", "activation", [hh], [junk, stat], out=junk[:], in_=hh[:], func=AF.Square, accum_out=stat[:, sc_:sc_ + 1])
            op("dve", "tensor_scalar", [stat], [stat], out=stat[:, sc_ + 1:sc_ + 2], in0=stat[:, sc_:sc_ + 1], scalar1=1.0 / D, scalar2=EPS, op0=ALU.mult, op1=ALU.add)
            rsqrt(stat, stat[:, sc_ + 1:sc_ + 2], stat, stat[:, sc_ + 2:sc_ + 3], 1)
            xo = xin[gt % 2]
            op("dve", "scalar_tensor_tensor", [hh, stat, fg_bc], [xo], out=xo[:], in0=hh[:], scalar=stat[:, sc_ + 2:sc_ + 3], in1=fg_bc[:], op0=ALU.mult, op1=ALU.mult)
            dma("sp", s_xin[gt % 2], [xo], [out_k], out=out_d[b, ti * 128:(ti + 1) * 128, :], in_=xo[:])

        F_pre(0)
        for gt in range(NTT):
            if gt + 1 < NTT:
                F_pre(gt + 1)
            F_main(gt)
        SC.wait_all("sp", [out_k.k])
        SC.emit()
    return nc


_CACHE = {}


def kernel(**inputs):
    NCORES = 8
    x = np.asarray(inputs["x"], dtype=np.float32)
    B, S, _ = x.shape
    NSEQ = B // NCORES
    key = (S, NSEQ)
    if key not in _CACHE:
        _CACHE[key] = build_nc(S, NSEQ)
    nc = _CACHE[key]
    T = S * NSEQ
    NBLK = (2 * T + NE * (BLK - 1) + BLK - 1) // BLK
    cst_np, _ = make_consts(NBLK)
    f = lambda k: np.ascontiguousarray(np.asarray(inputs[k], dtype=np.float32))
    shared = {
        "w_ada": f("w_ada")[0], "b_ada": f("b_ada"), "norm1_g": f("norm1_g"), "w_in": f("w_in")[0],
        "q_norm_g": f("q_norm_g"), "w_uq": f("w_uq")[0], "kv_norm_g": f("kv_norm_g"), "w_ukv": f("w_ukv")[0],
        "w_o": f("w_o")[0], "norm2_g": f("norm2_g"), "w_gr": f("w_gr")[0], "b_gr": f("b_gr"),
        "w_er": f("w_er")[0].reshape(D, 32), "b_er": f("b_er").reshape(1, 32), "w1": f("w1")[0], "w3": f("w3")[0],
        "w2": f("w2")[0], "final_g": f("final_g").reshape(1, D), "cst": cst_np,
    }
    c = f("c")
    pos = np.ascontiguousarray(np.asarray(inputs["positions"], dtype=np.int32))
    in_maps = []
    for i in range(NCORES):
        m = dict(shared)
        m["x"] = np.ascontiguousarray(x[i * NSEQ:(i + 1) * NSEQ])
        m["c"] = np.ascontiguousarray(c[i * NSEQ:(i + 1) * NSEQ])
        m["positions"] = np.ascontiguousarray(pos[i * NSEQ:(i + 1) * NSEQ])
        in_maps.append(m)
    res = run_bass_kernel_spmd(nc, in_maps, core_ids=list(range(NCORES)))
    return np.concatenate([np.asarray(r["out"]) for r in res.results], axis=0).astype(np.float32)
```

```python
import math
import numpy as np
from contextlib import ExitStack
import concourse.bass as bass
import concourse.mybir as mybir
from concourse.bass_utils import run_bass_kernel_spmd

F32 = mybir.dt.float32
BF16 = mybir.dt.bfloat16
I32 = mybir.dt.int32
ALU = mybir.AluOpType
AF = mybir.ActivationFunctionType
AX = mybir.AxisListType

ENGS = ("pe", "act", "dve", "pool", "sp")
D = 1024
NE = 32
BLK = 256
EPS = 1e-6
MAGIC_RN = 12582912.0
TWO_PI = float(2 * np.pi)


class Tk:
    __slots__ = ("w", "r", "acc", "wd")

    def __init__(self, acc=False):
        self.w = None
        self.r = []
        self.acc = acc
        self.wd = {}


class Sched:
    def __init__(self, nc, stack):
        self.nc = nc
        self.stack = stack
        self.cnt = {}
        self.sems = {}
        for e in ENGS:
            self._mksem("E_" + e)
        self.nd = 0
        self.all = []
        self.tk_sems = {}
        self.tok2op = {}

    def _mksem(self, key):
        self.sems[key] = self.stack.enter_context(self.nc.semaphore(key))
        self.cnt[key] = 0
        return key

    def dma_sem(self, name=""):
        self.nd += 1
        return self._mksem("D%d_%s" % (self.nd, name))

    def _deps(self, reads, writes):
        deps = set()

        def add(tok):
            if tok is None:
                return
            k, v = tok
            if k[0] == "D":
                v = self.cnt[k]
            deps.add((k, v))
        for t in reads:
            add(t.w)
            if t.acc:
                for kv in t.wd.items():
                    add(kv)
        for t in writes:
            if t.acc:
                continue
            add(t.w)
            for tok in t.r:
                add(tok)
        return deps

    def _commit(self, tok, reads, writes):
        for t in reads:
            if not t.acc:
                t.r.append(tok)
        for t in writes:
            if t.acc:
                if t.wd.get(tok[0], 0) < tok[1]:
                    t.wd[tok[0]] = tok[1]
            else:
                t.w = tok
                t.r = []

    def op(self, eng, fn, reads=(), writes=(), cost=0.5):
        deps = self._deps(reads, writes)
        key = "E_" + eng
        self.cnt[key] += 1
        tok = (key, self.cnt[key])
        self.tok2op[tok] = len(self.all)
        self.all.append(dict(eng=eng, fn=fn, dma=False, tok=tok, deps=deps, cost=cost, lat=0.0))
        self._commit(tok, reads, writes)

    def dma(self, eng, fn, sem, reads=(), writes=(), lat=4.0):
        anchor = None
        for t in list(writes) + list(reads):
            if not t.acc:
                anchor = t
                break
        if anchor is not None:
            key = "DT%d_%s" % (id(anchor), eng)
            if key not in self.tk_sems:
                self.tk_sems[key] = self.dma_sem("t")
            sem = self.tk_sems[key]
        elif eng == "pool":
            if sem + "_p" not in self.sems:
                self._mksem(sem + "_p")
            sem = sem + "_p"
        deps = self._deps(reads, writes)
        self.cnt[sem] += 16
        tok = (sem, self.cnt[sem])
        self.tok2op[tok] = len(self.all)
        self.all.append(dict(eng=eng, fn=fn, dma=True, tok=tok, deps=deps, cost=(1.2 if eng == "pool" else 0.12), lat=lat))
        self._commit(tok, reads, writes)

    def wait_all(self, eng, tks):
        deps = self._deps(tks, ())
        self.all.append(dict(eng=eng, fn=None, dma=False, tok=None, deps=deps, cost=0.0, lat=0.0))

    def _schedule(self, W=320):
        ops = self.all
        n = len(ops)
        prod = [None] * n
        dependents = [[] for _ in range(n)]
        ndeps = [0] * n
        for i, o in enumerate(ops):
            ps = set()
            for tok in o["deps"]:
                j = self.tok2op.get(tok)
                if j is not None:
                    ps.add(j)
            prod[i] = ps
            ndeps[i] = len(ps)
            for j in ps:
                dependents[j].append(i)
        pending = {e: [] for e in ENGS}
        for i, o in enumerate(ops):
            pending[o["eng"]].append(i)
        head = {e: 0 for e in ENGS}
        done = [False] * n
        comp = [0.0] * n
        ready = [0.0] * n
        free = {e: 0.0 for e in ENGS}
        semmax = {}
        order = {e: [] for e in ENGS}
        left = n
        while left:
            best = None
            for e in ENGS:
                lst = pending[e]
                h = head[e]
                while h < len(lst) and done[lst[h]]:
                    h += 1
                head[e] = h
                if h >= len(lst):
                    continue
                seen_sems = set()
                cnt = 0
                k = h
                fe = free[e]
                while k < len(lst) and cnt < W:
                    i = lst[k]
                    k += 1
                    if done[i]:
                        continue
                    cnt += 1
                    o = ops[i]
                    if o["fn"] is None:
                        if cnt > 1:
                            continue
                    elif o["dma"]:
                        sk_ = o["tok"][0]
                        if sk_ in seen_sems:
                            continue
                        seen_sems.add(sk_)
                    if ndeps[i]:
                        continue
                    st = ready[i] if ready[i] > fe else fe
                    if best is None or st < best[0] or (st == best[0] and i < best[1]):
                        best = (st, i, e)
                    if st <= fe:
                        break
            st, i, e = best
            o = ops[i]
            done[i] = True
            left -= 1
            order[e].append(i)
            fin = st + o["cost"]
            free[e] = fin
            c = fin + o["lat"]
            if o["dma"]:
                sk = o["tok"][0]
                if semmax.get(sk, 0.0) > c:
                    c = semmax[sk]
                semmax[sk] = c
            comp[i] = c
            for d in dependents[i]:
                ndeps[d] -= 1
                if comp[i] > ready[d]:
                    ready[d] = comp[i]
        self.sim_time = max(free.values())
        return order

    def emit(self):
        import os
        ops = self.all
        if os.environ.get("K_REORDER", "1") == "1":
            order = self._schedule()
        else:
            order = {e: [] for e in ENGS}
            for i, o in enumerate(ops):
                order[o["eng"]].append(i)
        newtok = {}
        for e in ENGS:
            c = 0
            for i in order[e]:
                o = ops[i]
                if o["fn"] is not None and not o["dma"]:
                    c += 1
                    newtok[o["tok"]] = ("E_" + e, c)
        plan = {e: [] for e in ENGS}
        needed = {}
        for e in ENGS:
            wd = {}
            for i in order[e]:
                o = ops[i]
                mx = {}
                for tok in o["deps"]:
                    k, v = newtok.get(tok, tok)
                    if mx.get(k, 0) < v:
                        mx[k] = v
                waits = []
                for k, v in mx.items():
                    if wd.get(k, 0) < v:
                        wd[k] = v
                        waits.append((k, v))
                        if k[0] == "E":
                            needed.setdefault(k, set()).add(v)
                plan[e].append((waits, o))
        rank = {k: {v: r + 1 for r, v in enumerate(sorted(vs))} for k, vs in needed.items()}
        sems = self.sems

        def run(name, eng):
            for waits, o in plan[name]:
                for k, v in waits:
                    eng.wait_ge(sems[k], rank[k][v] if k[0] == "E" else v)
                if o["fn"] is not None:
                    ins = o["fn"](eng)
                    if o["dma"]:
                        ins.then_inc(sems[o["tok"][0]], 16)
                    else:
                        nt = newtok[o["tok"]]
                        if nt[1] in needed.get(nt[0], ()):
                            ins.then_inc(sems[nt[0]], 1)
        with self.nc.Block() as block:
            @block.tensor
            def _(e):
                run("pe", e)

            @block.scalar
            def _(e):
                run("act", e)

            @block.vector
            def _(e):
                run("dve", e)

            @block.gpsimd
            def _(e):
                run("pool", e)

            @block.sync
            def _(e):
                run("sp", e)


def make_consts(nblk):
    H = 4
    gam = 1.0 - 2.0 ** (-5.0 - np.arange(H))
    lg = np.log(gam)
    p = np.arange(128)
    cols = {}
    cols["ident"] = np.eye(128, dtype=np.float64)
    cols["triu"] = (p[:, None] < p[None, :]).astype(np.float64)
    cm = np.zeros((128, 4, 512))
    q = np.arange(512)
    for m in range(4):
        cm[:, m, :] = ((128 * m + p)[:, None] <= q[None, :])
    cmask_np = cm.reshape(128, -1)
    dm = np.zeros((128, 4, 128))
    for h in range(4):
        d = p[None, :] - p[:, None]
        dm[:, h, :] = np.where(d >= 0, np.exp(np.maximum(d, 0) * lg[h]), 0.0)
    cols["dmaskT"] = dm.reshape(128, -1)
    xi = np.zeros((128, 2, 128))
    for j in range(2):
        for half in range(2):
            h = 2 * j + half
            xi[half * 64:(half + 1) * 64, j, :] = np.exp((p + 1.0) * lg[h])[None, :]
    cols["xi"] = xi.reshape(128, -1)
    zt = np.zeros((128, 4, 64))
    for h in range(4):
        zt[:, h, :] = np.exp((127.0 - p) * lg[h])[:, None]
    cols["zeta"] = zt.reshape(128, -1)
    rp = np.zeros((128, 6))
    jr = p % 32
    rp[:, 0] = 10000.0 ** (-(jr / 32.0))
    blk64 = (p % 64) // 32
    rp[:, 1] = np.where(blk64 == 0, np.pi / 2, 0.0)
    rp[:, 2] = np.where(blk64 == 0, np.pi, np.pi / 2)
    jm = (p - 64) % 16
    rp[:, 3] = 10000.0 ** (-(jm / 16.0))
    b16 = ((p - 64) // 16) % 2
    rp[:, 4] = np.where(b16 == 0, np.pi / 2, 0.0)
    rp[:, 5] = np.where(b16 == 0, np.pi, np.pi / 2)
    cols["rp"] = rp
    cols["blkstart"] = np.broadcast_to((np.arange(nblk) * float(BLK))[None, :], (128, nblk))
    cols["iotap"] = p[:, None].astype(np.float64)
    cols["cd"] = np.broadcast_to(np.exp(128.0 * lg)[None, :], (128, 4))
    cols["cmask"] = cmask_np
    off = {}
    o = 0
    arrs = []
    for k, v in cols.items():
        off[k] = (o, o + v.shape[1])
        o += v.shape[1]
        arrs.append(v)
    return np.concatenate(arrs, axis=1).astype(np.float32), off


def build_nc(S, NSEQ, dbg=None):
    T = S * NSEQ
    NT = S // 128
    NTT = T // 128
    NG = S // 512
    NBLK = (2 * T + NE * (BLK - 1) + BLK - 1) // BLK
    PT = NBLK * BLK
    cst_np, coff = make_consts(NBLK)
    NC = cst_np.shape[1]
    nc = bass.Bass("TRN2", target_bir_lowering=False)

    def din(name, shape, dt=F32):
        return nc.dram_tensor(name, list(shape), dt, kind="ExternalInput").ap()

    def dscr(name, shape, dt):
        return nc.dram_tensor(name, list(shape), dt, kind="Internal").ap()

    x_d = din("x", [NSEQ, S, D])
    c_d = din("c", [NSEQ, D])
    pos_d = din("positions", [NSEQ, S], I32)
    wada_d = din("w_ada", [D, 6 * D])
    bada_d = din("b_ada", [1, 6 * D])
    n1g_d = din("norm1_g", [1, D])
    win_d = din("w_in", [D, 1952])
    qng_d = din("q_norm_g", [1, 256])
    wuq_d = din("w_uq", [256, 768])
    kvng_d = din("kv_norm_g", [1, 128])
    wukv_d = din("w_ukv", [128, 1024])
    wo_d = din("w_o", [D, D])
    n2g_d = din("norm2_g", [1, D])
    wgr_d = din("w_gr", [D, 4])
    bgr_d = din("b_gr", [1, 4])
    wer_d = din("w_er", [D, 32])
    ber_d = din("b_er", [1, 32])
    w1_d = din("w1", [NE, D, 256])
    w3_d = din("w3", [NE, D, 256])
    w2_d = din("w2", [NE, 256, D])
    fg_d = din("final_g", [1, D])
    cst_d = din("cst", [128, NC])
    out_d = nc.dram_tensor("out", [NSEQ, S, D], F32, kind="ExternalOutput").ap()

    mods_d = dscr("mods", [NSEQ, 6 * D], F32)
    mixm_d = dscr("mixm", [NSEQ, 8, 64, S], BF16)
    mixr_d = dscr("mixr", [NSEQ, 4, 128, S], BF16)
    x1s_d = dscr("x1s", [T, D], F32)
    h2s_d = dscr("h2s", [T, D], BF16)
    xs_d = dscr("xs", [PT, D], BF16)
    ys_d = dscr("ys", [PT, D], BF16)
    wall_d = dscr("wall", [NE * 128, 6144], BF16)
    csms_d = dscr("csms", [NSEQ, 2, 32, S], F32)
    dbg_out = {}
    if dbg:
        for name, shape in dbg.items():
            dbg_out[name] = nc.dram_tensor("dbg_" + name, list(shape), F32, kind="ExternalOutput").ap()

    st = ExitStack()
    with st:
        SC = Sched(nc, st)

        class Buf:
            def __init__(self, name, shape, dt, psum=False):
                if psum:
                    self.t = st.enter_context(nc.psum_tensor(name, list(shape), dt))
                else:
                    self.t = st.enter_context(nc.sbuf_tensor("sb_" + name, list(shape), dt))
                self.k = Tk()

            def __getitem__(self, idx):
                return self.t[idx]

        def sb(name, shape, dt=F32):
            return Buf(name, shape, dt)

        class Alias:
            def __init__(self, parent, off, shape, dt, own=False):
                n = 1
                for d_ in shape[1:]:
                    n *= d_
                nb = n * (4 if dt in (F32, I32) else 2)
                a = parent.t[0:shape[0], off // 4:(off + nb) // 4]
                v = a if dt == F32 else a.bitcast(dt)
                if len(shape) == 3:
                    v = v.rearrange("p (a b) -> p a b", a=shape[1])
                elif len(shape) == 4:
                    v = v.rearrange("p (a b c) -> p a b c", a=shape[1], b=shape[2])
                self.v = v
                self.k = Tk() if own else parent.k

            def __getitem__(self, idx):
                return self.v[idx]

        def _fsz(ap):
            try:
                return float(ap.free_size())
            except Exception:
                return 256.0

        def op(eng, method, reads, writes, *a, **kw):
            if eng == "pe":
                if method == "matmul":
                    cost = 0.31 + _fsz(kw["rhs"]) / 1200.0
                else:
                    cost = 0.42
            else:
                o_ = kw.get("out") if kw.get("out") is not None else (a[0] if a else None)
                f_ = _fsz(o_) if o_ is not None else 64.0
                if eng == "dve":
                    cost = 0.12 + f_ / 1100.0
                elif eng == "act":
                    cost = 0.28 + f_ / 1200.0
                else:
                    cost = 0.6 + f_ / 600.0
            SC.op(eng, lambda e: getattr(e, method)(*a, **kw), [b.k for b in reads], [b.k for b in writes], cost=cost)
            if kw.get("accum_out") is not None:
                SC.op(eng, lambda e: e.copy(out=adum[0:1, 0:2], in_=adum[0:1, 2:4]), [], [b.k for b in writes] + [adum.k], cost=0.2)

        def dma(eng, sem, reads, writes, **kw):
            try:
                nb = float(kw["out"].nbytes())
            except Exception:
                nb = 65536.0
            SC.dma(eng, lambda e: e.dma_start(**kw), sem, [b.k for b in reads], [b.k for b in writes], lat=2.5 + nb / 150e3)

        class DR:
            def __init__(self):
                self.k = Tk(acc=True)

        PS = [Buf("ps%d" % i, [128, 512], F32, psum=True) for i in range(8)]
        adum = sb("adum", [128, 4])
        SC.op("dve", lambda e: e.memset(adum[:], 0.0), [], [adum.k])

        def psbf(i):
            return PS[i].t[:].bitcast(BF16)

        NC0 = coff["cmask"][0]
        cst = sb("cst", [128, NC0])
        s_c = SC.dma_sem("cst")
        dma("sp", s_c, [], [cst], out=cst[:], in_=cst_d[:, 0:NC0])

        def cc(name):
            a, b = coff[name]
            return cst[:, a:b]
        ident_f = cc("ident")
        ident_b = sb("ident_b", [128, 128], BF16)
        triu_b = sb("triu_b", [128, 128], BF16)
        ones_b = sb("ones_b", [128, 128], BF16)
        ones_f = sb("ones_f", [128, 128], F32)
        cmask_b = sb("cmask_b", [128, 4, 512], BF16)
        op("dve", "tensor_copy", [cst], [ident_b], out=ident_b[:], in_=ident_f)
        op("dve", "tensor_copy", [cst], [triu_b], out=triu_b[:], in_=cc("triu"))
        op("dve", "memset", [], [ones_b], ones_b[:], 1.0)
        op("dve", "memset", [], [ones_f], ones_f[:], 1.0)
        dma("pool", s_c, [], [cmask_b], out=cmask_b[:].rearrange("p a b -> p (a b)"), in_=cst_d[:, NC0:NC0 + 2048])
        dmaskT = cc("dmaskT").rearrange("p (h q) -> p h q", h=4)
        xi_c = cc("xi").rearrange("p (j q) -> p j q", j=2)
        zeta_c = cc("zeta")
        rp = cc("rp")
        cd_c = cc("cd")

        nhalf = sb("nhalf", [128, 16])
        op("pool", "memset", [], [nhalf], nhalf[:], -0.5)
        fdum = sb("fdum", [128, 2])

        rs_i = nhalf
        rs_t = nhalf
        import os as _os
        USE_POW = _os.environ.get("K_POW", "1") == "1"

        def rsqrt(vbuf, vap, outbuf, outap, n):
            if USE_POW:
                op("pool", "tensor_tensor", [vbuf, nhalf], [outbuf], out=outap, in0=vap, in1=nhalf[:, 0:n], op=ALU.pow)
                return
            yi = rs_i[:, 0:n]
            y = yi.bitcast(F32)
            tt = rs_t[:, 0:n]
            op("dve", "tensor_single_scalar", [vbuf], [rs_i], out=yi, in_=vap.bitcast(I32), scalar=1, op=ALU.arith_shift_right)
            op("dve", "tensor_scalar", [rs_i], [rs_i], out=yi, in0=yi, scalar1=-1.0, scalar2=float(0x5f3759df), op0=ALU.mult, op1=ALU.add)
            for it in range(3):
                op("dve", "tensor_tensor", [rs_i], [rs_t], out=tt, in0=y, in1=y, op=ALU.mult)
                op("dve", "tensor_tensor", [rs_t, vbuf], [rs_t], out=tt, in0=tt, in1=vap, op=ALU.mult)
                op("dve", "tensor_scalar", [rs_t], [rs_t], out=tt, in0=tt, scalar1=-0.5, scalar2=1.5, op0=ALU.mult, op1=ALU.add)
                if it < 2:
                    op("dve", "tensor_tensor", [rs_t, rs_i], [rs_i], out=y, in0=y, in1=tt, op=ALU.mult)
                else:
                    op("dve", "tensor_tensor", [rs_t, rs_i], [outbuf], out=outap, in0=y, in1=tt, op=ALU.mult)

        def fence(frm, to):
            SC.op("pool", lambda e: e.memset(fdum[0:1, 0:1], 0.0), [], [b_.k for b_ in frm] + [b_.k for b_ in to] + [fdum.k])

        s_m = SC.dma_sem("mods")
        mods_k = DR()
        cT = sb("cT", [128, 8, NSEQ])
        cTe = sb("cTe", [128, 8, NSEQ])
        siluT = sb("siluT", [128, 8, NSEQ], BF16)
        for b0 in range(NSEQ):
            dma("sp", s_m, [], [cT], out=cT[:, :, b0], in_=c_d[b0:b0 + 1, :].rearrange("o (c p) -> p (o c)", p=128), allow_slow_non_contiguous=True)
        op("act", "activation", [cT], [cTe], out=cTe[:], in_=cT[:], func=AF.Exp, scale=-1.0)
        op("dve", "tensor_scalar", [cTe], [cTe], out=cTe[:], in0=cTe[:], scalar1=1.0, scalar2=None, op0=ALU.add)
        op("dve", "reciprocal", [cTe], [cTe], out=cTe[:], in_=cTe[:])
        op("dve", "tensor_tensor", [cTe, cT], [siluT], out=siluT[:], in0=cTe[:], in1=cT[:], op=ALU.mult)
        BIGW = sb("BIGW", [128, 12448])
        P0 = sb("P0", [128, 2048])
        wa = [Alias(BIGW, 0, [128, 8, 512], BF16, own=True), Alias(BIGW, 8192, [128, 8, 512], BF16, own=True)]
        s_wa = [SC.dma_sem("wa%d" % i) for i in range(2)]
        ba = sb("ba", [NSEQ, 512])
        mrow = sb("mrow", [NSEQ, 512])
        s_ba = SC.dma_sem("ba")
        for j in range(12):
            w = wa[j % 2]
            dma("pool", s_wa[j % 2], [], [w], out=w[:], in_=wada_d[:, j * 512:(j + 1) * 512].rearrange("(c p) n -> p c n", p=128))
            dma("sp", s_ba, [], [ba], out=ba[:], in_=bada_d[:, j * 512:(j + 1) * 512].partition_broadcast(NSEQ))
            for k in range(8):
                op("pe", "matmul", [siluT, w], [PS[0]], PS[0][0:NSEQ, :], lhsT=siluT[:, k, :], rhs=w[:, k, :], start=(k == 0), stop=(k == 7))
            op("dve", "tensor_tensor", [PS[0], ba], [mrow], out=mrow[:], in0=PS[0][0:NSEQ, :], in1=ba[:], op=ALU.add)
            dma("sp", s_m, [mrow], [mods_k], out=mods_d[:, j * 512:(j + 1) * 512], in_=mrow[:])

        wblk = [Alias(BIGW, 0, [128, 6144], BF16, own=True), Alias(BIGW, 12288, [128, 6144], BF16, own=True)]
        s_wl = SC.dma_sem("wl")
        s_ws = SC.dma_sem("ws")
        wall_k = DR()

        def relayout_expert(e):
            stg = wblk[e % 2]
            v13 = stg[:, 0:4096].rearrange("p (c f) -> p c f", c=8)
            dma("pool", s_wl, [], [stg], out=v13[:, :, 0:256], in_=w1_d[e].rearrange("(c p) f -> p c f", p=128))
            dma("pool", s_wl, [], [stg], out=v13[:, :, 256:512], in_=w3_d[e].rearrange("(c p) f -> p c f", p=128))
            dma("pool", s_wl, [], [stg], out=stg[:, 4096:6144].rearrange("p (c f) -> p c f", c=2),
                in_=w2_d[e].rearrange("(c p) f -> p c f", p=128))
            dma("pool", s_ws, [stg], [wall_k], out=wall_d[e * 128:(e + 1) * 128, :], in_=stg[:])
        n_slots = NSEQ * 8 * NG
        per_slot = (NE + n_slots - 1) // n_slots
        relay_state = [0]

        fence(wa, [BIGW])
        s_w = SC.dma_sem("w")
        NFM = 8 * 128 + 2 * 96
        w_fm = Alias(BIGW, 0, [128, 8, NFM], BF16)
        w_tm = Alias(BIGW, 19456, [128, 8, 1408], BF16)
        wst = [Alias(BIGW, 41984, [128, 1952], F32)] * 2
        s_wst = [SC.dma_sem("wst%d" % i) for i in range(2)]
        wuq_a = sb("wuq_a", [128, 2, 8, 192], BF16)
        STG = P0
        wuq_s = Alias(STG, 0, [128, 2, 768], F32)
        qng_c = sb("qng_c", [128, 2])
        dma("sp", s_w, [], [wuq_s], out=wuq_s[:], in_=wuq_d.rearrange("(c p) n -> p c n", p=128))
        dma("sp", s_w, [], [qng_c], out=qng_c[:], in_=qng_d.rearrange("o (c p) -> p (o c)", p=128), allow_slow_non_contiguous=True)
        op("pool", "memset", [], [wuq_a], wuq_a[:], 0.0)
        for c in range(2):
            s4 = wuq_s[:, c, :].rearrange("p (h f) -> p h f", h=8)
            op("dve", "tensor_scalar", [wuq_s, qng_c], [wuq_a], out=wuq_a[:, c, :, 0:64], in0=s4[:, :, 0:64],
               scalar1=qng_c[:, c:c + 1], scalar2=None, op0=ALU.mult)
            for ab in range(2):
                dst = wuq_a[:, c, :, ab * 96 + 64:ab * 96 + 96].rearrange("p h (dup j) -> p h dup j", dup=2)
                src = s4[:, :, 64 + ab * 16:64 + ab * 16 + 16].unsqueeze(2).to_broadcast([128, 8, 2, 16])
                op("dve", "tensor_scalar", [wuq_s, qng_c], [wuq_a], out=dst, in0=src,
                   scalar1=qng_c[:, c:c + 1], scalar2=None, op0=ALU.mult)
        wukv_s = Alias(STG, 0, [128, 1024], F32)
        kvng_c = sb("kvng_c", [128, 1])
        wk_b = sb("wk_b", [128, 8, 64], BF16)
        wv_b = sb("wv_b", [128, 8, 64], BF16)
        dma("sp", s_w, [], [wukv_s], out=wukv_s[:], in_=wukv_d)
        dma("sp", s_w, [], [kvng_c], out=kvng_c[:], in_=kvng_d.rearrange("o p -> p o"), allow_slow_non_contiguous=True)
        s3 = wukv_s[:].rearrange("p (h f) -> p h f", h=8)
        op("dve", "tensor_scalar", [wukv_s, kvng_c], [wk_b], out=wk_b[:], in0=s3[:, :, 0:64], scalar1=kvng_c[:, 0:1], scalar2=None, op0=ALU.mult)
        op("dve", "tensor_scalar", [wukv_s, kvng_c], [wv_b], out=wv_b[:], in0=s3[:, :, 64:128], scalar1=kvng_c[:, 0:1], scalar2=None, op0=ALU.mult)
        wo_m = Alias(BIGW, 0, [64, 8, D], BF16)
        wo_r = Alias(BIGW, 16384, [128, 4, D], BF16)
        w_rt = sb("w_rt", [128, 8, 36])
        b_rt = sb("b_rt", [128, 36])
        dma("sp", s_w, [], [w_rt], out=w_rt[:, :, 0:4], in_=wgr_d.rearrange("(c p) n -> p c n", p=128), allow_slow_non_contiguous=True)
        dma("sp", s_w, [], [w_rt], out=w_rt[:, :, 4:36], in_=wer_d.rearrange("(c p) n -> p c n", p=128), allow_slow_non_contiguous=True)
        w_rtb = sb("w_rtb", [128, 8, 36], BF16)
        op("dve", "tensor_copy", [w_rt], [w_rtb], out=w_rtb[:], in_=w_rt[:])
        dma("sp", s_w, [], [b_rt], out=b_rt[:, 0:4], in_=bgr_d.partition_broadcast(128))
        dma("sp", s_w, [], [b_rt], out=b_rt[:, 4:36], in_=ber_d.partition_broadcast(128))
        n1g_c = sb("n1g_c", [128, 8])
        dma("sp", s_w, [], [n1g_c], out=n1g_c[:], in_=n1g_d.rearrange("o (c p) -> p (o c)", p=128), allow_slow_non_contiguous=True)

        OH1 = sb("OH1", [128, NTT, 32], BF16)
        OH2 = sb("OH2", [128, NTT, 32], BF16)
        CUM = sb("CUM", [128, NTT, 32])
        GATE = sb("GATE", [128, NTT, 2])
        Macc = sb("Macc", [128, 32], BF16)
        op("pool", "memset", [], [Macc], Macc[:], 0.0)

        cqnT = sb("cqnT", [128, 2, S], BF16)
        ckvnT = sb("ckvnT", [128, S], BF16)
        kT = sb("kT", [96, S], BF16)
        s_csm = SC.dma_sem("csm")
        csms_k = DR()
        xin = [sb("xin%d" % i, [128, D]) for i in range(2)]
        s_xin = [SC.dma_sem("xin%d" % i) for i in range(2)]
        junk = sb("junk", [128, D], BF16)
        stat = sb("stat", [128, 16])
        xsb = sb("xsb", [128, D], BF16)
        xsb2 = [xsb, sb("xsb1", [128, D], BF16)]
        P1 = sb("P1", [128, 2048])
        h1T = Alias(P1, 0, [128, 8, 512], BF16)
        P2b = sb("P2b", [128, 1024])
        posi = Alias(P2b, 0, [128, 512], I32)
        posf = Alias(P2b, 2048, [128, 512], F32)
        s_pos = SC.dma_sem("pos")
        P2a = sb("P2a", [128, 1024])
        targ = Alias(P2a, 0, [128, 512], F32)
        ttmp = Alias(P2a, 2048, [128, 512], F32)
        P3a = sb("P3a", [128, 1024])
        csr1 = Alias(P3a, 0, [128, 512], F32)
        csr2 = Alias(P3a, 2048, [128, 512], F32)
        P3b = sb("P3b", [128, 1024])
        csm1f = Alias(P3b, 0, [96, 512], F32)
        csm2f = Alias(P3b, 2048, [96, 512], F32)
        colv = sb("colv", [128, 8, 4])
        s_col = SC.dma_sem("col")
        P6 = sb("P6", [128, 1024])
        P7 = sb("P7", [128, 512])
        rqT = Alias(P6, 0, [128, 2, 512], BF16)
        rqxT = Alias(P6, 2048, [128, 2, 512], BF16)
        rkT = Alias(P7, 0, [128, 2, 512], BF16)
        P4a = sb("P4a", [128, 1024])
        rt1 = Alias(P4a, 0, [128, 512], F32)
        rt2 = Alias(P4a, 2048, [128, 512], F32)
        cqn = sb("cqn", [128, 384], BF16)
        P4b = sb("P4b", [128, 1024])
        P4c = sb("P4c", [128, 1024])
        RVG = [Alias(P4b, 0, [128, 4, 512], BF16), Alias(P4c, 0, [128, 4, 512], BF16)]
        GTG = [Alias(P0, 0, [128, 4, 512], BF16), Alias(P0, 4096, [128, 4, 512], BF16)]
        GT4 = GTG[0]
        gsg = sb("gsg", [128, 512])
        rkz = sb("rkz", [128, 256], BF16)
        sdT = sb("sdT", [128, 4, 128], BF16)
        state = sb("state", [128, 2, 128])
        state_b = sb("state_b", [128, 2, 128], BF16)
        P8 = sb("P8", [128, 1024])
        osb = Alias(P8, 0, [128, 4, 128], F32)
        osq = Alias(P8, 2048, [128, 4, 128], F32)
        gst = sb("gst", [128, 16])
        oretb = sb("oretb", [128, 512], BF16)
        P5 = sb("P5", [128, 1024])
        oretT = Alias(P5, 0, [128, 4, 512], BF16)
        s_mixr = SC.dma_sem("mixr")
        mixr_k = DR()
        mixm_k = DR()

        def rope_table(dst, dstap, prow, invf_col, ph_col):
            a = targ[prow, :]
            b = ttmp[prow, :]
            op("dve", "tensor_scalar", [posf, cst], [targ], out=a, in0=posf[prow, :], scalar1=rp[prow, invf_col:invf_col + 1],
               scalar2=rp[prow, ph_col:ph_col + 1], op0=ALU.mult, op1=ALU.add)
            op("dve", "tensor_scalar", [targ], [ttmp], out=b, in0=a, scalar1=1.0 / TWO_PI, scalar2=MAGIC_RN, op0=ALU.mult, op1=ALU.add)
            op("dve", "tensor_scalar", [ttmp], [ttmp], out=b, in0=b, scalar1=MAGIC_RN, scalar2=-TWO_PI, op0=ALU.subtract, op1=ALU.mult)
            op("dve", "tensor_tensor", [ttmp, targ], [targ], out=a, in0=a, in1=b, op=ALU.add)
            op("dve", "tensor_scalar", [targ], [targ], out=a, in0=a, scalar1=-3.1415925, scalar2=3.1415925, op0=ALU.max, op1=ALU.min)
            op("act", "activation", [targ], [dst], out=dstap, in_=a, func=AF.Sin)


        vh = [sb("vh0", [128, NT, 65], BF16)] * 2
        for i in range(1):
            op("pool", "memset", [], [vh[i]], vh[i][:, :, 64:65], 1.0)
        pT = [Alias(P5, 0, [128, 512], BF16, own=True), Alias(P5, 1024, [128, 512], BF16, own=True)]
        qT = [Alias(P5, 2048, [96, 512], BF16, own=True), Alias(P5, 3072, [96, 512], BF16, own=True)]
        rrow = Alias(P8, 0, [65, 512], F32)
        bcs = Alias(P8, 2048, [64, 512], F32)
        oTm = Alias(P7, 0, [64, 512], BF16)
        s_mixm = SC.dma_sem("mixm")
        s_bc = SC.dma_sem("bc")
        s_mm = SC.dma_sem("mm")
        s_x1 = SC.dma_sem("x1")
        s_h2 = SC.dma_sem("h2")
        x1s_k = DR()
        h2s_k = DR()
        g1bc = Alias(P2a, 0, [128, D], F32)
        sh2bc = Alias(P2b, 0, [128, D], F32)
        A2bc = Alias(P3a, 0, [128, D], F32)
        n2gbc = Alias(P3b, 0, [128, D], F32)
        mm_t = Alias(P6, 0, [64, 8, 128], BF16)
        mr_t = Alias(P6, 2048, [128, 4, 128], BF16)
        x1 = Alias(P4a, 0, [128, D], F32)
        h2 = Alias(P4b, 0, [128, D], F32)
        h2b = Alias(P7, 0, [128, D], BF16)
        h2T = Alias(P5, 0, [128, 8, 128], F32)
        x1_2 = [x1, Alias(P0, 0, [128, D], F32, own=True)]
        h2_2 = [h2, Alias(P0, 4096, [128, D], F32, own=True)]
        h2b_2 = [h2b, Alias(P4c, 0, [128, D], BF16, own=True)]
        h2T_2 = [h2T, Alias(P1, 0, [128, 8, 128], F32, own=True)]
        mm_t_2 = [mm_t, Alias(P1, 4096, [64, 8, 128], BF16, own=True)]
        mr_t_2 = [mr_t, Alias(P1, 6144, [128, 4, 128], BF16, own=True)]
        c_alts = [x1_2[1], h2_2[1], h2b_2[1], h2T_2[1], mm_t_2[1], mr_t_2[1]]
        h2Tb_2 = [Alias(P5, 0, [128, 8, 128], BF16), Alias(P1, 0, [128, 8, 128], BF16)]
        h2Tb_2[1].k = h2T_2[1].k
        s_mm2 = [s_mm, SC.dma_sem("mm1")]
        lgt2 = [sb("lgt%d" % i, [128, 40]) for i in range(2)]
        for i in range(2):
            op("dve", "memset", [], [lgt2[i]], lgt2[i][:], -1e30)
        m8_2 = [sb("m8_%d" % i, [128, 16]) for i in range(2)]
        rst_2 = [sb("rst_%d" % i, [128, 20]) for i in range(2)]
        lem_2 = [sb("lem_%d" % i, [128, 32]) for i in range(2)]
        Mt_2 = [sb("Mt_%d" % i, [128, 32], BF16) for i in range(2)]
        ra = Alias(P2a, 0, [128, 32], F32)
        rb = Alias(P2a, 128, [128, 32], F32)
        pad_ = Alias(P2a, 256, [128, 32], F32)
        pst = Alias(P2a, 384, [128, 32], F32)
        cmp3 = Alias(BIGW, 0, [128, NBLK, 32], F32)
        ebf = sb("ebf", [128, NBLK])
        WIDX = sb("WIDX", [128, NBLK], I32)
        cmpd = Alias(BIGW, 16384, [128, NTT, 32], F32)
        destf = Alias(P2b, 0, [128, NTT, 2], F32)
        DEST = sb("DEST", [128, NTT, 2], I32)
        h2r = [Alias(P6, 0, [128, D], BF16, own=True), Alias(P6, 2048, [128, D], BF16, own=True)]
        s_h2r = [SC.dma_sem("h2r%d" % i) for i in range(2)]
        s_sc = SC.dma_sem("sc")
        s_wg = [SC.dma_sem("wg%d" % i) for i in range(2)]
        xblk = [Alias(P1, 0, [128, 2, D], BF16, own=True), Alias(P1, 4096, [128, 2, D], BF16, own=True)]
        s_xb = [SC.dma_sem("xb%d" % i) for i in range(2)]
        xTb = Alias(P8, 0, [128, 2, 8, 128], BF16)
        sg = Alias(P2a, 0, [128, 256], F32)
        actb = Alias(P2a, 1024, [128, 256], BF16)
        actT = Alias(P2a, 1536, [128, 2, 128], BF16)
        ysb = [Alias(P0, 0, [128, D], BF16, own=True), Alias(P0, 4096, [128, D], BF16, own=True)]
        ysc = [Alias(P1, 0, [128, D], BF16, own=True), Alias(P1, 4096, [128, D], BF16, own=True)]
        s_ys = [SC.dma_sem("ys%d" % i) for i in range(2)]
        s_yg = [SC.dma_sem("yg%d" % i) for i in range(2)]
        for b in range(NSEQ):
            op("pool", "memset", [], [w_fm], w_fm[:, :, 1024:NFM], 0.0)
            for k in range(8):
                ws = wst[k % 2]
                dma("sp", s_wst[k % 2], [], [ws], out=ws[:], in_=win_d[k * 128:(k + 1) * 128, :])
                op("act", "copy", [ws], [w_tm], out=w_tm[:, k, 0:384], in_=ws[:, 0:384])
                op("act", "copy", [ws], [w_tm], out=w_tm[:, k, 384:1408], in_=ws[:, 928:1952])
                for which, base, scale in ((0, 416, 1.0), (1, 672, 0.125)):
                    src = ws[:, base:base + 256].rearrange("p (h two j) -> p h two j", h=4, two=2)
                    for ab in range(2):
                        dst = w_fm[:, k, (which * 4 + ab * 2) * 128:(which * 4 + ab * 2 + 2) * 128].rearrange(
                            "p (h dup j) -> p h dup j", h=4, dup=2)
                        op("dve", "tensor_scalar", [ws], [w_fm], out=dst,
                           in0=src[:, :, ab:ab + 1, :].to_broadcast([128, 4, 2, 32]), scalar1=scale, scalar2=None, op0=ALU.mult)
                srck = ws[:, 384:416].rearrange("p (two j) -> p two j", two=2)
                for ab in range(2):
                    dst = w_fm[:, k, 1024 + ab * 96 + 64:1024 + ab * 96 + 96].rearrange("p (dup j) -> p dup j", dup=2)
                    op("dve", "tensor_copy", [ws], [w_fm], out=dst, in_=srck[:, ab:ab + 1, :].to_broadcast([128, 2, 16]))
            dma("sp", s_col, [mods_k], [colv], out=colv[:, :, 0], in_=mods_d[b:b + 1, 0:D].rearrange("o (c p) -> p (o c)", p=128),
                allow_slow_non_contiguous=True)
            dma("sp", s_col, [mods_k], [colv], out=colv[:, :, 1], in_=mods_d[b:b + 1, D:2 * D].rearrange("o (c p) -> p (o c)", p=128),
                allow_slow_non_contiguous=True)
            op("dve", "scalar_tensor_tensor", [colv, n1g_c], [colv], out=colv[:, :, 2], in0=colv[:, :, 1], scalar=1.0, in1=n1g_c[:],
               op0=ALU.add, op1=ALU.mult)
            op("dve", "memset", [], [state], state[:], 0.0)
            op("dve", "memset", [], [state_b], state_b[:], 0.0)

            def A_pos(g):
                t0 = g * 512
                dma("sp", s_pos, [], [posi], out=posi[:], in_=pos_d[b:b + 1, t0:t0 + 512].partition_broadcast(128))
                op("dve", "tensor_copy", [posi], [posf], out=posf[:], in_=posi[:])

            def A_table(g, k):
                t0 = g * 512
                if k == 0:
                    rope_table(csr1, csr1[:], slice(0, 128), 0, 1)
                elif k == 1:
                    rope_table(csr2, csr2[:], slice(0, 128), 0, 2)
                elif k == 2:
                    rope_table(csm1f, csm1f[64:96, :], slice(64, 96), 3, 4)
                    dma("sp", s_csm, [csm1f], [csms_k], out=csms_d[b, 0, :, t0:t0 + 512], in_=csm1f[64:96, :])
                else:
                    rope_table(csm2f, csm2f[64:96, :], slice(64, 96), 3, 5)
                    dma("sp", s_csm, [csm2f], [csms_k], out=csms_d[b, 1, :, t0:t0 + 512], in_=csm2f[64:96, :])

            def A_S1(g, tl):
                ti = g * 4 + tl
                tok0 = ti * 128
                xi_ = xin[ti % 2]
                xs_ = xsb2[ti % 2]
                so = 10 + 3 * (ti % 2)
                dma("sp", s_xin[ti % 2], [], [xi_], out=xi_[:], in_=x_d[b, tok0:tok0 + 128, :])
                op("act", "activation", [xi_], [junk, stat], out=junk[:], in_=xi_[:], func=AF.Square, accum_out=stat[:, so:so + 1])
                op("dve", "tensor_scalar", [stat], [stat], out=stat[:, so + 1:so + 2], in0=stat[:, so:so + 1], scalar1=1.0 / D, scalar2=EPS,
                   op0=ALU.mult, op1=ALU.add)
                rsqrt(stat, stat[:, so + 1:so + 2], stat, stat[:, so + 2:so + 3], 1)
                op("dve", "tensor_scalar", [xi_, stat], [xs_], out=xs_[:], in0=xi_[:], scalar1=stat[:, so + 2:so + 3], scalar2=None, op0=ALU.mult)

            def A_T8(g, tl):
                ti = g * 4 + tl
                xs_ = xsb2[ti % 2]
                for c in range(8):
                    op("pe", "transpose", [xs_, ident_b], [PS[0]], out=psbf(0)[:, c * 128:(c + 1) * 128],
                       in_=xs_[:, c * 128:(c + 1) * 128], identity=ident_b[:])
                for c in range(8):
                    if c % 2 == 0:
                        op("dve", "tensor_scalar", [PS[0], colv], [h1T], out=h1T[:, c, tl * 128:(tl + 1) * 128],
                           in0=psbf(0)[:, c * 128:(c + 1) * 128], scalar1=colv[:, c, 2:3], scalar2=colv[:, c, 0:1],
                           op0=ALU.mult, op1=ALU.add)
                    else:
                        op("act", "activation", [PS[0], colv], [h1T], out=h1T[:, c, tl * 128:(tl + 1) * 128],
                           in_=psbf(0)[:, c * 128:(c + 1) * 128], func=AF.Identity, scale=colv[:, c, 2:3], bias=colv[:, c, 0:1])

            def A_MM(g, tl):
                RV4 = RVG[g % 2]
                GT4 = GTG[g % 2]
                ti = g * 4 + tl
                tok0 = ti * 128
                for (pb, c0, n) in ((1, 0, 384), (2, 384, 512), (3, 896, 512)):
                    for k in range(8):
                        op("pe", "matmul", [h1T, w_tm], [PS[pb]], PS[pb][:, 0:n], lhsT=h1T[:, k, tl * 128:(tl + 1) * 128],
                           rhs=w_tm[:, k, c0:c0 + n], start=(k == 0), stop=(k == 7))
                op("act", "activation", [PS[1]], [junk, stat], out=junk[:, 0:256], in_=PS[1][:, 0:256], func=AF.Square, accum_out=stat[:, 4:5])
                op("act", "activation", [PS[1]], [junk, stat], out=junk[:, 256:384], in_=PS[1][:, 256:384], func=AF.Square, accum_out=stat[:, 5:6])
                op("dve", "tensor_scalar", [stat], [stat], out=stat[:, 6:7], in0=stat[:, 4:5], scalar1=1.0 / 256, scalar2=EPS, op0=ALU.mult, op1=ALU.add)
                op("dve", "tensor_scalar", [stat], [stat], out=stat[:, 7:8], in0=stat[:, 5:6], scalar1=1.0 / 128, scalar2=EPS, op0=ALU.mult, op1=ALU.add)
                rsqrt(stat, stat[:, 6:8], stat, stat[:, 8:10], 2)
                op("dve", "tensor_scalar", [PS[1], stat], [cqn], out=cqn[:, 0:256], in0=PS[1][:, 0:256], scalar1=stat[:, 8:9], scalar2=None, op0=ALU.mult)
                op("dve", "tensor_scalar", [PS[1], stat], [cqn], out=cqn[:, 256:384], in0=PS[1][:, 256:384], scalar1=stat[:, 9:10], scalar2=None, op0=ALU.mult)
                for c in range(3):
                    op("pe", "transpose", [cqn, ident_b], [PS[0]], out=psbf(0)[:, c * 128:(c + 1) * 128],
                       in_=cqn[:, c * 128:(c + 1) * 128], identity=ident_b[:])
                op("act", "copy", [PS[0]], [cqnT], out=cqnT[:, :, tok0:tok0 + 128],
                   in_=psbf(0)[:, 0:256].rearrange("p (c t) -> p c t", c=2))
                op("act", "copy", [PS[0]], [ckvnT], out=ckvnT[:, tok0:tok0 + 128], in_=psbf(0)[:, 256:384])
                op("act", "copy", [PS[2]], [RV4], out=RV4[:, tl, :], in_=PS[2][:])
                op("act", "activation", [PS[3]], [gsg], out=gsg[:], in_=PS[3][:], func=AF.Tanh, scale=0.5)
                op("dve", "scalar_tensor_tensor", [gsg, PS[3]], [GT4], out=GT4[:, tl, :], in0=gsg[:], scalar=1.0, in1=PS[3][:], op0=ALU.add, op1=ALU.mult)

            def A_FM(g):
                t0 = g * 512

                def fm_mm(pb, col0, ncols):
                    for k in range(8):
                        op("pe", "matmul", [h1T, w_fm], [PS[pb]], PS[pb][0:ncols, :], lhsT=w_fm[:, k, col0:col0 + ncols],
                           rhs=h1T[:, k, :], start=(k == 0), stop=(k == 7))
                for which, dst in ((0, rqT), (1, rkT)):
                    for j in range(2):
                        fm_mm(4, (which * 4 + j) * 128, 128)
                        fm_mm(5, (which * 4 + 2 + j) * 128, 128)
                        op("dve", "tensor_tensor", [PS[4], csr1], [rt1], out=rt1[:], in0=PS[4][:], in1=csr1[:], op=ALU.mult)
                        op("dve", "tensor_tensor", [PS[5], csr2], [rt2], out=rt2[:], in0=PS[5][:], in1=csr2[:], op=ALU.mult)
                        op("pool", "tensor_tensor", [rt1, rt2], [dst], out=dst[:, j, :], in0=rt1[:], in1=rt2[:], op=ALU.add)
                op("pool", "tensor_tensor", [rqT, cst], [rqxT], out=rqxT[:].rearrange("p j (n q) -> p j n q", n=4),
                   in0=rqT[:].rearrange("p j (n q) -> p j n q", n=4), in1=xi_c.unsqueeze(2).to_broadcast([128, 2, 4, 128]), op=ALU.mult)
                fm_mm(4, 1024, 96)
                fm_mm(5, 1120, 96)
                op("dve", "tensor_tensor", [PS[4], csm1f], [rt1], out=rt1[64:96, :], in0=PS[4][64:96, :], in1=csm1f[64:96, :], op=ALU.mult)
                op("dve", "tensor_tensor", [PS[5], csm2f], [rt2], out=rt2[64:96, :], in0=PS[5][64:96, :], in1=csm2f[64:96, :], op=ALU.mult)
                op("pool", "tensor_tensor", [rt1, rt2], [kT], out=kT[64:96, t0:t0 + 512], in0=rt1[64:96, :], in1=rt2[64:96, :], op=ALU.add)

            def A_RETa(g, tl):
                RV4 = RVG[g % 2]
                qs = slice(tl * 128, (tl + 1) * 128)
                for j in range(2):
                    op("pe", "transpose", [rkT, ident_b], [PS[4]], out=psbf(4)[:, j * 128:(j + 1) * 128], in_=rkT[:, j, qs], identity=ident_b[:])
                op("dve", "tensor_tensor", [PS[4], cst], [rkz], out=rkz[:], in0=psbf(4)[:, 0:256], in1=zeta_c, op=ALU.mult)
                for h in range(4):
                    j, half = h // 2, h % 2
                    pr = slice(half * 64, half * 64 + 64)
                    op("pe", "matmul", [rkT, rqT], [PS[6]], PS[6][:, h * 128:(h + 1) * 128], lhsT=rkT[pr, j, qs], rhs=rqT[pr, j, qs],
                       start=True, stop=True)
                op("dve", "tensor_tensor", [PS[6], cst], [sdT], out=sdT[:], in0=PS[6][:].rearrange("p (h q) -> p h q", h=4), in1=dmaskT, op=ALU.mult)
                for h in range(4):
                    j, half = h // 2, h % 2
                    pr = slice(half * 64, half * 64 + 64)
                    op("pe", "matmul", [sdT, RV4], [PS[7]], PS[7][:, h * 128:(h + 1) * 128], lhsT=sdT[:, h, :], rhs=RV4[:, tl, h * 128:(h + 1) * 128],
                       start=True, stop=False)
                    op("pe", "matmul", [rqxT, state_b], [PS[7]], PS[7][:, h * 128:(h + 1) * 128], lhsT=rqxT[pr, j, qs], rhs=state_b[pr, j, :],
                       start=False, stop=True)
                for h in range(4):
                    j = h // 2
                    op("pe", "matmul", [rkz, RV4], [PS[6]], PS[6][:, h * 128:(h + 1) * 128], lhsT=rkz[:, j * 128:(j + 1) * 128],
                       rhs=RV4[:, tl, h * 128:(h + 1) * 128], start=True, stop=True)
                for h in range(4):
                    j, half = h // 2, h % 2
                    pr = slice(half * 64, half * 64 + 64)
                    op("dve", "scalar_tensor_tensor", [state, cst, PS[6]], [state], out=state[pr, j, :], in0=state[pr, j, :],
                       scalar=cd_c[pr, h:h + 1], in1=PS[6][pr, h * 128:(h + 1) * 128], op0=ALU.mult, op1=ALU.add)
                op("pool", "tensor_copy", [state], [state_b], out=state_b[:], in_=state[:])

            def A_RETb_dve(g, tl):
                GT4 = GTG[g % 2]
                op("act", "copy", [PS[7]], [osb], out=osb[:], in_=PS[7][:].rearrange("p (h d) -> p h d", h=4))
                op("dve", "tensor_reduce", [osb], [gst], out=gst[:, 0:4], in_=osb[:], axis=AX.X, op=ALU.add)
                op("pool", "tensor_tensor", [osb], [osq], out=osq[:], in0=osb[:], in1=osb[:], op=ALU.mult)
                op("dve", "tensor_reduce", [osq], [gst], out=gst[:, 4:8], in_=osq[:], axis=AX.X, op=ALU.add)
                op("dve", "tensor_scalar", [gst], [gst], out=gst[:, 0:4], in0=gst[:, 0:4], scalar1=1.0 / 128, scalar2=None, op0=ALU.mult)
                op("dve", "tensor_tensor", [gst], [gst], out=gst[:, 8:12], in0=gst[:, 0:4], in1=gst[:, 0:4], op=ALU.mult)
                op("dve", "scalar_tensor_tensor", [gst], [gst], out=gst[:, 8:12], in0=gst[:, 4:8], scalar=1.0 / 128, in1=gst[:, 8:12],
                   op0=ALU.mult, op1=ALU.subtract)
                op("dve", "tensor_scalar", [gst], [gst], out=gst[:, 8:12], in0=gst[:, 8:12], scalar1=EPS, scalar2=None, op0=ALU.add)
                rsqrt(gst, gst[:, 8:12], gst, gst[:, 12:16], 4)
                op("dve", "tensor_scalar", [gst], [gst], out=gst[:, 12:16], in0=gst[:, 12:16], scalar1=0.5, scalar2=None, op0=ALU.mult)
                op("dve", "tensor_tensor", [osb, gst], [osb], out=osb[:], in0=osb[:], in1=gst[:, 0:4].unsqueeze(2).to_broadcast([128, 4, 128]), op=ALU.subtract)
                op("dve", "tensor_tensor", [osb, gst], [osb], out=osb[:], in0=osb[:], in1=gst[:, 12:16].unsqueeze(2).to_broadcast([128, 4, 128]), op=ALU.mult)
                op("pool", "tensor_tensor", [osb, GT4], [oretb], out=oretb[:], in0=osb[:].rearrange("p h d -> p (h d)"), in1=GT4[:, tl, :], op=ALU.mult)

            def A_RETb_pe(g, tl):
                qs = slice(tl * 128, (tl + 1) * 128)
                for h in range(4):
                    op("pe", "transpose", [oretb, ident_b], [PS[5]], out=psbf(5)[:, h * 128:(h + 1) * 128], in_=oretb[:, h * 128:(h + 1) * 128], identity=ident_b[:])
                op("act", "copy", [PS[5]], [oretT], out=oretT[:, :, qs], in_=psbf(5)[:, 0:512].rearrange("p (h t) -> p h t", h=4))

            A_S1(0, 0)
            for g in range(NG + 1):
                if g < NG:
                    A_pos(g)
                for tl in range(4):
                    if g < NG:
                        A_T8(g, tl)
                    nxt = g * 4 + tl + 1
                    if nxt < NG * 4:
                        A_S1(nxt // 4, nxt % 4)
                    if g >= 1:
                        A_RETa(g - 1, tl)
                    if g >= 1 and tl > 0:
                        A_RETb_pe(g - 1, tl - 1)
                    if g < NG:
                        A_MM(g, tl)
                    if g >= 1:
                        A_RETb_dve(g - 1, tl)
                    if g < NG:
                        A_table(g, tl)
                if g < NG:
                    A_FM(g)
                if g >= 1:
                    A_RETb_pe(g - 1, 3)
                    dma("sp", s_mixr, [oretT], [mixr_k], out=mixr_d[b, :, :, (g - 1) * 512:g * 512].rearrange("h p t -> p h t"), in_=oretT[:])
            fence([oretT, BIGW], pT + qT + wblk)

            def qprep(h, i):
                qsl = slice(i * 512, (i + 1) * 512)
                for c in range(2):
                    op("pe", "matmul", [wuq_a, cqnT], [PS[4]], PS[4][0:96, :], lhsT=wuq_a[:, c, h, 0:96], rhs=cqnT[:, c, qsl], start=(c == 0), stop=(c == 1))
                for c in range(2):
                    op("pe", "matmul", [wuq_a, cqnT], [PS[5]], PS[5][0:96, :], lhsT=wuq_a[:, c, h, 96:192], rhs=cqnT[:, c, qsl], start=(c == 0), stop=(c == 1))
                qt = qT[(h * NG + i) % 2]
                op("act", "copy", [PS[4]], [qt], out=qt[0:64, :], in_=PS[4][0:64, :])
                dma("sp", s_csm, [csms_k], [csm1f], out=csm1f[64:96, :], in_=csms_d[b, 0, :, qsl])
                dma("sp", s_csm, [csms_k], [csm2f], out=csm2f[64:96, :], in_=csms_d[b, 1, :, qsl])
                op("dve", "tensor_tensor", [PS[4], csm1f], [rt1], out=rt1[64:96, :], in0=PS[4][64:96, :], in1=csm1f[64:96, :], op=ALU.mult)
                op("dve", "tensor_tensor", [PS[5], csm2f], [rt2], out=rt2[64:96, :], in0=PS[5][64:96, :], in1=csm2f[64:96, :], op=ALU.mult)
                op("dve", "tensor_tensor", [rt1, rt2], [qt], out=qt[64:96, :], in0=rt1[64:96, :], in1=rt2[64:96, :], op=ALU.add)

            pend_epi = []
            for h in range(8):
                for g in range(NG):
                    op("pe", "matmul", [wk_b, ckvnT], [PS[6]], PS[6][0:64, :], lhsT=wk_b[:, h, :], rhs=ckvnT[:, g * 512:(g + 1) * 512], start=True, stop=True)
                    op("act", "copy", [PS[6]], [kT], out=kT[0:64, g * 512:(g + 1) * 512], in_=PS[6][0:64, :])
                vb = vh[h % 2]
                for t8 in range((NT + 7) // 8):
                    n8 = min(8, NT - t8 * 8)
                    for tt_ in range(n8):
                        ti = t8 * 8 + tt_
                        op("pe", "matmul", [ckvnT, wv_b], [PS[7]], PS[7][:, tt_ * 64:(tt_ + 1) * 64], lhsT=ckvnT[:, ti * 128:(ti + 1) * 128], rhs=wv_b[:, h, :],
                           start=True, stop=True)
                    op("dve", "tensor_copy", [PS[7]], [vb], out=vb[:, t8 * 8:t8 * 8 + n8, 0:64], in_=PS[7][:, 0:n8 * 64].rearrange("p (t d) -> p t d", d=64))
                qprep(h, 0)
                for i in range(NG):
                    qsl = slice(i * 512, (i + 1) * 512)
                    qt = qT[(h * NG + i) % 2]
                    if i + 1 < NG:
                        qprep(h, i + 1)
                    nk = 4 * i + 4
                    ob = 2 + ((h * NG + i) % 2)

                    def c0_of(j):
                        return 128 * (j - 4 * i) if j > 4 * i else 0

                    def qk(j):
                        c0 = c0_of(j)
                        op("pe", "matmul", [kT, qt], [PS[j % 2]], PS[j % 2][:, c0:512], lhsT=kT[0:96, j * 128:(j + 1) * 128], rhs=qt[0:96, c0:512], start=True, stop=True)
                    qk(0)
                    for j in range(nk):
                        if j + 1 < nk:
                            qk(j + 1)
                        p_ = pT[j % 2]
                        c0 = c0_of(j)
                        op("act", "activation", [PS[j % 2]], [p_], out=p_[:, c0:512], in_=PS[j % 2][:, c0:512], func=AF.Exp, scale=float(96 ** -0.5))
                        if j >= 4 * i:
                            m_ = j - 4 * i
                            op("dve", "tensor_tensor", [p_, cmask_b], [p_], out=p_[:, c0:c0 + 128], in0=p_[:, c0:c0 + 128], in1=cmask_b[:, m_, c0:c0 + 128], op=ALU.mult)
                        op("pe", "matmul", [vb, p_], [PS[ob]], PS[ob][0:65, c0:512], lhsT=vb[:, j, 0:65], rhs=p_[:, c0:512], start=(j == 0), stop=(j == nk - 1))
                        if j == 1 and pend_epi:
                            pend_epi.pop(0)()
                    def epilogue(ob=ob, h=h, qsl=qsl):
                        op("dve", "reciprocal", [PS[ob]], [rrow], out=rrow[64:65, :], in_=PS[ob][64:65, :])
                        op("pe", "matmul", [ones_f, rrow], [PS[7]], PS[7][0:64, :], lhsT=ones_f[64:65, 0:64], rhs=rrow[64:65, :], start=True, stop=True)
                        op("act", "copy", [PS[7]], [bcs], out=bcs[:], in_=PS[7][0:64, :])
                        op("dve", "tensor_tensor", [PS[ob], bcs], [oTm], out=oTm[:], in0=PS[ob][0:64, :], in1=bcs[:], op=ALU.mult)
                        dma("sp", s_mixm, [oTm], [mixm_k], out=mixm_d[b, h, :, qsl], in_=oTm[:])
                    pend_epi.append(epilogue)
                    for _ in range(per_slot):
                        if relay_state[0] < NE:
                            relayout_expert(relay_state[0])
                            relay_state[0] += 1
            while pend_epi:
                pend_epi.pop(0)()
            fence(pT + qT + wblk, [h2T, BIGW])

            fence([GTG[0], h1T, RVG[1]], c_alts)
            dma("pool", s_w, [], [wo_m], out=wo_m[:], in_=wo_d[0:512, :].rearrange("(h p) n -> p h n", p=64))
            dma("pool", s_w, [], [wo_r], out=wo_r[:], in_=wo_d[512:1024, :].rearrange("(h p) n -> p h n", p=128))
            dma("sp", s_bc, [mods_k], [g1bc], out=g1bc[:], in_=mods_d[b:b + 1, 2 * D:3 * D].partition_broadcast(128))
            dma("sp", s_bc, [mods_k], [sh2bc], out=sh2bc[:], in_=mods_d[b:b + 1, 3 * D:4 * D].partition_broadcast(128))
            dma("sp", s_bc, [mods_k], [A2bc], out=A2bc[:], in_=mods_d[b:b + 1, 4 * D:5 * D].partition_broadcast(128))
            dma("sp", s_bc, [], [n2gbc], out=n2gbc[:], in_=n2g_d.partition_broadcast(128))
            op("dve", "scalar_tensor_tensor", [A2bc, n2gbc], [A2bc], out=A2bc[:], in0=A2bc[:], scalar=1.0, in1=n2gbc[:], op0=ALU.add, op1=ALU.mult)
            def C1(ti):
                tok0 = ti * 128
                gt = b * NT + ti
                p2 = ti % 2
                x1 = x1_2[p2]
                h2 = h2_2[p2]
                h2b = h2b_2[p2]
                h2T = h2T_2[p2]
                mm_t = mm_t_2[p2]
                mr_t = mr_t_2[p2]
                pa, pbk = (0, 1) if p2 == 0 else (6, 7)
                so = 0 if p2 == 0 else 10
                xi_ = xin[ti % 2]
                dma("sp", s_xin[ti % 2], [], [xi_], out=xi_[:], in_=x_d[b, tok0:tok0 + 128, :])
                dma("sp", s_mm2[p2], [mixm_k], [mm_t], out=mm_t[:], in_=mixm_d[b, :, :, tok0:tok0 + 128].rearrange("h p t -> p h t"))
                dma("sp", s_mm2[p2], [mixr_k], [mr_t], out=mr_t[:], in_=mixr_d[b, :, :, tok0:tok0 + 128].rearrange("h p t -> p h t"))
                for nh, pbank in ((0, pa), (1, pbk)):
                    for hh in range(8):
                        op("pe", "matmul", [mm_t, wo_m], [PS[pbank]], PS[pbank][:, :], lhsT=mm_t[:, hh, :], rhs=wo_m[:, hh, nh * 512:(nh + 1) * 512], start=(hh == 0), stop=False)
                    for hh in range(4):
                        op("pe", "matmul", [mr_t, wo_r], [PS[pbank]], PS[pbank][:, :], lhsT=mr_t[:, hh, :], rhs=wo_r[:, hh, nh * 512:(nh + 1) * 512], start=False, stop=(hh == 3))
                for nh, pbank in ((0, pa), (1, pbk)):
                    op("dve", "tensor_tensor", [PS[pbank], g1bc], [x1], out=x1[:, nh * 512:(nh + 1) * 512], in0=PS[pbank][:, :], in1=g1bc[:, nh * 512:(nh + 1) * 512], op=ALU.mult)
                op("pool", "tensor_tensor", [x1, xi_], [x1], out=x1[:], in0=x1[:], in1=xi_[:], op=ALU.add)
                dma("sp", s_x1, [x1], [x1s_k], out=x1s_d[gt * 128:(gt + 1) * 128, :], in_=x1[:])
                op("act", "activation", [x1], [junk, stat], out=junk[:], in_=x1[:], func=AF.Square, accum_out=stat[:, so:so + 1])
                op("dve", "tensor_scalar", [stat], [stat], out=stat[:, so + 1:so + 2], in0=stat[:, so:so + 1], scalar1=1.0 / D, scalar2=EPS, op0=ALU.mult, op1=ALU.add)
                rsqrt(stat, stat[:, so + 1:so + 2], stat, stat[:, so + 2:so + 3], 1)
                op("dve", "scalar_tensor_tensor", [x1, stat, A2bc], [h2], out=h2[:], in0=x1[:], scalar=stat[:, so + 2:so + 3], in1=A2bc[:], op0=ALU.mult, op1=ALU.mult)
                op("pool", "tensor_tensor", [h2, sh2bc], [h2], out=h2[:], in0=h2[:], in1=sh2bc[:], op=ALU.add)
                op("act", "copy", [h2], [h2b], out=h2b[:], in_=h2[:])
                dma("sp", s_h2, [h2b], [h2s_k], out=h2s_d[gt * 128:(gt + 1) * 128, :], in_=h2b[:])
                h2Tb = h2Tb_2[p2]
                for c in range(8):
                    op("pe", "transpose", [h2b, ident_b], [PS[2 + p2]], out=psbf(2 + p2)[:, c * 128:(c + 1) * 128], in_=h2b[:, c * 128:(c + 1) * 128], identity=ident_b[:])
                op("act", "copy", [PS[2 + p2]], [h2T], out=h2Tb[:], in_=psbf(2 + p2)[:, 0:1024].rearrange("p (c t) -> p c t", c=8))
                for c in range(8):
                    op("pe", "matmul", [h2T, w_rtb], [PS[4]], PS[4][:, 0:36], lhsT=h2Tb[:, c, :], rhs=w_rtb[:, c, :], start=(c == 0), stop=(c == 7))

                lgt = lgt2[ti % 2]
                op("dve", "tensor_tensor", [PS[4], b_rt], [lgt], out=lgt[:, 0:4], in0=PS[4][:, 0:4], in1=b_rt[:, 0:4], op=ALU.add)
                op("dve", "tensor_tensor", [PS[4], b_rt], [lgt], out=lgt[:, 8:40], in0=PS[4][:, 4:36], in1=b_rt[:, 4:36], op=ALU.add)

            def C2(ti):
                gt = b * NT + ti
                lgt = lgt2[ti % 2]
                m8 = m8_2[ti % 2]
                rst = rst_2[ti % 2]
                lem = lem_2[ti % 2]
                Mt = Mt_2[ti % 2]
                op("dve", "max", [lgt], [m8], out=m8[:, 0:8], in_=lgt[:, 0:8])
                op("dve", "tensor_scalar", [m8], [rst], out=rst[:, 0:1], in0=m8[:, 0:1], scalar1=-1.0, scalar2=None, op0=ALU.mult)
                op("act", "activation", [lgt, rst], [rst], out=rst[:, 8:16], in_=lgt[:, 0:8], func=AF.Exp, bias=rst[:, 0:1], scale=1.0, accum_out=rst[:, 1:2])
                op("dve", "reciprocal", [rst], [rst], out=rst[:, 2:3], in_=rst[:, 1:2])
                op("dve", "tensor_scalar", [lgt, m8], [rst], out=rst[:, 16:20], in0=lgt[:, 0:4], scalar1=m8[:, 0:1], scalar2=None, op0=ALU.is_equal)
                op("dve", "tensor_scalar", [rst], [rst], out=rst[:, 16:20], in0=rst[:, 16:20], scalar1=-1.0, scalar2=1e30, op0=ALU.add, op1=ALU.mult)
                op("dve", "tensor_tensor", [lgt, rst], [lem], out=lem[:].rearrange("p (g e) -> p g e", g=4), in0=lgt[:, 8:40].rearrange("p (g e) -> p g e", g=4),
                   in1=rst[:, 16:20].unsqueeze(2).to_broadcast([128, 4, 8]), op=ALU.add)
                op("dve", "max", [lem], [m8], out=m8[:, 8:16], in_=lem[:])
                op("dve", "tensor_scalar", [lem, m8], [OH1], out=OH1[:, gt, :], in0=lem[:], scalar1=m8[:, 8:9], scalar2=None, op0=ALU.is_equal)
                op("dve", "tensor_scalar", [lem, m8], [OH2], out=OH2[:, gt, :], in0=lem[:], scalar1=m8[:, 9:10], scalar2=None, op0=ALU.is_equal)
                op("dve", "tensor_tensor", [m8], [rst], out=rst[:, 3:4], in0=m8[:, 9:10], in1=m8[:, 8:9], op=ALU.subtract)
                op("act", "activation", [rst], [rst], out=rst[:, 4:5], in_=rst[:, 3:4], func=AF.Exp)
                op("dve", "tensor_scalar", [rst], [rst], out=rst[:, 4:5], in0=rst[:, 4:5], scalar1=1.0, scalar2=None, op0=ALU.add)
                op("dve", "reciprocal", [rst], [rst], out=rst[:, 5:6], in_=rst[:, 4:5])
                op("dve", "tensor_tensor", [rst], [GATE], out=GATE[:, gt, 0:1], in0=rst[:, 5:6], in1=rst[:, 2:3], op=ALU.mult)
                op("dve", "tensor_tensor", [rst, GATE], [GATE], out=GATE[:, gt, 1:2], in0=rst[:, 2:3], in1=GATE[:, gt, 0:1], op=ALU.subtract)
                op("pool", "tensor_tensor", [OH1, OH2], [Mt], out=Mt[:], in0=OH1[:, gt, :], in1=OH2[:, gt, :], op=ALU.add)
                op("pe", "matmul", [triu_b, Mt], [PS[5]], PS[5][:, 0:32], lhsT=triu_b[:], rhs=Mt[:], start=True, stop=False)
                op("pe", "matmul", [ones_b, Macc], [PS[5]], PS[5][:, 0:32], lhsT=ones_b[:], rhs=Macc[:], start=False, stop=True)
                op("act", "copy", [PS[5]], [CUM], out=CUM[:, gt, :], in_=PS[5][:, 0:32])
                op("pool", "tensor_tensor", [Macc, Mt], [Macc], out=Macc[:], in0=Macc[:], in1=Mt[:], op=ALU.add)


            import os as _os2
            if _os2.environ.get("K_CSKEW", "1") == "1":
                C1(0)
                for ti in range(NT):
                    if ti + 1 < NT:
                        C1(ti + 1)
                    C2(ti)
            else:
                for ti in range(NT):
                    C1(ti)
                    C2(ti)
            fence(c_alts, [P0, P1, P4c])

        op("pe", "matmul", [ones_b, Macc], [PS[5]], PS[5][:, 0:32], lhsT=ones_b[:], rhs=Macc[:], start=True, stop=True)
        op("dve", "tensor_scalar", [PS[5]], [ra], out=ra[:], in0=PS[5][:, 0:32], scalar1=1.0 / BLK, scalar2=(BLK - 1 - (BLK / 2 - 0.5)) / BLK, op0=ALU.mult, op1=ALU.add)
        op("dve", "tensor_scalar", [ra], [ra], out=ra[:], in0=ra[:], scalar1=MAGIC_RN, scalar2=None, op0=ALU.add)
        op("dve", "tensor_scalar", [ra], [pad_], out=pad_[:], in0=ra[:], scalar1=MAGIC_RN, scalar2=float(BLK), op0=ALU.subtract, op1=ALU.mult)
        op("dve", "tensor_copy", [pad_], [ra], out=ra[:], in_=pad_[:])
        cur, oth = ra, rb
        for sft in (1, 2, 4, 8, 16):
            op("dve", "tensor_copy", [cur], [oth], out=oth[:, 0:sft], in_=cur[:, 0:sft])
            op("dve", "tensor_tensor", [cur], [oth], out=oth[:, sft:32], in0=cur[:, sft:32], in1=cur[:, 0:32 - sft], op=ALU.add)
            cur, oth = oth, cur
        pend = cur
        op("dve", "tensor_tensor", [pend, pad_], [pst], out=pst[:], in0=pend[:], in1=pad_[:], op=ALU.subtract)
        a0, a1 = coff["blkstart"]
        op("dve", "tensor_tensor", [pend, cst], [cmp3], out=cmp3[:], in0=pend[:].unsqueeze(1).to_broadcast([128, NBLK, 32]),
           in1=cst[:, a0:a1].unsqueeze(2).to_broadcast([128, NBLK, 32]), op=ALU.is_le)
        op("dve", "tensor_reduce", [cmp3], [ebf], out=ebf[:], in_=cmp3[:], axis=AX.X, op=ALU.add)
        i0, i1 = coff["iotap"]
        op("dve", "tensor_scalar", [ebf], [ebf], out=ebf[:], in0=ebf[:], scalar1=31.0, scalar2=128.0, op0=ALU.min, op1=ALU.mult)
        op("dve", "tensor_scalar", [ebf, cst], [WIDX], out=WIDX[:], in0=ebf[:], scalar1=cst[:, i0:i1], scalar2=None, op0=ALU.add)
        op("pool", "tensor_tensor", [CUM, pst], [CUM], out=CUM[:], in0=CUM[:], in1=pst[:].unsqueeze(1).to_broadcast([128, NTT, 32]), op=ALU.add)
        for k_, OH in ((0, OH1), (1, OH2)):
            op("dve", "tensor_tensor", [OH, CUM], [cmpd], out=cmpd[:], in0=OH[:], in1=CUM[:], op=ALU.mult)
            op("dve", "tensor_reduce", [cmpd], [destf], out=destf[:, :, k_], in_=cmpd[:], axis=AX.X, op=ALU.add)
        op("dve", "tensor_copy", [destf], [DEST], out=DEST[:], in_=destf[:])

        xs_k = DR()
        ys_k = DR()
        h2r = h2r + [Alias(P7, 0, [128, D], BF16, own=True), Alias(P4c, 0, [128, D], BF16, own=True)]
        s_h2r = s_h2r + [SC.dma_sem("h2r2"), SC.dma_sem("h2r3")]
        fence([rqT, rkT, RVG[1], h1T, GT4, BIGW], h2r + xblk + ysb + wblk)
        for gt in range(NTT):
            hr = h2r[gt % 4]
            dma("sp", s_h2r[gt % 4], [h2s_k], [hr], out=hr[:], in_=h2s_d[gt * 128:(gt + 1) * 128, :])
            for k_ in range(2):
                SC.dma("pool", (lambda e, hr=hr, gt=gt, k_=k_: e.indirect_dma_start(
                    out=xs_d, out_offset=bass.IndirectOffsetOnAxis(ap=DEST[:, gt, k_:k_ + 1], axis=0), in_=hr[:, :], in_offset=None)),
                    s_sc, [hr.k, DEST.k], [xs_k.k], lat=6.0)

        xTb2 = [xTb, Alias(P4b, 0, [128, 2, 8, 128], BF16)]
        actb2 = [[Alias(P2a, 1024 + 512 * (2 * pq + r), [128, 256], BF16, own=True) for r in range(2)] for pq in range(2)]
        sg2 = [sg, Alias(P2a, 3072, [128, 256], F32, own=True)]
        actT2 = [Alias(P3a, 512 * r, [128, 2, 128], BF16, own=True) for r in range(2)]
        fence([g1bc, A2bc], [a_ for l_ in actb2 for a_ in l_] + sg2 + actT2)

        def stage1(blk):
            pq = blk % 2
            wb = wblk[pq]
            SC.dma("pool", (lambda e, wb=wb, blk=blk: e.indirect_dma_start(
                out=wb[:, :], out_offset=None, in_=wall_d, in_offset=bass.IndirectOffsetOnAxis(ap=WIDX[:, blk:blk + 1], axis=0))),
                s_wg[pq], [WIDX.k, wall_k.k], [wb.k], lat=14.0)
            xb_ = xblk[pq]
            dma("sp", s_xb[pq], [xs_k], [xb_], out=xb_[:], in_=xs_d[blk * BLK:(blk + 1) * BLK, :].rearrange("(r p) d -> p r d", p=128))
            xt_ = xTb2[pq]
            for r in range(2):
                for c in range(8):
                    op("pe", "transpose", [xb_, ident_b], [PS[0]], out=psbf(0)[:, c * 128:(c + 1) * 128], in_=xb_[:, r, c * 128:(c + 1) * 128], identity=ident_b[:])
                if r == 0:
                    op("act", "copy", [PS[0]], [xt_], out=xt_[:, r, :, :], in_=psbf(0)[:, 0:1024].rearrange("p (c t) -> p c t", c=8))
                else:
                    op("dve", "tensor_copy", [PS[0]], [xt_], out=xt_[:, r, :, :], in_=psbf(0)[:, 0:1024].rearrange("p (c t) -> p c t", c=8))
            for r in range(2):
                hb = 1 + 2 * pq + r
                for c in range(8):
                    op("pe", "matmul", [xt_, wb], [PS[hb]], PS[hb][:, :], lhsT=xt_[:, r, c, :], rhs=wb[:, c * 512:(c + 1) * 512], start=(c == 0), stop=(c == 7))
            for r in range(2):
                hb = 1 + 2 * pq + r
                sg_ = sg2[r]
                ab = actb2[pq][r]
                op("act", "activation", [PS[hb]], [sg_], out=sg_[:], in_=PS[hb][:, 0:256], func=AF.Tanh, scale=0.5)
                op("dve", "scalar_tensor_tensor", [sg_, PS[hb]], [sg_], out=sg_[:], in0=sg_[:], scalar=1.0, in1=PS[hb][:, 0:256], op0=ALU.add, op1=ALU.mult)
                op("dve", "scalar_tensor_tensor", [sg_, PS[hb]], [ab], out=ab[:], in0=sg_[:], scalar=0.5, in1=PS[hb][:, 256:512], op0=ALU.mult, op1=ALU.mult)

        def stage2(blk):
            pq = blk % 2
            wb = wblk[pq]
            for r in range(2):
                ab = actb2[pq][r]
                at = actT2[r]
                for fc in range(2):
                    op("pe", "transpose", [ab, ident_b], [PS[5]], out=psbf(5)[:, (2 * r + fc) * 128:(2 * r + fc + 1) * 128], in_=ab[:, fc * 128:(fc + 1) * 128], identity=ident_b[:])
                op("act", "copy", [PS[5]], [at], out=at[:], in_=psbf(5)[:, 2 * r * 128:(2 * r + 2) * 128].rearrange("p (c t) -> p c t", c=2))
            for r in range(2):
                at = actT2[r]
                yb = ysb[r]
                for nh in range(2):
                    for fc in range(2):
                        op("pe", "matmul", [at, wb], [PS[6 + nh]], PS[6 + nh][:, :], lhsT=at[:, fc, :],
                           rhs=wb[:, 4096 + fc * 1024 + nh * 512:4096 + fc * 1024 + (nh + 1) * 512], start=(fc == 0), stop=(fc == 1))
                    if nh == 0:
                        op("act", "copy", [PS[6]], [yb], out=yb[:, 0:512], in_=PS[6][:, :])
                    else:
                        op("dve", "tensor_copy", [PS[7]], [yb], out=yb[:, 512:1024], in_=PS[7][:, :])
                dma("sp", s_ys[r], [yb], [ys_k], out=ys_d[blk * BLK + r * 128:blk * BLK + (r + 1) * 128, :], in_=yb[:])

        stage1(0)
        for blk in range(NBLK):
            if blk + 1 < NBLK:
                stage1(blk + 1)
            stage2(blk)

        out_k = DR()
        fg_bc = Alias(P3b, 0, [128, D], F32)
        fence(xblk + [a_ for l_ in actb2 for a_ in l_] + sg2 + actT2, ysc + [g1bc, A2bc])
        dma("sp", s_bc, [], [fg_bc], out=fg_bc[:], in_=fg_d.partition_broadcast(128))
        s_yg2 = [SC.dma_sem("yg2_%d" % i) for i in range(2)]
        s_x1f = [SC.dma_sem("x1f%d" % i) for i in range(2)]
        h2alt = Alias(P2b, 0, [128, D], F32)
        x1alt = Alias(P5, 0, [128, D], F32)
        def F_pre(gt):
            yp = ysb if gt % 2 == 0 else ysc
            sy = s_yg if gt % 2 == 0 else s_yg2
            for k_ in range(2):
                SC.dma("pool", (lambda e, gt=gt, k_=k_, yy=yp[k_]: e.indirect_dma_start(
                    out=yy[:, :], out_offset=None, in_=ys_d, in_offset=bass.IndirectOffsetOnAxis(ap=DEST[:, gt, k_:k_ + 1], axis=0))),
                    sy[k_], [DEST.k, ys_k.k], [yp[k_].k], lat=7.0)
            xx = x1 if gt % 2 == 0 else x1alt
            dma("sp", s_x1f[gt % 2], [x1s_k], [xx], out=xx[:], in_=x1s_d[gt * 128:(gt + 1) * 128, :])

        def F_main(gt):
            b = gt // NT
            ti = gt % NT
            if ti == 0:
                dma("sp", s_bc, [mods_k], [g1bc], out=g1bc[:], in_=mods_d[b:b + 1, 5 * D:6 * D].partition_broadcast(128))
            yp = ysb if gt % 2 == 0 else ysc
            y1, y2 = yp[0], yp[1]
            hh = h2 if gt % 2 == 0 else h2alt
            xx = x1 if gt % 2 == 0 else x1alt
            sc_ = (gt % 2) * 4
            op("act", "activation", [y1, GATE], [hh], out=hh[:], in_=y1[:], func=AF.Identity, scale=GATE[:, gt, 0:1])
            op("dve", "scalar_tensor_tensor", [y2, GATE, hh], [hh], out=hh[:], in0=y2[:], scalar=GATE[:, gt, 1:2], in1=hh[:], op0=ALU.mult, op1=ALU.add)
            op("dve", "tensor_tensor", [hh, g1bc], [hh], out=hh[:], in0=hh[:], in1=g1bc[:], op=ALU.mult)
            op("pool", "tensor_tensor", [hh, xx], [hh], out=hh[:], in0=hh[:], in1=xx[:], op=ALU.add)
            op("act", "activation", [hh], [junk, stat], out=junk[:], in_=hh[:], func=AF.Square, accum_out=stat[:, sc_:sc_ + 1])
            op("dve", "tensor_scalar", [stat], [stat], out=stat[:, sc_ + 1:sc_ + 2], in0=stat[:, sc_:sc_ + 1], scalar1=1.0 / D, scalar2=EPS, op0=ALU.mult, op1=ALU.add)
            rsqrt(stat, stat[:, sc_ + 1:sc_ + 2], stat, stat[:, sc_ + 2:sc_ + 3], 1)
            xo = xin[gt % 2]
            op("dve", "scalar_tensor_tensor", [hh, stat, fg_bc], [xo], out=xo[:], in0=hh[:], scalar=stat[:, sc_ + 2:sc_ + 3], in1=fg_bc[:], op0=ALU.mult, op1=ALU.mult)
            dma("sp", s_xin[gt % 2], [xo], [out_k], out=out_d[b, ti * 128:(ti + 1) * 128, :], in_=xo[:])

        F_pre(0)
        for gt in range(NTT):
            if gt + 1 < NTT:
                F_pre(gt + 1)
            F_main(gt)
        SC.wait_all("sp", [out_k.k])
        SC.emit()
    return nc


_CACHE = {}


def kernel(**inputs):
    NCORES = 8
    x = np.asarray(inputs["x"], dtype=np.float32)
    B, S, _ = x.shape
    NSEQ = B // NCORES
    key = (S, NSEQ)
    if key not in _CACHE:
        _CACHE[key] = build_nc(S, NSEQ)
    nc = _CACHE[key]
    T = S * NSEQ
    NBLK = (2 * T + NE * (BLK - 1) + BLK - 1) // BLK
    cst_np, _ = make_consts(NBLK)
    f = lambda k: np.ascontiguousarray(np.asarray(inputs[k], dtype=np.float32))
    shared = {
        "w_ada": f("w_ada")[0], "b_ada": f("b_ada"), "norm1_g": f("norm1_g"), "w_in": f("w_in")[0],
        "q_norm_g": f("q_norm_g"), "w_uq": f("w_uq")[0], "kv_norm_g": f("kv_norm_g"), "w_ukv": f("w_ukv")[0],
        "w_o": f("w_o")[0], "norm2_g": f("norm2_g"), "w_gr": f("w_gr")[0], "b_gr": f("b_gr"),
        "w_er": f("w_er")[0].reshape(D, 32), "b_er": f("b_er").reshape(1, 32), "w1": f("w1")[0], "w3": f("w3")[0],
        "w2": f("w2")[0], "final_g": f("final_g").reshape(1, D), "cst": cst_np,
    }
    c = f("c")
    pos = np.ascontiguousarray(np.asarray(inputs["positions"], dtype=np.int32))
    in_maps = []
    for i in range(NCORES):
        m = dict(shared)
        m["x"] = np.ascontiguousarray(x[i * NSEQ:(i + 1) * NSEQ])
        m["c"] = np.ascontiguousarray(c[i * NSEQ:(i + 1) * NSEQ])
        m["positions"] = np.ascontiguousarray(pos[i * NSEQ:(i + 1) * NSEQ])
        in_maps.append(m)
    res = run_bass_kernel_spmd(nc, in_maps, core_ids=list(range(NCORES)))
    return np.concatenate([np.asarray(r["out"]) for r in res.results], axis=0).astype(np.float32)
```
